# Optimizing a Trainium2 kernel written in Bass

```python
import math
import jax, jax.numpy as jnp
from jax import lax
import numpy as np

D_MODEL = 1024
BATCH = 8
SEQ = 4096
DEPTH = 1

HEAD_DIM = 64
ROPE_THETA = 10000.0
EPS = 1e-6
NEG_INF = -1e30

DIL_GROUPS = ((128, 1), (512, 4), (2048, 16))
N_DIL_GROUPS = 3
DIL_HEADS_PER_GROUP = 4
DIL_HEADS = N_DIL_GROUPS * DIL_HEADS_PER_GROUP
DIL_WIDTH = DIL_HEADS * HEAD_DIM
DIL_OUT = DIL_HEADS_PER_GROUP * HEAD_DIM

DIFF_HEADS = D_MODEL // (2 * HEAD_DIM)
DIFF_QK_WIDTH = DIFF_HEADS * 2 * HEAD_DIM
DIFF_V_WIDTH = DIFF_HEADS * 2 * HEAD_DIM
DIFF_Q_BLOCK = 128

N_BRANCHES = 2
GATE_WIDTH = N_BRANCHES * D_MODEL
IN_WIDTH = 3 * DIL_WIDTH + 2 * DIFF_QK_WIDTH + DIFF_V_WIDTH + GATE_WIDTH
IN_SPLITS = (DIL_WIDTH, 2 * DIL_WIDTH, 3 * DIL_WIDTH,
             3 * DIL_WIDTH + DIFF_QK_WIDTH,
             3 * DIL_WIDTH + 2 * DIFF_QK_WIDTH,
             3 * DIL_WIDTH + 2 * DIFF_QK_WIDTH + DIFF_V_WIDTH)

PEER_HEADS = 8
PEER_N_KEYS = 128
PEER_N_EXPERTS = PEER_N_KEYS * PEER_N_KEYS
PEER_TOPK = 16
PEER_QUERY_DIM = 256
PEER_HALF = PEER_QUERY_DIM // 2
PEER_CHUNK = 128

kernel_name = "hybrid_dilated_diffattn_peer_block"


def rms_norm(x, g):
    xf = x.astype(jnp.float32)
    y = xf * lax.rsqrt(jnp.mean(xf * xf, axis=-1, keepdims=True) + EPS)
    return (y * g.astype(jnp.float32)).astype(x.dtype)


def rope(x, positions):
    hd = x.shape[-1]
    inv_freq = 1.0 / (ROPE_THETA ** (jnp.arange(0, hd, 2, dtype=jnp.float32) / hd))
    ang = positions.astype(jnp.float32)[:, :, None, None] * inv_freq
    cos, sin = jnp.cos(ang), jnp.sin(ang)
    xf = x.astype(jnp.float32)
    x1, x2 = xf[..., : hd // 2], xf[..., hd // 2:]
    return jnp.concatenate([x1 * cos - x2 * sin, x2 * cos + x1 * sin], axis=-1).astype(x.dtype)


def dilated_window_attention(q, k, v, window, dilation):
    b, s, h, hd = q.shape
    m = window // (2 * dilation)
    blk = m
    L = s // dilation
    nb = -(-L // blk)
    Lp = nb * blk

    def to_res(t):
        return t.reshape(b, L, dilation, h, hd).transpose(0, 2, 3, 1, 4)

    qr = jnp.pad(to_res(q), ((0, 0), (0, 0), (0, 0), (0, Lp - L), (0, 0)))
    kpad = ((0, 0), (0, 0), (0, 0), (blk, Lp - L + blk), (0, 0))
    kp = jnp.pad(to_res(k), kpad)
    vp = jnp.pad(to_res(v), kpad)
    qb = qr.reshape(b, dilation, h, nb, blk, hd)

    def band(t):
        tb = t.reshape(b, dilation, h, nb + 2, blk, hd)
        return jnp.concatenate([tb[:, :, :, :-2], tb[:, :, :, 1:-1], tb[:, :, :, 2:]], axis=4)

    kb, vb = band(kp), band(vp)
    scores = jnp.einsum('brhnqc,brhnkc->brhnqk', qb, kb).astype(jnp.float32) * (hd ** -0.5)
    qi = jnp.arange(nb)[:, None, None] * blk + jnp.arange(blk)[None, :, None]
    ki = jnp.arange(nb)[:, None, None] * blk - blk + jnp.arange(3 * blk)[None, None, :]
    mask = (jnp.abs(ki - qi) <= m) & (ki >= 0) & (ki < L)
    scores = jnp.where(mask, scores, NEG_INF)
    lse = jax.nn.logsumexp(scores, axis=-1)
    p = jnp.exp(scores - lse[..., None])
    out = jnp.einsum('brhnqk,brhnkc->brhnqc', p.astype(v.dtype), vb)
    out = out.reshape(b, dilation, h, Lp, hd)[:, :, :, :L]
    out = out.transpose(0, 3, 1, 2, 4).reshape(b, s, h, hd)
    lse = lse.reshape(b, dilation, h, Lp)[..., :L].transpose(0, 3, 1, 2).reshape(b, s, h)
    return out, lse


def differential_attention(q1, q2, k1, k2, v, lam):
    b, s, h, hd = q1.shape
    nqb = s // DIFF_Q_BLOCK
    scale = hd ** -0.5

    def blockify(t):
        return t.reshape(b, nqb, DIFF_Q_BLOCK, h, hd).transpose(1, 0, 2, 3, 4)

    def one_block(qs):
        qa, qb = qs
        s1 = jnp.einsum('bqhc,bkhc->bhqk', qa, k1).astype(jnp.float32) * scale
        s2 = jnp.einsum('bqhc,bkhc->bhqk', qb, k2).astype(jnp.float32) * scale
        a = jax.nn.softmax(s1, axis=-1) - lam * jax.nn.softmax(s2, axis=-1)
        return jnp.einsum('bhqk,bkhc->bqhc', a.astype(v.dtype), v)

    out = lax.map(one_block, (blockify(q1), blockify(q2)))
    return out.transpose(1, 0, 2, 3, 4).reshape(b, s, h, 2 * hd)


def peer_ffn(h, w_query, sub_keys, expert_u, expert_v):
    b, s, d = h.shape
    tokens = h.reshape((b * s) // PEER_CHUNK, PEER_CHUNK, d)

    def chunk(xc):
        q = (xc @ w_query).reshape(PEER_CHUNK, PEER_HEADS, 2, PEER_HALF)
        s1 = jnp.einsum('thc,hnc->thn', q[:, :, 0], sub_keys[:, 0]).astype(jnp.float32)
        s2 = jnp.einsum('thc,hnc->thn', q[:, :, 1], sub_keys[:, 1]).astype(jnp.float32)
        v1, i1 = lax.top_k(s1, PEER_TOPK)
        v2, i2 = lax.top_k(s2, PEER_TOPK)
        cand = (v1[..., :, None] + v2[..., None, :]).reshape(PEER_CHUNK, PEER_HEADS, PEER_TOPK * PEER_TOPK)
        top, ci = lax.top_k(cand, PEER_TOPK)
        e1 = jnp.take_along_axis(i1, ci // PEER_TOPK, axis=-1)
        e2 = jnp.take_along_axis(i2, ci % PEER_TOPK, axis=-1)
        eidx = e1 * PEER_N_KEYS + e2
        gate = jax.nn.softmax(top, axis=-1)
        u = expert_u[eidx]
        vv = expert_v[eidx]
        act = jax.nn.gelu(jnp.einsum('td,thkd->thk', xc, u).astype(jnp.float32), approximate=False)
        wts = (gate * act).astype(xc.dtype)
        return jnp.einsum('thk,thkd->td', wts, vv)

    return lax.map(chunk, tokens).reshape(b, s, d)


def hybrid_layer(x, c, positions, lam_init, w_ada, b_ada, norm1_g, w_in, b_gate,
                 qn_a, kn_a, w_proj_a, qn_b, kn_b, lam_q1, lam_k1, lam_q2, lam_k2,
                 subln_g, w_proj_b, w_out, norm2_g, w_query, sub_keys, expert_u, expert_v):
    b, s, d = x.shape
    mod = jax.nn.silu(c) @ w_ada + b_ada
    sh1, sc1, g1, sh2, sc2, g2 = [m[:, None, :] for m in jnp.split(mod, 6, axis=-1)]

    h = rms_norm(x, norm1_g) * (1 + sc1) + sh1
    proj = h @ w_in
    qa, ka, va, qb, kb, vb, gl = jnp.split(proj, IN_SPLITS, axis=-1)

    qa = rope(rms_norm(qa.reshape(b, s, DIL_HEADS, HEAD_DIM), qn_a), positions)
    ka = rope(rms_norm(ka.reshape(b, s, DIL_HEADS, HEAD_DIM), kn_a), positions)
    va = va.reshape(b, s, DIL_HEADS, HEAD_DIM)
    outs, lses = [], []
    for gi, (win, dil) in enumerate(DIL_GROUPS):
        sl = slice(gi * DIL_HEADS_PER_GROUP, (gi + 1) * DIL_HEADS_PER_GROUP)
        o, l = dilated_window_attention(qa[:, :, sl], ka[:, :, sl], va[:, :, sl], win, dil)
        outs.append(o)
        lses.append(l)
    wg = jax.nn.softmax(jnp.stack(lses), axis=0)
    oa = jnp.sum(wg[..., None] * jnp.stack(outs).astype(jnp.float32), axis=0).astype(x.dtype)
    branch_a = oa.reshape(b, s, DIL_OUT) @ w_proj_a

    qb = rms_norm(qb.reshape(b, s, DIFF_HEADS, 2, HEAD_DIM), qn_b)
    kb = rms_norm(kb.reshape(b, s, DIFF_HEADS, 2, HEAD_DIM), kn_b)
    q1, q2 = rope(qb[:, :, :, 0], positions), rope(qb[:, :, :, 1], positions)
    k1, k2 = rope(kb[:, :, :, 0], positions), rope(kb[:, :, :, 1], positions)
    f32 = jnp.float32
    lam = (jnp.exp(jnp.sum(lam_q1.astype(f32) * lam_k1.astype(f32)))
           - jnp.exp(jnp.sum(lam_q2.astype(f32) * lam_k2.astype(f32))) + lam_init)
    ob = differential_attention(q1, q2, k1, k2, vb.reshape(b, s, DIFF_HEADS, 2 * HEAD_DIM), lam)
    ob = rms_norm(ob, subln_g) * (1.0 - lam_init)
    branch_b = ob.reshape(b, s, DIFF_V_WIDTH) @ w_proj_b

    gates = jax.nn.sigmoid((gl + b_gate).astype(f32)).astype(x.dtype).reshape(b, s, N_BRANCHES, d)
    mixed = gates[:, :, 0] * branch_a + gates[:, :, 1] * branch_b
    x = x + g1 * (mixed @ w_out)

    h2 = rms_norm(x, norm2_g) * (1 + sc2) + sh2
    x = x + g2 * peer_ffn(h2, w_query, sub_keys, expert_u, expert_v)
    return x


def setup_inputs(seed: int = 0) -> dict:
    key = jax.random.key(seed)
    ks = jax.random.split(key, 32)
    f32 = jnp.float32

    def nrm(k, shape, scale):
        return jax.random.normal(k, shape, f32) * scale

    def gain(k, shape):
        return 1.0 + 0.02 * jax.random.normal(k, shape, f32)

    L = DEPTH
    D = D_MODEL
    return {
        "x": jax.random.normal(ks[0], (BATCH, SEQ, D), f32),
        "c": jax.random.normal(ks[1], (BATCH, D), f32),
        "positions": jnp.broadcast_to(jnp.arange(SEQ, dtype=jnp.int32), (BATCH, SEQ)),
        "w_ada": nrm(ks[2], (L, D, 6 * D), 0.5 * D ** -0.5),
        "b_ada": nrm(ks[3], (L, 6 * D), 0.02),
        "norm1_g": gain(ks[4], (L, D)),
        "w_in": nrm(ks[5], (L, D, IN_WIDTH), D ** -0.5),
        "b_gate": nrm(ks[6], (L, GATE_WIDTH), 0.02),
        "qn_a": gain(ks[7], (L, HEAD_DIM)),
        "kn_a": gain(ks[8], (L, HEAD_DIM)),
        "w_proj_a": nrm(ks[9], (L, DIL_OUT, D), DIL_OUT ** -0.5),
        "qn_b": gain(ks[10], (L, HEAD_DIM)),
        "kn_b": gain(ks[11], (L, HEAD_DIM)),
        "lam_q1": nrm(ks[12], (L, HEAD_DIM), 0.1),
        "lam_k1": nrm(ks[13], (L, HEAD_DIM), 0.1),
        "lam_q2": nrm(ks[14], (L, HEAD_DIM), 0.1),
        "lam_k2": nrm(ks[15], (L, HEAD_DIM), 0.1),
        "subln_g": gain(ks[16], (L, 2 * HEAD_DIM)),
        "w_proj_b": nrm(ks[17], (L, DIFF_V_WIDTH, D), DIFF_V_WIDTH ** -0.5),
        "w_out": nrm(ks[18], (L, D, D), D ** -0.5),
        "norm2_g": gain(ks[19], (L, D)),
        "w_query": nrm(ks[20], (L, D, PEER_HEADS * PEER_QUERY_DIM), D ** -0.5),
        "sub_keys": nrm(ks[21], (L, PEER_HEADS, 2, PEER_N_KEYS, PEER_HALF), PEER_HALF ** -0.5),
        "expert_u": nrm(ks[22], (L, PEER_N_EXPERTS, D), D ** -0.5),
        "expert_v": nrm(ks[23], (L, PEER_N_EXPERTS, D), PEER_HEADS ** -0.5),
    }


def reference(x, c, positions, w_ada, b_ada, norm1_g, w_in, b_gate, qn_a, kn_a, w_proj_a,
              qn_b, kn_b, lam_q1, lam_k1, lam_q2, lam_k2, subln_g, w_proj_b, w_out,
              norm2_g, w_query, sub_keys, expert_u, expert_v):
    for l in range(DEPTH):
        lam_init = 0.8 - 0.6 * math.exp(-0.3 * l)
        x = hybrid_layer(x, c, positions, lam_init, w_ada[l], b_ada[l], norm1_g[l], w_in[l],
                         b_gate[l], qn_a[l], kn_a[l], w_proj_a[l], qn_b[l], kn_b[l],
                         lam_q1[l], lam_k1[l], lam_q2[l], lam_k2[l], subln_g[l], w_proj_b[l],
                         w_out[l], norm2_g[l], w_query[l], sub_keys[l], expert_u[l], expert_v[l])
    return x
```

```python
import os
import numpy as np
from contextlib import ExitStack
import concourse.bass as bass
import concourse.mybir as mybir
from concourse.bass_utils import run_bass_kernel_spmd

F32 = mybir.dt.float32
BF16 = mybir.dt.bfloat16
I32 = mybir.dt.int32
AF = mybir.ActivationFunctionType
ALU = mybir.AluOpType
AX = mybir.AxisListType

S = 4096
D = 1024
NT = 32
EPS = 1e-6
LAM_INIT = 0.2
DIL = (1, 4, 16)
N_DMA_SEMS = 8
QUEUES = ("sp", "pool", "act")
TWO_PI = float(2 * np.pi)
C1 = 6.28125
C2 = float(2 * np.pi - 6.28125)


class Prog:
    def __init__(self):
        self.ops = []
        self.last_writer = {}
        self.readers = {}

    def add(self, eng, fn, reads=(), writes=(), dma=False):
        idx = len(self.ops)
        deps = set()
        raw = set()
        for t in reads:
            w = self.last_writer.get(t)
            if w is not None:
                deps.add(w)
                raw.add(w)
        for t in writes:
            w = self.last_writer.get(t)
            if w is not None:
                deps.add(w)
            for r in self.readers.get(t, ()):
                deps.add(r)
        op = dict(eng=eng, fn=fn, dma=dma, deps=deps, raw=raw, signal=False)
        self.ops.append(op)
        for t in reads:
            self.readers.setdefault(t, []).append(idx)
        for t in writes:
            self.last_writer[t] = idx
            self.readers[t] = []
        return idx

    def emit(self, nc, semsets, phase_sem, phase_idx, fin):
        sems, cnt = semsets[phase_idx % 3]
        ops = self.ops
        for op in ops:
            keep = set()
            for d in op["deps"]:
                p = ops[d]
                if p["dma"] or op["dma"] or p["eng"] != op["eng"]:
                    keep.add(d)
                elif op["eng"] != "pe" and d in op["raw"]:
                    keep.add(d)
            op["deps"] = keep
            for d in keep:
                ops[d]["signal"] = True
        dma_k = {}
        prev_slot = {}
        for op in ops:
            if op["dma"]:
                q = op["eng"]
                k = dma_k.get(q, 0)
                dma_k[q] = k + 1
                key = ("dma", q, k % N_DMA_SEMS)
                cnt[key] = cnt.get(key, 0) + 16
                op["prev"] = prev_slot.get(key)
                op["sig"] = (key, cnt[key])
                prev_slot[key] = op["sig"]
            elif op["signal"]:
                key = op["eng"]
                cnt[key] = cnt.get(key, 0) + 1
                op["sig"] = (key, cnt[key])

        def semof(key):
            if isinstance(key, tuple):
                return sems["dma_" + key[1]][key[2]]
            return sems[key]

        streams = {}
        for op in ops:
            streams.setdefault(op["eng"], []).append(op)

        def run_stream(engname, eng):
            if phase_idx > 0:
                eng.wait_ge(phase_sem, 19 * phase_idx)
            known = {}
            for op in streams.get(engname, []):
                waits = {}
                for d in op["deps"]:
                    key, val = ops[d]["sig"]
                    if waits.get(key, 0) < val:
                        waits[key] = val
                if op["dma"] and op["prev"] is not None:
                    key, val = op["prev"]
                    if waits.get(key, 0) < val:
                        waits[key] = val
                for key, val in waits.items():
                    if known.get(key, 0) >= val:
                        continue
                    eng.wait_ge(semof(key), val)
                    known[key] = val
                ins = op["fn"](eng)
                if "sig" in op:
                    ins.then_inc(semof(op["sig"][0]), 16 if op["dma"] else 1)
            if engname == "sp":
                for key, val in cnt.items():
                    if isinstance(key, tuple):
                        eng.wait_ge(semof(key), val)
                eng.dma_start(out=fin["d1"][:], in_=fin["d0"][:]).then_inc(phase_sem, 16)
            elif engname == "pe":
                pass
            elif engname == "act":
                eng.activation(out=fin["act"][:], in_=fin["d0"][0:1, 0:1].to_broadcast([1, 1]) if False else fin["act"][:], func=AF.Copy).then_inc(phase_sem, 1)
            else:
                eng.memset(fin[engname][:], 0.0).then_inc(phase_sem, 1)

        with nc.Block() as block:
            @block.sync
            def _(e):
                run_stream("sp", e)

            @block.tensor
            def _(e):
                run_stream("pe", e)

            @block.scalar
            def _(e):
                run_stream("act", e)

            @block.vector
            def _(e):
                run_stream("dve", e)

            @block.gpsimd
            def _(e):
                run_stream("pool", e)


class K:
    def __init__(self, P):
        self.P = P

    def dma(self, q, out, in_, r=(), w=(), **kw):
        self.P.add(q, lambda e: e.dma_start(out=out, in_=in_, **kw), r, w, dma=True)

    def mm(self, out, lhsT, rhs, start, stop, r=(), w=()):
        self.P.add("pe", lambda e: e.matmul(out, lhsT=lhsT, rhs=rhs, start=start, stop=stop), r, w)

    def tr(self, out, in_, ident, r=(), w=()):
        self.P.add("pe", lambda e: e.transpose(out=out, in_=in_, identity=ident), r, w)

    def act(self, out, in_, func, r=(), w=(), **kw):
        self.P.add("act", lambda e: e.activation(out=out, in_=in_, func=func, **kw), r, w)

    def tt(self, eng, out, in0, in1, op, r=(), w=()):
        self.P.add(eng, lambda e: e.tensor_tensor(out=out, in0=in0, in1=in1, op=op), r, w)

    def ts(self, eng, out, in0, s1, s2, op0, op1=None, r=(), w=()):
        if op1 is None:
            self.P.add(eng, lambda e: e.tensor_scalar(out=out, in0=in0, scalar1=s1, scalar2=None, op0=op0), r, w)
        else:
            self.P.add(eng, lambda e: e.tensor_scalar(out=out, in0=in0, scalar1=s1, scalar2=s2, op0=op0, op1=op1), r, w)

    def stt(self, eng, out, in0, scalar, in1, op0, op1, r=(), w=()):
        self.P.add(eng, lambda e: e.scalar_tensor_tensor(out=out, in0=in0, scalar=scalar, in1=in1, op0=op0, op1=op1), r, w)

    def cp(self, eng, out, in_, r=(), w=()):
        self.P.add(eng, lambda e: e.tensor_copy(out=out, in_=in_), r, w)

    def red(self, eng, out, in_, r=(), w=(), op=None):
        op = op or ALU.add
        self.P.add(eng, lambda e: e.tensor_reduce(out=out, in_=in_, axis=AX.X, op=op), r, w)

    def recip(self, out, in_, r=(), w=()):
        self.P.add("dve", lambda e: e.reciprocal(out=out, in_=in_), r, w)

    def memset(self, eng, out, val, r=(), w=()):
        self.P.add(eng, lambda e: e.memset(out, val), r, w)

    def max8(self, out, in_, r=(), w=()):
        self.P.add("dve", lambda e: e.max(out=out, in_=in_), r, w)

    def mrep(self, out, rep, vals, r=(), w=()):
        self.P.add("dve", lambda e: e.match_replace(out=out, in_to_replace=rep, in_values=vals, imm_value=-1e30), r, w)


def bc(ap, shape):
    return ap.to_broadcast(list(shape))


def build_program(debug=False):
    nc = bass.Bass("TRN2", target_bir_lowering=False)

    def din(name, shape, dt=F32):
        return nc.dram_tensor(name, list(shape), dt, kind="ExternalInput").ap()

    def dscr(name, shape, dt):
        kind = "ExternalOutput" if (debug and name in DEBUG_OUT) else "Internal"
        return nc.dram_tensor(name, list(shape), dt, kind=kind).ap()

    x = din("x", [S, D])
    pos = din("pos", [S], I32)
    cT = din("cT", [128, 8])
    w_ada = din("w_ada", [D, 6 * D])
    b_adaT = din("b_adaT", [128, 48])
    n1gT = din("n1gT", [128, 8])
    n2gT = din("n2gT", [128, 8])
    w_in = din("w_in", [D, 7424])
    b_gate_bc = din("b_gate_bc", [128, 2048])
    gainA = din("gainA", [128, 8, 64])
    gainQB = din("gainQB", [128, 8, 64])
    gainKB = din("gainKB", [128, 8, 64])
    lam_bc = din("lam_bc", [128, 4, 64])
    subln_bc = din("subln_bc", [128, 128])
    invf_bc = din("invf_bc", [128, 32])
    halfpi = din("halfpi", [128, 64])
    mask_std = din("mask_std", [128, 256])
    mask_bnd = din("mask_bnd", [128, 256])
    w_pa = din("w_pa", [256, D])
    w_pb = din("w_pb", [D, D])
    w_out = din("w_out", [D, D])
    wqT = din("wqT", [16, 128, D])
    skT = din("skT", [16, 128, 128])
    UT = din("UT", [16384, 1024])
    Vx = din("Vx", [16384, 1024])
    out = nc.dram_tensor("out", [S, D], F32, kind="ExternalOutput").ap()

    QTA = dscr("QTA", [3, 2, 128, S], BF16)
    KTA = dscr("KTA", [3, 2, 128, S], BF16)
    VA = dscr("VA", [3, S, 256], BF16)
    QTB = dscr("QTB", [8, 128, S], BF16)
    KTB = dscr("KTB", [8, 128, S], BF16)
    VB = dscr("VB", [S, 1024], BF16)
    GT = dscr("GT", [S, 2048], BF16)
    ND = dscr("ND", [S, 12, 65], F32)
    OBT = dscr("OBT", [8, 128, S], BF16)
    X1 = dscr("X1", [S, D], F32)
    H2T = dscr("H2T", [8, 128, S], BF16)
    PS = dscr("PS", [S, 8, 2, 128], F32)
    CF = dscr("CF", [S, 8], F32)
    UTb = dscr("UTb", [16384, 1024], BF16)
    Vb = dscr("Vb", [16384, 1024], BF16)

    with ExitStack() as top:
        def sbt(es, name, shape, dt):
            return es.enter_context(nc.sbuf_tensor(name, list(shape), dt))

        def pst(es, name, shape, dt):
            return es.enter_context(nc.psum_tensor(name, list(shape), dt))

        semsets = []
        for ph in range(3):
            ss = {}
            for e in ("pe", "act", "dve", "pool"):
                ss[e] = top.enter_context(nc.semaphore(f"s{ph}_{e}"))
            for q in QUEUES:
                ss["dma_" + q] = [top.enter_context(nc.semaphore(f"d{ph}_{q}_{i}")) for i in range(N_DMA_SEMS)]
            semsets.append((ss, {}))
        phase_sem = top.enter_context(nc.semaphore("phase"))
        conv_sem = top.enter_context(nc.semaphore("conv"))

        identb = sbt(top, "identb", [128, 128], BF16)
        identf = sbt(top, "identf", [128, 128], F32)
        onesf = sbt(top, "onesf", [128, 128], F32)
        s1T = sbt(top, "s1T", [128, 8], F32)
        sh1T = sbt(top, "sh1T", [128, 8], F32)
        s2T = sbt(top, "s2T", [128, 8], F32)
        sh2T = sbt(top, "sh2T", [128, 8], F32)
        G1row = sbt(top, "G1row", [128, D], F32)
        G2row = sbt(top, "G2row", [128, D], F32)
        neglam = sbt(top, "neglam", [128, 1], F32)
        sgain = sbt(top, "sgain", [128, 128], F32)
        epsb = sbt(top, "epsb", [128, 1], F32)
        fin = dict(
            d0=sbt(top, "fin_d0", [1, 16], F32), d1=sbt(top, "fin_d1", [1, 16], F32),
            act=sbt(top, "fin_act", [128, 1], F32), dve=sbt(top, "fin_dve", [128, 1], F32),
            pool=sbt(top, "fin_pool", [128, 1], F32), idb=identb,
        )
        psn = [0]

        def alloc_psum(es, nf=6, nb=2):
            psn[0] += 1
            bk = [pst(es, f"bank{psn[0]}_{i}", [128, 512], F32) for i in range(nf)]
            bt = [pst(es, f"bankT{psn[0]}_{i}", [128, 1024], BF16) for i in range(nb)]
            return bk, bt

        with ExitStack() as es:
            P = Prog()
            k = K(P)
            banks, bankT = alloc_psum(es)
            cTs = sbt(es, "cTs", [128, 8], F32)
            scs = sbt(es, "scs", [128, 8], F32)
            wada = [sbt(es, f"wada{i}", [128, 6 * D], F32) for i in range(2)]
            badas = sbt(es, "badas", [128, 48], F32)
            modT = sbt(es, "modT", [128, 48], F32)
            n1s = sbt(es, "n1s", [128, 8], F32)
            n2s = sbt(es, "n2s", [128, 8], F32)
            lams = sbt(es, "lams", [128, 4, 64], F32)
            lprod = sbt(es, "lprod", [128, 2, 64], F32)
            lsum = sbt(es, "lsum", [128, 2], F32)
            lexp = sbt(es, "lexp", [128, 2], F32)
            ltmp = sbt(es, "ltmp", [128, 1], F32)
            subs = sbt(es, "subs", [128, 128], F32)
            dg = [sbt(es, f"dg{i}", [128, 128], F32) for i in range(2)]
            NCONV = 16
            rows = 16384 // NCONV
            for i in range(NCONV):
                for (src, dst) in ((UT, UTb), (Vx, Vb)):
                    P.add("pool", (lambda s_, d_, i_: (lambda e: e.dma_start(out=d_[i_ * rows:(i_ + 1) * rows, :], in_=s_[i_ * rows:(i_ + 1) * rows, :]).then_inc(conv_sem, 16)))(src, dst, i))
            k.dma("sp", cTs[:], cT, w=["cTs"])
            k.dma("sp", badas[:], b_adaT, w=["badas"])
            k.dma("sp", n1s[:], n1gT, w=["n1s"])
            k.dma("sp", n2s[:], n2gT, w=["n2s"])
            k.dma("sp", lams[:], lam_bc, w=["lams"])
            k.dma("sp", subs[:], subln_bc, w=["subs"])
            k.memset("dve", identf[:], 1.0, w=["identf"])
            P.add("pool", lambda e: e.affine_select(out=identf[:], in_=identf[:], pattern=[[-1, 128]], compare_op=ALU.is_equal,
                                                   fill=0.0, base=0, channel_multiplier=1), ["identf"], ["identf"])
            k.cp("dve", identb[:], identf[:], r=["identf"], w=["identb"])
            k.memset("dve", onesf[:], 1.0, w=["onesf"])
            k.memset("dve", epsb[:], EPS, w=["epsb"])
            k.memset("dve", fin["d0"][:], 0.0, w=["find0"])
            k.act(scs[:], cTs[:], AF.Silu, r=["cTs"], w=["scs"])
            modps = banks[0]
            for kc in range(8):
                b = kc % 2
                k.dma("sp", wada[b][:], w_ada[kc * 128:(kc + 1) * 128, :], w=[f"wada{b}"])
                for f in range(48):
                    k.mm(modps[:, f:f + 1], wada[b][:, f * 128:(f + 1) * 128], scs[:, kc:kc + 1], kc == 0 and f == 0, kc == 7,
                         r=[f"wada{b}", "scs"], w=["modps"])
            k.tt("dve", modT[:], modps[:, 0:48], badas[:], ALU.add, r=["modps", "badas"], w=["modT"])
            k.stt("dve", s1T[:], modT[:, 8:16], 1.0, n1s[:], ALU.add, ALU.mult, r=["modT", "n1s"], w=["s1T"])
            k.cp("dve", sh1T[:], modT[:, 0:8], r=["modT"], w=["sh1T"])
            k.stt("dve", s2T[:], modT[:, 32:40], 1.0, n2s[:], ALU.add, ALU.mult, r=["modT", "n2s"], w=["s2T"])
            k.cp("dve", sh2T[:], modT[:, 24:32], r=["modT"], w=["sh2T"])
            for (row, base, bk) in ((G1row, 16, 1), (G2row, 40, 3)):
                for c in range(8):
                    b = c % 2
                    k.ts("dve", dg[b][:], identf[:], modT[:, base + c:base + c + 1], None, ALU.mult, r=["modT", "identf"], w=[f"dg{b}"])
                    bank = banks[bk + c // 4]
                    k.mm(bank[:, (c % 4) * 128:(c % 4 + 1) * 128], onesf[:], dg[b][:], True, True, r=[f"dg{b}", "onesf"], w=[f"gr{bk + c // 4}"])
                for hh in range(2):
                    k.cp("dve", row[:, hh * 512:(hh + 1) * 512], banks[bk + hh][:], r=[f"gr{bk + hh}"], w=[f"grow{base}"])
            k.tt("dve", lprod[:, 0, :], lams[:, 0, :], lams[:, 1, :], ALU.mult, r=["lams"], w=["lprod0"])
            k.tt("dve", lprod[:, 1, :], lams[:, 2, :], lams[:, 3, :], ALU.mult, r=["lams"], w=["lprod1"])
            k.red("dve", lsum[:], lprod[:], r=["lprod0", "lprod1"], w=["lsum"])
            k.act(lexp[:], lsum[:], AF.Exp, r=["lsum"], w=["lexp"])
            k.tt("dve", ltmp[:], lexp[:, 1:2], lexp[:, 0:1], ALU.subtract, r=["lexp"], w=["ltmp"])
            k.ts("dve", neglam[:], ltmp[:], -LAM_INIT, None, ALU.add, r=["ltmp"], w=["neglam"])
            k.ts("dve", sgain[:], subs[:], 1.0 - LAM_INIT, None, ALU.mult, r=["subs"], w=["sgain"])
            P.emit(nc, semsets, phase_sem, 0, fin)

        with ExitStack() as es:
            P = Prog()
            k = K(P)
            banks, bankT = alloc_psum(es)
            hT = sbt(es, "hT", [128, 8, 2048], BF16)
            xt = [sbt(es, f"xt{i}", [128, D], F32) for i in range(2)]
            sq = [sbt(es, f"sq{i}", [128, D], F32) for i in range(2)]
            xn = [sbt(es, f"xn{i}", [128, D], BF16) for i in range(2)]
            ssx = [sbt(es, f"ssx{i}", [128, 1], F32) for i in range(2)]
            rsx = [sbt(es, f"rsx{i}", [128, 1], F32) for i in range(2)]
            gA = sbt(es, "gA", [128, 8, 64], F32)
            gQB = sbt(es, "gQB", [128, 8, 64], F32)
            gKB = sbt(es, "gKB", [128, 8, 64], F32)
            bgs = sbt(es, "bgs", [128, 2048], F32)
            invfs = sbt(es, "invfs", [128, 32], F32)
            hps = sbt(es, "hps", [128, 64], F32)
            posi = sbt(es, "posi", [128, 16], I32)
            posf = sbt(es, "posf", [128, 16], F32)
            tabs = [sbt(es, f"tab{i}", [128, 16, 64], F32) for i in range(3)]
            arg = sbt(es, "arg", [128, 16, 64], F32)
            argk = sbt(es, "argk", [128, 16, 64], F32)
            argi = sbt(es, "argi", [128, 16, 64], I32)
            wb = [sbt(es, f"wb{i}", [128, 8, 512], BF16) for i in range(2)]
            sqq = [sbt(es, f"sqq{i}", [128, 8, 64], F32) for i in range(2)]
            ssq = [sbt(es, f"ssq{i}", [128, 8], F32) for i in range(2)]
            rsq = [sbt(es, f"rsq{i}", [128, 8], F32) for i in range(2)]
            qn = [sbt(es, f"qn{i}", [128, 8, 64], F32) for i in range(2)]
            qg = [sbt(es, f"qg{i}", [128, 8, 2, 32], F32) for i in range(2)]
            rt = [sbt(es, f"rt{i}", [128, 4, 8, 32], F32) for i in range(2)]
            qr = [sbt(es, f"qr{i}", [128, 8, 2, 32], BF16) for i in range(2)]
            stg = [sbt(es, f"stg{i}", [128, 4, 128], BF16) for i in range(2)]
            vst = [sbt(es, f"vst{i}", [128, 512], BF16) for i in range(2)]
            gpre = [sbt(es, f"gpre{i}", [128, 512], F32) for i in range(2)]

            k.dma("sp", gA[:], gainA, w=["gA"])
            k.dma("sp", gQB[:], gainQB, w=["gQB"])
            k.dma("sp", gKB[:], gainKB, w=["gKB"])
            k.dma("sp", bgs[:], b_gate_bc, w=["bgs"])
            k.dma("sp", invfs[:], invf_bc, w=["invfs"])
            k.dma("sp", hps[:], halfpi, w=["hps"])

            pos_pat = [
                pos.rearrange("(st u m) -> st m u", st=2, u=16, m=128),
                pos.rearrange("(st blk m r) -> st m blk r", st=2, blk=4, m=128, r=4),
                pos.rearrange("(st m r) -> st m r", st=2, m=128, r=16),
            ]

            def tokcols(pat, u):
                if pat == 0:
                    return slice(u * 128, (u + 1) * 128)
                if pat == 1:
                    blk, r = u // 4, u % 4
                    return slice(blk * 512 + r, blk * 512 + 512, 4)
                return slice(u, 2048, 16)

            def perm0(pat, st, u):
                if pat == 0:
                    return st * 2048 + u * 128
                if pat == 1:
                    blk, r = u // 4, u % 4
                    return r * 1024 + st * 512 + blk * 128
                return u * 256 + st * 128

            jobs = []
            for g in range(3):
                jobs.append((g, "qk", [(256 * g, 256), (768 + 256 * g, 256)], ("A", g)))
                jobs.append((g, "v", [(1536 + 256 * g, 256)], ("A", g)))
            for j in range(2):
                jobs.append((0, "qk", [(2304 + 512 * j, 512)], ("QB", j)))
                jobs.append((0, "qk", [(3328 + 512 * j, 512)], ("KB", j)))
                jobs.append((0, "v", [(4352 + 512 * j, 512)], ("B", j)))
            for j in range(4):
                jobs.append((0, "gate", [(5376 + 512 * j, 512)], ("G", j)))

            unit = 0
            for st in range(2):
                for u in range(16):
                    b = u % 2
                    t0 = st * 2048 + u * 128
                    k.dma("sp", xt[b][:], x[t0:t0 + 128, :], w=[f"xt{b}"])
                    k.act(sq[b][:], xt[b][:], AF.Square, r=[f"xt{b}"], w=[f"sq{b}"])
                    k.red("dve", ssx[b][:], sq[b][:], r=[f"sq{b}"], w=[f"ssx{b}"])
                    k.act(rsx[b][:], ssx[b][:], AF.Sqrt, r=[f"ssx{b}", "epsb"], w=[f"rsx{b}"], scale=1.0 / D, bias=epsb[:])
                    k.recip(rsx[b][:], rsx[b][:], r=[f"rsx{b}"], w=[f"rsx{b}"])
                    k.ts("dve", xn[b][:], xt[b][:], rsx[b][:, 0:1], None, ALU.mult, r=[f"xt{b}", f"rsx{b}"], w=[f"xn{b}"])
                    for c in range(8):
                        k.tr(bankT[b][:, c * 128:(c + 1) * 128], xn[b][:, c * 128:(c + 1) * 128], identb[:], r=[f"xn{b}"], w=[f"bT{b}"])
                    for c in range(8):
                        k.act(hT[:, c, u * 128:(u + 1) * 128], bankT[b][:, c * 128:(c + 1) * 128], AF.Identity,
                              r=[f"bT{b}"], w=[f"hT{u}"], scale=s1T[:, c:c + 1], bias=sh1T[:, c:c + 1])
                for pat in range(3):
                    if pat == 0:
                        k.dma("sp", posi[:], pos_pat[0][st], w=["posi"], allow_slow_non_contiguous=True)
                    elif pat == 1:
                        k.dma("sp", posi[:].rearrange("p (a b) -> p a b", a=4), pos_pat[1][st], w=["posi"])
                    else:
                        k.dma("sp", posi[:], pos_pat[2][st], w=["posi"])
                    k.cp("dve", posf[:], posi[:], r=["posi"], w=["posf"])
                    k.tt("dve", arg[:, :, 0:32], bc(posf[:].unsqueeze(2), [128, 16, 32]), bc(invfs[:].unsqueeze(1), [128, 16, 32]), ALU.mult,
                         r=["posf", "invfs"], w=["arg"])
                    k.cp("dve", arg[:, :, 32:64], arg[:, :, 0:32], r=["arg"], w=["arg"])
                    k.tt("dve", arg[:], arg[:], bc(hps[:].unsqueeze(1), [128, 16, 64]), ALU.add, r=["arg", "hps"], w=["arg"])
                    k.ts("dve", argk[:], arg[:], 1.0 / TWO_PI, None, ALU.mult, r=["arg"], w=["argk"])
                    k.cp("dve", argi[:], argk[:], r=["argk"], w=["argi"])
                    k.cp("dve", argk[:], argi[:], r=["argi"], w=["argk"])
                    k.stt("dve", arg[:], argk[:], -C1, arg[:], ALU.mult, ALU.add, r=["argk", "arg"], w=["arg"])
                    k.stt("dve", arg[:], argk[:], -C2, arg[:], ALU.mult, ALU.add, r=["argk", "arg"], w=["arg"])
                    k.ts("dve", argk[:], arg[:], float(np.pi), -TWO_PI, ALU.is_gt, ALU.mult, r=["arg"], w=["argk"])
                    k.tt("dve", arg[:], arg[:], argk[:], ALU.add, r=["arg", "argk"], w=["arg"])
                    k.ts("dve", argk[:], arg[:], float(-np.pi), TWO_PI, ALU.is_lt, ALU.mult, r=["arg"], w=["argk"])
                    k.tt("dve", arg[:], arg[:], argk[:], ALU.add, r=["arg", "argk"], w=["arg"])
                    k.act(tabs[pat][:], arg[:], AF.Sin, r=["arg"], w=[f"tab{pat}"])
                hall = [f"hT{u}" for u in range(16)]
                for ji, (pat, kind, cols, extra) in enumerate(jobs):
                    wbi = ji % 2
                    off = 0
                    for (c0, ncol) in cols:
                        k.dma("pool", wb[wbi][:, :, off:off + ncol], w_in[:, c0:c0 + ncol].rearrange("(c p) n -> p c n", p=128), w=[f"wb{wbi}"])
                        off += ncol
                    ncols = off
                    for u in range(16):
                        ub = unit % 2
                        unit += 1
                        bankP = banks[ub]
                        tc = tokcols(pat, u)
                        rtok = hall if pat else [f"hT{u}"]
                        for c in range(8):
                            k.mm(bankP[:, 0:ncols], hT[:, c, tc], wb[wbi][:, c, 0:ncols], c == 0, c == 7, r=rtok + [f"wb{wbi}"], w=[f"bP{ub}"])
                        p0 = perm0(pat, st, u)
                        if kind == "qk":
                            gt = {"A": gA, "QB": gQB, "KB": gKB}[extra[0]]
                            gtn = {"A": "gA", "QB": "gQB", "KB": "gKB"}[extra[0]]
                            pv = bankP[:].rearrange("p (h c) -> p h c", h=8)
                            k.act(sqq[ub][:], pv, AF.Square, r=[f"bP{ub}"], w=[f"sqq{ub}"])
                            k.red("dve", ssq[ub][:], sqq[ub][:], r=[f"sqq{ub}"], w=[f"ssq{ub}"])
                            k.act(rsq[ub][:], ssq[ub][:], AF.Sqrt, r=[f"ssq{ub}", "epsb"], w=[f"rsq{ub}"], scale=1.0 / 64, bias=epsb[:])
                            k.recip(rsq[ub][:], rsq[ub][:], r=[f"rsq{ub}"], w=[f"rsq{ub}"])
                            k.tt("dve", qn[ub][:], pv, bc(rsq[ub][:].unsqueeze(2), [128, 8, 64]), ALU.mult, r=[f"bP{ub}", f"rsq{ub}"], w=[f"qn{ub}"])
                            k.tt("pool", qg[ub][:].rearrange("p h a f -> p h (a f)"), qn[ub][:], gt[:], ALU.mult, r=[f"qn{ub}", gtn], w=[f"qg{ub}"])
                            sinb = bc(tabs[pat][:, u, 0:32].unsqueeze(1), [128, 8, 32])
                            cosb = bc(tabs[pat][:, u, 32:64].unsqueeze(1), [128, 8, 32])
                            x1 = qg[ub][:, :, 0, :]
                            x2 = qg[ub][:, :, 1, :]
                            tn = f"tab{pat}"
                            k.tt("dve", rt[ub][:, 0], x1, cosb, ALU.mult, r=[f"qg{ub}", tn], w=[f"rt0{ub}"])
                            k.tt("dve", rt[ub][:, 1], x2, sinb, ALU.mult, r=[f"qg{ub}", tn], w=[f"rt1{ub}"])
                            k.tt("dve", qr[ub][:, :, 0, :], rt[ub][:, 0], rt[ub][:, 1], ALU.subtract, r=[f"rt0{ub}", f"rt1{ub}"], w=[f"qr{ub}a"])
                            k.tt("pool", rt[ub][:, 2], x2, cosb, ALU.mult, r=[f"qg{ub}", tn], w=[f"rt2{ub}"])
                            k.tt("pool", rt[ub][:, 3], x1, sinb, ALU.mult, r=[f"qg{ub}", tn], w=[f"rt3{ub}"])
                            k.tt("pool", qr[ub][:, :, 1, :], rt[ub][:, 2], rt[ub][:, 3], ALU.add, r=[f"rt2{ub}", f"rt3{ub}"], w=[f"qr{ub}b"])
                            qflat = qr[ub][:].rearrange("p h a f -> p (h a f)")
                            for q4 in range(4):
                                k.tr(bankT[ub][:, q4 * 128:(q4 + 1) * 128], qflat[:, q4 * 128:(q4 + 1) * 128], identb[:],
                                     r=[f"qr{ub}a", f"qr{ub}b"], w=[f"bT{ub}"])
                            k.act(stg[ub][:].rearrange("p a t -> p (a t)"), bankT[ub][:, 0:512], AF.Copy, r=[f"bT{ub}"], w=[f"stg{ub}"])
                            if extra[0] == "A":
                                g = extra[1]
                                k.dma("sp", QTA[g, :, :, p0:p0 + 128].rearrange("a c t -> c a t"), stg[ub][:, 0:2, :], r=[f"stg{ub}"], w=["QTA"])
                                k.dma("sp", KTA[g, :, :, p0:p0 + 128].rearrange("a c t -> c a t"), stg[ub][:, 2:4, :], r=[f"stg{ub}"], w=["KTA"])
                            else:
                                dst = QTB if extra[0] == "QB" else KTB
                                j = extra[1]
                                k.dma("sp", dst[4 * j:4 * j + 4, :, p0:p0 + 128].rearrange("a c t -> c a t"), stg[ub][:], r=[f"stg{ub}"], w=[extra[0]])
                        elif kind == "v":
                            k.act(vst[ub][:, 0:ncols], bankP[:, 0:ncols], AF.Copy, r=[f"bP{ub}"], w=[f"vst{ub}"])
                            if extra[0] == "A":
                                k.dma("sp", VA[extra[1], p0:p0 + 128, :], vst[ub][:, 0:256], r=[f"vst{ub}"], w=["VA"])
                            else:
                                j = extra[1]
                                k.dma("sp", VB[p0:p0 + 128, 512 * j:512 * j + 512], vst[ub][:], r=[f"vst{ub}"], w=["VB"])
                        else:
                            j = extra[1]
                            k.tt("dve", gpre[ub][:], bankP[:], bgs[:, 512 * j:512 * j + 512], ALU.add, r=[f"bP{ub}", "bgs"], w=[f"gpre{ub}"])
                            k.act(vst[ub][:], gpre[ub][:], AF.Sigmoid, r=[f"gpre{ub}"], w=[f"vst{ub}"])
                            k.dma("sp", GT[p0:p0 + 128, 512 * j:512 * j + 512], vst[ub][:], r=[f"vst{ub}"], w=["GT"])
            P.emit(nc, semsets, phase_sem, 1, fin)

        with ExitStack() as es:
            P = Prog()
            k = K(P)
            banks, bankT = alloc_psum(es)
            KTs = [sbt(es, f"KTs{i}", [128, S + 128], BF16) for i in range(2)]
            QTs = [sbt(es, f"QTs{i}", [128, S + 128], BF16) for i in range(2)]
            V1 = [sbt(es, f"V1_{i}", [128, 33, 65], BF16) for i in range(2)]
            mstd = sbt(es, "mstd", [128, 256], F32)
            mbnd = sbt(es, "mbnd", [128, 256], F32)
            Eb = [sbt(es, f"Eb{i}", [128, 256], BF16) for i in range(2)]
            Pm = [sbt(es, f"Pm{i}", [128, 256], BF16) for i in range(2)]
            osb = [sbt(es, f"osb{i}", [128, 65], F32) for i in range(4)]
            k.dma("sp", mstd[:], mask_std, w=["mstd"])
            k.dma("sp", mbnd[:], mask_bnd, w=["mbnd"])
            for i in range(2):
                k.memset("pool", KTs[i][:, 0:64], 0.0, w=[f"KTs{i}"])
                k.memset("pool", KTs[i][:, S + 64:S + 128], 0.0, w=[f"KTs{i}"])
                k.memset("pool", QTs[i][:, 0:64], 0.0, w=[f"QTs{i}"])
                k.memset("pool", QTs[i][:, S + 64:S + 128], 0.0, w=[f"QTs{i}"])
                k.memset("pool", V1[i][:], 0.0, w=[f"V1_{i}"])
                k.memset("pool", V1[i][:, :, 64:65], 1.0, w=[f"V1_{i}"])
            bankSs = [banks[0], banks[2]]
            bankOs = [banks[1], banks[3], banks[4], banks[5]]
            hcount = 0
            ccount = 0
            for g in range(3):
                dil = DIL[g]
                L = S // dil
                for pr in range(2):
                    pb = (g * 2 + pr) % 2
                    k.dma("sp", KTs[pb][:, 64:64 + S], KTA[g, pr], r=["KTA"], w=[f"KTs{pb}"])
                    k.dma("sp", QTs[pb][:, 64:64 + S], QTA[g, pr], r=["QTA"], w=[f"QTs{pb}"])
                    for hh in range(2):
                        hs = pr * 2 + hh
                        bp = 64 * hh
                        vb = hcount % 2
                        hcount += 1
                        vsrc = VA[g, :, hs * 64:(hs + 1) * 64]
                        k.dma("sp", V1[vb][:, 1:32, 0:64], vsrc[64:S - 64, :].rearrange("(j p) c -> p j c", p=128), r=["VA"], w=[f"V1_{vb}"])
                        k.dma("sp", V1[vb][64:128, 0, 0:64], vsrc[0:64, :], r=["VA"], w=[f"V1_{vb}"])
                        k.dma("sp", V1[vb][0:64, 32, 0:64], vsrc[S - 64:S, :], r=["VA"], w=[f"V1_{vb}"])
                        for jc in range(33):
                            sb_ = ccount % 2
                            ccount += 1
                            bnd = (128 * jc) % L == 0
                            qlo = 128 if jc == 0 else 0
                            qhi = 128 if jc == 32 else 256
                            sv = bankSs[sb_][:, 0:256]
                            k.mm(sv[:, qlo:qhi], KTs[pb][bp:bp + 64, 128 * jc:128 * jc + 128],
                                 QTs[pb][bp:bp + 64, 128 * jc - 64 + qlo:128 * jc - 64 + qhi],
                                 True, True, r=[f"KTs{pb}", f"QTs{pb}"], w=[f"bS{sb_}"])
                            k.act(Eb[sb_][:, qlo:qhi], sv[:, qlo:qhi], AF.Exp, r=[f"bS{sb_}"], w=[f"Eb{sb_}"], scale=0.125)
                            mk = mbnd if bnd else mstd
                            k.tt("dve", Pm[sb_][:, qlo:qhi], Eb[sb_][:, qlo:qhi], mk[:, qlo:qhi], ALU.mult, r=[f"Eb{sb_}", "mstd", "mbnd"], w=[f"Pm{sb_}"])
                            for half in range(2):
                                if half * 128 < qlo or half * 128 >= qhi:
                                    continue
                                blk = jc - 1 + half
                                a = blk % 4
                                k.mm(bankOs[a][:, 0:65], Pm[sb_][:, half * 128:half * 128 + 128], V1[vb][:, jc, :],
                                     half == 1, half == 0, r=[f"Pm{sb_}", f"V1_{vb}"], w=[f"bO{a}"])
                            if jc >= 1:
                                blk = jc - 1
                                a = blk % 4
                                k.cp("dve", osb[a][:], bankOs[a][:, 0:65], r=[f"bO{a}"], w=[f"osb{a}"])
                                r_ = (128 * blk) // L
                                j0 = (128 * blk) % L
                                s0 = j0 * dil + r_
                                dst = ND[s0:s0 + 127 * dil + 1:dil, g * 4 + hs, :]
                                k.dma("sp", dst, osb[a][:], r=[f"osb{a}"], w=["ND"])
            P.emit(nc, semsets, phase_sem, 2, fin)

        with ExitStack() as es:
            P = Prog()
            k = K(P)
            banks, bankT = alloc_psum(es)
            KTs = [sbt(es, f"KTd{i}", [128, S], BF16) for i in range(2)]
            QTs = [sbt(es, f"QTd{i}", [128, S], BF16) for i in range(2)]
            V1 = [sbt(es, f"V1d{i}", [128, 32, 129], BF16) for i in range(2)]
            E = [[sbt(es, f"E{m}_{i}", [128, 512], BF16) for i in range(2)] for m in range(2)]
            osb = sbt(es, "osbd", [128, 8, 129], F32)
            rr = sbt(es, "rr", [128, 8], F32)
            o1 = sbt(es, "o1", [128, 4, 128], F32)
            o2 = sbt(es, "o2", [128, 4, 128], F32)
            ssd = sbt(es, "ssd", [128, 4], F32)
            obb = sbt(es, "obb", [128, 4, 128], BF16)
            obT = [sbt(es, f"obT{i}", [128, 512], BF16) for i in range(2)]
            for i in range(2):
                k.memset("pool", V1[i][:, :, 128:129], 1.0, w=[f"V1d{i}"])
            accb = [banks[4], banks[5], banks[3]]
            sbanks = [[banks[0], banks[1]], [banks[2], banks[2]]]
            cc = 0
            for h in range(8):
                hb = h % 2
                k.dma("sp", KTs[hb][:], KTB[h], r=["KB"], w=[f"KTd{hb}"])
                k.dma("sp", QTs[hb][:], QTB[h], r=["QB"], w=[f"QTd{hb}"])
                k.dma("sp", V1[hb][:, :, 0:128], VB[:, h * 128:(h + 1) * 128].rearrange("(j p) c -> p j c", p=128), r=["VB"], w=[f"V1d{hb}"])
                for qb in range(8):
                    for kc in range(32):
                        eb = cc % 2
                        cc += 1
                        for m in range(2):
                            bi = m if eb == 0 else (2 if m == 0 else 1)
                            tok = f"bS{bi}"
                            k.mm(banks[bi][:], KTs[hb][64 * m:64 * m + 64, kc * 128:(kc + 1) * 128], QTs[hb][64 * m:64 * m + 64, qb * 512:(qb + 1) * 512],
                                 True, True, r=[f"KTd{hb}", f"QTd{hb}"], w=[tok])
                            k.act(E[m][eb][:], banks[bi][:], AF.Exp, r=[tok], w=[f"E{m}_{eb}"], scale=0.125)
                        for m in range(2):
                            for sub in range(4):
                                a = m * 4 + sub
                                k.mm(accb[a // 3][:, (a % 3) * 129:(a % 3) * 129 + 129], E[m][eb][:, sub * 128:(sub + 1) * 128], V1[hb][:, kc, :],
                                     kc == 0 and a % 3 == 0, kc == 31, r=[f"E{m}_{eb}", f"V1d{hb}"], w=[f"accb{a // 3}"])
                    for a in range(8):
                        eng = "dve" if (a // 3) % 2 == 0 else "act"
                        src = accb[a // 3][:, (a % 3) * 129:(a % 3) * 129 + 129]
                        if eng == "dve":
                            k.cp("dve", osb[:, a, :], src, r=[f"accb{a // 3}"], w=[f"osbd{a}"])
                        else:
                            k.act(osb[:, a, :], src, AF.Copy, r=[f"accb{a // 3}"], w=[f"osbd{a}"])
                    k.recip(rr[:], osb[:, :, 128], r=[f"osbd{a_}" for a_ in range(8)], w=["rr"])
                    k.ts("dve", rr[:, 4:8], rr[:, 4:8], neglam[:, 0:1], None, ALU.mult, r=["rr"], w=["rr"])
                    k.tt("dve", o1[:], osb[:, 0:4, 0:128], bc(rr[:, 0:4].unsqueeze(2), [128, 4, 128]), ALU.mult, r=[f"osbd{a_}" for a_ in range(8)] + ["rr"], w=["o1"])
                    k.tt("pool", o2[:], osb[:, 4:8, 0:128], bc(rr[:, 4:8].unsqueeze(2), [128, 4, 128]), ALU.mult, r=[f"osbd{a_}" for a_ in range(8)] + ["rr"], w=["o2"])
                    k.tt("dve", o1[:], o1[:], o2[:], ALU.add, r=["o1", "o2"], w=["o1"])
                    k.tt("pool", o2[:], o1[:], o1[:], ALU.mult, r=["o1"], w=["o2"])
                    k.red("dve", ssd[:], o2[:], r=["o2"], w=["ssd"])
                    k.act(ssd[:], ssd[:], AF.Sqrt, r=["ssd", "epsb"], w=["ssd"], scale=1.0 / 128, bias=epsb[:])
                    k.recip(ssd[:], ssd[:], r=["ssd"], w=["ssd"])
                    k.tt("dve", o1[:], o1[:], bc(ssd[:].unsqueeze(2), [128, 4, 128]), ALU.mult, r=["o1", "ssd"], w=["o1"])
                    k.tt("pool", obb[:], o1[:], bc(sgain[:].unsqueeze(1), [128, 4, 128]), ALU.mult, r=["o1"], w=["obb"])
                    ob_i = (h * 8 + qb) % 2
                    for sub in range(4):
                        k.tr(bankT[0][:, sub * 128:(sub + 1) * 128], obb[:, sub, :], identb[:], r=["obb"], w=["bTd"])
                    k.act(obT[ob_i][:], bankT[0][:, 0:512], AF.Copy, r=["bTd"], w=[f"obT{ob_i}"])
                    k.dma("sp", OBT[h, :, qb * 512:(qb + 1) * 512], obT[ob_i][:], r=[f"obT{ob_i}"], w=["OBT"])
            P.emit(nc, semsets, phase_sem, 3, fin)

        with ExitStack() as es:
            P = Prog()
            k = K(P)
            banks, bankT = alloc_psum(es)
            Wc = sbt(es, "Wc", [128, 8, 2048], BF16)
            wpa = sbt(es, "wpa", [128, 2, D], BF16)
            wpb = sbt(es, "wpb", [128, 8, D], BF16)
            wo = sbt(es, "wo", [128, 8, D], BF16)
            wq = [sbt(es, f"wq{i}", [128, D], F32) for i in range(2)]
            sk = [sbt(es, f"sk{i}", [128, 128], F32) for i in range(2)]
            nd = [sbt(es, f"nd{i}", [128, 12, 65], F32) for i in range(2)]
            obt = [sbt(es, f"obt{i}", [128, 8, 128], BF16) for i in range(2)]
            gts = [sbt(es, f"gts{i}", [128, 2048], BF16) for i in range(2)]
            xts = [sbt(es, f"xts{i}", [128, D], F32) for i in range(2)]
            nsum = sbt(es, "nsum", [128, 4, 65], F32)
            rden = sbt(es, "rden", [128, 4], F32)
            oab = sbt(es, "oab", [128, 4, 64], BF16)
            oaT = sbt(es, "oaT", [128, 2, 128], BF16)
            bra = sbt(es, "bra", [128, D], F32)
            t1 = sbt(es, "t1", [128, D], F32)
            t2 = sbt(es, "t2", [128, D], F32)
            mixb = sbt(es, "mixb", [128, D], BF16)
            mixT = sbt(es, "mixT", [128, 8, 128], BF16)
            x1s = sbt(es, "x1s", [128, D], F32)
            sq2 = sbt(es, "sq2", [128, D], F32)
            ss2 = sbt(es, "ss2", [128, 1], F32)
            xn2 = sbt(es, "xn2", [128, D], BF16)
            h2T = sbt(es, "h2T", [128, 8, 128], BF16)
            Ssb = sbt(es, "Ssb", [128, 8, 2, 128], F32)
            mr = sbt(es, "mr", [128, 128], F32)
            t16 = sbt(es, "t16", [128, 8, 2, 16], F32)
            cand = sbt(es, "cand", [128, 8, 16, 16], F32)
            mr2 = sbt(es, "mr2", [128, 256], F32)
            c16 = sbt(es, "c16", [128, 8, 16], F32)
            dd = sbt(es, "dd", [128, 8, 16], F32)
            zz = sbt(es, "zz", [128, 8], F32)
            tau = sbt(es, "tau", [128, 8], F32)
            cf = sbt(es, "cf", [128, 8], F32)
            k.dma("pool", wpa[:], w_pa.rearrange("(c p) n -> p c n", p=128), w=["wpa"])
            k.dma("pool", wpb[:], w_pb.rearrange("(c p) n -> p c n", p=128), w=["wpb"])
            k.dma("pool", wo[:], w_out.rearrange("(c p) n -> p c n", p=128), w=["wo"])
            for kk in range(16):
                b = kk % 2
                k.dma("sp", wq[b][:], wqT[kk], w=[f"wq{b}"])
                k.dma("sp", sk[b][:], skT[kk], w=[f"sk{b}"])
                for dc in range(8):
                    k.mm(banks[dc // 4][:, (dc % 4) * 128:(dc % 4 + 1) * 128], wq[b][:, dc * 128:(dc + 1) * 128], sk[b][:], True, True,
                         r=[f"wq{b}", f"sk{b}"], w=[f"wcp{dc // 4}"])
                for hh in range(2):
                    k.act(Wc[:, 4 * hh:4 * hh + 4, kk * 128:(kk + 1) * 128], banks[hh][:].rearrange("p (a n) -> p a n", a=4), AF.Copy,
                          r=[f"wcp{hh}"], w=["Wc"])
            for t in range(NT):
                b = t % 2
                t0 = t * 128
                k.dma("sp", nd[b][:], ND[t0:t0 + 128], r=["ND"], w=[f"nd{b}"])
                k.dma("sp", obt[b][:], OBT[:, :, t0:t0 + 128].rearrange("h c t -> c h t"), r=["OBT"], w=[f"obt{b}"])
                k.dma("sp", gts[b][:], GT[t0:t0 + 128, :], r=["GT"], w=[f"gts{b}"])
                k.dma("sp", xts[b][:], x[t0:t0 + 128, :], w=[f"xts{b}"])
                k.tt("pool", nsum[:], nd[b][:, 0:4, :], nd[b][:, 4:8, :], ALU.add, r=[f"nd{b}"], w=["nsum"])
                k.tt("pool", nsum[:], nsum[:], nd[b][:, 8:12, :], ALU.add, r=[f"nd{b}", "nsum"], w=["nsum"])
                k.recip(rden[:], nsum[:, :, 64], r=["nsum"], w=["rden"])
                k.tt("dve", oab[:], nsum[:, :, 0:64], bc(rden[:].unsqueeze(2), [128, 4, 64]), ALU.mult, r=["nsum", "rden"], w=["oab"])
                oaf = oab[:].rearrange("p a c -> p (a c)")
                for c in range(2):
                    k.tr(bankT[0][:, c * 128:(c + 1) * 128], oaf[:, c * 128:(c + 1) * 128], identb[:], r=["oab"], w=["bT0"])
                k.act(oaT[:].rearrange("p a t -> p (a t)"), bankT[0][:, 0:256], AF.Copy, r=["bT0"], w=["oaT"])
                for hf in range(2):
                    for c in range(2):
                        k.mm(banks[hf][:], oaT[:, c, :], wpa[:, c, hf * 512:(hf + 1) * 512], c == 0, c == 1, r=["oaT", "wpa"], w=[f"bk{hf}"])
                for hf in range(2):
                    for c in range(8):
                        k.mm(banks[2 + hf][:], obt[b][:, c, :], wpb[:, c, hf * 512:(hf + 1) * 512], c == 0, c == 7, r=[f"obt{b}", "wpb"], w=[f"bk{2 + hf}"])
                for hf in range(2):
                    sl = slice(hf * 512, (hf + 1) * 512)
                    k.act(bra[:, sl], banks[hf][:], AF.Copy, r=[f"bk{hf}"], w=["bra"])
                    k.tt("dve", t2[:, sl], banks[2 + hf][:], gts[b][:, 1024 + hf * 512:1024 + (hf + 1) * 512], ALU.mult, r=[f"bk{2 + hf}", f"gts{b}"], w=["t2"])
                k.tt("pool", t1[:], bra[:], gts[b][:, 0:1024], ALU.mult, r=["bra", f"gts{b}"], w=["t1"])
                k.tt("pool", mixb[:], t1[:], t2[:], ALU.add, r=["t1", "t2"], w=["mixb"])
                for c in range(8):
                    k.tr(bankT[1][:, c * 128:(c + 1) * 128], mixb[:, c * 128:(c + 1) * 128], identb[:], r=["mixb"], w=["bT1"])
                k.act(mixT[:].rearrange("p a t -> p (a t)"), bankT[1][:], AF.Copy, r=["bT1"], w=["mixT"])
                for hf in range(2):
                    for c in range(8):
                        k.mm(banks[4 + hf][:], mixT[:, c, :], wo[:, c, hf * 512:(hf + 1) * 512], c == 0, c == 7, r=["mixT", "wo"], w=[f"bk{4 + hf}"])
                for hf in range(2):
                    sl = slice(hf * 512, (hf + 1) * 512)
                    k.tt("dve", t2[:, sl], banks[4 + hf][:], G1row[:, sl], ALU.mult, r=[f"bk{4 + hf}"], w=["t2"])
                k.tt("pool", x1s[:], t2[:], xts[b][:], ALU.add, r=["t2", f"xts{b}"], w=["x1s"])
                k.dma("sp", X1[t0:t0 + 128, :], x1s[:], r=["x1s"], w=["X1"])
                k.act(sq2[:], x1s[:], AF.Square, r=["x1s"], w=["sq2"])
                k.red("dve", ss2[:], sq2[:], r=["sq2"], w=["ss2"])
                k.act(ss2[:], ss2[:], AF.Sqrt, r=["ss2", "epsb"], w=["ss2"], scale=1.0 / D, bias=epsb[:])
                k.recip(ss2[:], ss2[:], r=["ss2"], w=["ss2"])
                k.ts("dve", xn2[:], x1s[:], ss2[:, 0:1], None, ALU.mult, r=["x1s", "ss2"], w=["xn2"])
                for c in range(8):
                    k.tr(bankT[0][:, c * 128:(c + 1) * 128], xn2[:, c * 128:(c + 1) * 128], identb[:], r=["xn2"], w=["bT0"])
                for c in range(8):
                    k.act(h2T[:, c, :], bankT[0][:, c * 128:(c + 1) * 128], AF.Identity, r=["bT0"], w=["h2T"], scale=s2T[:, c:c + 1], bias=sh2T[:, c:c + 1])
                k.dma("sp", H2T[:, :, t0:t0 + 128].rearrange("c p t -> p c t"), h2T[:], r=["h2T"], w=["H2T"])
                for nb in range(4):
                    for c in range(8):
                        k.mm(banks[nb][:], h2T[:, c, :], Wc[:, c, nb * 512:(nb + 1) * 512], c == 0, c == 7, r=["h2T", "Wc"], w=[f"bk{nb}"])
                Sf = Ssb[:].rearrange("p h a n -> p (h a n)")
                for nb in range(4):
                    k.act(Sf[:, nb * 512:(nb + 1) * 512], banks[nb][:], AF.Copy, r=[f"bk{nb}"], w=["Ssb"])
                for h in range(8):
                    for a in range(2):
                        k.max8(t16[:, h, a, 0:8], Ssb[:, h, a, :], r=["Ssb"], w=["t16"])
                        k.mrep(mr[:], t16[:, h, a, 0:8], Ssb[:, h, a, :], r=["Ssb", "t16"], w=["mr"])
                        k.max8(t16[:, h, a, 8:16], mr[:], r=["mr"], w=["t16"])
                k.tt("dve", cand[:], bc(t16[:, :, 0, :].unsqueeze(3), [128, 8, 16, 16]), bc(t16[:, :, 1, :].unsqueeze(2), [128, 8, 16, 16]), ALU.add,
                     r=["t16"], w=["cand"])
                for h in range(8):
                    cv = cand[:, h].rearrange("p a b -> p (a b)")
                    k.max8(c16[:, h, 0:8], cv, r=["cand"], w=["c16"])
                    k.mrep(mr2[:], c16[:, h, 0:8], cv, r=["cand", "c16"], w=["mr2"])
                    k.max8(c16[:, h, 8:16], mr2[:], r=["mr2"], w=["c16"])
                k.ts("dve", tau[:], c16[:, :, 15], -1e-5, None, ALU.add, r=["c16"], w=["tau"])
                k.tt("dve", dd[:], c16[:], bc(c16[:, :, 0:1], [128, 8, 16]), ALU.subtract, r=["c16"], w=["dd"])
                k.act(dd[:], dd[:], AF.Exp, r=["dd"], w=["dd"])
                k.red("dve", zz[:], dd[:], r=["dd"], w=["zz"])
                k.recip(zz[:], zz[:], r=["zz"], w=["zz"])
                k.tt("dve", cf[:], tau[:], c16[:, :, 0], ALU.subtract, r=["tau", "c16"], w=["cf"])
                k.act(cf[:], cf[:], AF.Exp, r=["cf"], w=["cf"])
                k.tt("dve", cf[:], cf[:], zz[:], ALU.mult, r=["cf", "zz"], w=["cf"])
                k.tt("dve", Ssb[:, :, 0, :], Ssb[:, :, 0, :], bc(tau[:].unsqueeze(2), [128, 8, 128]), ALU.subtract, r=["Ssb", "tau"], w=["Ssb"])
                k.dma("sp", PS[t0:t0 + 128], Ssb[:], r=["Ssb"], w=["PS"])
                k.dma("sp", CF[t0:t0 + 128, :], cf[:], r=["cf"], w=["CF"])
            P.emit(nc, semsets, phase_sem, 4, fin)

        with ExitStack() as es:
            P = Prog()
            k = K(P)
            banks, bankT = alloc_psum(es, 8, 0)
            NB = 4
            h2 = [sbt(es, f"h2_{i}", [128, 8, 256], BF16) for i in range(2)]
            pss = [sbt(es, f"pss{i}", [128, 2, 8, 2, 128], F32) for i in range(2)]
            cfs = [sbt(es, f"cfs{i}", [128, 2, 8], F32) for i in range(2)]
            x1t = sbt(es, "x1t", [128, 2, D], F32)
            Dm = [sbt(es, f"Dm{i}", [128, 2, 8, 128], BF16) for i in range(2)]
            UTi = [sbt(es, f"UTi{i}", [128, 8, 128], BF16) for i in range(NB)]
            Vi = [sbt(es, f"Vi{i}", [128, D], BF16) for i in range(NB)]
            Hg = [sbt(es, f"Hg{i}", [128, 256], BF16) for i in range(2)]
            zt = [sbt(es, f"zt{i}", [128, 2, 8, 128], F32) for i in range(2)]
            Et = [sbt(es, f"Et{i}", [128, 2, 8, 128], BF16) for i in range(2)]
            Gt = [sbt(es, f"Gt{i}", [128, 2, 8, 128], BF16) for i in range(2)]
            WT = [sbt(es, f"WT{i}", [128, 256], BF16) for i in range(2)]
            yo = [sbt(es, f"yo{i}", [128, D], F32) for i in range(2)]
            P.add("sp", lambda e: e.wait_ge(conv_sem, 16 * 32))
            UTv = UTb.rearrange("(i p) (c e) -> i p c e", p=128, c=8)
            step = 0
            for tt_ in range(16):
                tb = tt_ % 2
                t0 = tt_ * 256
                k.dma("sp", h2[tb][:], H2T[:, :, t0:t0 + 256].rearrange("c p t -> p c t"), w=[f"h2_{tb}"])
                k.dma("sp", pss[tb][:], PS[t0:t0 + 256].rearrange("(s p) h a n -> p s h a n", p=128), w=[f"pss{tb}"])
                k.dma("sp", cfs[tb][:], CF[t0:t0 + 256, :].rearrange("(s p) h -> p s h", p=128), w=[f"cfs{tb}"])
                for s_ in range(2):
                    for h in range(8):
                        k.ts("pool", Dm[tb][:, s_, h, :], identf[:], cfs[tb][:, s_, h:h + 1], None, ALU.mult, r=[f"cfs{tb}"], w=[f"Dm{tb}"])
                pend = None
                for i in range(128):
                    ub = step % NB
                    b2 = step % 2
                    step += 1
                    k.dma("sp", UTi[ub][:], UTv[i], w=[f"UTi{ub}"])
                    k.dma("act" if False else "sp", Vi[ub][:], Vb[i * 128:(i + 1) * 128, :], w=[f"Vi{ub}"])
                    hv = banks[4 + b2][:, 0:256]
                    gv = banks[6 + b2][:, 0:256]
                    for c in range(8):
                        k.mm(hv, UTi[ub][:, c, :], h2[tb][:, c, :], c == 0, c == 7, r=[f"UTi{ub}", f"h2_{tb}"], w=[f"bH{b2}"])
                    k.act(Hg[b2][:], hv, AF.Gelu, r=[f"bH{b2}"], w=[f"Hg{b2}"])
                    k.tt("pool", zt[b2][:], pss[tb][:, :, :, 1, :], bc(pss[tb][:, :, :, 0, i:i + 1], [128, 2, 8, 128]), ALU.add, r=[f"pss{tb}"], w=[f"zt{b2}"])
                    k.act(Et[b2][:], zt[b2][:], AF.Exp, r=[f"zt{b2}"], w=[f"Et{b2}"])
                    k.stt("dve", Gt[b2][:], zt[b2][:], 0.0, Et[b2][:], ALU.is_ge, ALU.mult, r=[f"zt{b2}", f"Et{b2}"], w=[f"Gt{b2}"])
                    for s_ in range(2):
                        for h in range(8):
                            k.mm(gv[:, s_ * 128:(s_ + 1) * 128], Gt[b2][:, s_, h, :], Dm[tb][:, s_, h, :], h == 0, h == 7,
                                 r=[f"Gt{b2}", f"Dm{tb}"], w=[f"bG{b2}"])
                    if pend is not None:
                        pend()
                    k.tt("dve", WT[b2][:], gv, Hg[b2][:], ALU.mult, r=[f"bG{b2}", f"Hg{b2}"], w=[f"WT{b2}"])

                    def ymm(i=i, ub=ub, b2=b2):
                        for s_ in range(2):
                            for hf in range(2):
                                k.mm(banks[s_ * 2 + hf][:], WT[b2][:, s_ * 128:(s_ + 1) * 128], Vi[ub][:, hf * 512:(hf + 1) * 512], i == 0, i == 127,
                                     r=[f"WT{b2}", f"Vi{ub}"], w=[f"bY{s_ * 2 + hf}"])
                    pend = ymm
                pend()
                k.dma("sp", x1t[:], X1[t0:t0 + 256, :].rearrange("(s p) d -> p s d", p=128), w=["x1t"])
                for s_ in range(2):
                    for hf in range(2):
                        sl = slice(hf * 512, (hf + 1) * 512)
                        k.tt("dve", yo[s_][:, sl], banks[s_ * 2 + hf][:], G2row[:, sl], ALU.mult, r=[f"bY{s_ * 2 + hf}"], w=[f"yo{s_}"])
                    k.tt("pool", yo[s_][:], yo[s_][:], x1t[:, s_, :], ALU.add, r=[f"yo{s_}", "x1t"], w=[f"yo{s_}"])
                    k.dma("sp", out[t0 + s_ * 128:t0 + (s_ + 1) * 128, :], yo[s_][:], r=[f"yo{s_}"], w=["out"])
            P.emit(nc, semsets, phase_sem, 5, fin)
    return nc


DEBUG_OUT = set()


def _masks():
    kk = np.arange(128)[:, None]
    qq = np.arange(256)[None, :]
    std = ((kk <= qq) & (qq <= kk + 128)).astype(np.float32)
    blk = (((kk < 64) & (qq < 128)) | ((kk >= 64) & (qq >= 128))).astype(np.float32)
    return std, std * blk


def prep_inputs(inputs):
    f = lambda a: np.ascontiguousarray(np.asarray(a), dtype=np.float32)
    x = f(inputs["x"])
    c = f(inputs["c"])
    pos = np.ascontiguousarray(np.asarray(inputs["positions"]), dtype=np.int32)
    L = 0
    rep = lambda v, n=128: np.ascontiguousarray(np.broadcast_to(f(v)[None], (n,) + f(v).shape))
    qn_a, kn_a = f(inputs["qn_a"])[L], f(inputs["kn_a"])[L]
    qn_b, kn_b = f(inputs["qn_b"])[L], f(inputs["kn_b"])[L]
    gainA = np.concatenate([np.tile(qn_a[None], (4, 1)), np.tile(kn_a[None], (4, 1))], 0)
    gainQB = np.tile(qn_b[None], (8, 1))
    gainKB = np.tile(kn_b[None], (8, 1))
    lam = np.stack([f(inputs["lam_q1"])[L], f(inputs["lam_k1"])[L], f(inputs["lam_q2"])[L], f(inputs["lam_k2"])[L]], 0)
    invf = (1.0 / (10000.0 ** (np.arange(0, 64, 2, dtype=np.float32) / 64))).astype(np.float32)
    hp = np.concatenate([np.zeros(32, np.float32), np.full(32, np.pi / 2, np.float32)])
    mstd, mbnd = _masks()
    wq = f(inputs["w_query"])[L]
    wqT = np.ascontiguousarray(wq.reshape(D, 16, 128).transpose(1, 2, 0))
    sk = f(inputs["sub_keys"])[L].reshape(16, 128, 128)
    skT = np.ascontiguousarray(sk.transpose(0, 2, 1))
    U = f(inputs["expert_u"])[L]
    UT = np.ascontiguousarray(U.reshape(128, 128, 8, 128).transpose(0, 3, 2, 1)).reshape(16384, 1024)
    shared = dict(
        w_ada=f(inputs["w_ada"])[L],
        b_adaT=np.ascontiguousarray(f(inputs["b_ada"])[L].reshape(48, 128).T),
        n1gT=np.ascontiguousarray(f(inputs["norm1_g"])[L].reshape(8, 128).T),
        n2gT=np.ascontiguousarray(f(inputs["norm2_g"])[L].reshape(8, 128).T),
        w_in=f(inputs["w_in"])[L],
        b_gate_bc=rep(f(inputs["b_gate"])[L]),
        gainA=rep(gainA), gainQB=rep(gainQB), gainKB=rep(gainKB),
        lam_bc=rep(lam), subln_bc=rep(f(inputs["subln_g"])[L]),
        invf_bc=rep(invf), halfpi=rep(hp), mask_std=mstd, mask_bnd=mbnd,
        w_pa=f(inputs["w_proj_a"])[L], w_pb=f(inputs["w_proj_b"])[L], w_out=f(inputs["w_out"])[L],
        wqT=wqT, skT=skT, UT=UT, Vx=f(inputs["expert_v"])[L],
    )
    in_maps = []
    for b in range(8):
        m = dict(shared)
        m["x"] = x[b]
        m["pos"] = pos[b]
        m["cT"] = np.ascontiguousarray(c[b].reshape(8, 128).T)
        in_maps.append(m)
    return in_maps


def kernel(**inputs):
    in_maps = prep_inputs(inputs)
    nc = build_program()
    res = run_bass_kernel_spmd(nc, in_maps, core_ids=list(range(8)))
    return np.stack([np.asarray(r["out"], dtype=np.float32) for r in res.results], 0)
```

```python
import os
import numpy as np
from contextlib import ExitStack
import concourse.bass as bass
import concourse.mybir as mybir
from concourse.bass_utils import run_bass_kernel_spmd

F32 = mybir.dt.float32
BF16 = mybir.dt.bfloat16
I32 = mybir.dt.int32
AF = mybir.ActivationFunctionType
ALU = mybir.AluOpType
AX = mybir.AxisListType

S = 4096
D = 1024
NT = 32
EPS = 1e-6
LAM_INIT = 0.2
DIL = (1, 4, 16)
N_DMA_SEMS = 8
QUEUES = ("sp", "pool", "act")
TWO_PI = float(2 * np.pi)
C1 = 6.28125
C2 = float(2 * np.pi - 6.28125)


class Prog:
    def __init__(self):
        self.ops = []
        self.last_writer = {}
        self.readers = {}

    def add(self, eng, fn, reads=(), writes=(), dma=False):
        idx = len(self.ops)
        deps = set()
        raw = set()
        for t in reads:
            w = self.last_writer.get(t)
            if w is not None:
                deps.add(w)
                raw.add(w)
        for t in writes:
            w = self.last_writer.get(t)
            if w is not None:
                deps.add(w)
            for r in self.readers.get(t, ()):
                deps.add(r)
        op = dict(eng=eng, fn=fn, dma=dma, deps=deps, raw=raw, signal=False)
        self.ops.append(op)
        for t in reads:
            self.readers.setdefault(t, []).append(idx)
        for t in writes:
            self.last_writer[t] = idx
            self.readers[t] = []
        return idx

    def emit(self, nc, semsets, phase_sem, phase_idx, fin):
        sems, cnt = semsets[phase_idx % 3]
        ops = self.ops
        for op in ops:
            keep = set()
            for d in op["deps"]:
                p = ops[d]
                if p["dma"] or op["dma"] or p["eng"] != op["eng"]:
                    keep.add(d)
                elif op["eng"] != "pe" and d in op["raw"]:
                    keep.add(d)
            op["deps"] = keep
            for d in keep:
                ops[d]["signal"] = True
        dma_k = {}
        prev_slot = {}
        for op in ops:
            if op["dma"]:
                q = op["eng"]
                k = dma_k.get(q, 0)
                dma_k[q] = k + 1
                key = ("dma", q, k % N_DMA_SEMS)
                cnt[key] = cnt.get(key, 0) + 16
                op["prev"] = prev_slot.get(key)
                op["sig"] = (key, cnt[key])
                prev_slot[key] = op["sig"]
            elif op["signal"]:
                key = op["eng"]
                cnt[key] = cnt.get(key, 0) + 1
                op["sig"] = (key, cnt[key])

        def semof(key):
            if isinstance(key, tuple):
                return sems["dma_" + key[1]][key[2]]
            return sems[key]

        streams = {}
        for op in ops:
            streams.setdefault(op["eng"], []).append(op)

        def run_stream(engname, eng):
            if phase_idx > 0:
                eng.wait_ge(phase_sem, 19 * phase_idx)
            known = {}
            for op in streams.get(engname, []):
                waits = {}
                for d in op["deps"]:
                    key, val = ops[d]["sig"]
                    if waits.get(key, 0) < val:
                        waits[key] = val
                if op["dma"] and op["prev"] is not None:
                    key, val = op["prev"]
                    if waits.get(key, 0) < val:
                        waits[key] = val
                for key, val in waits.items():
                    if known.get(key, 0) >= val:
                        continue
                    eng.wait_ge(semof(key), val)
                    known[key] = val
                ins = op["fn"](eng)
                if "sig" in op:
                    ins.then_inc(semof(op["sig"][0]), 16 if op["dma"] else 1)
            if engname == "sp":
                for key, val in cnt.items():
                    if isinstance(key, tuple):
                        eng.wait_ge(semof(key), val)
                eng.dma_start(out=fin["d1"][:], in_=fin["d0"][:]).then_inc(phase_sem, 16)
            elif engname == "pe":
                pass
            elif engname == "act":
                eng.activation(out=fin["act"][:], in_=fin["d0"][0:1, 0:1].to_broadcast([1, 1]) if False else fin["act"][:], func=AF.Copy).then_inc(phase_sem, 1)
            else:
                eng.memset(fin[engname][:], 0.0).then_inc(phase_sem, 1)

        with nc.Block() as block:
            @block.sync
            def _(e):
                run_stream("sp", e)

            @block.tensor
            def _(e):
                run_stream("pe", e)

            @block.scalar
            def _(e):
                run_stream("act", e)

            @block.vector
            def _(e):
                run_stream("dve", e)

            @block.gpsimd
            def _(e):
                run_stream("pool", e)


class K:
    def __init__(self, P):
        self.P = P

    def dma(self, q, out, in_, r=(), w=(), **kw):
        self.P.add(q, lambda e: e.dma_start(out=out, in_=in_, **kw), r, w, dma=True)

    def mm(self, out, lhsT, rhs, start, stop, r=(), w=()):
        self.P.add("pe", lambda e: e.matmul(out, lhsT=lhsT, rhs=rhs, start=start, stop=stop), r, w)

    def tr(self, out, in_, ident, r=(), w=()):
        self.P.add("pe", lambda e: e.transpose(out=out, in_=in_, identity=ident), r, w)

    def act(self, out, in_, func, r=(), w=(), **kw):
        self.P.add("act", lambda e: e.activation(out=out, in_=in_, func=func, **kw), r, w)

    def tt(self, eng, out, in0, in1, op, r=(), w=()):
        self.P.add(eng, lambda e: e.tensor_tensor(out=out, in0=in0, in1=in1, op=op), r, w)

    def ts(self, eng, out, in0, s1, s2, op0, op1=None, r=(), w=()):
        if op1 is None:
            self.P.add(eng, lambda e: e.tensor_scalar(out=out, in0=in0, scalar1=s1, scalar2=None, op0=op0), r, w)
        else:
            self.P.add(eng, lambda e: e.tensor_scalar(out=out, in0=in0, scalar1=s1, scalar2=s2, op0=op0, op1=op1), r, w)

    def stt(self, eng, out, in0, scalar, in1, op0, op1, r=(), w=()):
        self.P.add(eng, lambda e: e.scalar_tensor_tensor(out=out, in0=in0, scalar=scalar, in1=in1, op0=op0, op1=op1), r, w)

    def cp(self, eng, out, in_, r=(), w=()):
        self.P.add(eng, lambda e: e.tensor_copy(out=out, in_=in_), r, w)

    def red(self, eng, out, in_, r=(), w=(), op=None):
        op = op or ALU.add
        self.P.add(eng, lambda e: e.tensor_reduce(out=out, in_=in_, axis=AX.X, op=op), r, w)

    def recip(self, out, in_, r=(), w=()):
        self.P.add("dve", lambda e: e.reciprocal(out=out, in_=in_), r, w)

    def memset(self, eng, out, val, r=(), w=()):
        self.P.add(eng, lambda e: e.memset(out, val), r, w)

    def max8(self, out, in_, r=(), w=()):
        self.P.add("dve", lambda e: e.max(out=out, in_=in_), r, w)

    def mrep(self, out, rep, vals, r=(), w=()):
        self.P.add("dve", lambda e: e.match_replace(out=out, in_to_replace=rep, in_values=vals, imm_value=-1e30), r, w)


def bc(ap, shape):
    return ap.to_broadcast(list(shape))


def build_program(debug=False):
    nc = bass.Bass("TRN2", target_bir_lowering=False)

    def din(name, shape, dt=F32):
        return nc.dram_tensor(name, list(shape), dt, kind="ExternalInput").ap()

    def dscr(name, shape, dt):
        kind = "ExternalOutput" if (debug and name in DEBUG_OUT) else "Internal"
        return nc.dram_tensor(name, list(shape), dt, kind=kind).ap()

    x = din("x", [S, D])
    pos = din("pos", [S], I32)
    cT = din("cT", [128, 8])
    w_ada = din("w_ada", [D, 6 * D])
    b_adaT = din("b_adaT", [128, 48])
    n1gT = din("n1gT", [128, 8])
    n2gT = din("n2gT", [128, 8])
    w_in = din("w_in", [D, 7424])
    b_gate_bc = din("b_gate_bc", [128, 2048])
    gainA = din("gainA", [128, 8, 64])
    gainQB = din("gainQB", [128, 8, 64])
    gainKB = din("gainKB", [128, 8, 64])
    lam_bc = din("lam_bc", [128, 4, 64])
    subln_bc = din("subln_bc", [128, 128])
    invf_bc = din("invf_bc", [128, 32])
    halfpi = din("halfpi", [128, 64])
    mask_std = din("mask_std", [128, 256])
    mask_bnd = din("mask_bnd", [128, 256])
    w_pa = din("w_pa", [256, D])
    w_pb = din("w_pb", [D, D])
    w_out = din("w_out", [D, D])
    wqT = din("wqT", [16, 128, D])
    skT = din("skT", [16, 128, 128])
    UT = din("UT", [16384, 1024])
    Vx = din("Vx", [16384, 1024])
    out = nc.dram_tensor("out", [S, D], F32, kind="ExternalOutput").ap()

    QTA = dscr("QTA", [3, 2, 128, S], BF16)
    KTA = dscr("KTA", [3, 2, 128, S], BF16)
    VA = dscr("VA", [3, S, 256], BF16)
    QTB = dscr("QTB", [8, 128, S], BF16)
    KTB = dscr("KTB", [8, 128, S], BF16)
    VB = dscr("VB", [S, 1024], BF16)
    GT = dscr("GT", [S, 2048], BF16)
    ND = dscr("ND", [S, 12, 65], F32)
    OBT = dscr("OBT", [8, 128, S], BF16)
    X1 = dscr("X1", [S, D], F32)
    H2T = dscr("H2T", [8, 128, S], BF16)
    PS = dscr("PS", [S, 8, 2, 128], F32)
    CF = dscr("CF", [S, 8], F32)
    UTb = dscr("UTb", [16384, 1024], BF16)
    Vb = dscr("Vb", [16384, 1024], BF16)

    with ExitStack() as top:
        def sbt(es, name, shape, dt):
            return es.enter_context(nc.sbuf_tensor(name, list(shape), dt))

        def pst(es, name, shape, dt):
            return es.enter_context(nc.psum_tensor(name, list(shape), dt))

        semsets = []
        for ph in range(3):
            ss = {}
            for e in ("pe", "act", "dve", "pool"):
                ss[e] = top.enter_context(nc.semaphore(f"s{ph}_{e}"))
            for q in QUEUES:
                ss["dma_" + q] = [top.enter_context(nc.semaphore(f"d{ph}_{q}_{i}")) for i in range(N_DMA_SEMS)]
            semsets.append((ss, {}))
        phase_sem = top.enter_context(nc.semaphore("phase"))
        conv_sem = top.enter_context(nc.semaphore("conv"))

        identb = sbt(top, "identb", [128, 128], BF16)
        identf = sbt(top, "identf", [128, 128], F32)
        onesf = sbt(top, "onesf", [128, 128], F32)
        s1T = sbt(top, "s1T", [128, 8], F32)
        sh1T = sbt(top, "sh1T", [128, 8], F32)
        s2T = sbt(top, "s2T", [128, 8], F32)
        sh2T = sbt(top, "sh2T", [128, 8], F32)
        G1row = sbt(top, "G1row", [128, D], F32)
        G2row = sbt(top, "G2row", [128, D], F32)
        neglam = sbt(top, "neglam", [128, 1], F32)
        sgain = sbt(top, "sgain", [128, 128], F32)
        epsb = sbt(top, "epsb", [128, 1], F32)
        fin = dict(
            d0=sbt(top, "fin_d0", [1, 16], F32), d1=sbt(top, "fin_d1", [1, 16], F32),
            act=sbt(top, "fin_act", [128, 1], F32), dve=sbt(top, "fin_dve", [128, 1], F32),
            pool=sbt(top, "fin_pool", [128, 1], F32), idb=identb,
        )
        psn = [0]

        def alloc_psum(es, nf=6, nb=2):
            psn[0] += 1
            bk = [pst(es, f"bank{psn[0]}_{i}", [128, 512], F32) for i in range(nf)]
            bt = [pst(es, f"bankT{psn[0]}_{i}", [128, 1024], BF16) for i in range(nb)]
            return bk, bt

        with ExitStack() as es:
            P = Prog()
            k = K(P)
            banks, bankT = alloc_psum(es)
            cTs = sbt(es, "cTs", [128, 8], F32)
            scs = sbt(es, "scs", [128, 8], F32)
            wada = [sbt(es, f"wada{i}", [128, 6 * D], F32) for i in range(2)]
            badas = sbt(es, "badas", [128, 48], F32)
            modT = sbt(es, "modT", [128, 48], F32)
            n1s = sbt(es, "n1s", [128, 8], F32)
            n2s = sbt(es, "n2s", [128, 8], F32)
            lams = sbt(es, "lams", [128, 4, 64], F32)
            lprod = sbt(es, "lprod", [128, 2, 64], F32)
            lsum = sbt(es, "lsum", [128, 2], F32)
            lexp = sbt(es, "lexp", [128, 2], F32)
            ltmp = sbt(es, "ltmp", [128, 1], F32)
            subs = sbt(es, "subs", [128, 128], F32)
            dg = [sbt(es, f"dg{i}", [128, 128], F32) for i in range(2)]
            NCONV = 16
            rows = 16384 // NCONV
            for i in range(NCONV):
                for (src, dst) in ((UT, UTb), (Vx, Vb)):
                    P.add("pool", (lambda s_, d_, i_: (lambda e: e.dma_start(out=d_[i_ * rows:(i_ + 1) * rows, :], in_=s_[i_ * rows:(i_ + 1) * rows, :]).then_inc(conv_sem, 16)))(src, dst, i))
            k.dma("sp", cTs[:], cT, w=["cTs"])
            k.dma("sp", badas[:], b_adaT, w=["badas"])
            k.dma("sp", n1s[:], n1gT, w=["n1s"])
            k.dma("sp", n2s[:], n2gT, w=["n2s"])
            k.dma("sp", lams[:], lam_bc, w=["lams"])
            k.dma("sp", subs[:], subln_bc, w=["subs"])
            k.memset("dve", identf[:], 1.0, w=["identf"])
            P.add("pool", lambda e: e.affine_select(out=identf[:], in_=identf[:], pattern=[[-1, 128]], compare_op=ALU.is_equal,
                                                   fill=0.0, base=0, channel_multiplier=1), ["identf"], ["identf"])
            k.cp("dve", identb[:], identf[:], r=["identf"], w=["identb"])
            k.memset("dve", onesf[:], 1.0, w=["onesf"])
            k.memset("dve", epsb[:], EPS, w=["epsb"])
            k.memset("dve", fin["d0"][:], 0.0, w=["find0"])
            k.act(scs[:], cTs[:], AF.Silu, r=["cTs"], w=["scs"])
            modps = banks[0]
            for kc in range(8):
                b = kc % 2
                k.dma("sp", wada[b][:], w_ada[kc * 128:(kc + 1) * 128, :], w=[f"wada{b}"])
                for f in range(48):
                    k.mm(modps[:, f:f + 1], wada[b][:, f * 128:(f + 1) * 128], scs[:, kc:kc + 1], kc == 0 and f == 0, kc == 7,
                         r=[f"wada{b}", "scs"], w=["modps"])
            k.tt("dve", modT[:], modps[:, 0:48], badas[:], ALU.add, r=["modps", "badas"], w=["modT"])
            k.stt("dve", s1T[:], modT[:, 8:16], 1.0, n1s[:], ALU.add, ALU.mult, r=["modT", "n1s"], w=["s1T"])
            k.cp("dve", sh1T[:], modT[:, 0:8], r=["modT"], w=["sh1T"])
            k.stt("dve", s2T[:], modT[:, 32:40], 1.0, n2s[:], ALU.add, ALU.mult, r=["modT", "n2s"], w=["s2T"])
            k.cp("dve", sh2T[:], modT[:, 24:32], r=["modT"], w=["sh2T"])
            for (row, base, bk) in ((G1row, 16, 1), (G2row, 40, 3)):
                for c in range(8):
                    b = c % 2
                    k.ts("dve", dg[b][:], identf[:], modT[:, base + c:base + c + 1], None, ALU.mult, r=["modT", "identf"], w=[f"dg{b}"])
                    bank = banks[bk + c // 4]
                    k.mm(bank[:, (c % 4) * 128:(c % 4 + 1) * 128], onesf[:], dg[b][:], True, True, r=[f"dg{b}", "onesf"], w=[f"gr{bk + c // 4}"])
                for hh in range(2):
                    k.cp("dve", row[:, hh * 512:(hh + 1) * 512], banks[bk + hh][:], r=[f"gr{bk + hh}"], w=[f"grow{base}"])
            k.tt("dve", lprod[:, 0, :], lams[:, 0, :], lams[:, 1, :], ALU.mult, r=["lams"], w=["lprod0"])
            k.tt("dve", lprod[:, 1, :], lams[:, 2, :], lams[:, 3, :], ALU.mult, r=["lams"], w=["lprod1"])
            k.red("dve", lsum[:], lprod[:], r=["lprod0", "lprod1"], w=["lsum"])
            k.act(lexp[:], lsum[:], AF.Exp, r=["lsum"], w=["lexp"])
            k.tt("dve", ltmp[:], lexp[:, 1:2], lexp[:, 0:1], ALU.subtract, r=["lexp"], w=["ltmp"])
            k.ts("dve", neglam[:], ltmp[:], -LAM_INIT, None, ALU.add, r=["ltmp"], w=["neglam"])
            k.ts("dve", sgain[:], subs[:], 1.0 - LAM_INIT, None, ALU.mult, r=["subs"], w=["sgain"])
            P.emit(nc, semsets, phase_sem, 0, fin)

        with ExitStack() as es:
            P = Prog()
            k = K(P)
            banks, bankT = alloc_psum(es, 5, 3)
            hT = sbt(es, "hT", [128, 8, 2048], BF16)
            xt = [sbt(es, f"xt{i}", [128, D], F32) for i in range(2)]
            sq = [sbt(es, f"sq{i}", [128, D], F32) for i in range(2)]
            xn = [sbt(es, f"xn{i}", [128, D], BF16) for i in range(2)]
            ssx = [sbt(es, f"ssx{i}", [128, 1], F32) for i in range(2)]
            rsx = [sbt(es, f"rsx{i}", [128, 1], F32) for i in range(2)]
            gA = sbt(es, "gA", [128, 8, 64], F32)
            gQB = sbt(es, "gQB", [128, 8, 64], F32)
            gKB = sbt(es, "gKB", [128, 8, 64], F32)
            bgs = sbt(es, "bgs", [128, 2048], F32)
            invfs = sbt(es, "invfs", [128, 32], F32)
            hps = sbt(es, "hps", [128, 64], F32)
            posi = sbt(es, "posi", [128, 16], I32)
            posf = sbt(es, "posf", [128, 16], F32)
            tabs = [sbt(es, f"tab{i}", [128, 16, 64], F32) for i in range(3)]
            arg = sbt(es, "arg", [128, 16, 64], F32)
            argk = sbt(es, "argk", [128, 16, 64], F32)
            argi = sbt(es, "argi", [128, 16, 64], I32)
            wb = [sbt(es, f"wb{i}", [128, 8, 512], BF16) for i in range(2)]
            sqq = [sbt(es, f"sqq{i}", [128, 8, 64], F32) for i in range(3)]
            ssq = [sbt(es, f"ssq{i}", [128, 8], F32) for i in range(3)]
            rsq = [sbt(es, f"rsq{i}", [128, 8], F32) for i in range(3)]
            qn = [sbt(es, f"qn{i}", [128, 8, 64], F32) for i in range(3)]
            qg = [sbt(es, f"qg{i}", [128, 8, 2, 32], F32) for i in range(3)]
            rt = [sbt(es, f"rt{i}", [128, 4, 8, 32], F32) for i in range(3)]
            qr = [sbt(es, f"qr{i}", [128, 8, 2, 32], BF16) for i in range(3)]
            stg = [sbt(es, f"stg{i}", [128, 4, 128], BF16) for i in range(3)]
            vst = [sbt(es, f"vst{i}", [128, 512], BF16) for i in range(3)]
            gpre = [sbt(es, f"gpre{i}", [128, 512], F32) for i in range(3)]

            k.dma("sp", gA[:], gainA, w=["gA"])
            k.dma("sp", gQB[:], gainQB, w=["gQB"])
            k.dma("sp", gKB[:], gainKB, w=["gKB"])
            k.dma("sp", bgs[:], b_gate_bc, w=["bgs"])
            k.dma("sp", invfs[:], invf_bc, w=["invfs"])
            k.dma("sp", hps[:], halfpi, w=["hps"])

            pos_pat = [
                pos.rearrange("(st u m) -> st m u", st=2, u=16, m=128),
                pos.rearrange("(st blk m r) -> st m blk r", st=2, blk=4, m=128, r=4),
                pos.rearrange("(st m r) -> st m r", st=2, m=128, r=16),
            ]

            def tokcols(pat, u):
                if pat == 0:
                    return slice(u * 128, (u + 1) * 128)
                if pat == 1:
                    blk, r = u // 4, u % 4
                    return slice(blk * 512 + r, blk * 512 + 512, 4)
                return slice(u, 2048, 16)

            def perm0(pat, st, u):
                if pat == 0:
                    return st * 2048 + u * 128
                if pat == 1:
                    blk, r = u // 4, u % 4
                    return r * 1024 + st * 512 + blk * 128
                return u * 256 + st * 128

            jobs = []
            for g in range(3):
                jobs.append((g, "qk", [(256 * g, 256), (768 + 256 * g, 256)], ("A", g)))
                jobs.append((g, "v", [(1536 + 256 * g, 256)], ("A", g)))
            for j in range(2):
                jobs.append((0, "qk", [(2304 + 512 * j, 512)], ("QB", j)))
                jobs.append((0, "qk", [(3328 + 512 * j, 512)], ("KB", j)))
                jobs.append((0, "v", [(4352 + 512 * j, 512)], ("B", j)))
            for j in range(4):
                jobs.append((0, "gate", [(5376 + 512 * j, 512)], ("G", j)))

            unit = 0
            for st in range(2):
                for u in range(16):
                    b = u % 2
                    t0 = st * 2048 + u * 128
                    k.dma("sp", xt[b][:], x[t0:t0 + 128, :], w=[f"xt{b}"])
                    k.act(sq[b][:], xt[b][:], AF.Square, r=[f"xt{b}"], w=[f"sq{b}"])
                    k.red("dve", ssx[b][:], sq[b][:], r=[f"sq{b}"], w=[f"ssx{b}"])
                    k.act(rsx[b][:], ssx[b][:], AF.Sqrt, r=[f"ssx{b}", "epsb"], w=[f"rsx{b}"], scale=1.0 / D, bias=epsb[:])
                    k.recip(rsx[b][:], rsx[b][:], r=[f"rsx{b}"], w=[f"rsx{b}"])
                    k.ts("dve", xn[b][:], xt[b][:], rsx[b][:, 0:1], None, ALU.mult, r=[f"xt{b}", f"rsx{b}"], w=[f"xn{b}"])
                    for c in range(8):
                        k.tr(bankT[b][:, c * 128:(c + 1) * 128], xn[b][:, c * 128:(c + 1) * 128], identb[:], r=[f"xn{b}"], w=[f"bT{b}"])
                    for c in range(8):
                        k.act(hT[:, c, u * 128:(u + 1) * 128], bankT[b][:, c * 128:(c + 1) * 128], AF.Identity,
                              r=[f"bT{b}"], w=[f"hT{u}"], scale=s1T[:, c:c + 1], bias=sh1T[:, c:c + 1])
                for pat in range(3):
                    if pat == 0:
                        k.dma("sp", posi[:], pos_pat[0][st], w=["posi"], allow_slow_non_contiguous=True)
                    elif pat == 1:
                        k.dma("sp", posi[:].rearrange("p (a b) -> p a b", a=4), pos_pat[1][st], w=["posi"])
                    else:
                        k.dma("sp", posi[:], pos_pat[2][st], w=["posi"])
                    k.cp("dve", posf[:], posi[:], r=["posi"], w=["posf"])
                    k.tt("dve", arg[:, :, 0:32], bc(posf[:].unsqueeze(2), [128, 16, 32]), bc(invfs[:].unsqueeze(1), [128, 16, 32]), ALU.mult,
                         r=["posf", "invfs"], w=["arg"])
                    k.cp("dve", arg[:, :, 32:64], arg[:, :, 0:32], r=["arg"], w=["arg"])
                    k.tt("dve", arg[:], arg[:], bc(hps[:].unsqueeze(1), [128, 16, 64]), ALU.add, r=["arg", "hps"], w=["arg"])
                    k.ts("dve", argk[:], arg[:], 1.0 / TWO_PI, None, ALU.mult, r=["arg"], w=["argk"])
                    k.cp("dve", argi[:], argk[:], r=["argk"], w=["argi"])
                    k.cp("dve", argk[:], argi[:], r=["argi"], w=["argk"])
                    k.stt("dve", arg[:], argk[:], -C1, arg[:], ALU.mult, ALU.add, r=["argk", "arg"], w=["arg"])
                    k.stt("dve", arg[:], argk[:], -C2, arg[:], ALU.mult, ALU.add, r=["argk", "arg"], w=["arg"])
                    k.ts("dve", argk[:], arg[:], float(np.pi), -TWO_PI, ALU.is_gt, ALU.mult, r=["arg"], w=["argk"])
                    k.tt("dve", arg[:], arg[:], argk[:], ALU.add, r=["arg", "argk"], w=["arg"])
                    k.ts("dve", argk[:], arg[:], float(-np.pi), TWO_PI, ALU.is_lt, ALU.mult, r=["arg"], w=["argk"])
                    k.tt("dve", arg[:], arg[:], argk[:], ALU.add, r=["arg", "argk"], w=["arg"])
                    k.act(tabs[pat][:], arg[:], AF.Sin, r=["arg"], w=[f"tab{pat}"])
                hall = [f"hT{u}" for u in range(16)]
                pending = []
                for ji, (pat, kind, cols, extra) in enumerate(jobs):
                    wbi = ji % 2
                    off = 0
                    for (c0, ncol) in cols:
                        k.dma("pool", wb[wbi][:, :, off:off + ncol], w_in[:, c0:c0 + ncol].rearrange("(c p) n -> p c n", p=128), w=[f"wb{wbi}"])
                        off += ncol
                    ncols = off
                    for u in range(16):
                        ub = unit % 3
                        unit += 1
                        bankP = banks[ub]
                        tc = tokcols(pat, u)
                        rtok = hall if pat else [f"hT{u}"]
                        for c in range(8):
                            k.mm(bankP[:, 0:ncols], hT[:, c, tc], wb[wbi][:, c, 0:ncols], c == 0, c == 7, r=rtok + [f"wb{wbi}"], w=[f"bP{ub}"])
                        p0 = perm0(pat, st, u)

                        def post(ub=ub, bankP=bankP, pat=pat, kind=kind, extra=extra, u=u, p0=p0, ncols=ncols):
                            if kind == "qk":
                                gt = {"A": gA, "QB": gQB, "KB": gKB}[extra[0]]
                                gtn = {"A": "gA", "QB": "gQB", "KB": "gKB"}[extra[0]]
                                pv = bankP[:].rearrange("p (h c) -> p h c", h=8)
                                k.act(sqq[ub][:], pv, AF.Square, r=[f"bP{ub}"], w=[f"sqq{ub}"])
                                k.red("dve", ssq[ub][:], sqq[ub][:], r=[f"sqq{ub}"], w=[f"ssq{ub}"])
                                k.act(rsq[ub][:], ssq[ub][:], AF.Sqrt, r=[f"ssq{ub}", "epsb"], w=[f"rsq{ub}"], scale=1.0 / 64, bias=epsb[:])
                                k.recip(rsq[ub][:], rsq[ub][:], r=[f"rsq{ub}"], w=[f"rsq{ub}"])
                                k.tt("dve", qn[ub][:], pv, bc(rsq[ub][:].unsqueeze(2), [128, 8, 64]), ALU.mult, r=[f"bP{ub}", f"rsq{ub}"], w=[f"qn{ub}"])
                                k.tt("pool", qg[ub][:].rearrange("p h a f -> p h (a f)"), qn[ub][:], gt[:], ALU.mult, r=[f"qn{ub}", gtn], w=[f"qg{ub}"])
                                sinb = bc(tabs[pat][:, u, 0:32].unsqueeze(1), [128, 8, 32])
                                cosb = bc(tabs[pat][:, u, 32:64].unsqueeze(1), [128, 8, 32])
                                x1 = qg[ub][:, :, 0, :]
                                x2 = qg[ub][:, :, 1, :]
                                tn = f"tab{pat}"
                                k.tt("dve", rt[ub][:, 0], x1, cosb, ALU.mult, r=[f"qg{ub}", tn], w=[f"rt0{ub}"])
                                k.tt("dve", rt[ub][:, 1], x2, sinb, ALU.mult, r=[f"qg{ub}", tn], w=[f"rt1{ub}"])
                                k.tt("dve", qr[ub][:, :, 0, :], rt[ub][:, 0], rt[ub][:, 1], ALU.subtract, r=[f"rt0{ub}", f"rt1{ub}"], w=[f"qr{ub}a"])
                                k.tt("pool", rt[ub][:, 2], x2, cosb, ALU.mult, r=[f"qg{ub}", tn], w=[f"rt2{ub}"])
                                k.tt("pool", rt[ub][:, 3], x1, sinb, ALU.mult, r=[f"qg{ub}", tn], w=[f"rt3{ub}"])
                                k.tt("pool", qr[ub][:, :, 1, :], rt[ub][:, 2], rt[ub][:, 3], ALU.add, r=[f"rt2{ub}", f"rt3{ub}"], w=[f"qr{ub}b"])
                                qflat = qr[ub][:].rearrange("p h a f -> p (h a f)")
                                for q4 in range(4):
                                    k.tr(bankT[ub][:, q4 * 128:(q4 + 1) * 128], qflat[:, q4 * 128:(q4 + 1) * 128], identb[:],
                                         r=[f"qr{ub}a", f"qr{ub}b"], w=[f"bT{ub}"])
                                k.act(stg[ub][:].rearrange("p a t -> p (a t)"), bankT[ub][:, 0:512], AF.Copy, r=[f"bT{ub}"], w=[f"stg{ub}"])
                                if extra[0] == "A":
                                    g = extra[1]
                                    k.dma("sp", QTA[g, :, :, p0:p0 + 128].rearrange("a c t -> c a t"), stg[ub][:, 0:2, :], r=[f"stg{ub}"], w=["QTA"])
                                    k.dma("sp", KTA[g, :, :, p0:p0 + 128].rearrange("a c t -> c a t"), stg[ub][:, 2:4, :], r=[f"stg{ub}"], w=["KTA"])
                                else:
                                    dst = QTB if extra[0] == "QB" else KTB
                                    j = extra[1]
                                    k.dma("sp", dst[4 * j:4 * j + 4, :, p0:p0 + 128].rearrange("a c t -> c a t"), stg[ub][:], r=[f"stg{ub}"], w=[extra[0]])
                            elif kind == "v":
                                k.act(vst[ub][:, 0:ncols], bankP[:, 0:ncols], AF.Copy, r=[f"bP{ub}"], w=[f"vst{ub}"])
                                if extra[0] == "A":
                                    k.dma("sp", VA[extra[1], p0:p0 + 128, :], vst[ub][:, 0:256], r=[f"vst{ub}"], w=["VA"])
                                else:
                                    j = extra[1]
                                    k.dma("sp", VB[p0:p0 + 128, 512 * j:512 * j + 512], vst[ub][:], r=[f"vst{ub}"], w=["VB"])
                            else:
                                j = extra[1]
                                k.tt("dve", gpre[ub][:], bankP[:], bgs[:, 512 * j:512 * j + 512], ALU.add, r=[f"bP{ub}", "bgs"], w=[f"gpre{ub}"])
                                k.act(vst[ub][:], gpre[ub][:], AF.Sigmoid, r=[f"gpre{ub}"], w=[f"vst{ub}"])
                                k.dma("sp", GT[p0:p0 + 128, 512 * j:512 * j + 512], vst[ub][:], r=[f"vst{ub}"], w=["GT"])

                        pending.append(post)
                        if len(pending) > 2:
                            pending.pop(0)()
                while pending:
                    pending.pop(0)()
            P.emit(nc, semsets, phase_sem, 1, fin)

        with ExitStack() as es:
            P = Prog()
            k = K(P)
            banks, bankT = alloc_psum(es)
            KTs = [sbt(es, f"KTs{i}", [128, S + 128], BF16) for i in range(2)]
            QTs = [sbt(es, f"QTs{i}", [128, S + 128], BF16) for i in range(2)]
            V1 = [sbt(es, f"V1_{i}", [128, 33, 65], BF16) for i in range(2)]
            mstd = sbt(es, "mstd", [128, 256], F32)
            mbnd = sbt(es, "mbnd", [128, 256], F32)
            Eb = [sbt(es, f"Eb{i}", [128, 256], BF16) for i in range(2)]
            Pm = [sbt(es, f"Pm{i}", [128, 256], BF16) for i in range(2)]
            osb = [sbt(es, f"osb{i}", [128, 65], F32) for i in range(4)]
            k.dma("sp", mstd[:], mask_std, w=["mstd"])
            k.dma("sp", mbnd[:], mask_bnd, w=["mbnd"])
            for i in range(2):
                k.memset("pool", KTs[i][:, 0:64], 0.0, w=[f"KTs{i}"])
                k.memset("pool", KTs[i][:, S + 64:S + 128], 0.0, w=[f"KTs{i}"])
                k.memset("pool", QTs[i][:, 0:64], 0.0, w=[f"QTs{i}"])
                k.memset("pool", QTs[i][:, S + 64:S + 128], 0.0, w=[f"QTs{i}"])
                k.memset("pool", V1[i][:], 0.0, w=[f"V1_{i}"])
                k.memset("pool", V1[i][:, :, 64:65], 1.0, w=[f"V1_{i}"])
            bankSs = [banks[0], banks[2]]
            bankOs = [banks[1], banks[3], banks[4], banks[5]]
            hcount = 0
            ccount = 0
            for g in range(3):
                dil = DIL[g]
                L = S // dil
                for pr in range(2):
                    pb = (g * 2 + pr) % 2
                    k.dma("sp", KTs[pb][:, 64:64 + S], KTA[g, pr], r=["KTA"], w=[f"KTs{pb}"])
                    k.dma("sp", QTs[pb][:, 64:64 + S], QTA[g, pr], r=["QTA"], w=[f"QTs{pb}"])
                    for hh in range(2):
                        hs = pr * 2 + hh
                        bp = 64 * hh
                        vb = hcount % 2
                        hcount += 1
                        vsrc = VA[g, :, hs * 64:(hs + 1) * 64]
                        k.dma("sp", V1[vb][:, 1:32, 0:64], vsrc[64:S - 64, :].rearrange("(j p) c -> p j c", p=128), r=["VA"], w=[f"V1_{vb}"])
                        k.dma("sp", V1[vb][64:128, 0, 0:64], vsrc[0:64, :], r=["VA"], w=[f"V1_{vb}"])
                        k.dma("sp", V1[vb][0:64, 32, 0:64], vsrc[S - 64:S, :], r=["VA"], w=[f"V1_{vb}"])
                        for jc in range(33):
                            sb_ = ccount % 2
                            ccount += 1
                            bnd = (128 * jc) % L == 0
                            qlo = 128 if jc == 0 else 0
                            qhi = 128 if jc == 32 else 256
                            sv = bankSs[sb_][:, 0:256]
                            k.mm(sv[:, qlo:qhi], KTs[pb][bp:bp + 64, 128 * jc:128 * jc + 128],
                                 QTs[pb][bp:bp + 64, 128 * jc - 64 + qlo:128 * jc - 64 + qhi],
                                 True, True, r=[f"KTs{pb}", f"QTs{pb}"], w=[f"bS{sb_}"])
                            k.act(Eb[sb_][:, qlo:qhi], sv[:, qlo:qhi], AF.Exp, r=[f"bS{sb_}"], w=[f"Eb{sb_}"], scale=0.125)
                            mk = mbnd if bnd else mstd
                            k.tt("dve", Pm[sb_][:, qlo:qhi], Eb[sb_][:, qlo:qhi], mk[:, qlo:qhi], ALU.mult, r=[f"Eb{sb_}", "mstd", "mbnd"], w=[f"Pm{sb_}"])
                            for half in range(2):
                                if half * 128 < qlo or half * 128 >= qhi:
                                    continue
                                blk = jc - 1 + half
                                a = blk % 4
                                k.mm(bankOs[a][:, 0:65], Pm[sb_][:, half * 128:half * 128 + 128], V1[vb][:, jc, :],
                                     half == 1, half == 0, r=[f"Pm{sb_}", f"V1_{vb}"], w=[f"bO{a}"])
                            if jc >= 1:
                                blk = jc - 1
                                a = blk % 4
                                k.cp("dve", osb[a][:], bankOs[a][:, 0:65], r=[f"bO{a}"], w=[f"osb{a}"])
                                r_ = (128 * blk) // L
                                j0 = (128 * blk) % L
                                s0 = j0 * dil + r_
                                dst = ND[s0:s0 + 127 * dil + 1:dil, g * 4 + hs, :]
                                k.dma("sp", dst, osb[a][:], r=[f"osb{a}"], w=["ND"])
            P.emit(nc, semsets, phase_sem, 2, fin)

        with ExitStack() as es:
            P = Prog()
            k = K(P)
            banks, bankT = alloc_psum(es, 7, 1)
            KTs = [sbt(es, f"KTd{i}", [128, S], BF16) for i in range(2)]
            QTs = [sbt(es, f"QTd{i}", [128, S], BF16) for i in range(2)]
            V1 = [sbt(es, f"V1d{i}", [128, 32, 129], BF16) for i in range(2)]
            E = [[sbt(es, f"E{m}_{i}", [128, 512], BF16) for i in range(2)] for m in range(2)]
            osb = sbt(es, "osbd", [128, 8, 129], F32)
            rr = sbt(es, "rr", [128, 8], F32)
            o1 = sbt(es, "o1", [128, 4, 128], F32)
            o2 = sbt(es, "o2", [128, 4, 128], F32)
            ssd = sbt(es, "ssd", [128, 4], F32)
            obb = sbt(es, "obb", [128, 4, 128], BF16)
            obT = [sbt(es, f"obT{i}", [128, 512], BF16) for i in range(2)]
            for i in range(2):
                k.memset("pool", V1[i][:, :, 128:129], 1.0, w=[f"V1d{i}"])
            accb = [banks[4], banks[5], banks[6]]
            steps = [(h, qb, kc) for h in range(8) for qb in range(8) for kc in range(32)]

            def qk_exp(n):
                h, qb, kc = steps[n]
                hb = h % 2
                eb = n % 2
                if qb == 0 and kc == 0:
                    k.dma("sp", KTs[hb][:], KTB[h], r=["KB"], w=[f"KTd{hb}"])
                    k.dma("sp", QTs[hb][:], QTB[h], r=["QB"], w=[f"QTd{hb}"])
                    k.dma("sp", V1[hb][:, :, 0:128], VB[:, h * 128:(h + 1) * 128].rearrange("(j p) c -> p j c", p=128), r=["VB"], w=[f"V1d{hb}"])
                for m in range(2):
                    bi = m * 2 + eb
                    tok = f"bS{bi}"
                    k.mm(banks[bi][:], KTs[hb][64 * m:64 * m + 64, kc * 128:(kc + 1) * 128], QTs[hb][64 * m:64 * m + 64, qb * 512:(qb + 1) * 512],
                         True, True, r=[f"KTd{hb}", f"QTd{hb}"], w=[tok])
                    k.act(E[m][eb][:], banks[bi][:], AF.Exp, r=[tok], w=[f"E{m}_{eb}"], scale=0.125)

            deferred = []
            qk_exp(0)
            for n, (h, qb, kc) in enumerate(steps):
                hb = h % 2
                eb = n % 2
                if n + 1 < len(steps):
                    qk_exp(n + 1)
                for m in range(2):
                    for sub in range(4):
                        a = m * 4 + sub
                        k.mm(accb[a // 3][:, (a % 3) * 129:(a % 3) * 129 + 129], E[m][eb][:, sub * 128:(sub + 1) * 128], V1[hb][:, kc, :],
                             kc == 0 and a % 3 == 0, kc == 31, r=[f"E{m}_{eb}", f"V1d{hb}"], w=[f"accb{a // 3}"])
                if kc == 31:
                    for a in range(8):
                        eng = "dve" if (a // 3) % 2 == 0 else "act"
                        src = accb[a // 3][:, (a % 3) * 129:(a % 3) * 129 + 129]
                        if eng == "dve":
                            k.cp("dve", osb[:, a, :], src, r=[f"accb{a // 3}"], w=[f"osbd{a}"])
                        else:
                            k.act(osb[:, a, :], src, AF.Copy, r=[f"accb{a // 3}"], w=[f"osbd{a}"])
                    k.recip(rr[:], osb[:, :, 128], r=[f"osbd{a_}" for a_ in range(8)], w=["rr"])
                    k.ts("dve", rr[:, 4:8], rr[:, 4:8], neglam[:, 0:1], None, ALU.mult, r=["rr"], w=["rr"])
                    k.tt("dve", o1[:], osb[:, 0:4, 0:128], bc(rr[:, 0:4].unsqueeze(2), [128, 4, 128]), ALU.mult, r=[f"osbd{a_}" for a_ in range(8)] + ["rr"], w=["o1"])
                    k.tt("pool", o2[:], osb[:, 4:8, 0:128], bc(rr[:, 4:8].unsqueeze(2), [128, 4, 128]), ALU.mult, r=[f"osbd{a_}" for a_ in range(8)] + ["rr"], w=["o2"])
                    k.tt("dve", o1[:], o1[:], o2[:], ALU.add, r=["o1", "o2"], w=["o1"])
                    k.tt("pool", o2[:], o1[:], o1[:], ALU.mult, r=["o1"], w=["o2"])
                    k.red("dve", ssd[:], o2[:], r=["o2"], w=["ssd"])
                    k.act(ssd[:], ssd[:], AF.Sqrt, r=["ssd", "epsb"], w=["ssd"], scale=1.0 / 128, bias=epsb[:])
                    k.recip(ssd[:], ssd[:], r=["ssd"], w=["ssd"])
                    k.tt("dve", o1[:], o1[:], bc(ssd[:].unsqueeze(2), [128, 4, 128]), ALU.mult, r=["o1", "ssd"], w=["o1"])
                    k.tt("pool", obb[:], o1[:], bc(sgain[:].unsqueeze(1), [128, 4, 128]), ALU.mult, r=["o1"], w=["obb"])
                    def fin_tr(h=h, qb=qb):
                        ob_i = (h * 8 + qb) % 2
                        for sub in range(4):
                            k.tr(bankT[0][:, sub * 128:(sub + 1) * 128], obb[:, sub, :], identb[:], r=["obb"], w=["bTd"])
                        k.act(obT[ob_i][:], bankT[0][:, 0:512], AF.Copy, r=["bTd"], w=[f"obT{ob_i}"])
                        k.dma("sp", OBT[h, :, qb * 512:(qb + 1) * 512], obT[ob_i][:], r=[f"obT{ob_i}"], w=["OBT"])
                    deferred.append((n + 6, fin_tr))
                while deferred and (deferred[0][0] <= n or n == len(steps) - 1):
                    deferred.pop(0)[1]()
            P.emit(nc, semsets, phase_sem, 3, fin)

        with ExitStack() as es:
            P = Prog()
            k = K(P)
            banks, bankT = alloc_psum(es)
            Wc = sbt(es, "Wc", [128, 8, 2048], BF16)
            wpa = sbt(es, "wpa", [128, 2, D], BF16)
            wpb = sbt(es, "wpb", [128, 8, D], BF16)
            wo = sbt(es, "wo", [128, 8, D], BF16)
            wq = [sbt(es, f"wq{i}", [128, D], F32) for i in range(2)]
            sk = [sbt(es, f"sk{i}", [128, 128], F32) for i in range(2)]
            nd = [sbt(es, f"nd{i}", [128, 12, 65], F32) for i in range(2)]
            obt = [sbt(es, f"obt{i}", [128, 8, 128], BF16) for i in range(2)]
            gts = [sbt(es, f"gts{i}", [128, 2048], BF16) for i in range(2)]
            xts = [sbt(es, f"xts{i}", [128, D], F32) for i in range(2)]
            nsum = sbt(es, "nsum", [128, 4, 65], F32)
            rden = sbt(es, "rden", [128, 4], F32)
            oab = sbt(es, "oab", [128, 4, 64], BF16)
            oaT = sbt(es, "oaT", [128, 2, 128], BF16)
            bra = sbt(es, "bra", [128, D], F32)
            t1 = sbt(es, "t1", [128, D], F32)
            t2 = sbt(es, "t2", [128, D], F32)
            mixb = sbt(es, "mixb", [128, D], BF16)
            mixT = sbt(es, "mixT", [128, 8, 128], BF16)
            x1s = sbt(es, "x1s", [128, D], F32)
            sq2 = sbt(es, "sq2", [128, D], F32)
            ss2 = sbt(es, "ss2", [128, 1], F32)
            xn2 = sbt(es, "xn2", [128, D], BF16)
            h2T = sbt(es, "h2T", [128, 8, 128], BF16)
            Ssb = sbt(es, "Ssb", [128, 8, 2, 128], F32)
            mr = sbt(es, "mr", [128, 128], F32)
            t16 = sbt(es, "t16", [128, 8, 2, 16], F32)
            cand = sbt(es, "cand", [128, 8, 16, 16], F32)
            mr2 = sbt(es, "mr2", [128, 256], F32)
            c16 = sbt(es, "c16", [128, 8, 16], F32)
            dd = sbt(es, "dd", [128, 8, 16], F32)
            zz = sbt(es, "zz", [128, 8], F32)
            tau = sbt(es, "tau", [128, 8], F32)
            cf = sbt(es, "cf", [128, 8], F32)
            k.dma("pool", wpa[:], w_pa.rearrange("(c p) n -> p c n", p=128), w=["wpa"])
            k.dma("pool", wpb[:], w_pb.rearrange("(c p) n -> p c n", p=128), w=["wpb"])
            k.dma("pool", wo[:], w_out.rearrange("(c p) n -> p c n", p=128), w=["wo"])
            for kk in range(16):
                b = kk % 2
                k.dma("sp", wq[b][:], wqT[kk], w=[f"wq{b}"])
                k.dma("sp", sk[b][:], skT[kk], w=[f"sk{b}"])
                for dc in range(8):
                    k.mm(banks[dc // 4][:, (dc % 4) * 128:(dc % 4 + 1) * 128], wq[b][:, dc * 128:(dc + 1) * 128], sk[b][:], True, True,
                         r=[f"wq{b}", f"sk{b}"], w=[f"wcp{dc // 4}"])
                for hh in range(2):
                    k.act(Wc[:, 4 * hh:4 * hh + 4, kk * 128:(kk + 1) * 128], banks[hh][:].rearrange("p (a n) -> p a n", a=4), AF.Copy,
                          r=[f"wcp{hh}"], w=["Wc"])
            for t in range(NT):
                b = t % 2
                t0 = t * 128
                k.dma("sp", nd[b][:], ND[t0:t0 + 128], r=["ND"], w=[f"nd{b}"])
                k.dma("sp", obt[b][:], OBT[:, :, t0:t0 + 128].rearrange("h c t -> c h t"), r=["OBT"], w=[f"obt{b}"])
                k.dma("sp", gts[b][:], GT[t0:t0 + 128, :], r=["GT"], w=[f"gts{b}"])
                k.dma("sp", xts[b][:], x[t0:t0 + 128, :], w=[f"xts{b}"])
                k.tt("pool", nsum[:], nd[b][:, 0:4, :], nd[b][:, 4:8, :], ALU.add, r=[f"nd{b}"], w=["nsum"])
                k.tt("pool", nsum[:], nsum[:], nd[b][:, 8:12, :], ALU.add, r=[f"nd{b}", "nsum"], w=["nsum"])
                k.recip(rden[:], nsum[:, :, 64], r=["nsum"], w=["rden"])
                k.tt("dve", oab[:], nsum[:, :, 0:64], bc(rden[:].unsqueeze(2), [128, 4, 64]), ALU.mult, r=["nsum", "rden"], w=["oab"])
                oaf = oab[:].rearrange("p a c -> p (a c)")
                for c in range(2):
                    k.tr(bankT[0][:, c * 128:(c + 1) * 128], oaf[:, c * 128:(c + 1) * 128], identb[:], r=["oab"], w=["bT0"])
                k.act(oaT[:].rearrange("p a t -> p (a t)"), bankT[0][:, 0:256], AF.Copy, r=["bT0"], w=["oaT"])
                for hf in range(2):
                    for c in range(2):
                        k.mm(banks[hf][:], oaT[:, c, :], wpa[:, c, hf * 512:(hf + 1) * 512], c == 0, c == 1, r=["oaT", "wpa"], w=[f"bk{hf}"])
                for hf in range(2):
                    for c in range(8):
                        k.mm(banks[2 + hf][:], obt[b][:, c, :], wpb[:, c, hf * 512:(hf + 1) * 512], c == 0, c == 7, r=[f"obt{b}", "wpb"], w=[f"bk{2 + hf}"])
                for hf in range(2):
                    sl = slice(hf * 512, (hf + 1) * 512)
                    k.act(bra[:, sl], banks[hf][:], AF.Copy, r=[f"bk{hf}"], w=["bra"])
                    k.tt("dve", t2[:, sl], banks[2 + hf][:], gts[b][:, 1024 + hf * 512:1024 + (hf + 1) * 512], ALU.mult, r=[f"bk{2 + hf}", f"gts{b}"], w=["t2"])
                k.tt("pool", t1[:], bra[:], gts[b][:, 0:1024], ALU.mult, r=["bra", f"gts{b}"], w=["t1"])
                k.tt("pool", mixb[:], t1[:], t2[:], ALU.add, r=["t1", "t2"], w=["mixb"])
                for c in range(8):
                    k.tr(bankT[1][:, c * 128:(c + 1) * 128], mixb[:, c * 128:(c + 1) * 128], identb[:], r=["mixb"], w=["bT1"])
                k.act(mixT[:].rearrange("p a t -> p (a t)"), bankT[1][:], AF.Copy, r=["bT1"], w=["mixT"])
                for hf in range(2):
                    for c in range(8):
                        k.mm(banks[4 + hf][:], mixT[:, c, :], wo[:, c, hf * 512:(hf + 1) * 512], c == 0, c == 7, r=["mixT", "wo"], w=[f"bk{4 + hf}"])
                for hf in range(2):
                    sl = slice(hf * 512, (hf + 1) * 512)
                    k.tt("dve", t2[:, sl], banks[4 + hf][:], G1row[:, sl], ALU.mult, r=[f"bk{4 + hf}"], w=["t2"])
                k.tt("pool", x1s[:], t2[:], xts[b][:], ALU.add, r=["t2", f"xts{b}"], w=["x1s"])
                k.dma("sp", X1[t0:t0 + 128, :], x1s[:], r=["x1s"], w=["X1"])
                k.act(sq2[:], x1s[:], AF.Square, r=["x1s"], w=["sq2"])
                k.red("dve", ss2[:], sq2[:], r=["sq2"], w=["ss2"])
                k.act(ss2[:], ss2[:], AF.Sqrt, r=["ss2", "epsb"], w=["ss2"], scale=1.0 / D, bias=epsb[:])
                k.recip(ss2[:], ss2[:], r=["ss2"], w=["ss2"])
                k.ts("dve", xn2[:], x1s[:], ss2[:, 0:1], None, ALU.mult, r=["x1s", "ss2"], w=["xn2"])
                for c in range(8):
                    k.tr(bankT[0][:, c * 128:(c + 1) * 128], xn2[:, c * 128:(c + 1) * 128], identb[:], r=["xn2"], w=["bT0"])
                for c in range(8):
                    k.act(h2T[:, c, :], bankT[0][:, c * 128:(c + 1) * 128], AF.Identity, r=["bT0"], w=["h2T"], scale=s2T[:, c:c + 1], bias=sh2T[:, c:c + 1])
                k.dma("sp", H2T[:, :, t0:t0 + 128].rearrange("c p t -> p c t"), h2T[:], r=["h2T"], w=["H2T"])
                for nb in range(4):
                    for c in range(8):
                        k.mm(banks[nb][:], h2T[:, c, :], Wc[:, c, nb * 512:(nb + 1) * 512], c == 0, c == 7, r=["h2T", "Wc"], w=[f"bk{nb}"])
                Sf = Ssb[:].rearrange("p h a n -> p (h a n)")
                for nb in range(4):
                    k.act(Sf[:, nb * 512:(nb + 1) * 512], banks[nb][:], AF.Copy, r=[f"bk{nb}"], w=["Ssb"])
                for h in range(8):
                    for a in range(2):
                        k.max8(t16[:, h, a, 0:8], Ssb[:, h, a, :], r=["Ssb"], w=["t16"])
                        k.mrep(mr[:], t16[:, h, a, 0:8], Ssb[:, h, a, :], r=["Ssb", "t16"], w=["mr"])
                        k.max8(t16[:, h, a, 8:16], mr[:], r=["mr"], w=["t16"])
                k.tt("dve", cand[:], bc(t16[:, :, 0, :].unsqueeze(3), [128, 8, 16, 16]), bc(t16[:, :, 1, :].unsqueeze(2), [128, 8, 16, 16]), ALU.add,
                     r=["t16"], w=["cand"])
                for h in range(8):
                    cv = cand[:, h].rearrange("p a b -> p (a b)")
                    k.max8(c16[:, h, 0:8], cv, r=["cand"], w=["c16"])
                    k.mrep(mr2[:], c16[:, h, 0:8], cv, r=["cand", "c16"], w=["mr2"])
                    k.max8(c16[:, h, 8:16], mr2[:], r=["mr2"], w=["c16"])
                k.ts("dve", tau[:], c16[:, :, 15], -1e-5, None, ALU.add, r=["c16"], w=["tau"])
                k.tt("dve", dd[:], c16[:], bc(c16[:, :, 0:1], [128, 8, 16]), ALU.subtract, r=["c16"], w=["dd"])
                k.act(dd[:], dd[:], AF.Exp, r=["dd"], w=["dd"])
                k.red("dve", zz[:], dd[:], r=["dd"], w=["zz"])
                k.recip(zz[:], zz[:], r=["zz"], w=["zz"])
                k.tt("dve", cf[:], tau[:], c16[:, :, 0], ALU.subtract, r=["tau", "c16"], w=["cf"])
                k.act(cf[:], cf[:], AF.Exp, r=["cf"], w=["cf"])
                k.tt("dve", cf[:], cf[:], zz[:], ALU.mult, r=["cf", "zz"], w=["cf"])
                k.tt("dve", Ssb[:, :, 0, :], Ssb[:, :, 0, :], bc(tau[:].unsqueeze(2), [128, 8, 128]), ALU.subtract, r=["Ssb", "tau"], w=["Ssb"])
                k.dma("sp", PS[t0:t0 + 128], Ssb[:], r=["Ssb"], w=["PS"])
                k.dma("sp", CF[t0:t0 + 128, :], cf[:], r=["cf"], w=["CF"])
            P.emit(nc, semsets, phase_sem, 4, fin)

        with ExitStack() as es:
            P = Prog()
            k = K(P)
            banks, bankT = alloc_psum(es, 8, 0)
            NB = 4
            h2 = [sbt(es, f"h2_{i}", [128, 8, 256], BF16) for i in range(2)]
            pss = sbt(es, "pss", [128, 2, 8, 2, 128], F32)
            cfs = [sbt(es, f"cfs{i}", [128, 2, 8], F32) for i in range(2)]
            x1t = sbt(es, "x1t", [128, 2, D], F32)
            Dm = [sbt(es, f"Dm{i}", [128, 2, 8, 128], BF16) for i in range(2)]
            UTi = [sbt(es, f"UTi{i}", [128, 8, 128], BF16) for i in range(NB)]
            Vi = [sbt(es, f"Vi{i}", [128, D], BF16) for i in range(NB)]
            HgA = sbt(es, "HgA", [128, 128, 256], BF16)
            zt = [sbt(es, f"zt{i}", [128, 2, 8, 128], BF16) for i in range(3)]
            Et = [sbt(es, f"Et{i}", [128, 2, 8, 128], BF16) for i in range(2)]
            Gt = [sbt(es, f"Gt{i}", [128, 2, 8, 128], BF16) for i in range(3)]
            WT = [sbt(es, f"WT{i}", [128, 256], BF16) for i in range(2)]
            yo = [sbt(es, f"yo{i}", [128, D], F32) for i in range(2)]
            P.add("sp", lambda e: e.wait_ge(conv_sem, 16 * 32))
            UTv = UTb.rearrange("(i p) (c e) -> i p c e", p=128, c=8)
            ustep = 0
            vstep = 0
            for tt_ in range(16):
                tb = tt_ % 2
                t0 = tt_ * 256
                k.dma("sp", h2[tb][:], H2T[:, :, t0:t0 + 256].rearrange("c p t -> p c t"), w=[f"h2_{tb}"])
                k.dma("sp", cfs[tb][:], CF[t0:t0 + 256, :].rearrange("(s p) h -> p s h", p=128), w=[f"cfs{tb}"])
                for s_ in range(2):
                    for h in range(8):
                        k.ts("pool", Dm[tb][:, s_, h, :], identf[:], cfs[tb][:, s_, h:h + 1], None, ALU.mult, r=[f"cfs{tb}"], w=[f"Dm{tb}"])
                k.dma("sp", pss[:], PS[t0:t0 + 256].rearrange("(s p) h a n -> p s h a n", p=128), w=["pss"])
                for i in range(128):
                    ub = ustep % NB
                    hb_ = ustep % 2
                    ustep += 1
                    k.dma("sp", UTi[ub][:], UTv[i], w=[f"UTi{ub}"])
                    hv = banks[4 + hb_][:, 0:256]
                    for c in range(8):
                        k.mm(hv, UTi[ub][:, c, :], h2[tb][:, c, :], c == 0, c == 7, r=[f"UTi{ub}", f"h2_{tb}"], w=[f"bH{hb_}"])
                    k.act(HgA[:, i, :], hv, AF.Gelu, r=[f"bH{hb_}"], w=[f"HgA{i}"])

                def st_z(i):
                    zb = i % 3
                    k.tt("pool", zt[zb][:, 0], pss[:, 0, :, 1, :], bc(pss[:, 0, :, 0, i:i + 1], [128, 8, 128]), ALU.add, r=["pss"], w=[f"zt{zb}a"])
                    k.tt("dve", zt[zb][:, 1], pss[:, 1, :, 1, :], bc(pss[:, 1, :, 0, i:i + 1], [128, 8, 128]), ALU.add, r=["pss"], w=[f"zt{zb}b"])

                def st_eg(i):
                    zb = i % 3
                    eb = i % 2
                    k.act(Et[eb][:], zt[zb][:], AF.Exp, r=[f"zt{zb}a", f"zt{zb}b"], w=[f"Et{eb}"])
                    k.stt("dve", Gt[zb][:], zt[zb][:], 0.0, Et[eb][:], ALU.is_ge, ALU.mult, r=[f"zt{zb}a", f"zt{zb}b", f"Et{eb}"], w=[f"Gt{zb}"])

                def st_gt(i):
                    zb = i % 3
                    b2 = i % 2
                    gv = banks[6 + b2][:, 0:256]
                    for s_ in range(2):
                        for h in range(8):
                            k.mm(gv[:, s_ * 128:(s_ + 1) * 128], Gt[zb][:, s_, h, :], Dm[tb][:, s_, h, :], h == 0, h == 7,
                                 r=[f"Gt{zb}", f"Dm{tb}"], w=[f"bG{b2}"])

                def st_wt(i):
                    b2 = i % 2
                    gv = banks[6 + b2][:, 0:256]
                    k.tt("dve", WT[b2][:], gv, HgA[:, i, :], ALU.mult, r=[f"bG{b2}", f"HgA{i}"], w=[f"WT{b2}"])

                vbuf = {}

                def st_vload(i):
                    nonlocal vstep
                    vb_ = vstep % NB
                    vstep += 1
                    vbuf[i] = vb_
                    k.dma("sp", Vi[vb_][:], Vb[i * 128:(i + 1) * 128, :], w=[f"Vi{vb_}"])

                def st_y(i):
                    b2 = i % 2
                    vb_ = vbuf[i]
                    for s_ in range(2):
                        for hf in range(2):
                            k.mm(banks[s_ * 2 + hf][:], WT[b2][:, s_ * 128:(s_ + 1) * 128], Vi[vb_][:, hf * 512:(hf + 1) * 512], i == 0, i == 127,
                                 r=[f"WT{b2}", f"Vi{vb_}"], w=[f"bY{s_ * 2 + hf}"])

                for it in range(128 + 3):
                    if it < 128:
                        st_vload(it)
                        st_z(it)
                    if 1 <= it <= 128:
                        st_eg(it - 1)
                    if 2 <= it <= 129:
                        st_gt(it - 2)
                    if 3 <= it <= 130:
                        st_y(it - 3)
                    if 2 <= it <= 129:
                        st_wt(it - 2)
                k.dma("sp", x1t[:], X1[t0:t0 + 256, :].rearrange("(s p) d -> p s d", p=128), w=["x1t"])
                for s_ in range(2):
                    for hf in range(2):
                        sl = slice(hf * 512, (hf + 1) * 512)
                        k.tt("dve", yo[s_][:, sl], banks[s_ * 2 + hf][:], G2row[:, sl], ALU.mult, r=[f"bY{s_ * 2 + hf}"], w=[f"yo{s_}"])
                    k.tt("pool", yo[s_][:], yo[s_][:], x1t[:, s_, :], ALU.add, r=[f"yo{s_}", "x1t"], w=[f"yo{s_}"])
                    k.dma("sp", out[t0 + s_ * 128:t0 + (s_ + 1) * 128, :], yo[s_][:], r=[f"yo{s_}"], w=["out"])
            P.emit(nc, semsets, phase_sem, 5, fin)
    return nc


DEBUG_OUT = set()


def _masks():
    kk = np.arange(128)[:, None]
    qq = np.arange(256)[None, :]
    std = ((kk <= qq) & (qq <= kk + 128)).astype(np.float32)
    blk = (((kk < 64) & (qq < 128)) | ((kk >= 64) & (qq >= 128))).astype(np.float32)
    return std, std * blk


def prep_inputs(inputs):
    f = lambda a: np.ascontiguousarray(np.asarray(a), dtype=np.float32)
    x = f(inputs["x"])
    c = f(inputs["c"])
    pos = np.ascontiguousarray(np.asarray(inputs["positions"]), dtype=np.int32)
    L = 0
    rep = lambda v, n=128: np.ascontiguousarray(np.broadcast_to(f(v)[None], (n,) + f(v).shape))
    qn_a, kn_a = f(inputs["qn_a"])[L], f(inputs["kn_a"])[L]
    qn_b, kn_b = f(inputs["qn_b"])[L], f(inputs["kn_b"])[L]
    gainA = np.concatenate([np.tile(qn_a[None], (4, 1)), np.tile(kn_a[None], (4, 1))], 0)
    gainQB = np.tile(qn_b[None], (8, 1))
    gainKB = np.tile(kn_b[None], (8, 1))
    lam = np.stack([f(inputs["lam_q1"])[L], f(inputs["lam_k1"])[L], f(inputs["lam_q2"])[L], f(inputs["lam_k2"])[L]], 0)
    invf = (1.0 / (10000.0 ** (np.arange(0, 64, 2, dtype=np.float32) / 64))).astype(np.float32)
    hp = np.concatenate([np.zeros(32, np.float32), np.full(32, np.pi / 2, np.float32)])
    mstd, mbnd = _masks()
    wq = f(inputs["w_query"])[L]
    wqT = np.ascontiguousarray(wq.reshape(D, 16, 128).transpose(1, 2, 0))
    sk = f(inputs["sub_keys"])[L].reshape(16, 128, 128)
    skT = np.ascontiguousarray(sk.transpose(0, 2, 1))
    U = f(inputs["expert_u"])[L]
    UT = np.ascontiguousarray(U.reshape(128, 128, 8, 128).transpose(0, 3, 2, 1)).reshape(16384, 1024)
    shared = dict(
        w_ada=f(inputs["w_ada"])[L],
        b_adaT=np.ascontiguousarray(f(inputs["b_ada"])[L].reshape(48, 128).T),
        n1gT=np.ascontiguousarray(f(inputs["norm1_g"])[L].reshape(8, 128).T),
        n2gT=np.ascontiguousarray(f(inputs["norm2_g"])[L].reshape(8, 128).T),
        w_in=f(inputs["w_in"])[L],
        b_gate_bc=rep(f(inputs["b_gate"])[L]),
        gainA=rep(gainA), gainQB=rep(gainQB), gainKB=rep(gainKB),
        lam_bc=rep(lam), subln_bc=rep(f(inputs["subln_g"])[L]),
        invf_bc=rep(invf), halfpi=rep(hp), mask_std=mstd, mask_bnd=mbnd,
        w_pa=f(inputs["w_proj_a"])[L], w_pb=f(inputs["w_proj_b"])[L], w_out=f(inputs["w_out"])[L],
        wqT=wqT, skT=skT, UT=UT, Vx=f(inputs["expert_v"])[L],
    )
    in_maps = []
    for b in range(8):
        m = dict(shared)
        m["x"] = x[b]
        m["pos"] = pos[b]
        m["cT"] = np.ascontiguousarray(c[b].reshape(8, 128).T)
        in_maps.append(m)
    return in_maps


def kernel(**inputs):
    in_maps = prep_inputs(inputs)
    nc = build_program()
    res = run_bass_kernel_spmd(nc, in_maps, core_ids=list(range(8)))
    return np.stack([np.asarray(r["out"], dtype=np.float32) for r in res.results], 0)
```

```python
import os
import numpy as np
from contextlib import ExitStack
import concourse.bass as bass
import concourse.mybir as mybir
from concourse.bass_utils import run_bass_kernel_spmd

F32 = mybir.dt.float32
BF16 = mybir.dt.bfloat16
I32 = mybir.dt.int32
AF = mybir.ActivationFunctionType
ALU = mybir.AluOpType
AX = mybir.AxisListType

S = 4096
D = 1024
NT = 32
EPS = 1e-6
LAM_INIT = 0.2
DIL = (1, 4, 16)
N_DMA_SEMS = 8
QUEUES = ("sp", "pool", "act")
TWO_PI = float(2 * np.pi)
C1 = 6.28125
C2 = float(2 * np.pi - 6.28125)


class Prog:
    def __init__(self):
        self.ops = []
        self.last_writer = {}
        self.readers = {}

    def add(self, eng, fn, reads=(), writes=(), dma=False):
        idx = len(self.ops)
        deps = set()
        raw = set()
        for t in reads:
            w = self.last_writer.get(t)
            if w is not None:
                deps.add(w)
                raw.add(w)
        for t in writes:
            w = self.last_writer.get(t)
            if w is not None:
                deps.add(w)
            for r in self.readers.get(t, ()):
                deps.add(r)
        op = dict(eng=eng, fn=fn, dma=dma, deps=deps, raw=raw, signal=False)
        self.ops.append(op)
        for t in reads:
            self.readers.setdefault(t, []).append(idx)
        for t in writes:
            self.last_writer[t] = idx
            self.readers[t] = []
        return idx

    def emit(self, nc, semsets, phase_sem, phase_idx, fin):
        sems, cnt = semsets[phase_idx % 3]
        ops = self.ops
        for op in ops:
            keep = set()
            for d in op["deps"]:
                p = ops[d]
                if p["dma"] or op["dma"] or p["eng"] != op["eng"]:
                    keep.add(d)
                elif op["eng"] != "pe" and d in op["raw"]:
                    keep.add(d)
            op["deps"] = keep
            for d in keep:
                ops[d]["signal"] = True
        dma_k = {}
        prev_slot = {}
        for op in ops:
            if op["dma"]:
                q = op["eng"]
                k = dma_k.get(q, 0)
                dma_k[q] = k + 1
                key = ("dma", q, k % N_DMA_SEMS)
                cnt[key] = cnt.get(key, 0) + 16
                op["prev"] = prev_slot.get(key)
                op["sig"] = (key, cnt[key])
                prev_slot[key] = op["sig"]
            elif op["signal"]:
                key = op["eng"]
                cnt[key] = cnt.get(key, 0) + 1
                op["sig"] = (key, cnt[key])

        def semof(key):
            if isinstance(key, tuple):
                return sems["dma_" + key[1]][key[2]]
            return sems[key]

        streams = {}
        for op in ops:
            streams.setdefault(op["eng"], []).append(op)

        def run_stream(engname, eng):
            if phase_idx > 0:
                eng.wait_ge(phase_sem, 19 * phase_idx)
            known = {}
            for op in streams.get(engname, []):
                waits = {}
                for d in op["deps"]:
                    key, val = ops[d]["sig"]
                    if waits.get(key, 0) < val:
                        waits[key] = val
                if op["dma"] and op["prev"] is not None:
                    key, val = op["prev"]
                    if waits.get(key, 0) < val:
                        waits[key] = val
                for key, val in waits.items():
                    if known.get(key, 0) >= val:
                        continue
                    eng.wait_ge(semof(key), val)
                    known[key] = val
                ins = op["fn"](eng)
                if "sig" in op:
                    ins.then_inc(semof(op["sig"][0]), 16 if op["dma"] else 1)
            if engname == "sp":
                for key, val in cnt.items():
                    if isinstance(key, tuple):
                        eng.wait_ge(semof(key), val)
                eng.dma_start(out=fin["d1"][:], in_=fin["d0"][:]).then_inc(phase_sem, 16)
            elif engname == "pe":
                pass
            elif engname == "act":
                eng.activation(out=fin["act"][:], in_=fin["d0"][0:1, 0:1].to_broadcast([1, 1]) if False else fin["act"][:], func=AF.Copy).then_inc(phase_sem, 1)
            else:
                eng.memset(fin[engname][:], 0.0).then_inc(phase_sem, 1)

        with nc.Block() as block:
            @block.sync
            def _(e):
                run_stream("sp", e)

            @block.tensor
            def _(e):
                run_stream("pe", e)

            @block.scalar
            def _(e):
                run_stream("act", e)

            @block.vector
            def _(e):
                run_stream("dve", e)

            @block.gpsimd
            def _(e):
                run_stream("pool", e)


class K:
    def __init__(self, P):
        self.P = P

    def dma(self, q, out, in_, r=(), w=(), **kw):
        self.P.add(q, lambda e: e.dma_start(out=out, in_=in_, **kw), r, w, dma=True)

    def mm(self, out, lhsT, rhs, start, stop, r=(), w=()):
        self.P.add("pe", lambda e: e.matmul(out, lhsT=lhsT, rhs=rhs, start=start, stop=stop), r, w)

    def tr(self, out, in_, ident, r=(), w=()):
        self.P.add("pe", lambda e: e.transpose(out=out, in_=in_, identity=ident), r, w)

    def act(self, out, in_, func, r=(), w=(), **kw):
        self.P.add("act", lambda e: e.activation(out=out, in_=in_, func=func, **kw), r, w)

    def tt(self, eng, out, in0, in1, op, r=(), w=()):
        self.P.add(eng, lambda e: e.tensor_tensor(out=out, in0=in0, in1=in1, op=op), r, w)

    def ts(self, eng, out, in0, s1, s2, op0, op1=None, r=(), w=()):
        if op1 is None:
            self.P.add(eng, lambda e: e.tensor_scalar(out=out, in0=in0, scalar1=s1, scalar2=None, op0=op0), r, w)
        else:
            self.P.add(eng, lambda e: e.tensor_scalar(out=out, in0=in0, scalar1=s1, scalar2=s2, op0=op0, op1=op1), r, w)

    def stt(self, eng, out, in0, scalar, in1, op0, op1, r=(), w=()):
        self.P.add(eng, lambda e: e.scalar_tensor_tensor(out=out, in0=in0, scalar=scalar, in1=in1, op0=op0, op1=op1), r, w)

    def cp(self, eng, out, in_, r=(), w=()):
        self.P.add(eng, lambda e: e.tensor_copy(out=out, in_=in_), r, w)

    def red(self, eng, out, in_, r=(), w=(), op=None):
        op = op or ALU.add
        self.P.add(eng, lambda e: e.tensor_reduce(out=out, in_=in_, axis=AX.X, op=op), r, w)

    def recip(self, out, in_, r=(), w=()):
        self.P.add("dve", lambda e: e.reciprocal(out=out, in_=in_), r, w)

    def memset(self, eng, out, val, r=(), w=()):
        self.P.add(eng, lambda e: e.memset(out, val), r, w)

    def max8(self, out, in_, r=(), w=()):
        self.P.add("dve", lambda e: e.max(out=out, in_=in_), r, w)

    def mrep(self, out, rep, vals, r=(), w=()):
        self.P.add("dve", lambda e: e.match_replace(out=out, in_to_replace=rep, in_values=vals, imm_value=-1e30), r, w)


def bc(ap, shape):
    return ap.to_broadcast(list(shape))


def build_program(debug=False):
    nc = bass.Bass("TRN2", target_bir_lowering=False)

    def din(name, shape, dt=F32):
        return nc.dram_tensor(name, list(shape), dt, kind="ExternalInput").ap()

    def dscr(name, shape, dt):
        kind = "ExternalOutput" if (debug and name in DEBUG_OUT) else "Internal"
        return nc.dram_tensor(name, list(shape), dt, kind=kind).ap()

    x = din("x", [S, D])
    pos = din("pos", [S], I32)
    cT = din("cT", [128, 8])
    w_ada = din("w_ada", [D, 6 * D])
    b_adaT = din("b_adaT", [128, 48])
    n1gT = din("n1gT", [128, 8])
    n2gT = din("n2gT", [128, 8])
    w_in = din("w_in", [D, 7424])
    b_gate_bc = din("b_gate_bc", [128, 2048])
    gainA = din("gainA", [128, 8, 64])
    gainQB = din("gainQB", [128, 8, 64])
    gainKB = din("gainKB", [128, 8, 64])
    lam_bc = din("lam_bc", [128, 4, 64])
    subln_bc = din("subln_bc", [128, 128])
    invf_bc = din("invf_bc", [128, 32])
    halfpi = din("halfpi", [128, 64])
    mask_std = din("mask_std", [128, 256])
    mask_bnd = din("mask_bnd", [128, 256])
    w_pa = din("w_pa", [256, D])
    w_pb = din("w_pb", [D, D])
    w_out = din("w_out", [D, D])
    wqT = din("wqT", [16, 128, D])
    skT = din("skT", [16, 128, 128])
    UT = din("UT", [16384, 1024])
    Vx = din("Vx", [16384, 1024])
    out = nc.dram_tensor("out", [S, D], F32, kind="ExternalOutput").ap()

    QTA = dscr("QTA", [3, 2, 128, S], BF16)
    KTA = dscr("KTA", [3, 2, 128, S], BF16)
    VA = dscr("VA", [3, S, 256], BF16)
    QTB = dscr("QTB", [8, 128, S], BF16)
    KTB = dscr("KTB", [8, 128, S], BF16)
    VB = dscr("VB", [S, 1024], BF16)
    GT = dscr("GT", [S, 2048], BF16)
    ND = dscr("ND", [S, 12, 65], F32)
    OBT = dscr("OBT", [8, 128, S], BF16)
    X1 = dscr("X1", [S, D], F32)
    H2T = dscr("H2T", [8, 128, S], BF16)
    PS = dscr("PS", [S, 8, 2, 128], F32)
    CF = dscr("CF", [S, 8], F32)
    UTb = dscr("UTb", [16384, 1024], BF16)
    Vb = dscr("Vb", [16384, 1024], BF16)

    with ExitStack() as top:
        def sbt(es, name, shape, dt):
            return es.enter_context(nc.sbuf_tensor(name, list(shape), dt))

        def pst(es, name, shape, dt):
            return es.enter_context(nc.psum_tensor(name, list(shape), dt))

        semsets = []
        for ph in range(3):
            ss = {}
            for e in ("pe", "act", "dve", "pool"):
                ss[e] = top.enter_context(nc.semaphore(f"s{ph}_{e}"))
            for q in QUEUES:
                ss["dma_" + q] = [top.enter_context(nc.semaphore(f"d{ph}_{q}_{i}")) for i in range(N_DMA_SEMS)]
            semsets.append((ss, {}))
        phase_sem = top.enter_context(nc.semaphore("phase"))
        conv_sem = top.enter_context(nc.semaphore("conv"))

        identb = sbt(top, "identb", [128, 128], BF16)
        identf = sbt(top, "identf", [128, 128], F32)
        onesf = sbt(top, "onesf", [128, 128], F32)
        s1T = sbt(top, "s1T", [128, 8], F32)
        sh1T = sbt(top, "sh1T", [128, 8], F32)
        s2T = sbt(top, "s2T", [128, 8], F32)
        sh2T = sbt(top, "sh2T", [128, 8], F32)
        G1row = sbt(top, "G1row", [128, D], F32)
        G2row = sbt(top, "G2row", [128, D], F32)
        neglam = sbt(top, "neglam", [128, 1], F32)
        sgain = sbt(top, "sgain", [128, 128], F32)
        epsb = sbt(top, "epsb", [128, 1], F32)
        fin = dict(
            d0=sbt(top, "fin_d0", [1, 16], F32), d1=sbt(top, "fin_d1", [1, 16], F32),
            act=sbt(top, "fin_act", [128, 1], F32), dve=sbt(top, "fin_dve", [128, 1], F32),
            pool=sbt(top, "fin_pool", [128, 1], F32), idb=identb,
        )
        psn = [0]

        def alloc_psum(es, nf=6, nb=2):
            psn[0] += 1
            bk = [pst(es, f"bank{psn[0]}_{i}", [128, 512], F32) for i in range(nf)]
            bt = [pst(es, f"bankT{psn[0]}_{i}", [128, 1024], BF16) for i in range(nb)]
            return bk, bt

        with ExitStack() as es:
            P = Prog()
            k = K(P)
            banks, bankT = alloc_psum(es)
            cTs = sbt(es, "cTs", [128, 8], F32)
            scs = sbt(es, "scs", [128, 8], F32)
            wada = [sbt(es, f"wada{i}", [128, 6 * D], F32) for i in range(2)]
            badas = sbt(es, "badas", [128, 48], F32)
            modT = sbt(es, "modT", [128, 48], F32)
            n1s = sbt(es, "n1s", [128, 8], F32)
            n2s = sbt(es, "n2s", [128, 8], F32)
            lams = sbt(es, "lams", [128, 4, 64], F32)
            lprod = sbt(es, "lprod", [128, 2, 64], F32)
            lsum = sbt(es, "lsum", [128, 2], F32)
            lexp = sbt(es, "lexp", [128, 2], F32)
            ltmp = sbt(es, "ltmp", [128, 1], F32)
            subs = sbt(es, "subs", [128, 128], F32)
            dg = [sbt(es, f"dg{i}", [128, 128], F32) for i in range(2)]
            NCONV = 16
            rows = 16384 // NCONV
            for i in range(NCONV):
                for (src, dst) in ((UT, UTb), (Vx, Vb)):
                    P.add("pool", (lambda s_, d_, i_: (lambda e: e.dma_start(out=d_[i_ * rows:(i_ + 1) * rows, :], in_=s_[i_ * rows:(i_ + 1) * rows, :]).then_inc(conv_sem, 16)))(src, dst, i))
            k.dma("sp", cTs[:], cT, w=["cTs"])
            k.dma("sp", badas[:], b_adaT, w=["badas"])
            k.dma("sp", n1s[:], n1gT, w=["n1s"])
            k.dma("sp", n2s[:], n2gT, w=["n2s"])
            k.dma("sp", lams[:], lam_bc, w=["lams"])
            k.dma("sp", subs[:], subln_bc, w=["subs"])
            k.memset("dve", identf[:], 1.0, w=["identf"])
            P.add("pool", lambda e: e.affine_select(out=identf[:], in_=identf[:], pattern=[[-1, 128]], compare_op=ALU.is_equal,
                                                   fill=0.0, base=0, channel_multiplier=1), ["identf"], ["identf"])
            k.cp("dve", identb[:], identf[:], r=["identf"], w=["identb"])
            k.memset("dve", onesf[:], 1.0, w=["onesf"])
            k.memset("dve", epsb[:], EPS, w=["epsb"])
            k.memset("dve", fin["d0"][:], 0.0, w=["find0"])
            k.act(scs[:], cTs[:], AF.Silu, r=["cTs"], w=["scs"])
            modps = banks[0]
            for kc in range(8):
                b = kc % 2
                k.dma("sp", wada[b][:], w_ada[kc * 128:(kc + 1) * 128, :], w=[f"wada{b}"])
                for f in range(48):
                    k.mm(modps[:, f:f + 1], wada[b][:, f * 128:(f + 1) * 128], scs[:, kc:kc + 1], kc == 0 and f == 0, kc == 7,
                         r=[f"wada{b}", "scs"], w=["modps"])
            k.tt("dve", modT[:], modps[:, 0:48], badas[:], ALU.add, r=["modps", "badas"], w=["modT"])
            k.stt("dve", s1T[:], modT[:, 8:16], 1.0, n1s[:], ALU.add, ALU.mult, r=["modT", "n1s"], w=["s1T"])
            k.cp("dve", sh1T[:], modT[:, 0:8], r=["modT"], w=["sh1T"])
            k.stt("dve", s2T[:], modT[:, 32:40], 1.0, n2s[:], ALU.add, ALU.mult, r=["modT", "n2s"], w=["s2T"])
            k.cp("dve", sh2T[:], modT[:, 24:32], r=["modT"], w=["sh2T"])
            for (row, base, bk) in ((G1row, 16, 1), (G2row, 40, 3)):
                for c in range(8):
                    b = c % 2
                    k.ts("dve", dg[b][:], identf[:], modT[:, base + c:base + c + 1], None, ALU.mult, r=["modT", "identf"], w=[f"dg{b}"])
                    bank = banks[bk + c // 4]
                    k.mm(bank[:, (c % 4) * 128:(c % 4 + 1) * 128], onesf[:], dg[b][:], True, True, r=[f"dg{b}", "onesf"], w=[f"gr{bk + c // 4}"])
                for hh in range(2):
                    k.cp("dve", row[:, hh * 512:(hh + 1) * 512], banks[bk + hh][:], r=[f"gr{bk + hh}"], w=[f"grow{base}"])
            k.tt("dve", lprod[:, 0, :], lams[:, 0, :], lams[:, 1, :], ALU.mult, r=["lams"], w=["lprod0"])
            k.tt("dve", lprod[:, 1, :], lams[:, 2, :], lams[:, 3, :], ALU.mult, r=["lams"], w=["lprod1"])
            k.red("dve", lsum[:], lprod[:], r=["lprod0", "lprod1"], w=["lsum"])
            k.act(lexp[:], lsum[:], AF.Exp, r=["lsum"], w=["lexp"])
            k.tt("dve", ltmp[:], lexp[:, 1:2], lexp[:, 0:1], ALU.subtract, r=["lexp"], w=["ltmp"])
            k.ts("dve", neglam[:], ltmp[:], -LAM_INIT, None, ALU.add, r=["ltmp"], w=["neglam"])
            k.ts("dve", sgain[:], subs[:], 1.0 - LAM_INIT, None, ALU.mult, r=["subs"], w=["sgain"])
            P.emit(nc, semsets, phase_sem, 0, fin)

        with ExitStack() as es:
            P = Prog()
            k = K(P)
            banks, bankT = alloc_psum(es, 5, 3)
            hT = sbt(es, "hT", [128, 8, 2048], BF16)
            xt = [sbt(es, f"xt{i}", [128, D], F32) for i in range(2)]
            sq = [sbt(es, f"sq{i}", [128, D], F32) for i in range(2)]
            xn = [sbt(es, f"xn{i}", [128, D], BF16) for i in range(2)]
            ssx = [sbt(es, f"ssx{i}", [128, 1], F32) for i in range(2)]
            rsx = [sbt(es, f"rsx{i}", [128, 1], F32) for i in range(2)]
            gA = sbt(es, "gA", [128, 8, 64], F32)
            gQB = sbt(es, "gQB", [128, 8, 64], F32)
            gKB = sbt(es, "gKB", [128, 8, 64], F32)
            bgs = sbt(es, "bgs", [128, 2048], F32)
            invfs = sbt(es, "invfs", [128, 32], F32)
            hps = sbt(es, "hps", [128, 64], F32)
            posi = sbt(es, "posi", [128, 16], I32)
            posf = sbt(es, "posf", [128, 16], F32)
            tabs = [sbt(es, f"tab{i}", [128, 16, 64], F32) for i in range(3)]
            arg = sbt(es, "arg", [128, 16, 64], F32)
            argk = sbt(es, "argk", [128, 16, 64], F32)
            argi = sbt(es, "argi", [128, 16, 64], I32)
            wb = [sbt(es, f"wb{i}", [128, 8, 512], BF16) for i in range(2)]
            sqq = [sbt(es, f"sqq{i}", [128, 8, 64], F32) for i in range(3)]
            ssq = [sbt(es, f"ssq{i}", [128, 8], F32) for i in range(3)]
            rsq = [sbt(es, f"rsq{i}", [128, 8], F32) for i in range(3)]
            qn = [sbt(es, f"qn{i}", [128, 8, 64], F32) for i in range(3)]
            qg = [sbt(es, f"qg{i}", [128, 8, 2, 32], F32) for i in range(3)]
            rt = [sbt(es, f"rt{i}", [128, 4, 8, 32], F32) for i in range(3)]
            qr = [sbt(es, f"qr{i}", [128, 8, 2, 32], BF16) for i in range(3)]
            stg = [sbt(es, f"stg{i}", [128, 4, 128], BF16) for i in range(3)]
            vst = [sbt(es, f"vst{i}", [128, 512], BF16) for i in range(3)]
            gpre = [sbt(es, f"gpre{i}", [128, 512], F32) for i in range(3)]

            k.dma("sp", gA[:], gainA, w=["gA"])
            k.dma("sp", gQB[:], gainQB, w=["gQB"])
            k.dma("sp", gKB[:], gainKB, w=["gKB"])
            k.dma("sp", bgs[:], b_gate_bc, w=["bgs"])
            k.dma("sp", invfs[:], invf_bc, w=["invfs"])
            k.dma("sp", hps[:], halfpi, w=["hps"])

            pos_pat = [
                pos.rearrange("(st u m) -> st m u", st=2, u=16, m=128),
                pos.rearrange("(st blk m r) -> st m blk r", st=2, blk=4, m=128, r=4),
                pos.rearrange("(st m r) -> st m r", st=2, m=128, r=16),
            ]

            def tokcols(pat, u):
                if pat == 0:
                    return slice(u * 128, (u + 1) * 128)
                if pat == 1:
                    blk, r = u // 4, u % 4
                    return slice(blk * 512 + r, blk * 512 + 512, 4)
                return slice(u, 2048, 16)

            def perm0(pat, st, u):
                if pat == 0:
                    return st * 2048 + u * 128
                if pat == 1:
                    blk, r = u // 4, u % 4
                    return r * 1024 + st * 512 + blk * 128
                return u * 256 + st * 128

            jobs = []
            for g in range(3):
                jobs.append((g, "qk", [(256 * g, 256), (768 + 256 * g, 256)], ("A", g)))
                jobs.append((g, "v", [(1536 + 256 * g, 256)], ("A", g)))
            for j in range(2):
                jobs.append((0, "qk", [(2304 + 512 * j, 512)], ("QB", j)))
                jobs.append((0, "qk", [(3328 + 512 * j, 512)], ("KB", j)))
                jobs.append((0, "v", [(4352 + 512 * j, 512)], ("B", j)))
            for j in range(4):
                jobs.append((0, "gate", [(5376 + 512 * j, 512)], ("G", j)))

            unit = 0
            for st in range(2):
                for u in range(16):
                    b = u % 2
                    t0 = st * 2048 + u * 128
                    k.dma("sp", xt[b][:], x[t0:t0 + 128, :], w=[f"xt{b}"])
                    k.act(sq[b][:], xt[b][:], AF.Square, r=[f"xt{b}"], w=[f"sq{b}"])
                    k.red("dve", ssx[b][:], sq[b][:], r=[f"sq{b}"], w=[f"ssx{b}"])
                    k.act(rsx[b][:], ssx[b][:], AF.Sqrt, r=[f"ssx{b}", "epsb"], w=[f"rsx{b}"], scale=1.0 / D, bias=epsb[:])
                    k.recip(rsx[b][:], rsx[b][:], r=[f"rsx{b}"], w=[f"rsx{b}"])
                    k.ts("dve", xn[b][:], xt[b][:], rsx[b][:, 0:1], None, ALU.mult, r=[f"xt{b}", f"rsx{b}"], w=[f"xn{b}"])
                    for c in range(8):
                        k.tr(bankT[b][:, c * 128:(c + 1) * 128], xn[b][:, c * 128:(c + 1) * 128], identb[:], r=[f"xn{b}"], w=[f"bT{b}"])
                    for c in range(8):
                        k.act(hT[:, c, u * 128:(u + 1) * 128], bankT[b][:, c * 128:(c + 1) * 128], AF.Identity,
                              r=[f"bT{b}"], w=[f"hT{u}"], scale=s1T[:, c:c + 1], bias=sh1T[:, c:c + 1])
                for pat in range(3):
                    if pat == 0:
                        k.dma("sp", posi[:], pos_pat[0][st], w=["posi"], allow_slow_non_contiguous=True)
                    elif pat == 1:
                        k.dma("sp", posi[:].rearrange("p (a b) -> p a b", a=4), pos_pat[1][st], w=["posi"])
                    else:
                        k.dma("sp", posi[:], pos_pat[2][st], w=["posi"])
                    k.cp("dve", posf[:], posi[:], r=["posi"], w=["posf"])
                    k.tt("dve", arg[:, :, 0:32], bc(posf[:].unsqueeze(2), [128, 16, 32]), bc(invfs[:].unsqueeze(1), [128, 16, 32]), ALU.mult,
                         r=["posf", "invfs"], w=["arg"])
                    k.cp("dve", arg[:, :, 32:64], arg[:, :, 0:32], r=["arg"], w=["arg"])
                    k.tt("dve", arg[:], arg[:], bc(hps[:].unsqueeze(1), [128, 16, 64]), ALU.add, r=["arg", "hps"], w=["arg"])
                    k.ts("dve", argk[:], arg[:], 1.0 / TWO_PI, None, ALU.mult, r=["arg"], w=["argk"])
                    k.cp("dve", argi[:], argk[:], r=["argk"], w=["argi"])
                    k.cp("dve", argk[:], argi[:], r=["argi"], w=["argk"])
                    k.stt("dve", arg[:], argk[:], -C1, arg[:], ALU.mult, ALU.add, r=["argk", "arg"], w=["arg"])
                    k.stt("dve", arg[:], argk[:], -C2, arg[:], ALU.mult, ALU.add, r=["argk", "arg"], w=["arg"])
                    k.ts("dve", argk[:], arg[:], float(np.pi), -TWO_PI, ALU.is_gt, ALU.mult, r=["arg"], w=["argk"])
                    k.tt("dve", arg[:], arg[:], argk[:], ALU.add, r=["arg", "argk"], w=["arg"])
                    k.ts("dve", argk[:], arg[:], float(-np.pi), TWO_PI, ALU.is_lt, ALU.mult, r=["arg"], w=["argk"])
                    k.tt("dve", arg[:], arg[:], argk[:], ALU.add, r=["arg", "argk"], w=["arg"])
                    k.act(tabs[pat][:], arg[:], AF.Sin, r=["arg"], w=[f"tab{pat}"])
                hall = [f"hT{u}" for u in range(16)]
                pending = []
                for ji, (pat, kind, cols, extra) in enumerate(jobs):
                    wbi = ji % 2
                    off = 0
                    for (c0, ncol) in cols:
                        k.dma("pool", wb[wbi][:, :, off:off + ncol], w_in[:, c0:c0 + ncol].rearrange("(c p) n -> p c n", p=128), w=[f"wb{wbi}"])
                        off += ncol
                    ncols = off
                    for u in range(16):
                        ub = unit % 3
                        unit += 1
                        bankP = banks[ub]
                        tc = tokcols(pat, u)
                        rtok = hall if pat else [f"hT{u}"]
                        for c in range(8):
                            k.mm(bankP[:, 0:ncols], hT[:, c, tc], wb[wbi][:, c, 0:ncols], c == 0, c == 7, r=rtok + [f"wb{wbi}"], w=[f"bP{ub}"])
                        p0 = perm0(pat, st, u)

                        def post1(ub=ub, bankP=bankP, pat=pat, kind=kind, extra=extra, u=u, p0=p0, ncols=ncols):
                            if kind == "qk":
                                gt = {"A": gA, "QB": gQB, "KB": gKB}[extra[0]]
                                gtn = {"A": "gA", "QB": "gQB", "KB": "gKB"}[extra[0]]
                                pv = bankP[:].rearrange("p (h c) -> p h c", h=8)
                                k.act(sqq[ub][:], pv, AF.Square, r=[f"bP{ub}"], w=[f"sqq{ub}"])
                                k.red("dve", ssq[ub][:], sqq[ub][:], r=[f"sqq{ub}"], w=[f"ssq{ub}"])
                                k.act(rsq[ub][:], ssq[ub][:], AF.Sqrt, r=[f"ssq{ub}", "epsb"], w=[f"rsq{ub}"], scale=1.0 / 64, bias=epsb[:])
                                k.recip(rsq[ub][:], rsq[ub][:], r=[f"rsq{ub}"], w=[f"rsq{ub}"])
                                k.tt("dve", qn[ub][:], pv, bc(rsq[ub][:].unsqueeze(2), [128, 8, 64]), ALU.mult, r=[f"bP{ub}", f"rsq{ub}"], w=[f"qn{ub}"])
                                k.tt("pool", qg[ub][:].rearrange("p h a f -> p h (a f)"), qn[ub][:], gt[:], ALU.mult, r=[f"qn{ub}", gtn], w=[f"qg{ub}"])
                                sinb = bc(tabs[pat][:, u, 0:32].unsqueeze(1), [128, 8, 32])
                                cosb = bc(tabs[pat][:, u, 32:64].unsqueeze(1), [128, 8, 32])
                                x1 = qg[ub][:, :, 0, :]
                                x2 = qg[ub][:, :, 1, :]
                                tn = f"tab{pat}"
                                k.tt("dve", rt[ub][:, 0], x1, cosb, ALU.mult, r=[f"qg{ub}", tn], w=[f"rt0{ub}"])
                                k.tt("dve", rt[ub][:, 1], x2, sinb, ALU.mult, r=[f"qg{ub}", tn], w=[f"rt1{ub}"])
                                k.tt("dve", qr[ub][:, :, 0, :], rt[ub][:, 0], rt[ub][:, 1], ALU.subtract, r=[f"rt0{ub}", f"rt1{ub}"], w=[f"qr{ub}a"])
                                k.tt("pool", rt[ub][:, 2], x2, cosb, ALU.mult, r=[f"qg{ub}", tn], w=[f"rt2{ub}"])
                                k.tt("pool", rt[ub][:, 3], x1, sinb, ALU.mult, r=[f"qg{ub}", tn], w=[f"rt3{ub}"])
                                k.tt("pool", qr[ub][:, :, 1, :], rt[ub][:, 2], rt[ub][:, 3], ALU.add, r=[f"rt2{ub}", f"rt3{ub}"], w=[f"qr{ub}b"])
                                return
                            if kind == "v":
                                k.act(vst[ub][:, 0:ncols], bankP[:, 0:ncols], AF.Copy, r=[f"bP{ub}"], w=[f"vst{ub}"])
                                if extra[0] == "A":
                                    k.dma("sp", VA[extra[1], p0:p0 + 128, :], vst[ub][:, 0:256], r=[f"vst{ub}"], w=["VA"])
                                else:
                                    j = extra[1]
                                    k.dma("sp", VB[p0:p0 + 128, 512 * j:512 * j + 512], vst[ub][:], r=[f"vst{ub}"], w=["VB"])
                            else:
                                j = extra[1]
                                k.tt("dve", gpre[ub][:], bankP[:], bgs[:, 512 * j:512 * j + 512], ALU.add, r=[f"bP{ub}", "bgs"], w=[f"gpre{ub}"])
                                k.act(vst[ub][:], gpre[ub][:], AF.Sigmoid, r=[f"gpre{ub}"], w=[f"vst{ub}"])
                                k.dma("sp", GT[p0:p0 + 128, 512 * j:512 * j + 512], vst[ub][:], r=[f"vst{ub}"], w=["GT"])

                        def post2(ub=ub, bankP=bankP, pat=pat, kind=kind, extra=extra, u=u, p0=p0, ncols=ncols):
                            if kind == "qk":
                                qflat = qr[ub][:].rearrange("p h a f -> p (h a f)")
                                for q4 in range(4):
                                    k.tr(bankT[ub][:, q4 * 128:(q4 + 1) * 128], qflat[:, q4 * 128:(q4 + 1) * 128], identb[:],
                                         r=[f"qr{ub}a", f"qr{ub}b"], w=[f"bT{ub}"])
                                k.act(stg[ub][:].rearrange("p a t -> p (a t)"), bankT[ub][:, 0:512], AF.Copy, r=[f"bT{ub}"], w=[f"stg{ub}"])
                                if extra[0] == "A":
                                    g = extra[1]
                                    k.dma("sp", QTA[g, :, :, p0:p0 + 128].rearrange("a c t -> c a t"), stg[ub][:, 0:2, :], r=[f"stg{ub}"], w=["QTA"])
                                    k.dma("sp", KTA[g, :, :, p0:p0 + 128].rearrange("a c t -> c a t"), stg[ub][:, 2:4, :], r=[f"stg{ub}"], w=["KTA"])
                                else:
                                    dst = QTB if extra[0] == "QB" else KTB
                                    j = extra[1]
                                    k.dma("sp", dst[4 * j:4 * j + 4, :, p0:p0 + 128].rearrange("a c t -> c a t"), stg[ub][:], r=[f"stg{ub}"], w=[extra[0]])
                        pending.append((post1, post2))
                        if len(pending) >= 2:
                            pending[-2][0]()
                        if len(pending) >= 3:
                            pending.pop(0)[1]()
                if pending:
                    pending[-1][0]()
                while pending:
                    pending.pop(0)[1]()
            P.emit(nc, semsets, phase_sem, 1, fin)

        with ExitStack() as es:
            P = Prog()
            k = K(P)
            banks, bankT = alloc_psum(es)
            KTs = [sbt(es, f"KTs{i}", [128, S + 128], BF16) for i in range(2)]
            QTs = [sbt(es, f"QTs{i}", [128, S + 128], BF16) for i in range(2)]
            V1 = [sbt(es, f"V1_{i}", [128, 33, 65], BF16) for i in range(2)]
            mstd = sbt(es, "mstd", [128, 256], F32)
            mbnd = sbt(es, "mbnd", [128, 256], F32)
            Eb = [sbt(es, f"Eb{i}", [128, 256], BF16) for i in range(2)]
            Pm = [sbt(es, f"Pm{i}", [128, 256], BF16) for i in range(2)]
            osb = [sbt(es, f"osb{i}", [128, 65], F32) for i in range(4)]
            k.dma("sp", mstd[:], mask_std, w=["mstd"])
            k.dma("sp", mbnd[:], mask_bnd, w=["mbnd"])
            for i in range(2):
                k.memset("pool", KTs[i][:, 0:64], 0.0, w=[f"KTs{i}"])
                k.memset("pool", KTs[i][:, S + 64:S + 128], 0.0, w=[f"KTs{i}"])
                k.memset("pool", QTs[i][:, 0:64], 0.0, w=[f"QTs{i}"])
                k.memset("pool", QTs[i][:, S + 64:S + 128], 0.0, w=[f"QTs{i}"])
                k.memset("pool", V1[i][:], 0.0, w=[f"V1_{i}"])
                k.memset("pool", V1[i][:, :, 64:65], 1.0, w=[f"V1_{i}"])
            bankSs = [banks[0], banks[2]]
            bankOs = [banks[1], banks[3], banks[4], banks[5]]
            hcount = 0
            ccount = 0
            for g in range(3):
                dil = DIL[g]
                L = S // dil
                for pr in range(2):
                    pb = (g * 2 + pr) % 2
                    k.dma("sp", KTs[pb][:, 64:64 + S], KTA[g, pr], r=["KTA"], w=[f"KTs{pb}"])
                    k.dma("sp", QTs[pb][:, 64:64 + S], QTA[g, pr], r=["QTA"], w=[f"QTs{pb}"])
                    for hh in range(2):
                        hs = pr * 2 + hh
                        bp = 64 * hh
                        vb = hcount % 2
                        hcount += 1
                        vsrc = VA[g, :, hs * 64:(hs + 1) * 64]
                        k.dma("sp", V1[vb][:, 1:32, 0:64], vsrc[64:S - 64, :].rearrange("(j p) c -> p j c", p=128), r=["VA"], w=[f"V1_{vb}"])
                        k.dma("sp", V1[vb][64:128, 0, 0:64], vsrc[0:64, :], r=["VA"], w=[f"V1_{vb}"])
                        k.dma("sp", V1[vb][0:64, 32, 0:64], vsrc[S - 64:S, :], r=["VA"], w=[f"V1_{vb}"])
                        for jc in range(33):
                            sb_ = ccount % 2
                            ccount += 1
                            bnd = (128 * jc) % L == 0
                            qlo = 128 if jc == 0 else 0
                            qhi = 128 if jc == 32 else 256
                            sv = bankSs[sb_][:, 0:256]
                            k.mm(sv[:, qlo:qhi], KTs[pb][bp:bp + 64, 128 * jc:128 * jc + 128],
                                 QTs[pb][bp:bp + 64, 128 * jc - 64 + qlo:128 * jc - 64 + qhi],
                                 True, True, r=[f"KTs{pb}", f"QTs{pb}"], w=[f"bS{sb_}"])
                            k.act(Eb[sb_][:, qlo:qhi], sv[:, qlo:qhi], AF.Exp, r=[f"bS{sb_}"], w=[f"Eb{sb_}"], scale=0.125)
                            mk = mbnd if bnd else mstd
                            k.tt("dve", Pm[sb_][:, qlo:qhi], Eb[sb_][:, qlo:qhi], mk[:, qlo:qhi], ALU.mult, r=[f"Eb{sb_}", "mstd", "mbnd"], w=[f"Pm{sb_}"])
                            for half in range(2):
                                if half * 128 < qlo or half * 128 >= qhi:
                                    continue
                                blk = jc - 1 + half
                                a = blk % 4
                                k.mm(bankOs[a][:, 0:65], Pm[sb_][:, half * 128:half * 128 + 128], V1[vb][:, jc, :],
                                     half == 1, half == 0, r=[f"Pm{sb_}", f"V1_{vb}"], w=[f"bO{a}"])
                            if jc >= 1:
                                blk = jc - 1
                                a = blk % 4
                                k.cp("dve", osb[a][:], bankOs[a][:, 0:65], r=[f"bO{a}"], w=[f"osb{a}"])
                                r_ = (128 * blk) // L
                                j0 = (128 * blk) % L
                                s0 = j0 * dil + r_
                                dst = ND[s0:s0 + 127 * dil + 1:dil, g * 4 + hs, :]
                                k.dma("sp", dst, osb[a][:], r=[f"osb{a}"], w=["ND"])
            P.emit(nc, semsets, phase_sem, 2, fin)

        with ExitStack() as es:
            P = Prog()
            k = K(P)
            banks, bankT = alloc_psum(es, 7, 1)
            KTs = [sbt(es, f"KTd{i}", [128, S], BF16) for i in range(2)]
            QTs = [sbt(es, f"QTd{i}", [128, S], BF16) for i in range(2)]
            V1 = [sbt(es, f"V1d{i}", [128, 32, 129], BF16) for i in range(2)]
            E = [[sbt(es, f"E{m}_{i}", [128, 512], BF16) for i in range(2)] for m in range(2)]
            osb = sbt(es, "osbd", [128, 8, 129], F32)
            rr = sbt(es, "rr", [128, 8], F32)
            o1 = sbt(es, "o1", [128, 4, 128], F32)
            o2 = sbt(es, "o2", [128, 4, 128], F32)
            ssd = sbt(es, "ssd", [128, 4], F32)
            obb = sbt(es, "obb", [128, 4, 128], BF16)
            obT = [sbt(es, f"obT{i}", [128, 512], BF16) for i in range(2)]
            for i in range(2):
                k.memset("pool", V1[i][:, :, 128:129], 1.0, w=[f"V1d{i}"])
            accb = [banks[4], banks[5], banks[6]]
            steps = [(h, qb, kc) for h in range(8) for qb in range(8) for kc in range(32)]

            def qk_exp(n):
                h, qb, kc = steps[n]
                hb = h % 2
                eb = n % 2
                if qb == 0 and kc == 0:
                    k.dma("sp", KTs[hb][:], KTB[h], r=["KB"], w=[f"KTd{hb}"])
                    k.dma("sp", QTs[hb][:], QTB[h], r=["QB"], w=[f"QTd{hb}"])
                    k.dma("sp", V1[hb][:, :, 0:128], VB[:, h * 128:(h + 1) * 128].rearrange("(j p) c -> p j c", p=128), r=["VB"], w=[f"V1d{hb}"])
                for m in range(2):
                    bi = m * 2 + eb
                    tok = f"bS{bi}"
                    k.mm(banks[bi][:], KTs[hb][64 * m:64 * m + 64, kc * 128:(kc + 1) * 128], QTs[hb][64 * m:64 * m + 64, qb * 512:(qb + 1) * 512],
                         True, True, r=[f"KTd{hb}", f"QTd{hb}"], w=[tok])
                    k.act(E[m][eb][:], banks[bi][:], AF.Exp, r=[tok], w=[f"E{m}_{eb}"], scale=0.125)

            deferred = []
            qk_exp(0)
            for n, (h, qb, kc) in enumerate(steps):
                hb = h % 2
                eb = n % 2
                if n + 1 < len(steps):
                    qk_exp(n + 1)
                for m in range(2):
                    for sub in range(4):
                        a = m * 4 + sub
                        k.mm(accb[a // 3][:, (a % 3) * 129:(a % 3) * 129 + 129], E[m][eb][:, sub * 128:(sub + 1) * 128], V1[hb][:, kc, :],
                             kc == 0 and a % 3 == 0, kc == 31, r=[f"E{m}_{eb}", f"V1d{hb}"], w=[f"accb{a // 3}"])
                if kc == 31:
                    for a in range(8):
                        eng = "dve" if (a // 3) % 2 == 0 else "act"
                        src = accb[a // 3][:, (a % 3) * 129:(a % 3) * 129 + 129]
                        if eng == "dve":
                            k.cp("dve", osb[:, a, :], src, r=[f"accb{a // 3}"], w=[f"osbd{a}"])
                        else:
                            k.act(osb[:, a, :], src, AF.Copy, r=[f"accb{a // 3}"], w=[f"osbd{a}"])
                    k.recip(rr[:], osb[:, :, 128], r=[f"osbd{a_}" for a_ in range(8)], w=["rr"])
                    k.ts("dve", rr[:, 4:8], rr[:, 4:8], neglam[:, 0:1], None, ALU.mult, r=["rr"], w=["rr"])
                    k.tt("dve", o1[:], osb[:, 0:4, 0:128], bc(rr[:, 0:4].unsqueeze(2), [128, 4, 128]), ALU.mult, r=[f"osbd{a_}" for a_ in range(8)] + ["rr"], w=["o1"])
                    k.tt("pool", o2[:], osb[:, 4:8, 0:128], bc(rr[:, 4:8].unsqueeze(2), [128, 4, 128]), ALU.mult, r=[f"osbd{a_}" for a_ in range(8)] + ["rr"], w=["o2"])
                    k.tt("dve", o1[:], o1[:], o2[:], ALU.add, r=["o1", "o2"], w=["o1"])
                    k.tt("pool", o2[:], o1[:], o1[:], ALU.mult, r=["o1"], w=["o2"])
                    k.red("dve", ssd[:], o2[:], r=["o2"], w=["ssd"])
                    k.act(ssd[:], ssd[:], AF.Sqrt, r=["ssd", "epsb"], w=["ssd"], scale=1.0 / 128, bias=epsb[:])
                    k.recip(ssd[:], ssd[:], r=["ssd"], w=["ssd"])
                    k.tt("dve", o1[:], o1[:], bc(ssd[:].unsqueeze(2), [128, 4, 128]), ALU.mult, r=["o1", "ssd"], w=["o1"])
                    k.tt("pool", obb[:], o1[:], bc(sgain[:].unsqueeze(1), [128, 4, 128]), ALU.mult, r=["o1"], w=["obb"])
                    def fin_tr(h=h, qb=qb):
                        ob_i = (h * 8 + qb) % 2
                        for sub in range(4):
                            k.tr(bankT[0][:, sub * 128:(sub + 1) * 128], obb[:, sub, :], identb[:], r=["obb"], w=["bTd"])
                        k.act(obT[ob_i][:], bankT[0][:, 0:512], AF.Copy, r=["bTd"], w=[f"obT{ob_i}"])
                        k.dma("sp", OBT[h, :, qb * 512:(qb + 1) * 512], obT[ob_i][:], r=[f"obT{ob_i}"], w=["OBT"])
                    deferred.append((n + 6, fin_tr))
                while deferred and (deferred[0][0] <= n or n == len(steps) - 1):
                    deferred.pop(0)[1]()
            P.emit(nc, semsets, phase_sem, 3, fin)

        with ExitStack() as es:
            P = Prog()
            k = K(P)
            banks, bankT = alloc_psum(es)
            Wc = sbt(es, "Wc", [128, 8, 2048], BF16)
            wpa = sbt(es, "wpa", [128, 2, D], BF16)
            wpb = sbt(es, "wpb", [128, 8, D], BF16)
            wo = sbt(es, "wo", [128, 8, D], BF16)
            wq = [sbt(es, f"wq{i}", [128, D], F32) for i in range(2)]
            sk = [sbt(es, f"sk{i}", [128, 128], F32) for i in range(2)]
            nd = [sbt(es, f"nd{i}", [128, 12, 65], F32) for i in range(2)]
            obt = [sbt(es, f"obt{i}", [128, 8, 128], BF16) for i in range(2)]
            gts = [sbt(es, f"gts{i}", [128, 2048], BF16) for i in range(2)]
            xts = [sbt(es, f"xts{i}", [128, D], F32) for i in range(2)]
            nsum = sbt(es, "nsum", [128, 4, 65], F32)
            rden = sbt(es, "rden", [128, 4], F32)
            oab = sbt(es, "oab", [128, 4, 64], BF16)
            oaT = sbt(es, "oaT", [128, 2, 128], BF16)
            bra = sbt(es, "bra", [128, D], F32)
            t1 = sbt(es, "t1", [128, D], F32)
            t2 = sbt(es, "t2", [128, D], F32)
            mixb = sbt(es, "mixb", [128, D], BF16)
            mixT = sbt(es, "mixT", [128, 8, 128], BF16)
            x1s = sbt(es, "x1s", [128, D], F32)
            sq2 = sbt(es, "sq2", [128, D], F32)
            ss2 = sbt(es, "ss2", [128, 1], F32)
            xn2 = sbt(es, "xn2", [128, D], BF16)
            h2T = sbt(es, "h2T", [128, 8, 128], BF16)
            Ssbs = [sbt(es, f"Ssb{i}", [128, 8, 2, 128], F32) for i in range(2)]
            mr = sbt(es, "mr", [128, 128], F32)
            t16 = sbt(es, "t16", [128, 8, 2, 16], F32)
            cand = sbt(es, "cand", [128, 8, 16, 16], F32)
            mr2 = sbt(es, "mr2", [128, 256], F32)
            c16 = sbt(es, "c16", [128, 8, 16], F32)
            dd = sbt(es, "dd", [128, 8, 16], F32)
            zz = sbt(es, "zz", [128, 8], F32)
            tau = sbt(es, "tau", [128, 8], F32)
            cf = sbt(es, "cf", [128, 8], F32)
            k.dma("pool", wpa[:], w_pa.rearrange("(c p) n -> p c n", p=128), w=["wpa"])
            k.dma("pool", wpb[:], w_pb.rearrange("(c p) n -> p c n", p=128), w=["wpb"])
            k.dma("pool", wo[:], w_out.rearrange("(c p) n -> p c n", p=128), w=["wo"])
            for kk in range(16):
                b = kk % 2
                k.dma("sp", wq[b][:], wqT[kk], w=[f"wq{b}"])
                k.dma("sp", sk[b][:], skT[kk], w=[f"sk{b}"])
                for dc in range(8):
                    k.mm(banks[dc // 4][:, (dc % 4) * 128:(dc % 4 + 1) * 128], wq[b][:, dc * 128:(dc + 1) * 128], sk[b][:], True, True,
                         r=[f"wq{b}", f"sk{b}"], w=[f"wcp{dc // 4}"])
                for hh in range(2):
                    k.act(Wc[:, 4 * hh:4 * hh + 4, kk * 128:(kk + 1) * 128], banks[hh][:].rearrange("p (a n) -> p a n", a=4), AF.Copy,
                          r=[f"wcp{hh}"], w=["Wc"])
            def stageA(t):
                b = t % 2
                t0 = t * 128
                Ssb = Ssbs[b]
                sS = f"Ssb{b}"
                k.dma("sp", nd[b][:], ND[t0:t0 + 128], r=["ND"], w=[f"nd{b}"])
                k.dma("sp", obt[b][:], OBT[:, :, t0:t0 + 128].rearrange("h c t -> c h t"), r=["OBT"], w=[f"obt{b}"])
                k.dma("sp", gts[b][:], GT[t0:t0 + 128, :], r=["GT"], w=[f"gts{b}"])
                k.dma("sp", xts[b][:], x[t0:t0 + 128, :], w=[f"xts{b}"])
                k.tt("pool", nsum[:], nd[b][:, 0:4, :], nd[b][:, 4:8, :], ALU.add, r=[f"nd{b}"], w=["nsum"])
                k.tt("pool", nsum[:], nsum[:], nd[b][:, 8:12, :], ALU.add, r=[f"nd{b}", "nsum"], w=["nsum"])
                k.recip(rden[:], nsum[:, :, 64], r=["nsum"], w=["rden"])
                k.tt("dve", oab[:], nsum[:, :, 0:64], bc(rden[:].unsqueeze(2), [128, 4, 64]), ALU.mult, r=["nsum", "rden"], w=["oab"])
                oaf = oab[:].rearrange("p a c -> p (a c)")
                for c in range(2):
                    k.tr(bankT[0][:, c * 128:(c + 1) * 128], oaf[:, c * 128:(c + 1) * 128], identb[:], r=["oab"], w=["bT0"])
                k.act(oaT[:].rearrange("p a t -> p (a t)"), bankT[0][:, 0:256], AF.Copy, r=["bT0"], w=["oaT"])
                for hf in range(2):
                    for c in range(2):
                        k.mm(banks[hf][:], oaT[:, c, :], wpa[:, c, hf * 512:(hf + 1) * 512], c == 0, c == 1, r=["oaT", "wpa"], w=[f"bk{hf}"])
                for hf in range(2):
                    for c in range(8):
                        k.mm(banks[2 + hf][:], obt[b][:, c, :], wpb[:, c, hf * 512:(hf + 1) * 512], c == 0, c == 7, r=[f"obt{b}", "wpb"], w=[f"bk{2 + hf}"])
                for hf in range(2):
                    sl = slice(hf * 512, (hf + 1) * 512)
                    k.act(bra[:, sl], banks[hf][:], AF.Copy, r=[f"bk{hf}"], w=["bra"])
                    k.tt("dve", t2[:, sl], banks[2 + hf][:], gts[b][:, 1024 + hf * 512:1024 + (hf + 1) * 512], ALU.mult, r=[f"bk{2 + hf}", f"gts{b}"], w=["t2"])
                k.tt("pool", t1[:], bra[:], gts[b][:, 0:1024], ALU.mult, r=["bra", f"gts{b}"], w=["t1"])
                k.tt("pool", mixb[:], t1[:], t2[:], ALU.add, r=["t1", "t2"], w=["mixb"])
                for c in range(8):
                    k.tr(bankT[1][:, c * 128:(c + 1) * 128], mixb[:, c * 128:(c + 1) * 128], identb[:], r=["mixb"], w=["bT1"])
                k.act(mixT[:].rearrange("p a t -> p (a t)"), bankT[1][:], AF.Copy, r=["bT1"], w=["mixT"])
                for hf in range(2):
                    for c in range(8):
                        k.mm(banks[4 + hf][:], mixT[:, c, :], wo[:, c, hf * 512:(hf + 1) * 512], c == 0, c == 7, r=["mixT", "wo"], w=[f"bk{4 + hf}"])
                for hf in range(2):
                    sl = slice(hf * 512, (hf + 1) * 512)
                    k.tt("dve", t2[:, sl], banks[4 + hf][:], G1row[:, sl], ALU.mult, r=[f"bk{4 + hf}"], w=["t2"])
                k.tt("pool", x1s[:], t2[:], xts[b][:], ALU.add, r=["t2", f"xts{b}"], w=["x1s"])
                k.dma("sp", X1[t0:t0 + 128, :], x1s[:], r=["x1s"], w=["X1"])
                k.act(sq2[:], x1s[:], AF.Square, r=["x1s"], w=["sq2"])
                k.red("dve", ss2[:], sq2[:], r=["sq2"], w=["ss2"])
                k.act(ss2[:], ss2[:], AF.Sqrt, r=["ss2", "epsb"], w=["ss2"], scale=1.0 / D, bias=epsb[:])
                k.recip(ss2[:], ss2[:], r=["ss2"], w=["ss2"])
                k.ts("dve", xn2[:], x1s[:], ss2[:, 0:1], None, ALU.mult, r=["x1s", "ss2"], w=["xn2"])
                for c in range(8):
                    k.tr(bankT[0][:, c * 128:(c + 1) * 128], xn2[:, c * 128:(c + 1) * 128], identb[:], r=["xn2"], w=["bT0"])
                for c in range(8):
                    k.act(h2T[:, c, :], bankT[0][:, c * 128:(c + 1) * 128], AF.Identity, r=["bT0"], w=["h2T"], scale=s2T[:, c:c + 1], bias=sh2T[:, c:c + 1])
                k.dma("sp", H2T[:, :, t0:t0 + 128].rearrange("c p t -> p c t"), h2T[:], r=["h2T"], w=["H2T"])
                for nb in range(4):
                    for c in range(8):
                        k.mm(banks[nb][:], h2T[:, c, :], Wc[:, c, nb * 512:(nb + 1) * 512], c == 0, c == 7, r=["h2T", "Wc"], w=[f"bk{nb}"])
                Sf = Ssb[:].rearrange("p h a n -> p (h a n)")
                for nb in range(4):
                    k.act(Sf[:, nb * 512:(nb + 1) * 512], banks[nb][:], AF.Copy, r=[f"bk{nb}"], w=[sS])
            def stageB(t):
                b = t % 2
                t0 = t * 128
                Ssb = Ssbs[b]
                sS = f"Ssb{b}"
                for h in range(8):
                    for a in range(2):
                        k.max8(t16[:, h, a, 0:8], Ssb[:, h, a, :], r=[sS], w=["t16"])
                        k.mrep(mr[:], t16[:, h, a, 0:8], Ssb[:, h, a, :], r=[sS, "t16"], w=["mr"])
                        k.max8(t16[:, h, a, 8:16], mr[:], r=["mr"], w=["t16"])
                k.tt("dve", cand[:], bc(t16[:, :, 0, :].unsqueeze(3), [128, 8, 16, 16]), bc(t16[:, :, 1, :].unsqueeze(2), [128, 8, 16, 16]), ALU.add,
                     r=["t16"], w=["cand"])
                for h in range(8):
                    cv = cand[:, h].rearrange("p a b -> p (a b)")
                    k.max8(c16[:, h, 0:8], cv, r=["cand"], w=["c16"])
                    k.mrep(mr2[:], c16[:, h, 0:8], cv, r=["cand", "c16"], w=["mr2"])
                    k.max8(c16[:, h, 8:16], mr2[:], r=["mr2"], w=["c16"])
                k.ts("dve", tau[:], c16[:, :, 15], -1e-5, None, ALU.add, r=["c16"], w=["tau"])
                k.tt("dve", dd[:], c16[:], bc(c16[:, :, 0:1], [128, 8, 16]), ALU.subtract, r=["c16"], w=["dd"])
                k.act(dd[:], dd[:], AF.Exp, r=["dd"], w=["dd"])
                k.red("dve", zz[:], dd[:], r=["dd"], w=["zz"])
                k.recip(zz[:], zz[:], r=["zz"], w=["zz"])
                k.tt("dve", cf[:], tau[:], c16[:, :, 0], ALU.subtract, r=["tau", "c16"], w=["cf"])
                k.act(cf[:], cf[:], AF.Exp, r=["cf"], w=["cf"])
                k.tt("dve", cf[:], cf[:], zz[:], ALU.mult, r=["cf", "zz"], w=["cf"])
                k.tt("dve", Ssb[:, :, 0, :], Ssb[:, :, 0, :], bc(tau[:].unsqueeze(2), [128, 8, 128]), ALU.subtract, r=[sS, "tau"], w=[sS])
                k.dma("sp", PS[t0:t0 + 128], Ssb[:], r=[sS], w=["PS"])
                k.dma("sp", CF[t0:t0 + 128, :], cf[:], r=["cf"], w=["CF"])
            stageA(0)
            for t in range(NT):
                if t + 1 < NT:
                    stageA(t + 1)
                stageB(t)
            P.emit(nc, semsets, phase_sem, 4, fin)

        with ExitStack() as es:
            P = Prog()
            k = K(P)
            banks, bankT = alloc_psum(es, 8, 0)
            NB = 4
            h2 = [sbt(es, f"h2_{i}", [128, 8, 256], BF16) for i in range(2)]
            pss = sbt(es, "pss", [128, 2, 8, 2, 128], F32)
            cfs = [sbt(es, f"cfs{i}", [128, 2, 8], F32) for i in range(2)]
            x1t = sbt(es, "x1t", [128, 2, D], F32)
            Dm = [sbt(es, f"Dm{i}", [128, 2, 8, 128], BF16) for i in range(2)]
            UTi = [sbt(es, f"UTi{i}", [128, 8, 128], BF16) for i in range(NB)]
            Vi = [sbt(es, f"Vi{i}", [128, D], BF16) for i in range(NB)]
            HgA = sbt(es, "HgA", [128, 128, 256], BF16)
            zt = [sbt(es, f"zt{i}", [128, 2, 8, 128], BF16) for i in range(3)]
            Et = [sbt(es, f"Et{i}", [128, 2, 8, 128], BF16) for i in range(2)]
            Gt = [sbt(es, f"Gt{i}", [128, 2, 8, 128], BF16) for i in range(3)]
            WT = [sbt(es, f"WT{i}", [128, 256], BF16) for i in range(2)]
            yo = [sbt(es, f"yo{i}", [128, D], F32) for i in range(2)]
            P.add("sp", lambda e: e.wait_ge(conv_sem, 16 * 32))
            UTv = UTb.rearrange("(i p) (c e) -> i p c e", p=128, c=8)
            ustep = 0
            vstep = 0
            for tt_ in range(16):
                tb = tt_ % 2
                t0 = tt_ * 256
                k.dma("sp", h2[tb][:], H2T[:, :, t0:t0 + 256].rearrange("c p t -> p c t"), w=[f"h2_{tb}"])
                k.dma("sp", cfs[tb][:], CF[t0:t0 + 256, :].rearrange("(s p) h -> p s h", p=128), w=[f"cfs{tb}"])
                for s_ in range(2):
                    for h in range(8):
                        k.ts("pool", Dm[tb][:, s_, h, :], identf[:], cfs[tb][:, s_, h:h + 1], None, ALU.mult, r=[f"cfs{tb}"], w=[f"Dm{tb}"])
                k.dma("sp", pss[:], PS[t0:t0 + 256].rearrange("(s p) h a n -> p s h a n", p=128), w=["pss"])
                for i in range(128):
                    ub = ustep % NB
                    hb_ = ustep % 2
                    ustep += 1
                    k.dma("sp", UTi[ub][:], UTv[i], w=[f"UTi{ub}"])
                    hv = banks[4 + hb_][:, 0:256]
                    for c in range(8):
                        k.mm(hv, UTi[ub][:, c, :], h2[tb][:, c, :], c == 0, c == 7, r=[f"UTi{ub}", f"h2_{tb}"], w=[f"bH{hb_}"])
                    k.act(HgA[:, i, :], hv, AF.Gelu, r=[f"bH{hb_}"], w=[f"HgA{i}"])

                def st_z(i):
                    zb = i % 3
                    k.tt("pool", zt[zb][:], pss[:, :, :, 1, :], bc(pss[:, :, :, 0, i:i + 1], [128, 2, 8, 128]), ALU.add, r=["pss"], w=[f"zt{zb}a", f"zt{zb}b"])

                def st_eg(i):
                    zb = i % 3
                    eb = i % 2
                    k.act(Et[eb][:], zt[zb][:], AF.Exp, r=[f"zt{zb}a", f"zt{zb}b"], w=[f"Et{eb}"])
                    k.stt("dve", Gt[zb][:], zt[zb][:], 0.0, Et[eb][:], ALU.is_ge, ALU.mult, r=[f"zt{zb}a", f"zt{zb}b", f"Et{eb}"], w=[f"Gt{zb}"])

                def st_gt(i):
                    zb = i % 3
                    b2 = i % 2
                    gv = banks[6 + b2][:, 0:256]
                    for s_ in range(2):
                        for h in range(8):
                            k.mm(gv[:, s_ * 128:(s_ + 1) * 128], Gt[zb][:, s_, h, :], Dm[tb][:, s_, h, :], h == 0, h == 7,
                                 r=[f"Gt{zb}", f"Dm{tb}"], w=[f"bG{b2}"])

                def st_wt(i):
                    b2 = i % 2
                    gv = banks[6 + b2][:, 0:256]
                    k.tt("dve", WT[b2][:], gv, HgA[:, i, :], ALU.mult, r=[f"bG{b2}", f"HgA{i}"], w=[f"WT{b2}"])

                vbuf = {}

                def st_vload(i):
                    nonlocal vstep
                    vb_ = vstep % NB
                    vstep += 1
                    vbuf[i] = vb_
                    k.dma("sp", Vi[vb_][:], Vb[i * 128:(i + 1) * 128, :], w=[f"Vi{vb_}"])

                def st_y(i):
                    b2 = i % 2
                    vb_ = vbuf[i]
                    for s_ in range(2):
                        for hf in range(2):
                            k.mm(banks[s_ * 2 + hf][:], WT[b2][:, s_ * 128:(s_ + 1) * 128], Vi[vb_][:, hf * 512:(hf + 1) * 512], i == 0, i == 127,
                                 r=[f"WT{b2}", f"Vi{vb_}"], w=[f"bY{s_ * 2 + hf}"])

                for it in range(128 + 3):
                    if it < 128:
                        st_vload(it)
                        st_z(it)
                    if 1 <= it <= 128:
                        st_eg(it - 1)
                    if 2 <= it <= 129:
                        st_gt(it - 2)
                    if 3 <= it <= 130:
                        st_y(it - 3)
                    if 2 <= it <= 129:
                        st_wt(it - 2)
                k.dma("sp", x1t[:], X1[t0:t0 + 256, :].rearrange("(s p) d -> p s d", p=128), w=["x1t"])
                for s_ in range(2):
                    for hf in range(2):
                        sl = slice(hf * 512, (hf + 1) * 512)
                        k.tt("dve", yo[s_][:, sl], banks[s_ * 2 + hf][:], G2row[:, sl], ALU.mult, r=[f"bY{s_ * 2 + hf}"], w=[f"yo{s_}"])
                    k.tt("pool", yo[s_][:], yo[s_][:], x1t[:, s_, :], ALU.add, r=[f"yo{s_}", "x1t"], w=[f"yo{s_}"])
                    k.dma("sp", out[t0 + s_ * 128:t0 + (s_ + 1) * 128, :], yo[s_][:], r=[f"yo{s_}"], w=["out"])
            P.emit(nc, semsets, phase_sem, 5, fin)
    return nc


DEBUG_OUT = set()


def _masks():
    kk = np.arange(128)[:, None]
    qq = np.arange(256)[None, :]
    std = ((kk <= qq) & (qq <= kk + 128)).astype(np.float32)
    blk = (((kk < 64) & (qq < 128)) | ((kk >= 64) & (qq >= 128))).astype(np.float32)
    return std, std * blk


def prep_inputs(inputs):
    f = lambda a: np.ascontiguousarray(np.asarray(a), dtype=np.float32)
    x = f(inputs["x"])
    c = f(inputs["c"])
    pos = np.ascontiguousarray(np.asarray(inputs["positions"]), dtype=np.int32)
    L = 0
    rep = lambda v, n=128: np.ascontiguousarray(np.broadcast_to(f(v)[None], (n,) + f(v).shape))
    qn_a, kn_a = f(inputs["qn_a"])[L], f(inputs["kn_a"])[L]
    qn_b, kn_b = f(inputs["qn_b"])[L], f(inputs["kn_b"])[L]
    gainA = np.concatenate([np.tile(qn_a[None], (4, 1)), np.tile(kn_a[None], (4, 1))], 0)
    gainQB = np.tile(qn_b[None], (8, 1))
    gainKB = np.tile(kn_b[None], (8, 1))
    lam = np.stack([f(inputs["lam_q1"])[L], f(inputs["lam_k1"])[L], f(inputs["lam_q2"])[L], f(inputs["lam_k2"])[L]], 0)
    invf = (1.0 / (10000.0 ** (np.arange(0, 64, 2, dtype=np.float32) / 64))).astype(np.float32)
    hp = np.concatenate([np.zeros(32, np.float32), np.full(32, np.pi / 2, np.float32)])
    mstd, mbnd = _masks()
    wq = f(inputs["w_query"])[L]
    wqT = np.ascontiguousarray(wq.reshape(D, 16, 128).transpose(1, 2, 0))
    sk = f(inputs["sub_keys"])[L].reshape(16, 128, 128)
    skT = np.ascontiguousarray(sk.transpose(0, 2, 1))
    U = f(inputs["expert_u"])[L]
    UT = np.ascontiguousarray(U.reshape(128, 128, 8, 128).transpose(0, 3, 2, 1)).reshape(16384, 1024)
    shared = dict(
        w_ada=f(inputs["w_ada"])[L],
        b_adaT=np.ascontiguousarray(f(inputs["b_ada"])[L].reshape(48, 128).T),
        n1gT=np.ascontiguousarray(f(inputs["norm1_g"])[L].reshape(8, 128).T),
        n2gT=np.ascontiguousarray(f(inputs["norm2_g"])[L].reshape(8, 128).T),
        w_in=f(inputs["w_in"])[L],
        b_gate_bc=rep(f(inputs["b_gate"])[L]),
        gainA=rep(gainA), gainQB=rep(gainQB), gainKB=rep(gainKB),
        lam_bc=rep(lam), subln_bc=rep(f(inputs["subln_g"])[L]),
        invf_bc=rep(invf), halfpi=rep(hp), mask_std=mstd, mask_bnd=mbnd,
        w_pa=f(inputs["w_proj_a"])[L], w_pb=f(inputs["w_proj_b"])[L], w_out=f(inputs["w_out"])[L],
        wqT=wqT, skT=skT, UT=UT, Vx=f(inputs["expert_v"])[L],
    )
    in_maps = []
    for b in range(8):
        m = dict(shared)
        m["x"] = x[b]
        m["pos"] = pos[b]
        m["cT"] = np.ascontiguousarray(c[b].reshape(8, 128).T)
        in_maps.append(m)
    return in_maps


def kernel(**inputs):
    in_maps = prep_inputs(inputs)
    nc = build_program()
    res = run_bass_kernel_spmd(nc, in_maps, core_ids=list(range(8)))
    return np.stack([np.asarray(r["out"], dtype=np.float32) for r in res.results], 0)
```

```python
import os
import numpy as np
from contextlib import ExitStack
import concourse.bass as bass
import concourse.mybir as mybir
from concourse.bass_utils import run_bass_kernel_spmd

F32 = mybir.dt.float32
BF16 = mybir.dt.bfloat16
I32 = mybir.dt.int32
AF = mybir.ActivationFunctionType
ALU = mybir.AluOpType
AX = mybir.AxisListType

S = 4096
D = 1024
NT = 32
EPS = 1e-6
LAM_INIT = 0.2
DIL = (1, 4, 16)
N_DMA_SEMS = 8
QUEUES = ("sp", "pool", "act")
TWO_PI = float(2 * np.pi)
C1 = 6.28125
C2 = float(2 * np.pi - 6.28125)


class Prog:
    def __init__(self):
        self.ops = []
        self.last_writer = {}
        self.readers = {}

    def add(self, eng, fn, reads=(), writes=(), dma=False):
        idx = len(self.ops)
        deps = set()
        raw = set()
        for t in reads:
            w = self.last_writer.get(t)
            if w is not None:
                deps.add(w)
                raw.add(w)
        for t in writes:
            w = self.last_writer.get(t)
            if w is not None:
                deps.add(w)
            for r in self.readers.get(t, ()):
                deps.add(r)
        op = dict(eng=eng, fn=fn, dma=dma, deps=deps, raw=raw, signal=False)
        self.ops.append(op)
        for t in reads:
            self.readers.setdefault(t, []).append(idx)
        for t in writes:
            self.last_writer[t] = idx
            self.readers[t] = []
        return idx

    def emit(self, nc, semsets, phase_sem, phase_idx, fin):
        sems, cnt = semsets[phase_idx % 3]
        ops = self.ops
        for op in ops:
            keep = set()
            for d in op["deps"]:
                p = ops[d]
                if p["dma"] or op["dma"] or p["eng"] != op["eng"]:
                    keep.add(d)
                elif op["eng"] != "pe" and d in op["raw"]:
                    keep.add(d)
            op["deps"] = keep
            for d in keep:
                ops[d]["signal"] = True
        dma_k = {}
        prev_slot = {}
        for op in ops:
            if op["dma"]:
                q = op["eng"]
                k = dma_k.get(q, 0)
                dma_k[q] = k + 1
                key = ("dma", q, k % N_DMA_SEMS)
                cnt[key] = cnt.get(key, 0) + 16
                op["prev"] = prev_slot.get(key)
                op["sig"] = (key, cnt[key])
                prev_slot[key] = op["sig"]
            elif op["signal"]:
                key = op["eng"]
                cnt[key] = cnt.get(key, 0) + 1
                op["sig"] = (key, cnt[key])

        def semof(key):
            if isinstance(key, tuple):
                return sems["dma_" + key[1]][key[2]]
            return sems[key]

        streams = {}
        for op in ops:
            streams.setdefault(op["eng"], []).append(op)

        def run_stream(engname, eng):
            if phase_idx > 0:
                eng.wait_ge(phase_sem, 19 * phase_idx)
            known = {}
            for op in streams.get(engname, []):
                waits = {}
                for d in op["deps"]:
                    key, val = ops[d]["sig"]
                    if waits.get(key, 0) < val:
                        waits[key] = val
                if op["dma"] and op["prev"] is not None:
                    key, val = op["prev"]
                    if waits.get(key, 0) < val:
                        waits[key] = val
                for key, val in waits.items():
                    if known.get(key, 0) >= val:
                        continue
                    eng.wait_ge(semof(key), val)
                    known[key] = val
                ins = op["fn"](eng)
                if "sig" in op:
                    ins.then_inc(semof(op["sig"][0]), 16 if op["dma"] else 1)
            if engname == "sp":
                for key, val in cnt.items():
                    if isinstance(key, tuple):
                        eng.wait_ge(semof(key), val)
                eng.dma_start(out=fin["d1"][:], in_=fin["d0"][:]).then_inc(phase_sem, 16)
            elif engname == "pe":
                pass
            elif engname == "act":
                eng.activation(out=fin["act"][:], in_=fin["d0"][0:1, 0:1].to_broadcast([1, 1]) if False else fin["act"][:], func=AF.Copy).then_inc(phase_sem, 1)
            else:
                eng.memset(fin[engname][:], 0.0).then_inc(phase_sem, 1)

        with nc.Block() as block:
            @block.sync
            def _(e):
                run_stream("sp", e)

            @block.tensor
            def _(e):
                run_stream("pe", e)

            @block.scalar
            def _(e):
                run_stream("act", e)

            @block.vector
            def _(e):
                run_stream("dve", e)

            @block.gpsimd
            def _(e):
                run_stream("pool", e)


class K:
    def __init__(self, P):
        self.P = P

    def dma(self, q, out, in_, r=(), w=(), **kw):
        self.P.add(q, lambda e: e.dma_start(out=out, in_=in_, **kw), r, w, dma=True)

    def mm(self, out, lhsT, rhs, start, stop, r=(), w=()):
        self.P.add("pe", lambda e: e.matmul(out, lhsT=lhsT, rhs=rhs, start=start, stop=stop), r, w)

    def tr(self, out, in_, ident, r=(), w=()):
        self.P.add("pe", lambda e: e.transpose(out=out, in_=in_, identity=ident), r, w)

    def act(self, out, in_, func, r=(), w=(), **kw):
        self.P.add("act", lambda e: e.activation(out=out, in_=in_, func=func, **kw), r, w)

    def tt(self, eng, out, in0, in1, op, r=(), w=()):
        self.P.add(eng, lambda e: e.tensor_tensor(out=out, in0=in0, in1=in1, op=op), r, w)

    def ts(self, eng, out, in0, s1, s2, op0, op1=None, r=(), w=()):
        if op1 is None:
            self.P.add(eng, lambda e: e.tensor_scalar(out=out, in0=in0, scalar1=s1, scalar2=None, op0=op0), r, w)
        else:
            self.P.add(eng, lambda e: e.tensor_scalar(out=out, in0=in0, scalar1=s1, scalar2=s2, op0=op0, op1=op1), r, w)

    def stt(self, eng, out, in0, scalar, in1, op0, op1, r=(), w=()):
        self.P.add(eng, lambda e: e.scalar_tensor_tensor(out=out, in0=in0, scalar=scalar, in1=in1, op0=op0, op1=op1), r, w)

    def cp(self, eng, out, in_, r=(), w=()):
        self.P.add(eng, lambda e: e.tensor_copy(out=out, in_=in_), r, w)

    def red(self, eng, out, in_, r=(), w=(), op=None):
        op = op or ALU.add
        self.P.add(eng, lambda e: e.tensor_reduce(out=out, in_=in_, axis=AX.X, op=op), r, w)

    def recip(self, out, in_, r=(), w=()):
        self.P.add("dve", lambda e: e.reciprocal(out=out, in_=in_), r, w)

    def memset(self, eng, out, val, r=(), w=()):
        self.P.add(eng, lambda e: e.memset(out, val), r, w)

    def max8(self, out, in_, r=(), w=()):
        self.P.add("dve", lambda e: e.max(out=out, in_=in_), r, w)

    def mrep(self, out, rep, vals, r=(), w=()):
        self.P.add("dve", lambda e: e.match_replace(out=out, in_to_replace=rep, in_values=vals, imm_value=-1e30), r, w)


def bc(ap, shape):
    return ap.to_broadcast(list(shape))


def build_program(debug=False):
    nc = bass.Bass("TRN2", target_bir_lowering=False)

    def din(name, shape, dt=F32):
        return nc.dram_tensor(name, list(shape), dt, kind="ExternalInput").ap()

    def dscr(name, shape, dt):
        kind = "ExternalOutput" if (debug and name in DEBUG_OUT) else "Internal"
        return nc.dram_tensor(name, list(shape), dt, kind=kind).ap()

    x = din("x", [S, D])
    pos = din("pos", [S], I32)
    cT = din("cT", [128, 8])
    w_ada = din("w_ada", [D, 6 * D])
    b_adaT = din("b_adaT", [128, 48])
    n1gT = din("n1gT", [128, 8])
    n2gT = din("n2gT", [128, 8])
    w_in = din("w_in", [D, 7424])
    b_gate_bc = din("b_gate_bc", [128, 2048])
    gainA = din("gainA", [128, 8, 64])
    gainQB = din("gainQB", [128, 8, 64])
    gainKB = din("gainKB", [128, 8, 64])
    lam_bc = din("lam_bc", [128, 4, 64])
    subln_bc = din("subln_bc", [128, 128])
    invf_bc = din("invf_bc", [128, 32])
    halfpi = din("halfpi", [128, 64])
    mask_std = din("mask_std", [128, 256])
    mask_bnd = din("mask_bnd", [128, 256])
    w_pa = din("w_pa", [256, D])
    w_pb = din("w_pb", [D, D])
    w_out = din("w_out", [D, D])
    wqT = din("wqT", [16, 128, D])
    skT = din("skT", [16, 128, 128])
    UT = din("UT", [16384, 1024])
    Vx = din("Vx", [16384, 1024])
    out = nc.dram_tensor("out", [S, D], F32, kind="ExternalOutput").ap()

    QTA = dscr("QTA", [3, 2, 128, S], BF16)
    KTA = dscr("KTA", [3, 2, 128, S], BF16)
    VA = dscr("VA", [3, S, 256], BF16)
    QTB = dscr("QTB", [8, 128, S], BF16)
    KTB = dscr("KTB", [8, 128, S], BF16)
    VB = dscr("VB", [S, 1024], BF16)
    GT = dscr("GT", [S, 2048], BF16)
    ND = dscr("ND", [S, 12, 65], F32)
    OBT = dscr("OBT", [8, 128, S], BF16)
    X1 = dscr("X1", [S, D], F32)
    H2T = dscr("H2T", [8, 128, S], BF16)
    PS = dscr("PS", [S, 8, 2, 128], F32)
    CF = dscr("CF", [S, 8], F32)
    UTb = dscr("UTb", [16384, 1024], BF16)
    Vb = dscr("Vb", [16384, 1024], BF16)

    with ExitStack() as top:
        def sbt(es, name, shape, dt):
            return es.enter_context(nc.sbuf_tensor(name, list(shape), dt))

        def pst(es, name, shape, dt):
            return es.enter_context(nc.psum_tensor(name, list(shape), dt))

        semsets = []
        for ph in range(3):
            ss = {}
            for e in ("pe", "act", "dve", "pool"):
                ss[e] = top.enter_context(nc.semaphore(f"s{ph}_{e}"))
            for q in QUEUES:
                ss["dma_" + q] = [top.enter_context(nc.semaphore(f"d{ph}_{q}_{i}")) for i in range(N_DMA_SEMS)]
            semsets.append((ss, {}))
        phase_sem = top.enter_context(nc.semaphore("phase"))
        conv_sem = top.enter_context(nc.semaphore("conv"))

        identb = sbt(top, "identb", [128, 128], BF16)
        identf = sbt(top, "identf", [128, 128], F32)
        onesf = sbt(top, "onesf", [128, 128], F32)
        s1T = sbt(top, "s1T", [128, 8], F32)
        sh1T = sbt(top, "sh1T", [128, 8], F32)
        s2T = sbt(top, "s2T", [128, 8], F32)
        sh2T = sbt(top, "sh2T", [128, 8], F32)
        G1row = sbt(top, "G1row", [128, D], F32)
        G2row = sbt(top, "G2row", [128, D], F32)
        neglam = sbt(top, "neglam", [128, 1], F32)
        sgain = sbt(top, "sgain", [128, 128], F32)
        epsb = sbt(top, "epsb", [128, 1], F32)
        fin = dict(
            d0=sbt(top, "fin_d0", [1, 16], F32), d1=sbt(top, "fin_d1", [1, 16], F32),
            act=sbt(top, "fin_act", [128, 1], F32), dve=sbt(top, "fin_dve", [128, 1], F32),
            pool=sbt(top, "fin_pool", [128, 1], F32), idb=identb,
        )
        psn = [0]

        def alloc_psum(es, nf=6, nb=2):
            psn[0] += 1
            bk = [pst(es, f"bank{psn[0]}_{i}", [128, 512], F32) for i in range(nf)]
            bt = [pst(es, f"bankT{psn[0]}_{i}", [128, 1024], BF16) for i in range(nb)]
            return bk, bt

        with ExitStack() as es:
            P = Prog()
            k = K(P)
            banks, bankT = alloc_psum(es)
            cTs = sbt(es, "cTs", [128, 8], F32)
            scs = sbt(es, "scs", [128, 8], F32)
            wada = [sbt(es, f"wada{i}", [128, 6 * D], F32) for i in range(2)]
            badas = sbt(es, "badas", [128, 48], F32)
            modT = sbt(es, "modT", [128, 48], F32)
            n1s = sbt(es, "n1s", [128, 8], F32)
            n2s = sbt(es, "n2s", [128, 8], F32)
            lams = sbt(es, "lams", [128, 4, 64], F32)
            lprod = sbt(es, "lprod", [128, 2, 64], F32)
            lsum = sbt(es, "lsum", [128, 2], F32)
            lexp = sbt(es, "lexp", [128, 2], F32)
            ltmp = sbt(es, "ltmp", [128, 1], F32)
            subs = sbt(es, "subs", [128, 128], F32)
            dg = [sbt(es, f"dg{i}", [128, 128], F32) for i in range(2)]
            NCONV = 16
            rows = 16384 // NCONV
            for i in range(NCONV):
                for (src, dst) in ((UT, UTb), (Vx, Vb)):
                    P.add("pool", (lambda s_, d_, i_: (lambda e: e.dma_start(out=d_[i_ * rows:(i_ + 1) * rows, :], in_=s_[i_ * rows:(i_ + 1) * rows, :]).then_inc(conv_sem, 16)))(src, dst, i))
            k.dma("sp", cTs[:], cT, w=["cTs"])
            k.dma("sp", badas[:], b_adaT, w=["badas"])
            k.dma("sp", n1s[:], n1gT, w=["n1s"])
            k.dma("sp", n2s[:], n2gT, w=["n2s"])
            k.dma("sp", lams[:], lam_bc, w=["lams"])
            k.dma("sp", subs[:], subln_bc, w=["subs"])
            k.memset("dve", identf[:], 1.0, w=["identf"])
            P.add("pool", lambda e: e.affine_select(out=identf[:], in_=identf[:], pattern=[[-1, 128]], compare_op=ALU.is_equal,
                                                   fill=0.0, base=0, channel_multiplier=1), ["identf"], ["identf"])
            k.cp("dve", identb[:], identf[:], r=["identf"], w=["identb"])
            k.memset("dve", onesf[:], 1.0, w=["onesf"])
            k.memset("dve", epsb[:], EPS, w=["epsb"])
            k.memset("dve", fin["d0"][:], 0.0, w=["find0"])
            k.act(scs[:], cTs[:], AF.Silu, r=["cTs"], w=["scs"])
            modps = banks[0]
            for kc in range(8):
                b = kc % 2
                k.dma("sp", wada[b][:], w_ada[kc * 128:(kc + 1) * 128, :], w=[f"wada{b}"])
                for f in range(48):
                    k.mm(modps[:, f:f + 1], wada[b][:, f * 128:(f + 1) * 128], scs[:, kc:kc + 1], kc == 0 and f == 0, kc == 7,
                         r=[f"wada{b}", "scs"], w=["modps"])
            k.tt("dve", modT[:], modps[:, 0:48], badas[:], ALU.add, r=["modps", "badas"], w=["modT"])
            k.stt("dve", s1T[:], modT[:, 8:16], 1.0, n1s[:], ALU.add, ALU.mult, r=["modT", "n1s"], w=["s1T"])
            k.cp("dve", sh1T[:], modT[:, 0:8], r=["modT"], w=["sh1T"])
            k.stt("dve", s2T[:], modT[:, 32:40], 1.0, n2s[:], ALU.add, ALU.mult, r=["modT", "n2s"], w=["s2T"])
            k.cp("dve", sh2T[:], modT[:, 24:32], r=["modT"], w=["sh2T"])
            for (row, base, bk) in ((G1row, 16, 1), (G2row, 40, 3)):
                for c in range(8):
                    b = c % 2
                    k.ts("dve", dg[b][:], identf[:], modT[:, base + c:base + c + 1], None, ALU.mult, r=["modT", "identf"], w=[f"dg{b}"])
                    bank = banks[bk + c // 4]
                    k.mm(bank[:, (c % 4) * 128:(c % 4 + 1) * 128], onesf[:], dg[b][:], True, True, r=[f"dg{b}", "onesf"], w=[f"gr{bk + c // 4}"])
                for hh in range(2):
                    k.cp("dve", row[:, hh * 512:(hh + 1) * 512], banks[bk + hh][:], r=[f"gr{bk + hh}"], w=[f"grow{base}"])
            k.tt("dve", lprod[:, 0, :], lams[:, 0, :], lams[:, 1, :], ALU.mult, r=["lams"], w=["lprod0"])
            k.tt("dve", lprod[:, 1, :], lams[:, 2, :], lams[:, 3, :], ALU.mult, r=["lams"], w=["lprod1"])
            k.red("dve", lsum[:], lprod[:], r=["lprod0", "lprod1"], w=["lsum"])
            k.act(lexp[:], lsum[:], AF.Exp, r=["lsum"], w=["lexp"])
            k.tt("dve", ltmp[:], lexp[:, 1:2], lexp[:, 0:1], ALU.subtract, r=["lexp"], w=["ltmp"])
            k.ts("dve", neglam[:], ltmp[:], -LAM_INIT, None, ALU.add, r=["ltmp"], w=["neglam"])
            k.ts("dve", sgain[:], subs[:], 1.0 - LAM_INIT, None, ALU.mult, r=["subs"], w=["sgain"])
            P.emit(nc, semsets, phase_sem, 0, fin)

        with ExitStack() as es:
            P = Prog()
            k = K(P)
            banks, bankT = alloc_psum(es, 5, 3)
            hT = sbt(es, "hT", [128, 8, 2048], BF16)
            xt = [sbt(es, f"xt{i}", [128, D], F32) for i in range(2)]
            sq = [sbt(es, f"sq{i}", [128, D], F32) for i in range(2)]
            xn = [sbt(es, f"xn{i}", [128, D], BF16) for i in range(2)]
            ssx = [sbt(es, f"ssx{i}", [128, 1], F32) for i in range(2)]
            rsx = [sbt(es, f"rsx{i}", [128, 1], F32) for i in range(2)]
            gA = sbt(es, "gA", [128, 8, 64], F32)
            gQB = sbt(es, "gQB", [128, 8, 64], F32)
            gKB = sbt(es, "gKB", [128, 8, 64], F32)
            bgs = sbt(es, "bgs", [128, 2048], F32)
            invfs = sbt(es, "invfs", [128, 32], F32)
            hps = sbt(es, "hps", [128, 64], F32)
            posi = sbt(es, "posi", [128, 16], I32)
            posf = sbt(es, "posf", [128, 16], F32)
            tabs = [sbt(es, f"tab{i}", [128, 16, 64], F32) for i in range(3)]
            arg = sbt(es, "arg", [128, 16, 64], F32)
            argk = sbt(es, "argk", [128, 16, 64], F32)
            argi = sbt(es, "argi", [128, 16, 64], I32)
            wb = [sbt(es, f"wb{i}", [128, 8, 512], BF16) for i in range(2)]
            sqq = [sbt(es, f"sqq{i}", [128, 8, 64], F32) for i in range(3)]
            ssq = [sbt(es, f"ssq{i}", [128, 8], F32) for i in range(3)]
            rsq = [sbt(es, f"rsq{i}", [128, 8], F32) for i in range(3)]
            qn = [sbt(es, f"qn{i}", [128, 8, 64], F32) for i in range(3)]
            qg = [sbt(es, f"qg{i}", [128, 8, 2, 32], F32) for i in range(3)]
            rt = [sbt(es, f"rt{i}", [128, 4, 8, 32], F32) for i in range(3)]
            qr = [sbt(es, f"qr{i}", [128, 8, 2, 32], BF16) for i in range(3)]
            stg = [sbt(es, f"stg{i}", [128, 4, 128], BF16) for i in range(3)]
            vst = [sbt(es, f"vst{i}", [128, 512], BF16) for i in range(3)]
            gpre = [sbt(es, f"gpre{i}", [128, 512], F32) for i in range(3)]

            k.dma("sp", gA[:], gainA, w=["gA"])
            k.dma("sp", gQB[:], gainQB, w=["gQB"])
            k.dma("sp", gKB[:], gainKB, w=["gKB"])
            k.dma("sp", bgs[:], b_gate_bc, w=["bgs"])
            k.dma("sp", invfs[:], invf_bc, w=["invfs"])
            k.dma("sp", hps[:], halfpi, w=["hps"])

            pos_pat = [
                pos.rearrange("(st u m) -> st m u", st=2, u=16, m=128),
                pos.rearrange("(st blk m r) -> st m blk r", st=2, blk=4, m=128, r=4),
                pos.rearrange("(st m r) -> st m r", st=2, m=128, r=16),
            ]

            def tokcols(pat, u):
                if pat == 0:
                    return slice(u * 128, (u + 1) * 128)
                if pat == 1:
                    blk, r = u // 4, u % 4
                    return slice(blk * 512 + r, blk * 512 + 512, 4)
                return slice(u, 2048, 16)

            def perm0(pat, st, u):
                if pat == 0:
                    return st * 2048 + u * 128
                if pat == 1:
                    blk, r = u // 4, u % 4
                    return r * 1024 + st * 512 + blk * 128
                return u * 256 + st * 128

            jobs = []
            for g in range(3):
                jobs.append((g, "qk", [(256 * g, 256), (768 + 256 * g, 256)], ("A", g)))
                jobs.append((g, "v", [(1536 + 256 * g, 256)], ("A", g)))
            for j in range(2):
                jobs.append((0, "qk", [(2304 + 512 * j, 512)], ("QB", j)))
                jobs.append((0, "qk", [(3328 + 512 * j, 512)], ("KB", j)))
                jobs.append((0, "v", [(4352 + 512 * j, 512)], ("B", j)))
            for j in range(4):
                jobs.append((0, "gate", [(5376 + 512 * j, 512)], ("G", j)))

            unit = 0
            for st in range(2):
                for u in range(16):
                    b = u % 2
                    t0 = st * 2048 + u * 128
                    k.dma("sp", xt[b][:], x[t0:t0 + 128, :], w=[f"xt{b}"])
                    k.act(sq[b][:], xt[b][:], AF.Square, r=[f"xt{b}"], w=[f"sq{b}"])
                    k.red("dve", ssx[b][:], sq[b][:], r=[f"sq{b}"], w=[f"ssx{b}"])
                    k.act(rsx[b][:], ssx[b][:], AF.Sqrt, r=[f"ssx{b}", "epsb"], w=[f"rsx{b}"], scale=1.0 / D, bias=epsb[:])
                    k.recip(rsx[b][:], rsx[b][:], r=[f"rsx{b}"], w=[f"rsx{b}"])
                    k.ts("dve", xn[b][:], xt[b][:], rsx[b][:, 0:1], None, ALU.mult, r=[f"xt{b}", f"rsx{b}"], w=[f"xn{b}"])
                    for c in range(8):
                        k.tr(bankT[b][:, c * 128:(c + 1) * 128], xn[b][:, c * 128:(c + 1) * 128], identb[:], r=[f"xn{b}"], w=[f"bT{b}"])
                    for c in range(8):
                        k.act(hT[:, c, u * 128:(u + 1) * 128], bankT[b][:, c * 128:(c + 1) * 128], AF.Identity,
                              r=[f"bT{b}"], w=[f"hT{u}"], scale=s1T[:, c:c + 1], bias=sh1T[:, c:c + 1])
                for pat in range(3):
                    if pat == 0:
                        k.dma("sp", posi[:], pos_pat[0][st], w=["posi"], allow_slow_non_contiguous=True)
                    elif pat == 1:
                        k.dma("sp", posi[:].rearrange("p (a b) -> p a b", a=4), pos_pat[1][st], w=["posi"])
                    else:
                        k.dma("sp", posi[:], pos_pat[2][st], w=["posi"])
                    k.cp("dve", posf[:], posi[:], r=["posi"], w=["posf"])
                    k.tt("dve", arg[:, :, 0:32], bc(posf[:].unsqueeze(2), [128, 16, 32]), bc(invfs[:].unsqueeze(1), [128, 16, 32]), ALU.mult,
                         r=["posf", "invfs"], w=["arg"])
                    k.cp("dve", arg[:, :, 32:64], arg[:, :, 0:32], r=["arg"], w=["arg"])
                    k.tt("dve", arg[:], arg[:], bc(hps[:].unsqueeze(1), [128, 16, 64]), ALU.add, r=["arg", "hps"], w=["arg"])
                    k.ts("dve", argk[:], arg[:], 1.0 / TWO_PI, None, ALU.mult, r=["arg"], w=["argk"])
                    k.cp("dve", argi[:], argk[:], r=["argk"], w=["argi"])
                    k.cp("dve", argk[:], argi[:], r=["argi"], w=["argk"])
                    k.stt("dve", arg[:], argk[:], -C1, arg[:], ALU.mult, ALU.add, r=["argk", "arg"], w=["arg"])
                    k.stt("dve", arg[:], argk[:], -C2, arg[:], ALU.mult, ALU.add, r=["argk", "arg"], w=["arg"])
                    k.ts("dve", argk[:], arg[:], float(np.pi), -TWO_PI, ALU.is_gt, ALU.mult, r=["arg"], w=["argk"])
                    k.tt("dve", arg[:], arg[:], argk[:], ALU.add, r=["arg", "argk"], w=["arg"])
                    k.ts("dve", argk[:], arg[:], float(-np.pi), TWO_PI, ALU.is_lt, ALU.mult, r=["arg"], w=["argk"])
                    k.tt("dve", arg[:], arg[:], argk[:], ALU.add, r=["arg", "argk"], w=["arg"])
                    k.act(tabs[pat][:], arg[:], AF.Sin, r=["arg"], w=[f"tab{pat}"])
                hall = [f"hT{u}" for u in range(16)]
                pending = []
                for ji, (pat, kind, cols, extra) in enumerate(jobs):
                    wbi = ji % 2
                    off = 0
                    for (c0, ncol) in cols:
                        k.dma("pool", wb[wbi][:, :, off:off + ncol], w_in[:, c0:c0 + ncol].rearrange("(c p) n -> p c n", p=128), w=[f"wb{wbi}"])
                        off += ncol
                    ncols = off
                    for u in range(16):
                        ub = unit % 3
                        unit += 1
                        bankP = banks[ub]
                        tc = tokcols(pat, u)
                        rtok = hall if pat else [f"hT{u}"]
                        for c in range(8):
                            k.mm(bankP[:, 0:ncols], hT[:, c, tc], wb[wbi][:, c, 0:ncols], c == 0, c == 7, r=rtok + [f"wb{wbi}"], w=[f"bP{ub}"])
                        p0 = perm0(pat, st, u)

                        def post1(ub=ub, bankP=bankP, pat=pat, kind=kind, extra=extra, u=u, p0=p0, ncols=ncols):
                            if kind == "qk":
                                gt = {"A": gA, "QB": gQB, "KB": gKB}[extra[0]]
                                gtn = {"A": "gA", "QB": "gQB", "KB": "gKB"}[extra[0]]
                                pv = bankP[:].rearrange("p (h c) -> p h c", h=8)
                                k.act(sqq[ub][:], pv, AF.Square, r=[f"bP{ub}"], w=[f"sqq{ub}"])
                                k.red("dve", ssq[ub][:], sqq[ub][:], r=[f"sqq{ub}"], w=[f"ssq{ub}"])
                                k.act(rsq[ub][:], ssq[ub][:], AF.Sqrt, r=[f"ssq{ub}", "epsb"], w=[f"rsq{ub}"], scale=1.0 / 64, bias=epsb[:])
                                k.recip(rsq[ub][:], rsq[ub][:], r=[f"rsq{ub}"], w=[f"rsq{ub}"])
                                k.tt("dve", qn[ub][:], pv, bc(rsq[ub][:].unsqueeze(2), [128, 8, 64]), ALU.mult, r=[f"bP{ub}", f"rsq{ub}"], w=[f"qn{ub}"])
                                k.tt("pool", qg[ub][:].rearrange("p h a f -> p h (a f)"), qn[ub][:], gt[:], ALU.mult, r=[f"qn{ub}", gtn], w=[f"qg{ub}"])
                                sinb = bc(tabs[pat][:, u, 0:32].unsqueeze(1), [128, 8, 32])
                                cosb = bc(tabs[pat][:, u, 32:64].unsqueeze(1), [128, 8, 32])
                                x1 = qg[ub][:, :, 0, :]
                                x2 = qg[ub][:, :, 1, :]
                                tn = f"tab{pat}"
                                k.tt("dve", rt[ub][:, 0], x1, cosb, ALU.mult, r=[f"qg{ub}", tn], w=[f"rt0{ub}"])
                                k.tt("dve", rt[ub][:, 1], x2, sinb, ALU.mult, r=[f"qg{ub}", tn], w=[f"rt1{ub}"])
                                k.tt("dve", qr[ub][:, :, 0, :], rt[ub][:, 0], rt[ub][:, 1], ALU.subtract, r=[f"rt0{ub}", f"rt1{ub}"], w=[f"qr{ub}a"])
                                k.tt("pool", rt[ub][:, 2], x2, cosb, ALU.mult, r=[f"qg{ub}", tn], w=[f"rt2{ub}"])
                                k.tt("pool", rt[ub][:, 3], x1, sinb, ALU.mult, r=[f"qg{ub}", tn], w=[f"rt3{ub}"])
                                k.tt("pool", qr[ub][:, :, 1, :], rt[ub][:, 2], rt[ub][:, 3], ALU.add, r=[f"rt2{ub}", f"rt3{ub}"], w=[f"qr{ub}b"])
                                return
                            if kind == "v":
                                k.act(vst[ub][:, 0:ncols], bankP[:, 0:ncols], AF.Copy, r=[f"bP{ub}"], w=[f"vst{ub}"])
                                if extra[0] == "A":
                                    k.dma("sp", VA[extra[1], p0:p0 + 128, :], vst[ub][:, 0:256], r=[f"vst{ub}"], w=["VA"])
                                else:
                                    j = extra[1]
                                    k.dma("sp", VB[p0:p0 + 128, 512 * j:512 * j + 512], vst[ub][:], r=[f"vst{ub}"], w=["VB"])
                            else:
                                j = extra[1]
                                k.tt("dve", gpre[ub][:], bankP[:], bgs[:, 512 * j:512 * j + 512], ALU.add, r=[f"bP{ub}", "bgs"], w=[f"gpre{ub}"])
                                k.act(vst[ub][:], gpre[ub][:], AF.Sigmoid, r=[f"gpre{ub}"], w=[f"vst{ub}"])
                                k.dma("sp", GT[p0:p0 + 128, 512 * j:512 * j + 512], vst[ub][:], r=[f"vst{ub}"], w=["GT"])

                        def post2(ub=ub, bankP=bankP, pat=pat, kind=kind, extra=extra, u=u, p0=p0, ncols=ncols):
                            if kind == "qk":
                                qflat = qr[ub][:].rearrange("p h a f -> p (h a f)")
                                for q4 in range(4):
                                    k.tr(bankT[ub][:, q4 * 128:(q4 + 1) * 128], qflat[:, q4 * 128:(q4 + 1) * 128], identb[:],
                                         r=[f"qr{ub}a", f"qr{ub}b"], w=[f"bT{ub}"])
                                k.act(stg[ub][:].rearrange("p a t -> p (a t)"), bankT[ub][:, 0:512], AF.Copy, r=[f"bT{ub}"], w=[f"stg{ub}"])
                                if extra[0] == "A":
                                    g = extra[1]
                                    k.dma("sp", QTA[g, :, :, p0:p0 + 128].rearrange("a c t -> c a t"), stg[ub][:, 0:2, :], r=[f"stg{ub}"], w=["QTA"])
                                    k.dma("sp", KTA[g, :, :, p0:p0 + 128].rearrange("a c t -> c a t"), stg[ub][:, 2:4, :], r=[f"stg{ub}"], w=["KTA"])
                                else:
                                    dst = QTB if extra[0] == "QB" else KTB
                                    j = extra[1]
                                    k.dma("sp", dst[4 * j:4 * j + 4, :, p0:p0 + 128].rearrange("a c t -> c a t"), stg[ub][:], r=[f"stg{ub}"], w=[extra[0]])
                        pending.append((post1, post2))
                        if len(pending) >= 2:
                            pending[-2][0]()
                        if len(pending) >= 3:
                            pending.pop(0)[1]()
                if pending:
                    pending[-1][0]()
                while pending:
                    pending.pop(0)[1]()
            P.emit(nc, semsets, phase_sem, 1, fin)

        with ExitStack() as es:
            P = Prog()
            k = K(P)
            banks, bankT = alloc_psum(es)
            KTs = [sbt(es, f"KTs{i}", [128, S + 128], BF16) for i in range(2)]
            QTs = [sbt(es, f"QTs{i}", [128, S + 128], BF16) for i in range(2)]
            V1 = [sbt(es, f"V1_{i}", [128, 33, 65], BF16) for i in range(2)]
            mstd = sbt(es, "mstd", [128, 256], F32)
            mbnd = sbt(es, "mbnd", [128, 256], F32)
            Eb = [sbt(es, f"Eb{i}", [128, 256], BF16) for i in range(2)]
            Pm = [sbt(es, f"Pm{i}", [128, 256], BF16) for i in range(2)]
            osb = [sbt(es, f"osb{i}", [128, 65], F32) for i in range(4)]
            k.dma("sp", mstd[:], mask_std, w=["mstd"])
            k.dma("sp", mbnd[:], mask_bnd, w=["mbnd"])
            for i in range(2):
                k.memset("pool", KTs[i][:, 0:64], 0.0, w=[f"KTs{i}"])
                k.memset("pool", KTs[i][:, S + 64:S + 128], 0.0, w=[f"KTs{i}"])
                k.memset("pool", QTs[i][:, 0:64], 0.0, w=[f"QTs{i}"])
                k.memset("pool", QTs[i][:, S + 64:S + 128], 0.0, w=[f"QTs{i}"])
                k.memset("pool", V1[i][:], 0.0, w=[f"V1_{i}"])
                k.memset("pool", V1[i][:, :, 64:65], 1.0, w=[f"V1_{i}"])
            bankSs = [banks[0], banks[2]]
            bankOs = [banks[1], banks[3], banks[4], banks[5]]
            hcount = 0
            ccount = 0
            for g in range(3):
                dil = DIL[g]
                L = S // dil
                for pr in range(2):
                    pb = (g * 2 + pr) % 2
                    k.dma("sp", KTs[pb][:, 64:64 + S], KTA[g, pr], r=["KTA"], w=[f"KTs{pb}"])
                    k.dma("sp", QTs[pb][:, 64:64 + S], QTA[g, pr], r=["QTA"], w=[f"QTs{pb}"])
                    for hh in range(2):
                        hs = pr * 2 + hh
                        bp = 64 * hh
                        vb = hcount % 2
                        hcount += 1
                        vsrc = VA[g, :, hs * 64:(hs + 1) * 64]
                        k.dma("sp", V1[vb][:, 1:32, 0:64], vsrc[64:S - 64, :].rearrange("(j p) c -> p j c", p=128), r=["VA"], w=[f"V1_{vb}"])
                        k.dma("sp", V1[vb][64:128, 0, 0:64], vsrc[0:64, :], r=["VA"], w=[f"V1_{vb}"])
                        k.dma("sp", V1[vb][0:64, 32, 0:64], vsrc[S - 64:S, :], r=["VA"], w=[f"V1_{vb}"])
                        for jc in range(33):
                            sb_ = ccount % 2
                            ccount += 1
                            bnd = (128 * jc) % L == 0
                            qlo = 128 if jc == 0 else 0
                            qhi = 128 if jc == 32 else 256
                            sv = bankSs[sb_][:, 0:256]
                            k.mm(sv[:, qlo:qhi], KTs[pb][bp:bp + 64, 128 * jc:128 * jc + 128],
                                 QTs[pb][bp:bp + 64, 128 * jc - 64 + qlo:128 * jc - 64 + qhi],
                                 True, True, r=[f"KTs{pb}", f"QTs{pb}"], w=[f"bS{sb_}"])
                            k.act(Eb[sb_][:, qlo:qhi], sv[:, qlo:qhi], AF.Exp, r=[f"bS{sb_}"], w=[f"Eb{sb_}"], scale=0.125)
                            mk = mbnd if bnd else mstd
                            k.tt("dve", Pm[sb_][:, qlo:qhi], Eb[sb_][:, qlo:qhi], mk[:, qlo:qhi], ALU.mult, r=[f"Eb{sb_}", "mstd", "mbnd"], w=[f"Pm{sb_}"])
                            for half in range(2):
                                if half * 128 < qlo or half * 128 >= qhi:
                                    continue
                                blk = jc - 1 + half
                                a = blk % 4
                                k.mm(bankOs[a][:, 0:65], Pm[sb_][:, half * 128:half * 128 + 128], V1[vb][:, jc, :],
                                     half == 1, half == 0, r=[f"Pm{sb_}", f"V1_{vb}"], w=[f"bO{a}"])
                            if jc >= 1:
                                blk = jc - 1
                                a = blk % 4
                                k.cp("dve", osb[a][:], bankOs[a][:, 0:65], r=[f"bO{a}"], w=[f"osb{a}"])
                                r_ = (128 * blk) // L
                                j0 = (128 * blk) % L
                                s0 = j0 * dil + r_
                                dst = ND[s0:s0 + 127 * dil + 1:dil, g * 4 + hs, :]
                                k.dma("sp", dst, osb[a][:], r=[f"osb{a}"], w=["ND"])
            P.emit(nc, semsets, phase_sem, 2, fin)

        with ExitStack() as es:
            P = Prog()
            k = K(P)
            banks, bankT = alloc_psum(es, 7, 1)
            KTs = [sbt(es, f"KTd{i}", [128, S], BF16) for i in range(2)]
            QTs = [sbt(es, f"QTd{i}", [128, S], BF16) for i in range(2)]
            V1 = [sbt(es, f"V1d{i}", [128, 32, 129], BF16) for i in range(2)]
            E = [[sbt(es, f"E{m}_{i}", [128, 512], BF16) for i in range(2)] for m in range(2)]
            osb = sbt(es, "osbd", [128, 8, 129], F32)
            rr = sbt(es, "rr", [128, 8], F32)
            o1 = sbt(es, "o1", [128, 4, 128], F32)
            o2 = sbt(es, "o2", [128, 4, 128], F32)
            ssd = sbt(es, "ssd", [128, 4], F32)
            obb = sbt(es, "obb", [128, 4, 128], BF16)
            obT = [sbt(es, f"obT{i}", [128, 512], BF16) for i in range(2)]
            for i in range(2):
                k.memset("pool", V1[i][:, :, 128:129], 1.0, w=[f"V1d{i}"])
            accb = [banks[4], banks[5], banks[6]]
            steps = [(h, qb, kc) for h in range(8) for qb in range(8) for kc in range(32)]

            def qk_exp(n):
                h, qb, kc = steps[n]
                hb = h % 2
                eb = n % 2
                if qb == 0 and kc == 0:
                    k.dma("sp", KTs[hb][:], KTB[h], r=["KB"], w=[f"KTd{hb}"])
                    k.dma("sp", QTs[hb][:], QTB[h], r=["QB"], w=[f"QTd{hb}"])
                    k.dma("sp", V1[hb][:, :, 0:128], VB[:, h * 128:(h + 1) * 128].rearrange("(j p) c -> p j c", p=128), r=["VB"], w=[f"V1d{hb}"])
                for m in range(2):
                    bi = m * 2 + eb
                    tok = f"bS{bi}"
                    k.mm(banks[bi][:], KTs[hb][64 * m:64 * m + 64, kc * 128:(kc + 1) * 128], QTs[hb][64 * m:64 * m + 64, qb * 512:(qb + 1) * 512],
                         True, True, r=[f"KTd{hb}", f"QTd{hb}"], w=[tok])
                    k.act(E[m][eb][:], banks[bi][:], AF.Exp, r=[tok], w=[f"E{m}_{eb}"], scale=0.125)

            deferred = []
            qk_exp(0)
            for n, (h, qb, kc) in enumerate(steps):
                hb = h % 2
                eb = n % 2
                if n + 1 < len(steps):
                    qk_exp(n + 1)
                for m in range(2):
                    for sub in range(4):
                        a = m * 4 + sub
                        k.mm(accb[a // 3][:, (a % 3) * 129:(a % 3) * 129 + 129], E[m][eb][:, sub * 128:(sub + 1) * 128], V1[hb][:, kc, :],
                             kc == 0 and a % 3 == 0, kc == 31, r=[f"E{m}_{eb}", f"V1d{hb}"], w=[f"accb{a // 3}"])
                if kc == 31:
                    for a in range(8):
                        eng = "dve" if (a // 3) % 2 == 0 else "act"
                        src = accb[a // 3][:, (a % 3) * 129:(a % 3) * 129 + 129]
                        if eng == "dve":
                            k.cp("dve", osb[:, a, :], src, r=[f"accb{a // 3}"], w=[f"osbd{a}"])
                        else:
                            k.act(osb[:, a, :], src, AF.Copy, r=[f"accb{a // 3}"], w=[f"osbd{a}"])
                    k.recip(rr[:], osb[:, :, 128], r=[f"osbd{a_}" for a_ in range(8)], w=["rr"])
                    k.ts("dve", rr[:, 4:8], rr[:, 4:8], neglam[:, 0:1], None, ALU.mult, r=["rr"], w=["rr"])
                    k.tt("dve", o1[:], osb[:, 0:4, 0:128], bc(rr[:, 0:4].unsqueeze(2), [128, 4, 128]), ALU.mult, r=[f"osbd{a_}" for a_ in range(8)] + ["rr"], w=["o1"])
                    k.tt("pool", o2[:], osb[:, 4:8, 0:128], bc(rr[:, 4:8].unsqueeze(2), [128, 4, 128]), ALU.mult, r=[f"osbd{a_}" for a_ in range(8)] + ["rr"], w=["o2"])
                    k.tt("dve", o1[:], o1[:], o2[:], ALU.add, r=["o1", "o2"], w=["o1"])
                    k.tt("pool", o2[:], o1[:], o1[:], ALU.mult, r=["o1"], w=["o2"])
                    k.red("dve", ssd[:], o2[:], r=["o2"], w=["ssd"])
                    k.act(ssd[:], ssd[:], AF.Sqrt, r=["ssd", "epsb"], w=["ssd"], scale=1.0 / 128, bias=epsb[:])
                    k.recip(ssd[:], ssd[:], r=["ssd"], w=["ssd"])
                    k.tt("dve", o1[:], o1[:], bc(ssd[:].unsqueeze(2), [128, 4, 128]), ALU.mult, r=["o1", "ssd"], w=["o1"])
                    k.tt("pool", obb[:], o1[:], bc(sgain[:].unsqueeze(1), [128, 4, 128]), ALU.mult, r=["o1"], w=["obb"])
                    def fin_tr(h=h, qb=qb):
                        ob_i = (h * 8 + qb) % 2
                        for sub in range(4):
                            k.tr(bankT[0][:, sub * 128:(sub + 1) * 128], obb[:, sub, :], identb[:], r=["obb"], w=["bTd"])
                        k.act(obT[ob_i][:], bankT[0][:, 0:512], AF.Copy, r=["bTd"], w=[f"obT{ob_i}"])
                        k.dma("sp", OBT[h, :, qb * 512:(qb + 1) * 512], obT[ob_i][:], r=[f"obT{ob_i}"], w=["OBT"])
                    deferred.append((n + 6, fin_tr))
                while deferred and (deferred[0][0] <= n or n == len(steps) - 1):
                    deferred.pop(0)[1]()
            P.emit(nc, semsets, phase_sem, 3, fin)

        with ExitStack() as es:
            P = Prog()
            k = K(P)
            banks, bankT = alloc_psum(es)
            Wc = sbt(es, "Wc", [128, 8, 2048], BF16)
            wpa = sbt(es, "wpa", [128, 2, D], BF16)
            wpb = sbt(es, "wpb", [128, 8, D], BF16)
            wo = sbt(es, "wo", [128, 8, D], BF16)
            wq = [sbt(es, f"wq{i}", [128, D], F32) for i in range(2)]
            sk = [sbt(es, f"sk{i}", [128, 128], F32) for i in range(2)]
            nd = [sbt(es, f"nd{i}", [128, 12, 65], F32) for i in range(2)]
            obt = [sbt(es, f"obt{i}", [128, 8, 128], BF16) for i in range(2)]
            gts = [sbt(es, f"gts{i}", [128, 2048], BF16) for i in range(2)]
            xts = [sbt(es, f"xts{i}", [128, D], F32) for i in range(2)]
            nsum = sbt(es, "nsum", [128, 4, 65], F32)
            rden = sbt(es, "rden", [128, 4], F32)
            oab = sbt(es, "oab", [128, 4, 64], BF16)
            oaT = sbt(es, "oaT", [128, 2, 128], BF16)
            bra = sbt(es, "bra", [128, D], F32)
            t1 = sbt(es, "t1", [128, D], F32)
            t2 = sbt(es, "t2", [128, D], F32)
            mixb = sbt(es, "mixb", [128, D], BF16)
            mixT = sbt(es, "mixT", [128, 8, 128], BF16)
            x1s = sbt(es, "x1s", [128, D], F32)
            sq2 = sbt(es, "sq2", [128, D], F32)
            ss2 = sbt(es, "ss2", [128, 1], F32)
            xn2 = sbt(es, "xn2", [128, D], BF16)
            h2T = sbt(es, "h2T", [128, 8, 128], BF16)
            Ssbs = [sbt(es, f"Ssb{i}", [128, 8, 2, 128], F32) for i in range(2)]
            mr = sbt(es, "mr", [128, 128], F32)
            t16 = sbt(es, "t16", [128, 8, 2, 16], F32)
            cand = sbt(es, "cand", [128, 8, 16, 16], F32)
            mr2 = sbt(es, "mr2", [128, 256], F32)
            c16 = sbt(es, "c16", [128, 8, 16], F32)
            dd = sbt(es, "dd", [128, 8, 16], F32)
            zz = sbt(es, "zz", [128, 8], F32)
            tau = sbt(es, "tau", [128, 8], F32)
            cf = sbt(es, "cf", [128, 8], F32)
            k.dma("pool", wpa[:], w_pa.rearrange("(c p) n -> p c n", p=128), w=["wpa"])
            k.dma("pool", wpb[:], w_pb.rearrange("(c p) n -> p c n", p=128), w=["wpb"])
            k.dma("pool", wo[:], w_out.rearrange("(c p) n -> p c n", p=128), w=["wo"])
            for kk in range(16):
                b = kk % 2
                k.dma("sp", wq[b][:], wqT[kk], w=[f"wq{b}"])
                k.dma("sp", sk[b][:], skT[kk], w=[f"sk{b}"])
                for dc in range(8):
                    k.mm(banks[dc // 4][:, (dc % 4) * 128:(dc % 4 + 1) * 128], wq[b][:, dc * 128:(dc + 1) * 128], sk[b][:], True, True,
                         r=[f"wq{b}", f"sk{b}"], w=[f"wcp{dc // 4}"])
                for hh in range(2):
                    k.act(Wc[:, 4 * hh:4 * hh + 4, kk * 128:(kk + 1) * 128], banks[hh][:].rearrange("p (a n) -> p a n", a=4), AF.Copy,
                          r=[f"wcp{hh}"], w=["Wc"])
            def stageA(t):
                b = t % 2
                t0 = t * 128
                Ssb = Ssbs[b]
                sS = f"Ssb{b}"
                k.dma("sp", nd[b][:], ND[t0:t0 + 128], r=["ND"], w=[f"nd{b}"])
                k.dma("sp", obt[b][:], OBT[:, :, t0:t0 + 128].rearrange("h c t -> c h t"), r=["OBT"], w=[f"obt{b}"])
                k.dma("sp", gts[b][:], GT[t0:t0 + 128, :], r=["GT"], w=[f"gts{b}"])
                k.dma("sp", xts[b][:], x[t0:t0 + 128, :], w=[f"xts{b}"])
                k.tt("pool", nsum[:], nd[b][:, 0:4, :], nd[b][:, 4:8, :], ALU.add, r=[f"nd{b}"], w=["nsum"])
                k.tt("pool", nsum[:], nsum[:], nd[b][:, 8:12, :], ALU.add, r=[f"nd{b}", "nsum"], w=["nsum"])
                k.recip(rden[:], nsum[:, :, 64], r=["nsum"], w=["rden"])
                k.tt("dve", oab[:], nsum[:, :, 0:64], bc(rden[:].unsqueeze(2), [128, 4, 64]), ALU.mult, r=["nsum", "rden"], w=["oab"])
                oaf = oab[:].rearrange("p a c -> p (a c)")
                for c in range(2):
                    k.tr(bankT[0][:, c * 128:(c + 1) * 128], oaf[:, c * 128:(c + 1) * 128], identb[:], r=["oab"], w=["bT0"])
                k.act(oaT[:].rearrange("p a t -> p (a t)"), bankT[0][:, 0:256], AF.Copy, r=["bT0"], w=["oaT"])
                for hf in range(2):
                    for c in range(2):
                        k.mm(banks[hf][:], oaT[:, c, :], wpa[:, c, hf * 512:(hf + 1) * 512], c == 0, c == 1, r=["oaT", "wpa"], w=[f"bk{hf}"])
                for hf in range(2):
                    for c in range(8):
                        k.mm(banks[2 + hf][:], obt[b][:, c, :], wpb[:, c, hf * 512:(hf + 1) * 512], c == 0, c == 7, r=[f"obt{b}", "wpb"], w=[f"bk{2 + hf}"])
                for hf in range(2):
                    sl = slice(hf * 512, (hf + 1) * 512)
                    k.act(bra[:, sl], banks[hf][:], AF.Copy, r=[f"bk{hf}"], w=["bra"])
                    k.tt("dve", t2[:, sl], banks[2 + hf][:], gts[b][:, 1024 + hf * 512:1024 + (hf + 1) * 512], ALU.mult, r=[f"bk{2 + hf}", f"gts{b}"], w=["t2"])
                k.tt("pool", t1[:], bra[:], gts[b][:, 0:1024], ALU.mult, r=["bra", f"gts{b}"], w=["t1"])
                k.tt("pool", mixb[:], t1[:], t2[:], ALU.add, r=["t1", "t2"], w=["mixb"])
                for c in range(8):
                    k.tr(bankT[1][:, c * 128:(c + 1) * 128], mixb[:, c * 128:(c + 1) * 128], identb[:], r=["mixb"], w=["bT1"])
                k.act(mixT[:].rearrange("p a t -> p (a t)"), bankT[1][:], AF.Copy, r=["bT1"], w=["mixT"])
                for hf in range(2):
                    for c in range(8):
                        k.mm(banks[4 + hf][:], mixT[:, c, :], wo[:, c, hf * 512:(hf + 1) * 512], c == 0, c == 7, r=["mixT", "wo"], w=[f"bk{4 + hf}"])
                for hf in range(2):
                    sl = slice(hf * 512, (hf + 1) * 512)
                    k.tt("dve", t2[:, sl], banks[4 + hf][:], G1row[:, sl], ALU.mult, r=[f"bk{4 + hf}"], w=["t2"])
                k.tt("pool", x1s[:], t2[:], xts[b][:], ALU.add, r=["t2", f"xts{b}"], w=["x1s"])
                k.dma("sp", X1[t0:t0 + 128, :], x1s[:], r=["x1s"], w=["X1"])
                k.act(sq2[:], x1s[:], AF.Square, r=["x1s"], w=["sq2"])
                k.red("dve", ss2[:], sq2[:], r=["sq2"], w=["ss2"])
                k.act(ss2[:], ss2[:], AF.Sqrt, r=["ss2", "epsb"], w=["ss2"], scale=1.0 / D, bias=epsb[:])
                k.recip(ss2[:], ss2[:], r=["ss2"], w=["ss2"])
                k.ts("dve", xn2[:], x1s[:], ss2[:, 0:1], None, ALU.mult, r=["x1s", "ss2"], w=["xn2"])
                for c in range(8):
                    k.tr(bankT[0][:, c * 128:(c + 1) * 128], xn2[:, c * 128:(c + 1) * 128], identb[:], r=["xn2"], w=["bT0"])
                for c in range(8):
                    k.act(h2T[:, c, :], bankT[0][:, c * 128:(c + 1) * 128], AF.Identity, r=["bT0"], w=["h2T"], scale=s2T[:, c:c + 1], bias=sh2T[:, c:c + 1])
                k.dma("sp", H2T[:, :, t0:t0 + 128].rearrange("c p t -> p c t"), h2T[:], r=["h2T"], w=["H2T"])
                for nb in range(4):
                    for c in range(8):
                        k.mm(banks[nb][:], h2T[:, c, :], Wc[:, c, nb * 512:(nb + 1) * 512], c == 0, c == 7, r=["h2T", "Wc"], w=[f"bk{nb}"])
                Sf = Ssb[:].rearrange("p h a n -> p (h a n)")
                for nb in range(4):
                    k.act(Sf[:, nb * 512:(nb + 1) * 512], banks[nb][:], AF.Copy, r=[f"bk{nb}"], w=[sS])
            def stageB(t):
                b = t % 2
                t0 = t * 128
                Ssb = Ssbs[b]
                sS = f"Ssb{b}"
                for h in range(8):
                    for a in range(2):
                        k.max8(t16[:, h, a, 0:8], Ssb[:, h, a, :], r=[sS], w=["t16"])
                        k.mrep(mr[:], t16[:, h, a, 0:8], Ssb[:, h, a, :], r=[sS, "t16"], w=["mr"])
                        k.max8(t16[:, h, a, 8:16], mr[:], r=["mr"], w=["t16"])
                k.tt("dve", cand[:], bc(t16[:, :, 0, :].unsqueeze(3), [128, 8, 16, 16]), bc(t16[:, :, 1, :].unsqueeze(2), [128, 8, 16, 16]), ALU.add,
                     r=["t16"], w=["cand"])
                for h in range(8):
                    cv = cand[:, h].rearrange("p a b -> p (a b)")
                    k.max8(c16[:, h, 0:8], cv, r=["cand"], w=["c16"])
                    k.mrep(mr2[:], c16[:, h, 0:8], cv, r=["cand", "c16"], w=["mr2"])
                    k.max8(c16[:, h, 8:16], mr2[:], r=["mr2"], w=["c16"])
                k.ts("dve", tau[:], c16[:, :, 15], -1e-3, None, ALU.add, r=["c16"], w=["tau"])
                k.tt("dve", dd[:], c16[:], bc(c16[:, :, 0:1], [128, 8, 16]), ALU.subtract, r=["c16"], w=["dd"])
                k.act(dd[:], dd[:], AF.Exp, r=["dd"], w=["dd"])
                k.red("dve", zz[:], dd[:], r=["dd"], w=["zz"])
                k.recip(zz[:], zz[:], r=["zz"], w=["zz"])
                k.tt("dve", cf[:], tau[:], c16[:, :, 0], ALU.subtract, r=["tau", "c16"], w=["cf"])
                k.act(cf[:], cf[:], AF.Exp, r=["cf"], w=["cf"])
                k.tt("dve", cf[:], cf[:], zz[:], ALU.mult, r=["cf", "zz"], w=["cf"])
                k.tt("dve", Ssb[:, :, 0, :], Ssb[:, :, 0, :], bc(tau[:].unsqueeze(2), [128, 8, 128]), ALU.subtract, r=[sS, "tau"], w=[sS])
                k.dma("sp", PS[t0:t0 + 128], Ssb[:], r=[sS], w=["PS"])
                k.dma("sp", CF[t0:t0 + 128, :], cf[:], r=["cf"], w=["CF"])
            stageA(0)
            for t in range(NT):
                if t + 1 < NT:
                    stageA(t + 1)
                stageB(t)
            P.emit(nc, semsets, phase_sem, 4, fin)

        with ExitStack() as es:
            P = Prog()
            k = K(P)
            banks, bankT = alloc_psum(es, 8, 0)
            NB = 4
            h2 = [sbt(es, f"h2_{i}", [128, 8, 256], BF16) for i in range(2)]
            pss = sbt(es, "pss", [128, 2, 8, 2, 128], F32)
            cfs = [sbt(es, f"cfs{i}", [128, 2, 8], F32) for i in range(2)]
            x1t = sbt(es, "x1t", [128, 2, D], F32)
            Dm = [sbt(es, f"Dm{i}", [128, 2, 8, 128], BF16) for i in range(2)]
            UTi = [sbt(es, f"UTi{i}", [128, 8, 128], BF16) for i in range(NB)]
            Vi = [sbt(es, f"Vi{i}", [128, D], BF16) for i in range(NB)]
            HgA = sbt(es, "HgA", [128, 128, 256], BF16)
            zt = [sbt(es, f"zt{i}", [128, 6, 128], F32) for i in range(3)]
            E32 = [sbt(es, f"E32_{i}", [128, 2, 8, 128], F32) for i in range(2)]
            Gt = [sbt(es, f"Gt{i}", [128, 2, 8, 128], BF16) for i in range(3)]
            WT = [sbt(es, f"WT{i}", [128, 256], BF16) for i in range(2)]
            yo = [sbt(es, f"yo{i}", [128, D], F32) for i in range(2)]
            P.add("sp", lambda e: e.wait_ge(conv_sem, 16 * 32))
            UTv = UTb.rearrange("(i p) (c e) -> i p c e", p=128, c=8)
            ustep = 0
            vstep = 0
            for tt_ in range(16):
                tb = tt_ % 2
                t0 = tt_ * 256
                k.dma("sp", h2[tb][:], H2T[:, :, t0:t0 + 256].rearrange("c p t -> p c t"), w=[f"h2_{tb}"])
                k.dma("sp", cfs[tb][:], CF[t0:t0 + 256, :].rearrange("(s p) h -> p s h", p=128), w=[f"cfs{tb}"])
                for s_ in range(2):
                    for h in range(8):
                        k.ts("pool", Dm[tb][:, s_, h, :], identf[:], cfs[tb][:, s_, h:h + 1], None, ALU.mult, r=[f"cfs{tb}"], w=[f"Dm{tb}"])
                k.dma("sp", pss[:], PS[t0:t0 + 256].rearrange("(s p) h a n -> p s h a n", p=128), w=["pss"])
                for i in range(128):
                    ub = ustep % NB
                    hb_ = ustep % 2
                    ustep += 1
                    k.dma("sp", UTi[ub][:], UTv[i], w=[f"UTi{ub}"])
                    hv = banks[4 + hb_][:, 0:256]
                    for c in range(8):
                        k.mm(hv, UTi[ub][:, c, :], h2[tb][:, c, :], c == 0, c == 7, r=[f"UTi{ub}", f"h2_{tb}"], w=[f"bH{hb_}"])
                    k.act(HgA[:, i, :], hv, AF.Gelu, r=[f"bH{hb_}"], w=[f"HgA{i}"])

                NA = 10

                def st_z(i):
                    zb = i % 3
                    h0 = NA - 8
                    k.tt("pool", zt[zb][:], pss[:, 1, h0:8, 1, :], bc(pss[:, 1, h0:8, 0, i:i + 1], [128, 8 - h0, 128]), ALU.add, r=["pss"], w=[f"zt{zb}"])

                def st_eg(i):
                    zb = i % 3
                    eb = i % 2
                    h0 = NA - 8
                    for g_ in range(NA):
                        s_, h = g_ // 8, g_ % 8
                        k.act(E32[eb][:, s_, h, :], pss[:, s_, h, 1, :], AF.Exp, r=["pss"], w=[f"E32_{eb}"], bias=pss[:, s_, h, 0, i:i + 1])
                    k.act(E32[eb][:, 1, h0:8, :], zt[zb][:], AF.Exp, r=[f"zt{zb}"], w=[f"E32_{eb}"])
                    k.stt("dve", Gt[zb][:], E32[eb][:], 1.0, E32[eb][:], ALU.is_ge, ALU.mult, r=[f"E32_{eb}"], w=[f"Gt{zb}"])

                def st_gt(i):
                    zb = i % 3
                    b2 = i % 2
                    gv = banks[6 + b2][:, 0:256]
                    for s_ in range(2):
                        for h in range(8):
                            k.mm(gv[:, s_ * 128:(s_ + 1) * 128], Gt[zb][:, s_, h, :], Dm[tb][:, s_, h, :], h == 0, h == 7,
                                 r=[f"Gt{zb}", f"Dm{tb}"], w=[f"bG{b2}"])

                def st_wt(i):
                    b2 = i % 2
                    gv = banks[6 + b2][:, 0:256]
                    k.tt("dve", WT[b2][:], gv, HgA[:, i, :], ALU.mult, r=[f"bG{b2}", f"HgA{i}"], w=[f"WT{b2}"])

                vbuf = {}

                def st_vload(i):
                    nonlocal vstep
                    vb_ = vstep % NB
                    vstep += 1
                    vbuf[i] = vb_
                    k.dma("sp", Vi[vb_][:], Vb[i * 128:(i + 1) * 128, :], w=[f"Vi{vb_}"])

                def st_y(i):
                    b2 = i % 2
                    vb_ = vbuf[i]
                    for s_ in range(2):
                        for hf in range(2):
                            k.mm(banks[s_ * 2 + hf][:], WT[b2][:, s_ * 128:(s_ + 1) * 128], Vi[vb_][:, hf * 512:(hf + 1) * 512], i == 0, i == 127,
                                 r=[f"WT{b2}", f"Vi{vb_}"], w=[f"bY{s_ * 2 + hf}"])

                for it in range(128 + 3):
                    if it < 128:
                        st_vload(it)
                        st_z(it)
                    if 1 <= it <= 128:
                        st_eg(it - 1)
                    if 2 <= it <= 129:
                        st_gt(it - 2)
                    if 3 <= it <= 130:
                        st_y(it - 3)
                    if 2 <= it <= 129:
                        st_wt(it - 2)
                k.dma("sp", x1t[:], X1[t0:t0 + 256, :].rearrange("(s p) d -> p s d", p=128), w=["x1t"])
                for s_ in range(2):
                    for hf in range(2):
                        sl = slice(hf * 512, (hf + 1) * 512)
                        k.tt("dve", yo[s_][:, sl], banks[s_ * 2 + hf][:], G2row[:, sl], ALU.mult, r=[f"bY{s_ * 2 + hf}"], w=[f"yo{s_}"])
                    k.tt("pool", yo[s_][:], yo[s_][:], x1t[:, s_, :], ALU.add, r=[f"yo{s_}", "x1t"], w=[f"yo{s_}"])
                    k.dma("sp", out[t0 + s_ * 128:t0 + (s_ + 1) * 128, :], yo[s_][:], r=[f"yo{s_}"], w=["out"])
            P.emit(nc, semsets, phase_sem, 5, fin)
    return nc


DEBUG_OUT = set()


def _masks():
    kk = np.arange(128)[:, None]
    qq = np.arange(256)[None, :]
    std = ((kk <= qq) & (qq <= kk + 128)).astype(np.float32)
    blk = (((kk < 64) & (qq < 128)) | ((kk >= 64) & (qq >= 128))).astype(np.float32)
    return std, std * blk


def prep_inputs(inputs):
    f = lambda a: np.ascontiguousarray(np.asarray(a), dtype=np.float32)
    x = f(inputs["x"])
    c = f(inputs["c"])
    pos = np.ascontiguousarray(np.asarray(inputs["positions"]), dtype=np.int32)
    L = 0
    rep = lambda v, n=128: np.ascontiguousarray(np.broadcast_to(f(v)[None], (n,) + f(v).shape))
    qn_a, kn_a = f(inputs["qn_a"])[L], f(inputs["kn_a"])[L]
    qn_b, kn_b = f(inputs["qn_b"])[L], f(inputs["kn_b"])[L]
    gainA = np.concatenate([np.tile(qn_a[None], (4, 1)), np.tile(kn_a[None], (4, 1))], 0)
    gainQB = np.tile(qn_b[None], (8, 1))
    gainKB = np.tile(kn_b[None], (8, 1))
    lam = np.stack([f(inputs["lam_q1"])[L], f(inputs["lam_k1"])[L], f(inputs["lam_q2"])[L], f(inputs["lam_k2"])[L]], 0)
    invf = (1.0 / (10000.0 ** (np.arange(0, 64, 2, dtype=np.float32) / 64))).astype(np.float32)
    hp = np.concatenate([np.zeros(32, np.float32), np.full(32, np.pi / 2, np.float32)])
    mstd, mbnd = _masks()
    wq = f(inputs["w_query"])[L]
    wqT = np.ascontiguousarray(wq.reshape(D, 16, 128).transpose(1, 2, 0))
    sk = f(inputs["sub_keys"])[L].reshape(16, 128, 128)
    skT = np.ascontiguousarray(sk.transpose(0, 2, 1))
    U = f(inputs["expert_u"])[L]
    UT = np.ascontiguousarray(U.reshape(128, 128, 8, 128).transpose(0, 3, 2, 1)).reshape(16384, 1024)
    shared = dict(
        w_ada=f(inputs["w_ada"])[L],
        b_adaT=np.ascontiguousarray(f(inputs["b_ada"])[L].reshape(48, 128).T),
        n1gT=np.ascontiguousarray(f(inputs["norm1_g"])[L].reshape(8, 128).T),
        n2gT=np.ascontiguousarray(f(inputs["norm2_g"])[L].reshape(8, 128).T),
        w_in=f(inputs["w_in"])[L],
        b_gate_bc=rep(f(inputs["b_gate"])[L]),
        gainA=rep(gainA), gainQB=rep(gainQB), gainKB=rep(gainKB),
        lam_bc=rep(lam), subln_bc=rep(f(inputs["subln_g"])[L]),
        invf_bc=rep(invf), halfpi=rep(hp), mask_std=mstd, mask_bnd=mbnd,
        w_pa=f(inputs["w_proj_a"])[L], w_pb=f(inputs["w_proj_b"])[L], w_out=f(inputs["w_out"])[L],
        wqT=wqT, skT=skT, UT=UT, Vx=f(inputs["expert_v"])[L],
    )
    in_maps = []
    for b in range(8):
        m = dict(shared)
        m["x"] = x[b]
        m["pos"] = pos[b]
        m["cT"] = np.ascontiguousarray(c[b].reshape(8, 128).T)
        in_maps.append(m)
    return in_maps


def kernel(**inputs):
    in_maps = prep_inputs(inputs)
    nc = build_program()
    res = run_bass_kernel_spmd(nc, in_maps, core_ids=list(range(8)))
    return np.stack([np.asarray(r["out"], dtype=np.float32) for r in res.results], 0)
```

```python
import os
import numpy as np
from contextlib import ExitStack
import concourse.bass as bass
import concourse.mybir as mybir
from concourse.bass_utils import run_bass_kernel_spmd

F32 = mybir.dt.float32
BF16 = mybir.dt.bfloat16
I32 = mybir.dt.int32
AF = mybir.ActivationFunctionType
ALU = mybir.AluOpType
AX = mybir.AxisListType

S = 4096
D = 1024
NT = 32
EPS = 1e-6
LAM_INIT = 0.2
DIL = (1, 4, 16)
N_DMA_SEMS = 8
QUEUES = ("sp", "pool", "act")
TWO_PI = float(2 * np.pi)
C1 = 6.28125
C2 = float(2 * np.pi - 6.28125)


class Prog:
    def __init__(self):
        self.ops = []
        self.last_writer = {}
        self.readers = {}

    def add(self, eng, fn, reads=(), writes=(), dma=False):
        idx = len(self.ops)
        deps = set()
        raw = set()
        for t in reads:
            w = self.last_writer.get(t)
            if w is not None:
                deps.add(w)
                raw.add(w)
        for t in writes:
            w = self.last_writer.get(t)
            if w is not None:
                deps.add(w)
            for r in self.readers.get(t, ()):
                deps.add(r)
        op = dict(eng=eng, fn=fn, dma=dma, deps=deps, raw=raw, signal=False)
        self.ops.append(op)
        for t in reads:
            self.readers.setdefault(t, []).append(idx)
        for t in writes:
            self.last_writer[t] = idx
            self.readers[t] = []
        return idx

    def emit(self, nc, semsets, phase_sem, phase_idx, fin):
        sems, cnt = semsets[phase_idx % 3]
        ops = self.ops
        for op in ops:
            keep = set()
            for d in op["deps"]:
                p = ops[d]
                if p["dma"] or op["dma"] or p["eng"] != op["eng"]:
                    keep.add(d)
                elif op["eng"] != "pe" and d in op["raw"]:
                    keep.add(d)
            op["deps"] = keep
            for d in keep:
                ops[d]["signal"] = True
        dma_k = {}
        prev_slot = {}
        for op in ops:
            if op["dma"]:
                q = op["eng"]
                k = dma_k.get(q, 0)
                dma_k[q] = k + 1
                key = ("dma", q, k % N_DMA_SEMS)
                cnt[key] = cnt.get(key, 0) + 16
                op["prev"] = prev_slot.get(key)
                op["sig"] = (key, cnt[key])
                prev_slot[key] = op["sig"]
            elif op["signal"]:
                key = op["eng"]
                cnt[key] = cnt.get(key, 0) + 1
                op["sig"] = (key, cnt[key])

        def semof(key):
            if isinstance(key, tuple):
                return sems["dma_" + key[1]][key[2]]
            return sems[key]

        streams = {}
        for op in ops:
            streams.setdefault(op["eng"], []).append(op)

        def run_stream(engname, eng):
            if phase_idx > 0:
                eng.wait_ge(phase_sem, 19 * phase_idx)
            known = {}
            for op in streams.get(engname, []):
                waits = {}
                for d in op["deps"]:
                    key, val = ops[d]["sig"]
                    if waits.get(key, 0) < val:
                        waits[key] = val
                if op["dma"] and op["prev"] is not None:
                    key, val = op["prev"]
                    if waits.get(key, 0) < val:
                        waits[key] = val
                for key, val in waits.items():
                    if known.get(key, 0) >= val:
                        continue
                    eng.wait_ge(semof(key), val)
                    known[key] = val
                ins = op["fn"](eng)
                if "sig" in op:
                    ins.then_inc(semof(op["sig"][0]), 16 if op["dma"] else 1)
            if engname == "sp":
                for key, val in cnt.items():
                    if isinstance(key, tuple):
                        eng.wait_ge(semof(key), val)
                eng.dma_start(out=fin["d1"][:], in_=fin["d0"][:]).then_inc(phase_sem, 16)
            elif engname == "pe":
                pass
            elif engname == "act":
                eng.activation(out=fin["act"][:], in_=fin["d0"][0:1, 0:1].to_broadcast([1, 1]) if False else fin["act"][:], func=AF.Copy).then_inc(phase_sem, 1)
            else:
                eng.memset(fin[engname][:], 0.0).then_inc(phase_sem, 1)

        with nc.Block() as block:
            @block.sync
            def _(e):
                run_stream("sp", e)

            @block.tensor
            def _(e):
                run_stream("pe", e)

            @block.scalar
            def _(e):
                run_stream("act", e)

            @block.vector
            def _(e):
                run_stream("dve", e)

            @block.gpsimd
            def _(e):
                run_stream("pool", e)


class K:
    def __init__(self, P):
        self.P = P

    def dma(self, q, out, in_, r=(), w=(), **kw):
        self.P.add(q, lambda e: e.dma_start(out=out, in_=in_, **kw), r, w, dma=True)

    def mm(self, out, lhsT, rhs, start, stop, r=(), w=()):
        self.P.add("pe", lambda e: e.matmul(out, lhsT=lhsT, rhs=rhs, start=start, stop=stop), r, w)

    def tr(self, out, in_, ident, r=(), w=()):
        self.P.add("pe", lambda e: e.transpose(out=out, in_=in_, identity=ident), r, w)

    def act(self, out, in_, func, r=(), w=(), **kw):
        self.P.add("act", lambda e: e.activation(out=out, in_=in_, func=func, **kw), r, w)

    def tt(self, eng, out, in0, in1, op, r=(), w=()):
        self.P.add(eng, lambda e: e.tensor_tensor(out=out, in0=in0, in1=in1, op=op), r, w)

    def ts(self, eng, out, in0, s1, s2, op0, op1=None, r=(), w=()):
        if op1 is None:
            self.P.add(eng, lambda e: e.tensor_scalar(out=out, in0=in0, scalar1=s1, scalar2=None, op0=op0), r, w)
        else:
            self.P.add(eng, lambda e: e.tensor_scalar(out=out, in0=in0, scalar1=s1, scalar2=s2, op0=op0, op1=op1), r, w)

    def stt(self, eng, out, in0, scalar, in1, op0, op1, r=(), w=()):
        self.P.add(eng, lambda e: e.scalar_tensor_tensor(out=out, in0=in0, scalar=scalar, in1=in1, op0=op0, op1=op1), r, w)

    def cp(self, eng, out, in_, r=(), w=()):
        self.P.add(eng, lambda e: e.tensor_copy(out=out, in_=in_), r, w)

    def red(self, eng, out, in_, r=(), w=(), op=None):
        op = op or ALU.add
        self.P.add(eng, lambda e: e.tensor_reduce(out=out, in_=in_, axis=AX.X, op=op), r, w)

    def recip(self, out, in_, r=(), w=()):
        self.P.add("dve", lambda e: e.reciprocal(out=out, in_=in_), r, w)

    def memset(self, eng, out, val, r=(), w=()):
        self.P.add(eng, lambda e: e.memset(out, val), r, w)

    def max8(self, out, in_, r=(), w=()):
        self.P.add("dve", lambda e: e.max(out=out, in_=in_), r, w)

    def mrep(self, out, rep, vals, r=(), w=()):
        self.P.add("dve", lambda e: e.match_replace(out=out, in_to_replace=rep, in_values=vals, imm_value=-1e30), r, w)


def bc(ap, shape):
    return ap.to_broadcast(list(shape))


def build_program(debug=False):
    nc = bass.Bass("TRN2", target_bir_lowering=False)

    def din(name, shape, dt=F32):
        return nc.dram_tensor(name, list(shape), dt, kind="ExternalInput").ap()

    def dscr(name, shape, dt):
        kind = "ExternalOutput" if (debug and name in DEBUG_OUT) else "Internal"
        return nc.dram_tensor(name, list(shape), dt, kind=kind).ap()

    x = din("x", [S, D])
    pos = din("pos", [S], I32)
    cT = din("cT", [128, 8])
    w_ada = din("w_ada", [D, 6 * D])
    b_adaT = din("b_adaT", [128, 48])
    n1gT = din("n1gT", [128, 8])
    n2gT = din("n2gT", [128, 8])
    w_in = din("w_in", [D, 7424])
    b_gate_bc = din("b_gate_bc", [128, 2048])
    gainA = din("gainA", [128, 8, 64])
    gainQB = din("gainQB", [128, 8, 64])
    gainKB = din("gainKB", [128, 8, 64])
    lam_bc = din("lam_bc", [128, 4, 64])
    subln_bc = din("subln_bc", [128, 128])
    invf_bc = din("invf_bc", [128, 32])
    halfpi = din("halfpi", [128, 64])
    mask_std = din("mask_std", [128, 256])
    mask_bnd = din("mask_bnd", [128, 256])
    w_pa = din("w_pa", [256, D])
    w_pb = din("w_pb", [D, D])
    w_out = din("w_out", [D, D])
    wqT = din("wqT", [16, 128, D])
    skT = din("skT", [16, 128, 128])
    UT = din("UT", [16384, 1024])
    Vx = din("Vx", [16384, 1024])
    out = nc.dram_tensor("out", [S, D], F32, kind="ExternalOutput").ap()

    QTA = dscr("QTA", [3, 2, 128, S], BF16)
    KTA = dscr("KTA", [3, 2, 128, S], BF16)
    VA = dscr("VA", [3, S, 256], BF16)
    QTB = dscr("QTB", [8, 128, S], BF16)
    KTB = dscr("KTB", [8, 128, S], BF16)
    VB = dscr("VB", [S, 1024], BF16)
    GT = dscr("GT", [S, 2048], BF16)
    ND = dscr("ND", [S, 12, 65], F32)
    OBT = dscr("OBT", [8, 128, S], BF16)
    X1 = dscr("X1", [S, D], F32)
    H2T = dscr("H2T", [8, 128, S], BF16)
    PS = dscr("PS", [S, 8, 2, 128], F32)
    CF = dscr("CF", [S, 8], F32)
    UTb = dscr("UTb", [16384, 1024], BF16)
    Vb = dscr("Vb", [16384, 1024], BF16)

    with ExitStack() as top:
        def sbt(es, name, shape, dt):
            return es.enter_context(nc.sbuf_tensor(name, list(shape), dt))

        def pst(es, name, shape, dt):
            return es.enter_context(nc.psum_tensor(name, list(shape), dt))

        semsets = []
        for ph in range(3):
            ss = {}
            for e in ("pe", "act", "dve", "pool"):
                ss[e] = top.enter_context(nc.semaphore(f"s{ph}_{e}"))
            for q in QUEUES:
                ss["dma_" + q] = [top.enter_context(nc.semaphore(f"d{ph}_{q}_{i}")) for i in range(N_DMA_SEMS)]
            semsets.append((ss, {}))
        phase_sem = top.enter_context(nc.semaphore("phase"))
        conv_sem = top.enter_context(nc.semaphore("conv"))

        identb = sbt(top, "identb", [128, 128], BF16)
        identf = sbt(top, "identf", [128, 128], F32)
        onesf = sbt(top, "onesf", [128, 128], F32)
        s1T = sbt(top, "s1T", [128, 8], F32)
        sh1T = sbt(top, "sh1T", [128, 8], F32)
        s2T = sbt(top, "s2T", [128, 8], F32)
        sh2T = sbt(top, "sh2T", [128, 8], F32)
        G1row = sbt(top, "G1row", [128, D], F32)
        G2row = sbt(top, "G2row", [128, D], F32)
        neglam = sbt(top, "neglam", [128, 1], F32)
        sgain = sbt(top, "sgain", [128, 128], F32)
        epsb = sbt(top, "epsb", [128, 1], F32)
        fin = dict(
            d0=sbt(top, "fin_d0", [1, 16], F32), d1=sbt(top, "fin_d1", [1, 16], F32),
            act=sbt(top, "fin_act", [128, 1], F32), dve=sbt(top, "fin_dve", [128, 1], F32),
            pool=sbt(top, "fin_pool", [128, 1], F32), idb=identb,
        )
        psn = [0]

        def alloc_psum(es, nf=6, nb=2):
            psn[0] += 1
            bk = [pst(es, f"bank{psn[0]}_{i}", [128, 512], F32) for i in range(nf)]
            bt = [pst(es, f"bankT{psn[0]}_{i}", [128, 1024], BF16) for i in range(nb)]
            return bk, bt

        with ExitStack() as es:
            P = Prog()
            k = K(P)
            banks, bankT = alloc_psum(es)
            cTs = sbt(es, "cTs", [128, 8], F32)
            scs = sbt(es, "scs", [128, 8], F32)
            wada = [sbt(es, f"wada{i}", [128, 6 * D], F32) for i in range(2)]
            badas = sbt(es, "badas", [128, 48], F32)
            modT = sbt(es, "modT", [128, 48], F32)
            n1s = sbt(es, "n1s", [128, 8], F32)
            n2s = sbt(es, "n2s", [128, 8], F32)
            lams = sbt(es, "lams", [128, 4, 64], F32)
            lprod = sbt(es, "lprod", [128, 2, 64], F32)
            lsum = sbt(es, "lsum", [128, 2], F32)
            lexp = sbt(es, "lexp", [128, 2], F32)
            ltmp = sbt(es, "ltmp", [128, 1], F32)
            subs = sbt(es, "subs", [128, 128], F32)
            dg = [sbt(es, f"dg{i}", [128, 128], F32) for i in range(2)]
            NCONV = 16
            rows = 16384 // NCONV
            for i in range(NCONV):
                for (src, dst) in ((UT, UTb), (Vx, Vb)):
                    P.add("pool", (lambda s_, d_, i_: (lambda e: e.dma_start(out=d_[i_ * rows:(i_ + 1) * rows, :], in_=s_[i_ * rows:(i_ + 1) * rows, :]).then_inc(conv_sem, 16)))(src, dst, i))
            k.dma("sp", cTs[:], cT, w=["cTs"])
            k.dma("sp", badas[:], b_adaT, w=["badas"])
            k.dma("sp", n1s[:], n1gT, w=["n1s"])
            k.dma("sp", n2s[:], n2gT, w=["n2s"])
            k.dma("sp", lams[:], lam_bc, w=["lams"])
            k.dma("sp", subs[:], subln_bc, w=["subs"])
            k.memset("dve", identf[:], 1.0, w=["identf"])
            P.add("pool", lambda e: e.affine_select(out=identf[:], in_=identf[:], pattern=[[-1, 128]], compare_op=ALU.is_equal,
                                                   fill=0.0, base=0, channel_multiplier=1), ["identf"], ["identf"])
            k.cp("dve", identb[:], identf[:], r=["identf"], w=["identb"])
            k.memset("dve", onesf[:], 1.0, w=["onesf"])
            k.memset("dve", epsb[:], EPS, w=["epsb"])
            k.memset("dve", fin["d0"][:], 0.0, w=["find0"])
            k.act(scs[:], cTs[:], AF.Silu, r=["cTs"], w=["scs"])
            modps = banks[0]
            for kc in range(8):
                b = kc % 2
                k.dma("sp", wada[b][:], w_ada[kc * 128:(kc + 1) * 128, :], w=[f"wada{b}"])
                for f in range(48):
                    k.mm(modps[:, f:f + 1], wada[b][:, f * 128:(f + 1) * 128], scs[:, kc:kc + 1], kc == 0 and f == 0, kc == 7,
                         r=[f"wada{b}", "scs"], w=["modps"])
            k.tt("dve", modT[:], modps[:, 0:48], badas[:], ALU.add, r=["modps", "badas"], w=["modT"])
            k.stt("dve", s1T[:], modT[:, 8:16], 1.0, n1s[:], ALU.add, ALU.mult, r=["modT", "n1s"], w=["s1T"])
            k.cp("dve", sh1T[:], modT[:, 0:8], r=["modT"], w=["sh1T"])
            k.stt("dve", s2T[:], modT[:, 32:40], 1.0, n2s[:], ALU.add, ALU.mult, r=["modT", "n2s"], w=["s2T"])
            k.cp("dve", sh2T[:], modT[:, 24:32], r=["modT"], w=["sh2T"])
            for (row, base, bk) in ((G1row, 16, 1), (G2row, 40, 3)):
                for c in range(8):
                    b = c % 2
                    k.ts("dve", dg[b][:], identf[:], modT[:, base + c:base + c + 1], None, ALU.mult, r=["modT", "identf"], w=[f"dg{b}"])
                    bank = banks[bk + c // 4]
                    k.mm(bank[:, (c % 4) * 128:(c % 4 + 1) * 128], onesf[:], dg[b][:], True, True, r=[f"dg{b}", "onesf"], w=[f"gr{bk + c // 4}"])
                for hh in range(2):
                    k.cp("dve", row[:, hh * 512:(hh + 1) * 512], banks[bk + hh][:], r=[f"gr{bk + hh}"], w=[f"grow{base}"])
            k.tt("dve", lprod[:, 0, :], lams[:, 0, :], lams[:, 1, :], ALU.mult, r=["lams"], w=["lprod0"])
            k.tt("dve", lprod[:, 1, :], lams[:, 2, :], lams[:, 3, :], ALU.mult, r=["lams"], w=["lprod1"])
            k.red("dve", lsum[:], lprod[:], r=["lprod0", "lprod1"], w=["lsum"])
            k.act(lexp[:], lsum[:], AF.Exp, r=["lsum"], w=["lexp"])
            k.tt("dve", ltmp[:], lexp[:, 1:2], lexp[:, 0:1], ALU.subtract, r=["lexp"], w=["ltmp"])
            k.ts("dve", neglam[:], ltmp[:], -LAM_INIT, None, ALU.add, r=["ltmp"], w=["neglam"])
            k.ts("dve", sgain[:], subs[:], 1.0 - LAM_INIT, None, ALU.mult, r=["subs"], w=["sgain"])
            P.emit(nc, semsets, phase_sem, 0, fin)

        with ExitStack() as es:
            P = Prog()
            k = K(P)
            banks, bankT = alloc_psum(es, 5, 3)
            hT = sbt(es, "hT", [128, 8, 2048], BF16)
            xt = [sbt(es, f"xt{i}", [128, D], F32) for i in range(2)]
            sq = [sbt(es, f"sq{i}", [128, D], F32) for i in range(2)]
            xn = [sbt(es, f"xn{i}", [128, D], BF16) for i in range(2)]
            ssx = [sbt(es, f"ssx{i}", [128, 1], F32) for i in range(2)]
            rsx = [sbt(es, f"rsx{i}", [128, 1], F32) for i in range(2)]
            gA = sbt(es, "gA", [128, 8, 64], F32)
            gQB = sbt(es, "gQB", [128, 8, 64], F32)
            gKB = sbt(es, "gKB", [128, 8, 64], F32)
            bgs = sbt(es, "bgs", [128, 2048], F32)
            invfs = sbt(es, "invfs", [128, 32], F32)
            hps = sbt(es, "hps", [128, 64], F32)
            posi = sbt(es, "posi", [128, 16], I32)
            posf = sbt(es, "posf", [128, 16], F32)
            tabs = [sbt(es, f"tab{i}", [128, 16, 64], F32) for i in range(3)]
            arg = sbt(es, "arg", [128, 16, 64], F32)
            argk = sbt(es, "argk", [128, 16, 64], F32)
            argi = sbt(es, "argi", [128, 16, 64], I32)
            wb = [sbt(es, f"wb{i}", [128, 8, 512], BF16) for i in range(2)]
            sqq = [sbt(es, f"sqq{i}", [128, 8, 64], F32) for i in range(3)]
            ssq = [sbt(es, f"ssq{i}", [128, 8], F32) for i in range(3)]
            rsq = [sbt(es, f"rsq{i}", [128, 8], F32) for i in range(3)]
            qn = [sbt(es, f"qn{i}", [128, 8, 64], F32) for i in range(3)]
            qg = [sbt(es, f"qg{i}", [128, 8, 2, 32], F32) for i in range(3)]
            rt = [sbt(es, f"rt{i}", [128, 4, 8, 32], F32) for i in range(3)]
            qr = [sbt(es, f"qr{i}", [128, 8, 2, 32], BF16) for i in range(3)]
            stg = [sbt(es, f"stg{i}", [128, 4, 128], BF16) for i in range(3)]
            vst = [sbt(es, f"vst{i}", [128, 512], BF16) for i in range(3)]
            gpre = [sbt(es, f"gpre{i}", [128, 512], F32) for i in range(3)]

            k.dma("sp", gA[:], gainA, w=["gA"])
            k.dma("sp", gQB[:], gainQB, w=["gQB"])
            k.dma("sp", gKB[:], gainKB, w=["gKB"])
            k.dma("sp", bgs[:], b_gate_bc, w=["bgs"])
            k.dma("sp", invfs[:], invf_bc, w=["invfs"])
            k.dma("sp", hps[:], halfpi, w=["hps"])

            pos_pat = [
                pos.rearrange("(st u m) -> st m u", st=2, u=16, m=128),
                pos.rearrange("(st blk m r) -> st m blk r", st=2, blk=4, m=128, r=4),
                pos.rearrange("(st m r) -> st m r", st=2, m=128, r=16),
            ]

            def tokcols(pat, u):
                if pat == 0:
                    return slice(u * 128, (u + 1) * 128)
                if pat == 1:
                    blk, r = u // 4, u % 4
                    return slice(blk * 512 + r, blk * 512 + 512, 4)
                return slice(u, 2048, 16)

            def perm0(pat, st, u):
                if pat == 0:
                    return st * 2048 + u * 128
                if pat == 1:
                    blk, r = u // 4, u % 4
                    return r * 1024 + st * 512 + blk * 128
                return u * 256 + st * 128

            jobs = []
            for g in range(3):
                jobs.append((g, "qk", [(256 * g, 256), (768 + 256 * g, 256)], ("A", g)))
                jobs.append((g, "v", [(1536 + 256 * g, 256)], ("A", g)))
            for j in range(2):
                jobs.append((0, "qk", [(2304 + 512 * j, 512)], ("QB", j)))
                jobs.append((0, "qk", [(3328 + 512 * j, 512)], ("KB", j)))
                jobs.append((0, "v", [(4352 + 512 * j, 512)], ("B", j)))
            for j in range(4):
                jobs.append((0, "gate", [(5376 + 512 * j, 512)], ("G", j)))

            unit = 0
            for st in range(2):
                for u in range(16):
                    b = u % 2
                    t0 = st * 2048 + u * 128
                    k.dma("sp", xt[b][:], x[t0:t0 + 128, :], w=[f"xt{b}"])
                    k.act(sq[b][:], xt[b][:], AF.Square, r=[f"xt{b}"], w=[f"sq{b}"])
                    k.red("dve", ssx[b][:], sq[b][:], r=[f"sq{b}"], w=[f"ssx{b}"])
                    k.act(rsx[b][:], ssx[b][:], AF.Sqrt, r=[f"ssx{b}", "epsb"], w=[f"rsx{b}"], scale=1.0 / D, bias=epsb[:])
                    k.recip(rsx[b][:], rsx[b][:], r=[f"rsx{b}"], w=[f"rsx{b}"])
                    k.ts("dve", xn[b][:], xt[b][:], rsx[b][:, 0:1], None, ALU.mult, r=[f"xt{b}", f"rsx{b}"], w=[f"xn{b}"])
                    for c in range(8):
                        k.tr(bankT[b][:, c * 128:(c + 1) * 128], xn[b][:, c * 128:(c + 1) * 128], identb[:], r=[f"xn{b}"], w=[f"bT{b}"])
                    for c in range(8):
                        k.act(hT[:, c, u * 128:(u + 1) * 128], bankT[b][:, c * 128:(c + 1) * 128], AF.Identity,
                              r=[f"bT{b}"], w=[f"hT{u}"], scale=s1T[:, c:c + 1], bias=sh1T[:, c:c + 1])
                for pat in range(3):
                    if pat == 0:
                        k.dma("sp", posi[:], pos_pat[0][st], w=["posi"], allow_slow_non_contiguous=True)
                    elif pat == 1:
                        k.dma("sp", posi[:].rearrange("p (a b) -> p a b", a=4), pos_pat[1][st], w=["posi"])
                    else:
                        k.dma("sp", posi[:], pos_pat[2][st], w=["posi"])
                    k.cp("dve", posf[:], posi[:], r=["posi"], w=["posf"])
                    k.tt("dve", arg[:, :, 0:32], bc(posf[:].unsqueeze(2), [128, 16, 32]), bc(invfs[:].unsqueeze(1), [128, 16, 32]), ALU.mult,
                         r=["posf", "invfs"], w=["arg"])
                    k.cp("dve", arg[:, :, 32:64], arg[:, :, 0:32], r=["arg"], w=["arg"])
                    k.tt("dve", arg[:], arg[:], bc(hps[:].unsqueeze(1), [128, 16, 64]), ALU.add, r=["arg", "hps"], w=["arg"])
                    k.ts("dve", argk[:], arg[:], 1.0 / TWO_PI, None, ALU.mult, r=["arg"], w=["argk"])
                    k.cp("dve", argi[:], argk[:], r=["argk"], w=["argi"])
                    k.cp("dve", argk[:], argi[:], r=["argi"], w=["argk"])
                    k.stt("dve", arg[:], argk[:], -C1, arg[:], ALU.mult, ALU.add, r=["argk", "arg"], w=["arg"])
                    k.stt("dve", arg[:], argk[:], -C2, arg[:], ALU.mult, ALU.add, r=["argk", "arg"], w=["arg"])
                    k.ts("dve", argk[:], arg[:], float(np.pi), -TWO_PI, ALU.is_gt, ALU.mult, r=["arg"], w=["argk"])
                    k.tt("dve", arg[:], arg[:], argk[:], ALU.add, r=["arg", "argk"], w=["arg"])
                    k.ts("dve", argk[:], arg[:], float(-np.pi), TWO_PI, ALU.is_lt, ALU.mult, r=["arg"], w=["argk"])
                    k.tt("dve", arg[:], arg[:], argk[:], ALU.add, r=["arg", "argk"], w=["arg"])
                    k.act(tabs[pat][:], arg[:], AF.Sin, r=["arg"], w=[f"tab{pat}"])
                hall = [f"hT{u}" for u in range(16)]
                pending = []
                for ji, (pat, kind, cols, extra) in enumerate(jobs):
                    wbi = ji % 2
                    off = 0
                    for (c0, ncol) in cols:
                        k.dma("pool", wb[wbi][:, :, off:off + ncol], w_in[:, c0:c0 + ncol].rearrange("(c p) n -> p c n", p=128), w=[f"wb{wbi}"])
                        off += ncol
                    ncols = off
                    for u in range(16):
                        ub = unit % 3
                        unit += 1
                        bankP = banks[ub]
                        tc = tokcols(pat, u)
                        rtok = hall if pat else [f"hT{u}"]
                        for c in range(8):
                            k.mm(bankP[:, 0:ncols], hT[:, c, tc], wb[wbi][:, c, 0:ncols], c == 0, c == 7, r=rtok + [f"wb{wbi}"], w=[f"bP{ub}"])
                        p0 = perm0(pat, st, u)

                        def post1(ub=ub, bankP=bankP, pat=pat, kind=kind, extra=extra, u=u, p0=p0, ncols=ncols):
                            if kind == "qk":
                                gt = {"A": gA, "QB": gQB, "KB": gKB}[extra[0]]
                                gtn = {"A": "gA", "QB": "gQB", "KB": "gKB"}[extra[0]]
                                pv = bankP[:].rearrange("p (h c) -> p h c", h=8)
                                k.act(sqq[ub][:], pv, AF.Square, r=[f"bP{ub}"], w=[f"sqq{ub}"])
                                k.red("dve", ssq[ub][:], sqq[ub][:], r=[f"sqq{ub}"], w=[f"ssq{ub}"])
                                k.act(rsq[ub][:], ssq[ub][:], AF.Sqrt, r=[f"ssq{ub}", "epsb"], w=[f"rsq{ub}"], scale=1.0 / 64, bias=epsb[:])
                                k.recip(rsq[ub][:], rsq[ub][:], r=[f"rsq{ub}"], w=[f"rsq{ub}"])
                                k.tt("dve", qn[ub][:], pv, bc(rsq[ub][:].unsqueeze(2), [128, 8, 64]), ALU.mult, r=[f"bP{ub}", f"rsq{ub}"], w=[f"qn{ub}"])
                                k.tt("pool", qg[ub][:].rearrange("p h a f -> p h (a f)"), qn[ub][:], gt[:], ALU.mult, r=[f"qn{ub}", gtn], w=[f"qg{ub}"])
                                sinb = bc(tabs[pat][:, u, 0:32].unsqueeze(1), [128, 8, 32])
                                cosb = bc(tabs[pat][:, u, 32:64].unsqueeze(1), [128, 8, 32])
                                x1 = qg[ub][:, :, 0, :]
                                x2 = qg[ub][:, :, 1, :]
                                tn = f"tab{pat}"
                                k.tt("dve", rt[ub][:, 0], x1, cosb, ALU.mult, r=[f"qg{ub}", tn], w=[f"rt0{ub}"])
                                k.tt("dve", rt[ub][:, 1], x2, sinb, ALU.mult, r=[f"qg{ub}", tn], w=[f"rt1{ub}"])
                                k.tt("dve", qr[ub][:, :, 0, :], rt[ub][:, 0], rt[ub][:, 1], ALU.subtract, r=[f"rt0{ub}", f"rt1{ub}"], w=[f"qr{ub}a"])
                                k.tt("pool", rt[ub][:, 2], x2, cosb, ALU.mult, r=[f"qg{ub}", tn], w=[f"rt2{ub}"])
                                k.tt("pool", rt[ub][:, 3], x1, sinb, ALU.mult, r=[f"qg{ub}", tn], w=[f"rt3{ub}"])
                                k.tt("pool", qr[ub][:, :, 1, :], rt[ub][:, 2], rt[ub][:, 3], ALU.add, r=[f"rt2{ub}", f"rt3{ub}"], w=[f"qr{ub}b"])
                                return
                            if kind == "v":
                                k.act(vst[ub][:, 0:ncols], bankP[:, 0:ncols], AF.Copy, r=[f"bP{ub}"], w=[f"vst{ub}"])
                                if extra[0] == "A":
                                    k.dma("sp", VA[extra[1], p0:p0 + 128, :], vst[ub][:, 0:256], r=[f"vst{ub}"], w=["VA"])
                                else:
                                    j = extra[1]
                                    k.dma("sp", VB[p0:p0 + 128, 512 * j:512 * j + 512], vst[ub][:], r=[f"vst{ub}"], w=["VB"])
                            else:
                                j = extra[1]
                                k.tt("dve", gpre[ub][:], bankP[:], bgs[:, 512 * j:512 * j + 512], ALU.add, r=[f"bP{ub}", "bgs"], w=[f"gpre{ub}"])
                                k.act(vst[ub][:], gpre[ub][:], AF.Sigmoid, r=[f"gpre{ub}"], w=[f"vst{ub}"])
                                k.dma("sp", GT[p0:p0 + 128, 512 * j:512 * j + 512], vst[ub][:], r=[f"vst{ub}"], w=["GT"])

                        def post2(ub=ub, bankP=bankP, pat=pat, kind=kind, extra=extra, u=u, p0=p0, ncols=ncols):
                            if kind == "qk":
                                qflat = qr[ub][:].rearrange("p h a f -> p (h a f)")
                                for q4 in range(4):
                                    k.tr(bankT[ub][:, q4 * 128:(q4 + 1) * 128], qflat[:, q4 * 128:(q4 + 1) * 128], identb[:],
                                         r=[f"qr{ub}a", f"qr{ub}b"], w=[f"bT{ub}"])
                                k.act(stg[ub][:].rearrange("p a t -> p (a t)"), bankT[ub][:, 0:512], AF.Copy, r=[f"bT{ub}"], w=[f"stg{ub}"])
                                if extra[0] == "A":
                                    g = extra[1]
                                    k.dma("sp", QTA[g, :, :, p0:p0 + 128].rearrange("a c t -> c a t"), stg[ub][:, 0:2, :], r=[f"stg{ub}"], w=["QTA"])
                                    k.dma("sp", KTA[g, :, :, p0:p0 + 128].rearrange("a c t -> c a t"), stg[ub][:, 2:4, :], r=[f"stg{ub}"], w=["KTA"])
                                else:
                                    dst = QTB if extra[0] == "QB" else KTB
                                    j = extra[1]
                                    k.dma("sp", dst[4 * j:4 * j + 4, :, p0:p0 + 128].rearrange("a c t -> c a t"), stg[ub][:], r=[f"stg{ub}"], w=[extra[0]])
                        pending.append((post1, post2))
                        if len(pending) >= 2:
                            pending[-2][0]()
                        if len(pending) >= 3:
                            pending.pop(0)[1]()
                if pending:
                    pending[-1][0]()
                while pending:
                    pending.pop(0)[1]()
            P.emit(nc, semsets, phase_sem, 1, fin)

        with ExitStack() as es:
            P = Prog()
            k = K(P)
            banks, bankT = alloc_psum(es)
            KTs = [sbt(es, f"KTs{i}", [128, S + 128], BF16) for i in range(2)]
            QTs = [sbt(es, f"QTs{i}", [128, S + 128], BF16) for i in range(2)]
            V1 = [sbt(es, f"V1_{i}", [128, 33, 65], BF16) for i in range(2)]
            mstd = sbt(es, "mstd", [128, 256], F32)
            mbnd = sbt(es, "mbnd", [128, 256], F32)
            Eb = [sbt(es, f"Eb{i}", [128, 256], BF16) for i in range(2)]
            Pm = [sbt(es, f"Pm{i}", [128, 256], BF16) for i in range(2)]
            osb = [sbt(es, f"osb{i}", [128, 65], F32) for i in range(4)]
            k.dma("sp", mstd[:], mask_std, w=["mstd"])
            k.dma("sp", mbnd[:], mask_bnd, w=["mbnd"])
            for i in range(2):
                k.memset("pool", KTs[i][:, 0:64], 0.0, w=[f"KTs{i}"])
                k.memset("pool", KTs[i][:, S + 64:S + 128], 0.0, w=[f"KTs{i}"])
                k.memset("pool", QTs[i][:, 0:64], 0.0, w=[f"QTs{i}"])
                k.memset("pool", QTs[i][:, S + 64:S + 128], 0.0, w=[f"QTs{i}"])
                k.memset("pool", V1[i][:], 0.0, w=[f"V1_{i}"])
                k.memset("pool", V1[i][:, :, 64:65], 1.0, w=[f"V1_{i}"])
            bankSs = [banks[0], banks[2]]
            bankOs = [banks[1], banks[3], banks[4], banks[5]]
            hcount = 0
            ccount = 0
            for g in range(3):
                dil = DIL[g]
                L = S // dil
                for pr in range(2):
                    pb = (g * 2 + pr) % 2
                    k.dma("sp", KTs[pb][:, 64:64 + S], KTA[g, pr], r=["KTA"], w=[f"KTs{pb}"])
                    k.dma("sp", QTs[pb][:, 64:64 + S], QTA[g, pr], r=["QTA"], w=[f"QTs{pb}"])
                    for hh in range(2):
                        hs = pr * 2 + hh
                        bp = 64 * hh
                        vb = hcount % 2
                        hcount += 1
                        vsrc = VA[g, :, hs * 64:(hs + 1) * 64]
                        k.dma("sp", V1[vb][:, 1:32, 0:64], vsrc[64:S - 64, :].rearrange("(j p) c -> p j c", p=128), r=["VA"], w=[f"V1_{vb}"])
                        k.dma("sp", V1[vb][64:128, 0, 0:64], vsrc[0:64, :], r=["VA"], w=[f"V1_{vb}"])
                        k.dma("sp", V1[vb][0:64, 32, 0:64], vsrc[S - 64:S, :], r=["VA"], w=[f"V1_{vb}"])
                        for jc in range(33):
                            sb_ = ccount % 2
                            ccount += 1
                            bnd = (128 * jc) % L == 0
                            qlo = 128 if jc == 0 else 0
                            qhi = 128 if jc == 32 else 256
                            sv = bankSs[sb_][:, 0:256]
                            k.mm(sv[:, qlo:qhi], KTs[pb][bp:bp + 64, 128 * jc:128 * jc + 128],
                                 QTs[pb][bp:bp + 64, 128 * jc - 64 + qlo:128 * jc - 64 + qhi],
                                 True, True, r=[f"KTs{pb}", f"QTs{pb}"], w=[f"bS{sb_}"])
                            k.act(Eb[sb_][:, qlo:qhi], sv[:, qlo:qhi], AF.Exp, r=[f"bS{sb_}"], w=[f"Eb{sb_}"], scale=0.125)
                            mk = mbnd if bnd else mstd
                            k.tt("dve", Pm[sb_][:, qlo:qhi], Eb[sb_][:, qlo:qhi], mk[:, qlo:qhi], ALU.mult, r=[f"Eb{sb_}", "mstd", "mbnd"], w=[f"Pm{sb_}"])
                            for half in range(2):
                                if half * 128 < qlo or half * 128 >= qhi:
                                    continue
                                blk = jc - 1 + half
                                a = blk % 4
                                k.mm(bankOs[a][:, 0:65], Pm[sb_][:, half * 128:half * 128 + 128], V1[vb][:, jc, :],
                                     half == 1, half == 0, r=[f"Pm{sb_}", f"V1_{vb}"], w=[f"bO{a}"])
                            if jc >= 1:
                                blk = jc - 1
                                a = blk % 4
                                k.cp("dve", osb[a][:], bankOs[a][:, 0:65], r=[f"bO{a}"], w=[f"osb{a}"])
                                r_ = (128 * blk) // L
                                j0 = (128 * blk) % L
                                s0 = j0 * dil + r_
                                dst = ND[s0:s0 + 127 * dil + 1:dil, g * 4 + hs, :]
                                k.dma("sp", dst, osb[a][:], r=[f"osb{a}"], w=["ND"])
            P.emit(nc, semsets, phase_sem, 2, fin)

        with ExitStack() as es:
            P = Prog()
            k = K(P)
            banks, bankT = alloc_psum(es, 7, 1)
            KTs = [sbt(es, f"KTd{i}", [128, S], BF16) for i in range(2)]
            QTs = [sbt(es, f"QTd{i}", [128, S], BF16) for i in range(2)]
            V1 = [sbt(es, f"V1d{i}", [128, 32, 129], BF16) for i in range(2)]
            E = [[sbt(es, f"E{m}_{i}", [128, 512], BF16) for i in range(2)] for m in range(2)]
            osb = sbt(es, "osbd", [128, 8, 129], F32)
            rr = sbt(es, "rr", [128, 8], F32)
            o1 = sbt(es, "o1", [128, 4, 128], F32)
            o2 = sbt(es, "o2", [128, 4, 128], F32)
            ssd = sbt(es, "ssd", [128, 4], F32)
            obb = sbt(es, "obb", [128, 4, 128], BF16)
            obT = [sbt(es, f"obT{i}", [128, 512], BF16) for i in range(2)]
            for i in range(2):
                k.memset("pool", V1[i][:, :, 128:129], 1.0, w=[f"V1d{i}"])
            accb = [banks[4], banks[5], banks[6]]
            steps = [(h, qb, kc) for h in range(8) for qb in range(8) for kc in range(32)]

            def qk_exp(n):
                h, qb, kc = steps[n]
                hb = h % 2
                eb = n % 2
                if qb == 0 and kc == 0:
                    k.dma("sp", KTs[hb][:], KTB[h], r=["KB"], w=[f"KTd{hb}"])
                    k.dma("sp", QTs[hb][:], QTB[h], r=["QB"], w=[f"QTd{hb}"])
                    k.dma("sp", V1[hb][:, :, 0:128], VB[:, h * 128:(h + 1) * 128].rearrange("(j p) c -> p j c", p=128), r=["VB"], w=[f"V1d{hb}"])
                for m in range(2):
                    bi = m * 2 + eb
                    tok = f"bS{bi}"
                    k.mm(banks[bi][:], KTs[hb][64 * m:64 * m + 64, kc * 128:(kc + 1) * 128], QTs[hb][64 * m:64 * m + 64, qb * 512:(qb + 1) * 512],
                         True, True, r=[f"KTd{hb}", f"QTd{hb}"], w=[tok])
                    k.act(E[m][eb][:], banks[bi][:], AF.Exp, r=[tok], w=[f"E{m}_{eb}"], scale=0.125)

            deferred = []
            qk_exp(0)
            for n, (h, qb, kc) in enumerate(steps):
                hb = h % 2
                eb = n % 2
                if n + 1 < len(steps):
                    qk_exp(n + 1)
                for m in range(2):
                    for sub in range(4):
                        a = m * 4 + sub
                        k.mm(accb[a // 3][:, (a % 3) * 129:(a % 3) * 129 + 129], E[m][eb][:, sub * 128:(sub + 1) * 128], V1[hb][:, kc, :],
                             kc == 0 and a % 3 == 0, kc == 31, r=[f"E{m}_{eb}", f"V1d{hb}"], w=[f"accb{a // 3}"])
                if kc == 31:
                    def ep1(h=h, qb=qb):
                        for a in range(8):
                            src = accb[a // 3][:, (a % 3) * 129:(a % 3) * 129 + 129]
                            k.cp("dve", osb[:, a, :], src, r=[f"accb{a // 3}"], w=[f"osbd{a}"])
                        allo = [f"osbd{a_}" for a_ in range(8)]
                        k.recip(rr[:], osb[:, :, 128], r=allo, w=["rr"])
                        k.ts("dve", rr[:, 4:8], rr[:, 4:8], neglam[:, 0:1], None, ALU.mult, r=["rr"], w=["rr"])
                        k.tt("dve", o1[:], osb[:, 0:4, 0:128], bc(rr[:, 0:4].unsqueeze(2), [128, 4, 128]), ALU.mult, r=allo + ["rr"], w=["o1"])
                        k.tt("pool", o2[:], osb[:, 4:8, 0:128], bc(rr[:, 4:8].unsqueeze(2), [128, 4, 128]), ALU.mult, r=allo + ["rr"], w=["o2"])
                        k.tt("dve", o1[:], o1[:], o2[:], ALU.add, r=["o1", "o2"], w=["o1"])
                        k.tt("pool", o2[:], o1[:], o1[:], ALU.mult, r=["o1"], w=["o2"])
                        k.red("dve", ssd[:], o2[:], r=["o2"], w=["ssd"])

                    def ep2(h=h, qb=qb):
                        k.act(ssd[:], ssd[:], AF.Sqrt, r=["ssd", "epsb"], w=["ssd"], scale=1.0 / 128, bias=epsb[:])
                        k.recip(ssd[:], ssd[:], r=["ssd"], w=["ssd"])
                        k.tt("dve", o1[:], o1[:], bc(ssd[:].unsqueeze(2), [128, 4, 128]), ALU.mult, r=["o1", "ssd"], w=["o1"])
                        k.tt("pool", obb[:], o1[:], bc(sgain[:].unsqueeze(1), [128, 4, 128]), ALU.mult, r=["o1"], w=["obb"])

                    def ep3(h=h, qb=qb):
                        ob_i = (h * 8 + qb) % 2
                        for sub in range(4):
                            k.tr(bankT[0][:, sub * 128:(sub + 1) * 128], obb[:, sub, :], identb[:], r=["obb"], w=["bTd"])
                        k.cp("dve", obT[ob_i][:], bankT[0][:, 0:512], r=["bTd"], w=[f"obT{ob_i}"])
                        k.dma("sp", OBT[h, :, qb * 512:(qb + 1) * 512], obT[ob_i][:], r=[f"obT{ob_i}"], w=["OBT"])
                    ep1()
                    deferred.append((n + 3, ep2))
                    deferred.append((n + 6, ep3))
                while deferred and (deferred[0][0] <= n or n == len(steps) - 1):
                    deferred.pop(0)[1]()
            P.emit(nc, semsets, phase_sem, 3, fin)

        with ExitStack() as es:
            P = Prog()
            k = K(P)
            banks, bankT = alloc_psum(es)
            Wc = sbt(es, "Wc", [128, 8, 2048], BF16)
            wpa = sbt(es, "wpa", [128, 2, D], BF16)
            wpb = sbt(es, "wpb", [128, 8, D], BF16)
            wo = sbt(es, "wo", [128, 8, D], BF16)
            wq = [sbt(es, f"wq{i}", [128, D], F32) for i in range(2)]
            sk = [sbt(es, f"sk{i}", [128, 128], F32) for i in range(2)]
            nd = [sbt(es, f"nd{i}", [128, 12, 65], F32) for i in range(2)]
            obt = [sbt(es, f"obt{i}", [128, 8, 128], BF16) for i in range(2)]
            gts = [sbt(es, f"gts{i}", [128, 2048], BF16) for i in range(2)]
            xts = [sbt(es, f"xts{i}", [128, D], F32) for i in range(2)]
            nsum = sbt(es, "nsum", [128, 4, 65], F32)
            rden = sbt(es, "rden", [128, 4], F32)
            oab = sbt(es, "oab", [128, 4, 64], BF16)
            oaT = sbt(es, "oaT", [128, 2, 128], BF16)
            bra = sbt(es, "bra", [128, D], F32)
            t1 = sbt(es, "t1", [128, D], F32)
            t2 = sbt(es, "t2", [128, D], F32)
            mixb = sbt(es, "mixb", [128, D], BF16)
            mixT = sbt(es, "mixT", [128, 8, 128], BF16)
            x1s = sbt(es, "x1s", [128, D], F32)
            sq2 = sbt(es, "sq2", [128, D], F32)
            ss2 = sbt(es, "ss2", [128, 1], F32)
            xn2 = sbt(es, "xn2", [128, D], BF16)
            h2T = sbt(es, "h2T", [128, 8, 128], BF16)
            Ssbs = [sbt(es, f"Ssb{i}", [128, 8, 2, 128], F32) for i in range(2)]
            mr = sbt(es, "mr", [128, 128], F32)
            t16 = sbt(es, "t16", [128, 8, 2, 16], F32)
            cand = sbt(es, "cand", [128, 8, 16, 16], F32)
            mr2 = sbt(es, "mr2", [128, 256], F32)
            c16 = sbt(es, "c16", [128, 8, 16], F32)
            dd = sbt(es, "dd", [128, 8, 16], F32)
            zz = sbt(es, "zz", [128, 8], F32)
            tau = sbt(es, "tau", [128, 8], F32)
            cf = sbt(es, "cf", [128, 8], F32)
            k.dma("pool", wpa[:], w_pa.rearrange("(c p) n -> p c n", p=128), w=["wpa"])
            k.dma("pool", wpb[:], w_pb.rearrange("(c p) n -> p c n", p=128), w=["wpb"])
            k.dma("pool", wo[:], w_out.rearrange("(c p) n -> p c n", p=128), w=["wo"])
            for kk in range(16):
                b = kk % 2
                k.dma("sp", wq[b][:], wqT[kk], w=[f"wq{b}"])
                k.dma("sp", sk[b][:], skT[kk], w=[f"sk{b}"])
                for dc in range(8):
                    k.mm(banks[dc // 4][:, (dc % 4) * 128:(dc % 4 + 1) * 128], wq[b][:, dc * 128:(dc + 1) * 128], sk[b][:], True, True,
                         r=[f"wq{b}", f"sk{b}"], w=[f"wcp{dc // 4}"])
                for hh in range(2):
                    k.act(Wc[:, 4 * hh:4 * hh + 4, kk * 128:(kk + 1) * 128], banks[hh][:].rearrange("p (a n) -> p a n", a=4), AF.Copy,
                          r=[f"wcp{hh}"], w=["Wc"])
            def stageA(t):
                b = t % 2
                t0 = t * 128
                Ssb = Ssbs[b]
                sS = f"Ssb{b}"
                k.dma("sp", nd[b][:], ND[t0:t0 + 128], r=["ND"], w=[f"nd{b}"])
                k.dma("sp", obt[b][:], OBT[:, :, t0:t0 + 128].rearrange("h c t -> c h t"), r=["OBT"], w=[f"obt{b}"])
                k.dma("sp", gts[b][:], GT[t0:t0 + 128, :], r=["GT"], w=[f"gts{b}"])
                k.dma("sp", xts[b][:], x[t0:t0 + 128, :], w=[f"xts{b}"])
                k.tt("pool", nsum[:], nd[b][:, 0:4, :], nd[b][:, 4:8, :], ALU.add, r=[f"nd{b}"], w=["nsum"])
                k.tt("pool", nsum[:], nsum[:], nd[b][:, 8:12, :], ALU.add, r=[f"nd{b}", "nsum"], w=["nsum"])
                k.recip(rden[:], nsum[:, :, 64], r=["nsum"], w=["rden"])
                k.tt("dve", oab[:], nsum[:, :, 0:64], bc(rden[:].unsqueeze(2), [128, 4, 64]), ALU.mult, r=["nsum", "rden"], w=["oab"])
                oaf = oab[:].rearrange("p a c -> p (a c)")
                for c in range(2):
                    k.tr(bankT[0][:, c * 128:(c + 1) * 128], oaf[:, c * 128:(c + 1) * 128], identb[:], r=["oab"], w=["bT0"])
                k.act(oaT[:].rearrange("p a t -> p (a t)"), bankT[0][:, 0:256], AF.Copy, r=["bT0"], w=["oaT"])
                for hf in range(2):
                    for c in range(2):
                        k.mm(banks[hf][:], oaT[:, c, :], wpa[:, c, hf * 512:(hf + 1) * 512], c == 0, c == 1, r=["oaT", "wpa"], w=[f"bk{hf}"])
                for hf in range(2):
                    for c in range(8):
                        k.mm(banks[2 + hf][:], obt[b][:, c, :], wpb[:, c, hf * 512:(hf + 1) * 512], c == 0, c == 7, r=[f"obt{b}", "wpb"], w=[f"bk{2 + hf}"])
                for hf in range(2):
                    sl = slice(hf * 512, (hf + 1) * 512)
                    k.act(bra[:, sl], banks[hf][:], AF.Copy, r=[f"bk{hf}"], w=["bra"])
                    k.tt("dve", t2[:, sl], banks[2 + hf][:], gts[b][:, 1024 + hf * 512:1024 + (hf + 1) * 512], ALU.mult, r=[f"bk{2 + hf}", f"gts{b}"], w=["t2"])
                k.tt("pool", t1[:], bra[:], gts[b][:, 0:1024], ALU.mult, r=["bra", f"gts{b}"], w=["t1"])
                k.tt("pool", mixb[:], t1[:], t2[:], ALU.add, r=["t1", "t2"], w=["mixb"])
                for c in range(8):
                    k.tr(bankT[1][:, c * 128:(c + 1) * 128], mixb[:, c * 128:(c + 1) * 128], identb[:], r=["mixb"], w=["bT1"])
                k.act(mixT[:].rearrange("p a t -> p (a t)"), bankT[1][:], AF.Copy, r=["bT1"], w=["mixT"])
                for hf in range(2):
                    for c in range(8):
                        k.mm(banks[4 + hf][:], mixT[:, c, :], wo[:, c, hf * 512:(hf + 1) * 512], c == 0, c == 7, r=["mixT", "wo"], w=[f"bk{4 + hf}"])
                for hf in range(2):
                    sl = slice(hf * 512, (hf + 1) * 512)
                    k.tt("dve", t2[:, sl], banks[4 + hf][:], G1row[:, sl], ALU.mult, r=[f"bk{4 + hf}"], w=["t2"])
                k.tt("pool", x1s[:], t2[:], xts[b][:], ALU.add, r=["t2", f"xts{b}"], w=["x1s"])
                k.dma("sp", X1[t0:t0 + 128, :], x1s[:], r=["x1s"], w=["X1"])
                k.act(sq2[:], x1s[:], AF.Square, r=["x1s"], w=["sq2"])
                k.red("dve", ss2[:], sq2[:], r=["sq2"], w=["ss2"])
                k.act(ss2[:], ss2[:], AF.Sqrt, r=["ss2", "epsb"], w=["ss2"], scale=1.0 / D, bias=epsb[:])
                k.recip(ss2[:], ss2[:], r=["ss2"], w=["ss2"])
                k.ts("dve", xn2[:], x1s[:], ss2[:, 0:1], None, ALU.mult, r=["x1s", "ss2"], w=["xn2"])
                for c in range(8):
                    k.tr(bankT[0][:, c * 128:(c + 1) * 128], xn2[:, c * 128:(c + 1) * 128], identb[:], r=["xn2"], w=["bT0"])
                for c in range(8):
                    k.act(h2T[:, c, :], bankT[0][:, c * 128:(c + 1) * 128], AF.Identity, r=["bT0"], w=["h2T"], scale=s2T[:, c:c + 1], bias=sh2T[:, c:c + 1])
                k.dma("sp", H2T[:, :, t0:t0 + 128].rearrange("c p t -> p c t"), h2T[:], r=["h2T"], w=["H2T"])
                for nb in range(4):
                    for c in range(8):
                        k.mm(banks[nb][:], h2T[:, c, :], Wc[:, c, nb * 512:(nb + 1) * 512], c == 0, c == 7, r=["h2T", "Wc"], w=[f"bk{nb}"])
                Sf = Ssb[:].rearrange("p h a n -> p (h a n)")
                for nb in range(4):
                    k.act(Sf[:, nb * 512:(nb + 1) * 512], banks[nb][:], AF.Copy, r=[f"bk{nb}"], w=[sS])
            def stageB(t):
                b = t % 2
                t0 = t * 128
                Ssb = Ssbs[b]
                sS = f"Ssb{b}"
                for h in range(8):
                    for a in range(2):
                        k.max8(t16[:, h, a, 0:8], Ssb[:, h, a, :], r=[sS], w=["t16"])
                        k.mrep(mr[:], t16[:, h, a, 0:8], Ssb[:, h, a, :], r=[sS, "t16"], w=["mr"])
                        k.max8(t16[:, h, a, 8:16], mr[:], r=["mr"], w=["t16"])
                k.tt("dve", cand[:], bc(t16[:, :, 0, :].unsqueeze(3), [128, 8, 16, 16]), bc(t16[:, :, 1, :].unsqueeze(2), [128, 8, 16, 16]), ALU.add,
                     r=["t16"], w=["cand"])
                for h in range(8):
                    cv = cand[:, h].rearrange("p a b -> p (a b)")
                    k.max8(c16[:, h, 0:8], cv, r=["cand"], w=["c16"])
                    k.mrep(mr2[:], c16[:, h, 0:8], cv, r=["cand", "c16"], w=["mr2"])
                    k.max8(c16[:, h, 8:16], mr2[:], r=["mr2"], w=["c16"])
                k.ts("dve", tau[:], c16[:, :, 15], -1e-3, None, ALU.add, r=["c16"], w=["tau"])
                k.tt("dve", dd[:], c16[:], bc(c16[:, :, 0:1], [128, 8, 16]), ALU.subtract, r=["c16"], w=["dd"])
                k.act(dd[:], dd[:], AF.Exp, r=["dd"], w=["dd"])
                k.red("dve", zz[:], dd[:], r=["dd"], w=["zz"])
                k.recip(zz[:], zz[:], r=["zz"], w=["zz"])
                k.tt("dve", cf[:], tau[:], c16[:, :, 0], ALU.subtract, r=["tau", "c16"], w=["cf"])
                k.act(cf[:], cf[:], AF.Exp, r=["cf"], w=["cf"])
                k.tt("dve", cf[:], cf[:], zz[:], ALU.mult, r=["cf", "zz"], w=["cf"])
                k.tt("dve", Ssb[:, :, 0, :], Ssb[:, :, 0, :], bc(tau[:].unsqueeze(2), [128, 8, 128]), ALU.subtract, r=[sS, "tau"], w=[sS])
                k.dma("sp", PS[t0:t0 + 128], Ssb[:], r=[sS], w=["PS"])
                k.dma("sp", CF[t0:t0 + 128, :], cf[:], r=["cf"], w=["CF"])
            stageA(0)
            for t in range(NT):
                if t + 1 < NT:
                    stageA(t + 1)
                stageB(t)
            P.emit(nc, semsets, phase_sem, 4, fin)

        with ExitStack() as es:
            P = Prog()
            k = K(P)
            banks, bankT = alloc_psum(es, 8, 0)
            NB = 4
            h2 = [sbt(es, f"h2_{i}", [128, 8, 256], BF16) for i in range(2)]
            pss = sbt(es, "pss", [128, 2, 8, 2, 128], F32)
            cfs = [sbt(es, f"cfs{i}", [128, 2, 8], F32) for i in range(2)]
            x1t = sbt(es, "x1t", [128, 2, D], F32)
            Dm = [sbt(es, f"Dm{i}", [128, 2, 8, 128], BF16) for i in range(2)]
            UTi = [sbt(es, f"UTi{i}", [128, 8, 128], BF16) for i in range(NB)]
            Vi = [sbt(es, f"Vi{i}", [128, D], BF16) for i in range(NB)]
            HgA = sbt(es, "HgA", [128, 128, 256], BF16)
            zt = [sbt(es, f"zt{i}", [128, 6, 128], F32) for i in range(3)]
            E32 = [sbt(es, f"E32_{i}", [128, 2, 8, 128], F32) for i in range(2)]
            Gt = [sbt(es, f"Gt{i}", [128, 2, 8, 128], BF16) for i in range(3)]
            WT = [sbt(es, f"WT{i}", [128, 256], BF16) for i in range(2)]
            yo = [sbt(es, f"yo{i}", [128, D], F32) for i in range(2)]
            P.add("sp", lambda e: e.wait_ge(conv_sem, 16 * 32))
            UTv = UTb.rearrange("(i p) (c e) -> i p c e", p=128, c=8)
            ustep = 0
            vstep = 0
            for tt_ in range(16):
                tb = tt_ % 2
                t0 = tt_ * 256
                k.dma("sp", h2[tb][:], H2T[:, :, t0:t0 + 256].rearrange("c p t -> p c t"), w=[f"h2_{tb}"])
                k.dma("sp", cfs[tb][:], CF[t0:t0 + 256, :].rearrange("(s p) h -> p s h", p=128), w=[f"cfs{tb}"])
                for s_ in range(2):
                    for h in range(8):
                        k.ts("pool", Dm[tb][:, s_, h, :], identf[:], cfs[tb][:, s_, h:h + 1], None, ALU.mult, r=[f"cfs{tb}"], w=[f"Dm{tb}"])
                k.dma("sp", pss[:], PS[t0:t0 + 256].rearrange("(s p) h a n -> p s h a n", p=128), w=["pss"])
                for i in range(128):
                    ub = ustep % NB
                    hb_ = ustep % 2
                    ustep += 1
                    k.dma("sp", UTi[ub][:], UTv[i], w=[f"UTi{ub}"])
                    hv = banks[4 + hb_][:, 0:256]
                    for c in range(8):
                        k.mm(hv, UTi[ub][:, c, :], h2[tb][:, c, :], c == 0, c == 7, r=[f"UTi{ub}", f"h2_{tb}"], w=[f"bH{hb_}"])
                    k.act(HgA[:, i, :], hv, AF.Gelu, r=[f"bH{hb_}"], w=[f"HgA{i}"])

                NA = 10

                def st_z(i):
                    zb = i % 3
                    h0 = NA - 8
                    k.tt("pool", zt[zb][:], pss[:, 1, h0:8, 1, :], bc(pss[:, 1, h0:8, 0, i:i + 1], [128, 8 - h0, 128]), ALU.add, r=["pss"], w=[f"zt{zb}"])

                def st_eg(i):
                    zb = i % 3
                    eb = i % 2
                    h0 = NA - 8
                    for g_ in range(NA):
                        s_, h = g_ // 8, g_ % 8
                        k.act(E32[eb][:, s_, h, :], pss[:, s_, h, 1, :], AF.Exp, r=["pss"], w=[f"E32_{eb}"], bias=pss[:, s_, h, 0, i:i + 1])
                    k.act(E32[eb][:, 1, h0:8, :], zt[zb][:], AF.Exp, r=[f"zt{zb}"], w=[f"E32_{eb}"])
                    k.stt("dve", Gt[zb][:], E32[eb][:], 1.0, E32[eb][:], ALU.is_ge, ALU.mult, r=[f"E32_{eb}"], w=[f"Gt{zb}"])

                def st_gt(i):
                    zb = i % 3
                    b2 = i % 2
                    gv = banks[6 + b2][:, 0:256]
                    for s_ in range(2):
                        for h in range(8):
                            k.mm(gv[:, s_ * 128:(s_ + 1) * 128], Gt[zb][:, s_, h, :], Dm[tb][:, s_, h, :], h == 0, h == 7,
                                 r=[f"Gt{zb}", f"Dm{tb}"], w=[f"bG{b2}"])

                def st_wt(i):
                    b2 = i % 2
                    gv = banks[6 + b2][:, 0:256]
                    k.tt("dve", WT[b2][:], gv, HgA[:, i, :], ALU.mult, r=[f"bG{b2}", f"HgA{i}"], w=[f"WT{b2}"])

                vbuf = {}

                def st_vload(i):
                    nonlocal vstep
                    vb_ = vstep % NB
                    vstep += 1
                    vbuf[i] = vb_
                    k.dma("sp", Vi[vb_][:], Vb[i * 128:(i + 1) * 128, :], w=[f"Vi{vb_}"])

                def st_y(i):
                    b2 = i % 2
                    vb_ = vbuf[i]
                    for s_ in range(2):
                        for hf in range(2):
                            k.mm(banks[s_ * 2 + hf][:], WT[b2][:, s_ * 128:(s_ + 1) * 128], Vi[vb_][:, hf * 512:(hf + 1) * 512], i == 0, i == 127,
                                 r=[f"WT{b2}", f"Vi{vb_}"], w=[f"bY{s_ * 2 + hf}"])

                for it in range(128 + 3):
                    if it < 128:
                        st_vload(it)
                        st_z(it)
                    if 1 <= it <= 128:
                        st_eg(it - 1)
                    if 2 <= it <= 129:
                        st_gt(it - 2)
                    if 3 <= it <= 130:
                        st_y(it - 3)
                    if 2 <= it <= 129:
                        st_wt(it - 2)
                k.dma("sp", x1t[:], X1[t0:t0 + 256, :].rearrange("(s p) d -> p s d", p=128), w=["x1t"])
                for s_ in range(2):
                    for hf in range(2):
                        sl = slice(hf * 512, (hf + 1) * 512)
                        k.tt("dve", yo[s_][:, sl], banks[s_ * 2 + hf][:], G2row[:, sl], ALU.mult, r=[f"bY{s_ * 2 + hf}"], w=[f"yo{s_}"])
                    k.tt("pool", yo[s_][:], yo[s_][:], x1t[:, s_, :], ALU.add, r=[f"yo{s_}", "x1t"], w=[f"yo{s_}"])
                    k.dma("sp", out[t0 + s_ * 128:t0 + (s_ + 1) * 128, :], yo[s_][:], r=[f"yo{s_}"], w=["out"])
            P.emit(nc, semsets, phase_sem, 5, fin)
    return nc


DEBUG_OUT = set()


def _masks():
    kk = np.arange(128)[:, None]
    qq = np.arange(256)[None, :]
    std = ((kk <= qq) & (qq <= kk + 128)).astype(np.float32)
    blk = (((kk < 64) & (qq < 128)) | ((kk >= 64) & (qq >= 128))).astype(np.float32)
    return std, std * blk


def prep_inputs(inputs):
    f = lambda a: np.ascontiguousarray(np.asarray(a), dtype=np.float32)
    x = f(inputs["x"])
    c = f(inputs["c"])
    pos = np.ascontiguousarray(np.asarray(inputs["positions"]), dtype=np.int32)
    L = 0
    rep = lambda v, n=128: np.ascontiguousarray(np.broadcast_to(f(v)[None], (n,) + f(v).shape))
    qn_a, kn_a = f(inputs["qn_a"])[L], f(inputs["kn_a"])[L]
    qn_b, kn_b = f(inputs["qn_b"])[L], f(inputs["kn_b"])[L]
    gainA = np.concatenate([np.tile(qn_a[None], (4, 1)), np.tile(kn_a[None], (4, 1))], 0)
    gainQB = np.tile(qn_b[None], (8, 1))
    gainKB = np.tile(kn_b[None], (8, 1))
    lam = np.stack([f(inputs["lam_q1"])[L], f(inputs["lam_k1"])[L], f(inputs["lam_q2"])[L], f(inputs["lam_k2"])[L]], 0)
    invf = (1.0 / (10000.0 ** (np.arange(0, 64, 2, dtype=np.float32) / 64))).astype(np.float32)
    hp = np.concatenate([np.zeros(32, np.float32), np.full(32, np.pi / 2, np.float32)])
    mstd, mbnd = _masks()
    wq = f(inputs["w_query"])[L]
    wqT = np.ascontiguousarray(wq.reshape(D, 16, 128).transpose(1, 2, 0))
    sk = f(inputs["sub_keys"])[L].reshape(16, 128, 128)
    skT = np.ascontiguousarray(sk.transpose(0, 2, 1))
    U = f(inputs["expert_u"])[L]
    UT = np.ascontiguousarray(U.reshape(128, 128, 8, 128).transpose(0, 3, 2, 1)).reshape(16384, 1024)
    shared = dict(
        w_ada=f(inputs["w_ada"])[L],
        b_adaT=np.ascontiguousarray(f(inputs["b_ada"])[L].reshape(48, 128).T),
        n1gT=np.ascontiguousarray(f(inputs["norm1_g"])[L].reshape(8, 128).T),
        n2gT=np.ascontiguousarray(f(inputs["norm2_g"])[L].reshape(8, 128).T),
        w_in=f(inputs["w_in"])[L],
        b_gate_bc=rep(f(inputs["b_gate"])[L]),
        gainA=rep(gainA), gainQB=rep(gainQB), gainKB=rep(gainKB),
        lam_bc=rep(lam), subln_bc=rep(f(inputs["subln_g"])[L]),
        invf_bc=rep(invf), halfpi=rep(hp), mask_std=mstd, mask_bnd=mbnd,
        w_pa=f(inputs["w_proj_a"])[L], w_pb=f(inputs["w_proj_b"])[L], w_out=f(inputs["w_out"])[L],
        wqT=wqT, skT=skT, UT=UT, Vx=f(inputs["expert_v"])[L],
    )
    in_maps = []
    for b in range(8):
        m = dict(shared)
        m["x"] = x[b]
        m["pos"] = pos[b]
        m["cT"] = np.ascontiguousarray(c[b].reshape(8, 128).T)
        in_maps.append(m)
    return in_maps


def kernel(**inputs):
    in_maps = prep_inputs(inputs)
    nc = build_program()
    res = run_bass_kernel_spmd(nc, in_maps, core_ids=list(range(8)))
    return np.stack([np.asarray(r["out"], dtype=np.float32) for r in res.results], 0)
```

```python
import os
import numpy as np
from contextlib import ExitStack
import concourse.bass as bass
import concourse.mybir as mybir
from concourse.bass_utils import run_bass_kernel_spmd

F32 = mybir.dt.float32
BF16 = mybir.dt.bfloat16
I32 = mybir.dt.int32
AF = mybir.ActivationFunctionType
ALU = mybir.AluOpType
AX = mybir.AxisListType

S = 4096
D = 1024
NT = 32
EPS = 1e-6
LAM_INIT = 0.2
DIL = (1, 4, 16)
N_DMA_SEMS = 8
QUEUES = ("sp", "pool", "act")
TWO_PI = float(2 * np.pi)
C1 = 6.28125
C2 = float(2 * np.pi - 6.28125)


class Prog:
    def __init__(self):
        self.ops = []
        self.last_writer = {}
        self.readers = {}

    def add(self, eng, fn, reads=(), writes=(), dma=False):
        idx = len(self.ops)
        deps = set()
        raw = set()
        for t in reads:
            w = self.last_writer.get(t)
            if w is not None:
                deps.add(w)
                raw.add(w)
        for t in writes:
            w = self.last_writer.get(t)
            if w is not None:
                deps.add(w)
            for r in self.readers.get(t, ()):
                deps.add(r)
        op = dict(eng=eng, fn=fn, dma=dma, deps=deps, raw=raw, signal=False)
        self.ops.append(op)
        for t in reads:
            self.readers.setdefault(t, []).append(idx)
        for t in writes:
            self.last_writer[t] = idx
            self.readers[t] = []
        return idx

    def emit(self, nc, semsets, phase_sem, phase_idx, fin):
        sems, cnt = semsets[phase_idx % 3]
        ops = self.ops
        for op in ops:
            keep = set()
            for d in op["deps"]:
                p = ops[d]
                if p["dma"] or op["dma"] or p["eng"] != op["eng"]:
                    keep.add(d)
                elif op["eng"] != "pe" and d in op["raw"]:
                    keep.add(d)
            op["deps"] = keep
            for d in keep:
                ops[d]["signal"] = True
        dma_k = {}
        prev_slot = {}
        for op in ops:
            if op["dma"]:
                q = op["eng"]
                k = dma_k.get(q, 0)
                dma_k[q] = k + 1
                key = ("dma", q, k % N_DMA_SEMS)
                cnt[key] = cnt.get(key, 0) + 16
                op["prev"] = prev_slot.get(key)
                op["sig"] = (key, cnt[key])
                prev_slot[key] = op["sig"]
            elif op["signal"]:
                key = op["eng"]
                cnt[key] = cnt.get(key, 0) + 1
                op["sig"] = (key, cnt[key])

        def semof(key):
            if isinstance(key, tuple):
                return sems["dma_" + key[1]][key[2]]
            return sems[key]

        streams = {}
        for op in ops:
            streams.setdefault(op["eng"], []).append(op)

        def run_stream(engname, eng):
            if phase_idx > 0:
                eng.wait_ge(phase_sem, 19 * phase_idx)
            known = {}
            for op in streams.get(engname, []):
                waits = {}
                for d in op["deps"]:
                    key, val = ops[d]["sig"]
                    if waits.get(key, 0) < val:
                        waits[key] = val
                if op["dma"] and op["prev"] is not None:
                    key, val = op["prev"]
                    if waits.get(key, 0) < val:
                        waits[key] = val
                for key, val in waits.items():
                    if known.get(key, 0) >= val:
                        continue
                    eng.wait_ge(semof(key), val)
                    known[key] = val
                ins = op["fn"](eng)
                if "sig" in op:
                    ins.then_inc(semof(op["sig"][0]), 16 if op["dma"] else 1)
            if engname == "sp":
                for key, val in cnt.items():
                    if isinstance(key, tuple):
                        eng.wait_ge(semof(key), val)
                eng.dma_start(out=fin["d1"][:], in_=fin["d0"][:]).then_inc(phase_sem, 16)
            elif engname == "pe":
                pass
            elif engname == "act":
                eng.activation(out=fin["act"][:], in_=fin["d0"][0:1, 0:1].to_broadcast([1, 1]) if False else fin["act"][:], func=AF.Copy).then_inc(phase_sem, 1)
            else:
                eng.memset(fin[engname][:], 0.0).then_inc(phase_sem, 1)

        with nc.Block() as block:
            @block.sync
            def _(e):
                run_stream("sp", e)

            @block.tensor
            def _(e):
                run_stream("pe", e)

            @block.scalar
            def _(e):
                run_stream("act", e)

            @block.vector
            def _(e):
                run_stream("dve", e)

            @block.gpsimd
            def _(e):
                run_stream("pool", e)


class K:
    def __init__(self, P):
        self.P = P

    def dma(self, q, out, in_, r=(), w=(), **kw):
        self.P.add(q, lambda e: e.dma_start(out=out, in_=in_, **kw), r, w, dma=True)

    def mm(self, out, lhsT, rhs, start, stop, r=(), w=()):
        self.P.add("pe", lambda e: e.matmul(out, lhsT=lhsT, rhs=rhs, start=start, stop=stop), r, w)

    def tr(self, out, in_, ident, r=(), w=()):
        self.P.add("pe", lambda e: e.transpose(out=out, in_=in_, identity=ident), r, w)

    def act(self, out, in_, func, r=(), w=(), **kw):
        self.P.add("act", lambda e: e.activation(out=out, in_=in_, func=func, **kw), r, w)

    def tt(self, eng, out, in0, in1, op, r=(), w=()):
        self.P.add(eng, lambda e: e.tensor_tensor(out=out, in0=in0, in1=in1, op=op), r, w)

    def ts(self, eng, out, in0, s1, s2, op0, op1=None, r=(), w=()):
        if op1 is None:
            self.P.add(eng, lambda e: e.tensor_scalar(out=out, in0=in0, scalar1=s1, scalar2=None, op0=op0), r, w)
        else:
            self.P.add(eng, lambda e: e.tensor_scalar(out=out, in0=in0, scalar1=s1, scalar2=s2, op0=op0, op1=op1), r, w)

    def stt(self, eng, out, in0, scalar, in1, op0, op1, r=(), w=()):
        self.P.add(eng, lambda e: e.scalar_tensor_tensor(out=out, in0=in0, scalar=scalar, in1=in1, op0=op0, op1=op1), r, w)

    def cp(self, eng, out, in_, r=(), w=()):
        self.P.add(eng, lambda e: e.tensor_copy(out=out, in_=in_), r, w)

    def red(self, eng, out, in_, r=(), w=(), op=None):
        op = op or ALU.add
        self.P.add(eng, lambda e: e.tensor_reduce(out=out, in_=in_, axis=AX.X, op=op), r, w)

    def recip(self, out, in_, r=(), w=()):
        self.P.add("dve", lambda e: e.reciprocal(out=out, in_=in_), r, w)

    def memset(self, eng, out, val, r=(), w=()):
        self.P.add(eng, lambda e: e.memset(out, val), r, w)

    def max8(self, out, in_, r=(), w=()):
        self.P.add("dve", lambda e: e.max(out=out, in_=in_), r, w)

    def mrep(self, out, rep, vals, r=(), w=()):
        self.P.add("dve", lambda e: e.match_replace(out=out, in_to_replace=rep, in_values=vals, imm_value=-1e30), r, w)


def bc(ap, shape):
    return ap.to_broadcast(list(shape))


def build_program(debug=False):
    nc = bass.Bass("TRN2", target_bir_lowering=False)

    def din(name, shape, dt=F32):
        return nc.dram_tensor(name, list(shape), dt, kind="ExternalInput").ap()

    def dscr(name, shape, dt):
        kind = "ExternalOutput" if (debug and name in DEBUG_OUT) else "Internal"
        return nc.dram_tensor(name, list(shape), dt, kind=kind).ap()

    x = din("x", [S, D])
    pos = din("pos", [S], I32)
    cT = din("cT", [128, 8])
    w_ada = din("w_ada", [D, 6 * D])
    b_adaT = din("b_adaT", [128, 48])
    n1gT = din("n1gT", [128, 8])
    n2gT = din("n2gT", [128, 8])
    w_in = din("w_in", [D, 7424])
    b_gate_bc = din("b_gate_bc", [128, 2048])
    gainA = din("gainA", [128, 8, 64])
    gainQB = din("gainQB", [128, 8, 64])
    gainKB = din("gainKB", [128, 8, 64])
    lam_bc = din("lam_bc", [128, 4, 64])
    subln_bc = din("subln_bc", [128, 128])
    invf_bc = din("invf_bc", [128, 32])
    halfpi = din("halfpi", [128, 64])
    mask_std = din("mask_std", [128, 256])
    mask_bnd = din("mask_bnd", [128, 256])
    w_pa = din("w_pa", [256, D])
    w_pb = din("w_pb", [D, D])
    w_out = din("w_out", [D, D])
    wqT = din("wqT", [16, 128, D])
    skT = din("skT", [16, 128, 128])
    UT = din("UT", [16384, 1024])
    Vx = din("Vx", [16384, 1024])
    out = nc.dram_tensor("out", [S, D], F32, kind="ExternalOutput").ap()

    QTA = dscr("QTA", [3, 2, 128, S], BF16)
    KTA = dscr("KTA", [3, 2, 128, S], BF16)
    VA = dscr("VA", [3, S, 256], BF16)
    QTB = dscr("QTB", [8, 128, S], BF16)
    KTB = dscr("KTB", [8, 128, S], BF16)
    VB = dscr("VB", [S, 1024], BF16)
    GT = dscr("GT", [S, 2048], BF16)
    ND = dscr("ND", [S, 12, 65], F32)
    OBT = dscr("OBT", [8, 128, S], BF16)
    X1 = dscr("X1", [S, D], F32)
    H2T = dscr("H2T", [8, 128, S], BF16)
    PS = dscr("PS", [S, 8, 2, 128], F32)
    CF = dscr("CF", [S, 8], F32)
    UTb = dscr("UTb", [16384, 1024], BF16)
    Vb = dscr("Vb", [16384, 1024], BF16)

    with ExitStack() as top:
        def sbt(es, name, shape, dt):
            return es.enter_context(nc.sbuf_tensor(name, list(shape), dt))

        def pst(es, name, shape, dt):
            return es.enter_context(nc.psum_tensor(name, list(shape), dt))

        semsets = []
        for ph in range(3):
            ss = {}
            for e in ("pe", "act", "dve", "pool"):
                ss[e] = top.enter_context(nc.semaphore(f"s{ph}_{e}"))
            for q in QUEUES:
                ss["dma_" + q] = [top.enter_context(nc.semaphore(f"d{ph}_{q}_{i}")) for i in range(N_DMA_SEMS)]
            semsets.append((ss, {}))
        phase_sem = top.enter_context(nc.semaphore("phase"))
        conv_sem = top.enter_context(nc.semaphore("conv"))

        identb = sbt(top, "identb", [128, 128], BF16)
        identf = sbt(top, "identf", [128, 128], F32)
        onesf = sbt(top, "onesf", [128, 128], F32)
        s1T = sbt(top, "s1T", [128, 8], F32)
        sh1T = sbt(top, "sh1T", [128, 8], F32)
        s2T = sbt(top, "s2T", [128, 8], F32)
        sh2T = sbt(top, "sh2T", [128, 8], F32)
        G1row = sbt(top, "G1row", [128, D], F32)
        G2row = sbt(top, "G2row", [128, D], F32)
        neglam = sbt(top, "neglam", [128, 1], F32)
        sgain = sbt(top, "sgain", [128, 128], F32)
        epsb = sbt(top, "epsb", [128, 1], F32)
        fin = dict(
            d0=sbt(top, "fin_d0", [1, 16], F32), d1=sbt(top, "fin_d1", [1, 16], F32),
            act=sbt(top, "fin_act", [128, 1], F32), dve=sbt(top, "fin_dve", [128, 1], F32),
            pool=sbt(top, "fin_pool", [128, 1], F32), idb=identb,
        )
        psn = [0]

        def alloc_psum(es, nf=6, nb=2):
            psn[0] += 1
            bk = [pst(es, f"bank{psn[0]}_{i}", [128, 512], F32) for i in range(nf)]
            bt = [pst(es, f"bankT{psn[0]}_{i}", [128, 1024], BF16) for i in range(nb)]
            return bk, bt

        with ExitStack() as es:
            P = Prog()
            k = K(P)
            banks, bankT = alloc_psum(es)
            cTs = sbt(es, "cTs", [128, 8], F32)
            scs = sbt(es, "scs", [128, 8], F32)
            wada = [sbt(es, f"wada{i}", [128, 6 * D], F32) for i in range(2)]
            badas = sbt(es, "badas", [128, 48], F32)
            modT = sbt(es, "modT", [128, 48], F32)
            n1s = sbt(es, "n1s", [128, 8], F32)
            n2s = sbt(es, "n2s", [128, 8], F32)
            lams = sbt(es, "lams", [128, 4, 64], F32)
            lprod = sbt(es, "lprod", [128, 2, 64], F32)
            lsum = sbt(es, "lsum", [128, 2], F32)
            lexp = sbt(es, "lexp", [128, 2], F32)
            ltmp = sbt(es, "ltmp", [128, 1], F32)
            subs = sbt(es, "subs", [128, 128], F32)
            dg = [sbt(es, f"dg{i}", [128, 128], F32) for i in range(2)]
            NCONV = 16
            rows = 16384 // NCONV
            for i in range(NCONV):
                for (src, dst) in ((UT, UTb), (Vx, Vb)):
                    P.add("pool", (lambda s_, d_, i_: (lambda e: e.dma_start(out=d_[i_ * rows:(i_ + 1) * rows, :], in_=s_[i_ * rows:(i_ + 1) * rows, :]).then_inc(conv_sem, 16)))(src, dst, i))
            k.dma("sp", cTs[:], cT, w=["cTs"])
            k.dma("sp", badas[:], b_adaT, w=["badas"])
            k.dma("sp", n1s[:], n1gT, w=["n1s"])
            k.dma("sp", n2s[:], n2gT, w=["n2s"])
            k.dma("sp", lams[:], lam_bc, w=["lams"])
            k.dma("sp", subs[:], subln_bc, w=["subs"])
            k.memset("dve", identf[:], 1.0, w=["identf"])
            P.add("pool", lambda e: e.affine_select(out=identf[:], in_=identf[:], pattern=[[-1, 128]], compare_op=ALU.is_equal,
                                                   fill=0.0, base=0, channel_multiplier=1), ["identf"], ["identf"])
            k.cp("dve", identb[:], identf[:], r=["identf"], w=["identb"])
            k.memset("dve", onesf[:], 1.0, w=["onesf"])
            k.memset("dve", epsb[:], EPS, w=["epsb"])
            k.dma("sp", fin["d0"][:], invf_bc[0:1, 0:16], w=["find0"])
            k.act(scs[:], cTs[:], AF.Silu, r=["cTs"], w=["scs"])
            modps = banks[0]
            for kc in range(8):
                b = kc % 2
                k.dma("sp", wada[b][:], w_ada[kc * 128:(kc + 1) * 128, :], w=[f"wada{b}"])
                for f in range(48):
                    k.mm(modps[:, f:f + 1], wada[b][:, f * 128:(f + 1) * 128], scs[:, kc:kc + 1], kc == 0 and f == 0, kc == 7,
                         r=[f"wada{b}", "scs"], w=["modps"])
            k.tt("dve", modT[:], modps[:, 0:48], badas[:], ALU.add, r=["modps", "badas"], w=["modT"])
            k.stt("dve", s1T[:], modT[:, 8:16], 1.0, n1s[:], ALU.add, ALU.mult, r=["modT", "n1s"], w=["s1T"])
            k.cp("dve", sh1T[:], modT[:, 0:8], r=["modT"], w=["sh1T"])
            k.stt("dve", s2T[:], modT[:, 32:40], 1.0, n2s[:], ALU.add, ALU.mult, r=["modT", "n2s"], w=["s2T"])
            k.cp("dve", sh2T[:], modT[:, 24:32], r=["modT"], w=["sh2T"])
            for (row, base, bk) in ((G1row, 16, 1), (G2row, 40, 3)):
                for c in range(8):
                    b = c % 2
                    k.ts("dve", dg[b][:], identf[:], modT[:, base + c:base + c + 1], None, ALU.mult, r=["modT", "identf"], w=[f"dg{b}"])
                    bank = banks[bk + c // 4]
                    k.mm(bank[:, (c % 4) * 128:(c % 4 + 1) * 128], onesf[:], dg[b][:], True, True, r=[f"dg{b}", "onesf"], w=[f"gr{bk + c // 4}"])
                for hh in range(2):
                    k.cp("dve", row[:, hh * 512:(hh + 1) * 512], banks[bk + hh][:], r=[f"gr{bk + hh}"], w=[f"grow{base}"])
            k.tt("dve", lprod[:, 0, :], lams[:, 0, :], lams[:, 1, :], ALU.mult, r=["lams"], w=["lprod0"])
            k.tt("dve", lprod[:, 1, :], lams[:, 2, :], lams[:, 3, :], ALU.mult, r=["lams"], w=["lprod1"])
            k.red("dve", lsum[:], lprod[:], r=["lprod0", "lprod1"], w=["lsum"])
            k.act(lexp[:], lsum[:], AF.Exp, r=["lsum"], w=["lexp"])
            k.tt("dve", ltmp[:], lexp[:, 1:2], lexp[:, 0:1], ALU.subtract, r=["lexp"], w=["ltmp"])
            k.ts("dve", neglam[:], ltmp[:], -LAM_INIT, None, ALU.add, r=["ltmp"], w=["neglam"])
            k.ts("dve", sgain[:], subs[:], 1.0 - LAM_INIT, None, ALU.mult, r=["subs"], w=["sgain"])
            P.emit(nc, semsets, phase_sem, 0, fin)

        with ExitStack() as es:
            P = Prog()
            k = K(P)
            banks, bankT = alloc_psum(es, 5, 3)
            hT = sbt(es, "hT", [128, 8, 2048], BF16)
            xt = [sbt(es, f"xt{i}", [128, D], F32) for i in range(2)]
            sq = [sbt(es, f"sq{i}", [128, D], F32) for i in range(2)]
            xn = [sbt(es, f"xn{i}", [128, D], BF16) for i in range(2)]
            ssx = [sbt(es, f"ssx{i}", [128, 1], F32) for i in range(2)]
            rsx = [sbt(es, f"rsx{i}", [128, 1], F32) for i in range(2)]
            gA = sbt(es, "gA", [128, 8, 64], F32)
            gQB = sbt(es, "gQB", [128, 8, 64], F32)
            gKB = sbt(es, "gKB", [128, 8, 64], F32)
            bgs = sbt(es, "bgs", [128, 2048], F32)
            invfs = sbt(es, "invfs", [128, 32], F32)
            hps = sbt(es, "hps", [128, 64], F32)
            posi = sbt(es, "posi", [128, 16], I32)
            posf = sbt(es, "posf", [128, 16], F32)
            tabs = [sbt(es, f"tab{i}", [128, 16, 64], F32) for i in range(3)]
            arg = sbt(es, "arg", [128, 16, 64], F32)
            argk = sbt(es, "argk", [128, 16, 64], F32)
            argi = sbt(es, "argi", [128, 16, 64], I32)
            wb = [sbt(es, f"wb{i}", [128, 8, 512], BF16) for i in range(2)]
            sqq = [sbt(es, f"sqq{i}", [128, 8, 64], F32) for i in range(3)]
            ssq = [sbt(es, f"ssq{i}", [128, 8], F32) for i in range(3)]
            rsq = [sbt(es, f"rsq{i}", [128, 8], F32) for i in range(3)]
            qn = [sbt(es, f"qn{i}", [128, 8, 64], F32) for i in range(3)]
            qg = [sbt(es, f"qg{i}", [128, 8, 2, 32], F32) for i in range(3)]
            rt = [sbt(es, f"rt{i}", [128, 4, 8, 32], F32) for i in range(3)]
            qr = [sbt(es, f"qr{i}", [128, 8, 2, 32], BF16) for i in range(3)]
            stg = [sbt(es, f"stg{i}", [128, 4, 128], BF16) for i in range(3)]
            vst = [sbt(es, f"vst{i}", [128, 512], BF16) for i in range(3)]
            gpre = [sbt(es, f"gpre{i}", [128, 512], F32) for i in range(3)]

            k.dma("sp", gA[:], gainA, w=["gA"])
            k.dma("sp", gQB[:], gainQB, w=["gQB"])
            k.dma("sp", gKB[:], gainKB, w=["gKB"])
            k.dma("sp", bgs[:], b_gate_bc, w=["bgs"])
            k.dma("sp", invfs[:], invf_bc, w=["invfs"])
            k.dma("sp", hps[:], halfpi, w=["hps"])

            pos_pat = [
                pos.rearrange("(st u m) -> st m u", st=2, u=16, m=128),
                pos.rearrange("(st blk m r) -> st m blk r", st=2, blk=4, m=128, r=4),
                pos.rearrange("(st m r) -> st m r", st=2, m=128, r=16),
            ]

            def tokcols(pat, u):
                if pat == 0:
                    return slice(u * 128, (u + 1) * 128)
                if pat == 1:
                    blk, r = u // 4, u % 4
                    return slice(blk * 512 + r, blk * 512 + 512, 4)
                return slice(u, 2048, 16)

            def perm0(pat, st, u):
                if pat == 0:
                    return st * 2048 + u * 128
                if pat == 1:
                    blk, r = u // 4, u % 4
                    return r * 1024 + st * 512 + blk * 128
                return u * 256 + st * 128

            jobs = []
            for g in range(3):
                jobs.append((g, "qk", [(256 * g, 256), (768 + 256 * g, 256)], ("A", g)))
                jobs.append((g, "v", [(1536 + 256 * g, 256)], ("A", g)))
            for j in range(2):
                jobs.append((0, "qk", [(2304 + 512 * j, 512)], ("QB", j)))
                jobs.append((0, "qk", [(3328 + 512 * j, 512)], ("KB", j)))
                jobs.append((0, "v", [(4352 + 512 * j, 512)], ("B", j)))
            for j in range(4):
                jobs.append((0, "gate", [(5376 + 512 * j, 512)], ("G", j)))

            unit = 0
            for st in range(2):
                for u in range(16):
                    b = u % 2
                    t0 = st * 2048 + u * 128
                    k.dma("sp", xt[b][:], x[t0:t0 + 128, :], w=[f"xt{b}"])
                    k.act(sq[b][:], xt[b][:], AF.Square, r=[f"xt{b}"], w=[f"sq{b}"])
                    k.red("dve", ssx[b][:], sq[b][:], r=[f"sq{b}"], w=[f"ssx{b}"])
                    k.act(rsx[b][:], ssx[b][:], AF.Sqrt, r=[f"ssx{b}", "epsb"], w=[f"rsx{b}"], scale=1.0 / D, bias=epsb[:])
                    k.recip(rsx[b][:], rsx[b][:], r=[f"rsx{b}"], w=[f"rsx{b}"])
                    k.ts("dve", xn[b][:], xt[b][:], rsx[b][:, 0:1], None, ALU.mult, r=[f"xt{b}", f"rsx{b}"], w=[f"xn{b}"])
                    for c in range(8):
                        k.tr(bankT[b][:, c * 128:(c + 1) * 128], xn[b][:, c * 128:(c + 1) * 128], identb[:], r=[f"xn{b}"], w=[f"bT{b}"])
                    for c in range(8):
                        k.act(hT[:, c, u * 128:(u + 1) * 128], bankT[b][:, c * 128:(c + 1) * 128], AF.Identity,
                              r=[f"bT{b}"], w=[f"hT{u}"], scale=s1T[:, c:c + 1], bias=sh1T[:, c:c + 1])
                for pat in range(3):
                    if pat == 0:
                        k.dma("sp", posi[:], pos_pat[0][st], w=["posi"], allow_slow_non_contiguous=True)
                    elif pat == 1:
                        k.dma("sp", posi[:].rearrange("p (a b) -> p a b", a=4), pos_pat[1][st], w=["posi"])
                    else:
                        k.dma("sp", posi[:], pos_pat[2][st], w=["posi"])
                    k.cp("dve", posf[:], posi[:], r=["posi"], w=["posf"])
                    k.tt("dve", arg[:, :, 0:32], bc(posf[:].unsqueeze(2), [128, 16, 32]), bc(invfs[:].unsqueeze(1), [128, 16, 32]), ALU.mult,
                         r=["posf", "invfs"], w=["arg"])
                    k.cp("dve", arg[:, :, 32:64], arg[:, :, 0:32], r=["arg"], w=["arg"])
                    k.tt("dve", arg[:], arg[:], bc(hps[:].unsqueeze(1), [128, 16, 64]), ALU.add, r=["arg", "hps"], w=["arg"])
                    k.ts("dve", argk[:], arg[:], 1.0 / TWO_PI, None, ALU.mult, r=["arg"], w=["argk"])
                    k.cp("dve", argi[:], argk[:], r=["argk"], w=["argi"])
                    k.cp("dve", argk[:], argi[:], r=["argi"], w=["argk"])
                    k.stt("dve", arg[:], argk[:], -C1, arg[:], ALU.mult, ALU.add, r=["argk", "arg"], w=["arg"])
                    k.stt("dve", arg[:], argk[:], -C2, arg[:], ALU.mult, ALU.add, r=["argk", "arg"], w=["arg"])
                    k.ts("dve", argk[:], arg[:], float(np.pi), -TWO_PI, ALU.is_gt, ALU.mult, r=["arg"], w=["argk"])
                    k.tt("dve", arg[:], arg[:], argk[:], ALU.add, r=["arg", "argk"], w=["arg"])
                    k.ts("dve", argk[:], arg[:], float(-np.pi), TWO_PI, ALU.is_lt, ALU.mult, r=["arg"], w=["argk"])
                    k.tt("dve", arg[:], arg[:], argk[:], ALU.add, r=["arg", "argk"], w=["arg"])
                    k.act(tabs[pat][:], arg[:], AF.Sin, r=["arg"], w=[f"tab{pat}"])
                hall = [f"hT{u}" for u in range(16)]
                pending = []
                for ji, (pat, kind, cols, extra) in enumerate(jobs):
                    wbi = ji % 2
                    off = 0
                    for (c0, ncol) in cols:
                        k.dma("pool", wb[wbi][:, :, off:off + ncol], w_in[:, c0:c0 + ncol].rearrange("(c p) n -> p c n", p=128), w=[f"wb{wbi}"])
                        off += ncol
                    ncols = off
                    for u in range(16):
                        ub = unit % 3
                        unit += 1
                        bankP = banks[ub]
                        tc = tokcols(pat, u)
                        rtok = hall if pat else [f"hT{u}"]
                        for c in range(8):
                            k.mm(bankP[:, 0:ncols], hT[:, c, tc], wb[wbi][:, c, 0:ncols], c == 0, c == 7, r=rtok + [f"wb{wbi}"], w=[f"bP{ub}"])
                        p0 = perm0(pat, st, u)

                        def post1(ub=ub, bankP=bankP, pat=pat, kind=kind, extra=extra, u=u, p0=p0, ncols=ncols):
                            if kind == "qk":
                                gt = {"A": gA, "QB": gQB, "KB": gKB}[extra[0]]
                                gtn = {"A": "gA", "QB": "gQB", "KB": "gKB"}[extra[0]]
                                pv = bankP[:].rearrange("p (h c) -> p h c", h=8)
                                k.act(sqq[ub][:], pv, AF.Square, r=[f"bP{ub}"], w=[f"sqq{ub}"])
                                k.red("dve", ssq[ub][:], sqq[ub][:], r=[f"sqq{ub}"], w=[f"ssq{ub}"])
                                k.act(rsq[ub][:], ssq[ub][:], AF.Sqrt, r=[f"ssq{ub}", "epsb"], w=[f"rsq{ub}"], scale=1.0 / 64, bias=epsb[:])
                                k.recip(rsq[ub][:], rsq[ub][:], r=[f"rsq{ub}"], w=[f"rsq{ub}"])
                                k.tt("dve", qn[ub][:], pv, bc(rsq[ub][:].unsqueeze(2), [128, 8, 64]), ALU.mult, r=[f"bP{ub}", f"rsq{ub}"], w=[f"qn{ub}"])
                                k.tt("pool", qg[ub][:].rearrange("p h a f -> p h (a f)"), qn[ub][:], gt[:], ALU.mult, r=[f"qn{ub}", gtn], w=[f"qg{ub}"])
                                sinb = bc(tabs[pat][:, u, 0:32].unsqueeze(1), [128, 8, 32])
                                cosb = bc(tabs[pat][:, u, 32:64].unsqueeze(1), [128, 8, 32])
                                x1 = qg[ub][:, :, 0, :]
                                x2 = qg[ub][:, :, 1, :]
                                tn = f"tab{pat}"
                                k.tt("dve", rt[ub][:, 0], x1, cosb, ALU.mult, r=[f"qg{ub}", tn], w=[f"rt0{ub}"])
                                k.tt("dve", rt[ub][:, 1], x2, sinb, ALU.mult, r=[f"qg{ub}", tn], w=[f"rt1{ub}"])
                                k.tt("dve", qr[ub][:, :, 0, :], rt[ub][:, 0], rt[ub][:, 1], ALU.subtract, r=[f"rt0{ub}", f"rt1{ub}"], w=[f"qr{ub}a"])
                                k.tt("pool", rt[ub][:, 2], x2, cosb, ALU.mult, r=[f"qg{ub}", tn], w=[f"rt2{ub}"])
                                k.tt("pool", rt[ub][:, 3], x1, sinb, ALU.mult, r=[f"qg{ub}", tn], w=[f"rt3{ub}"])
                                k.tt("pool", qr[ub][:, :, 1, :], rt[ub][:, 2], rt[ub][:, 3], ALU.add, r=[f"rt2{ub}", f"rt3{ub}"], w=[f"qr{ub}b"])
                                return
                            if kind == "v":
                                k.act(vst[ub][:, 0:ncols], bankP[:, 0:ncols], AF.Copy, r=[f"bP{ub}"], w=[f"vst{ub}"])
                                if extra[0] == "A":
                                    k.dma("sp", VA[extra[1], p0:p0 + 128, :], vst[ub][:, 0:256], r=[f"vst{ub}"], w=["VA"])
                                else:
                                    j = extra[1]
                                    k.dma("sp", VB[p0:p0 + 128, 512 * j:512 * j + 512], vst[ub][:], r=[f"vst{ub}"], w=["VB"])
                            else:
                                j = extra[1]
                                k.tt("dve", gpre[ub][:], bankP[:], bgs[:, 512 * j:512 * j + 512], ALU.add, r=[f"bP{ub}", "bgs"], w=[f"gpre{ub}"])
                                k.act(vst[ub][:], gpre[ub][:], AF.Sigmoid, r=[f"gpre{ub}"], w=[f"vst{ub}"])
                                k.dma("sp", GT[p0:p0 + 128, 512 * j:512 * j + 512], vst[ub][:], r=[f"vst{ub}"], w=["GT"])

                        def post2(ub=ub, bankP=bankP, pat=pat, kind=kind, extra=extra, u=u, p0=p0, ncols=ncols):
                            if kind == "qk":
                                qflat = qr[ub][:].rearrange("p h a f -> p (h a f)")
                                for q4 in range(4):
                                    k.tr(bankT[ub][:, q4 * 128:(q4 + 1) * 128], qflat[:, q4 * 128:(q4 + 1) * 128], identb[:],
                                         r=[f"qr{ub}a", f"qr{ub}b"], w=[f"bT{ub}"])
                                k.act(stg[ub][:].rearrange("p a t -> p (a t)"), bankT[ub][:, 0:512], AF.Copy, r=[f"bT{ub}"], w=[f"stg{ub}"])
                                if extra[0] == "A":
                                    g = extra[1]
                                    k.dma("sp", QTA[g, :, :, p0:p0 + 128].rearrange("a c t -> c a t"), stg[ub][:, 0:2, :], r=[f"stg{ub}"], w=["QTA"])
                                    k.dma("sp", KTA[g, :, :, p0:p0 + 128].rearrange("a c t -> c a t"), stg[ub][:, 2:4, :], r=[f"stg{ub}"], w=["KTA"])
                                else:
                                    dst = QTB if extra[0] == "QB" else KTB
                                    j = extra[1]
                                    k.dma("sp", dst[4 * j:4 * j + 4, :, p0:p0 + 128].rearrange("a c t -> c a t"), stg[ub][:], r=[f"stg{ub}"], w=[extra[0]])
                        pending.append((post1, post2))
                        if len(pending) >= 2:
                            pending[-2][0]()
                        if len(pending) >= 3:
                            pending.pop(0)[1]()
                if pending:
                    pending[-1][0]()
                while pending:
                    pending.pop(0)[1]()
            P.emit(nc, semsets, phase_sem, 1, fin)

        with ExitStack() as es:
            P = Prog()
            k = K(P)
            banks, bankT = alloc_psum(es)
            KTs = [sbt(es, f"KTs{i}", [128, S + 128], BF16) for i in range(2)]
            QTs = [sbt(es, f"QTs{i}", [128, S + 128], BF16) for i in range(2)]
            V1 = [sbt(es, f"V1_{i}", [128, 33, 65], BF16) for i in range(2)]
            mstd = sbt(es, "mstd", [128, 256], F32)
            mbnd = sbt(es, "mbnd", [128, 256], F32)
            Eb = [sbt(es, f"Eb{i}", [128, 256], BF16) for i in range(2)]
            Pm = [sbt(es, f"Pm{i}", [128, 256], BF16) for i in range(2)]
            osb = [sbt(es, f"osb{i}", [128, 65], F32) for i in range(4)]
            k.dma("sp", mstd[:], mask_std, w=["mstd"])
            k.dma("sp", mbnd[:], mask_bnd, w=["mbnd"])
            for i in range(2):
                k.memset("pool", KTs[i][:, 0:64], 0.0, w=[f"KTs{i}"])
                k.memset("pool", KTs[i][:, S + 64:S + 128], 0.0, w=[f"KTs{i}"])
                k.memset("pool", QTs[i][:, 0:64], 0.0, w=[f"QTs{i}"])
                k.memset("pool", QTs[i][:, S + 64:S + 128], 0.0, w=[f"QTs{i}"])
                k.memset("pool", V1[i][:], 0.0, w=[f"V1_{i}"])
                k.memset("pool", V1[i][:, :, 64:65], 1.0, w=[f"V1_{i}"])
            bankSs = [banks[0], banks[2]]
            bankOs = [banks[1], banks[3], banks[4], banks[5]]
            hcount = 0
            ccount = 0
            for g in range(3):
                dil = DIL[g]
                L = S // dil
                for pr in range(2):
                    pb = (g * 2 + pr) % 2
                    k.dma("sp", KTs[pb][:, 64:64 + S], KTA[g, pr], r=["KTA"], w=[f"KTs{pb}"])
                    k.dma("sp", QTs[pb][:, 64:64 + S], QTA[g, pr], r=["QTA"], w=[f"QTs{pb}"])
                    for hh in range(2):
                        hs = pr * 2 + hh
                        bp = 64 * hh
                        vb = hcount % 2
                        hcount += 1
                        vsrc = VA[g, :, hs * 64:(hs + 1) * 64]
                        k.dma("sp", V1[vb][:, 1:32, 0:64], vsrc[64:S - 64, :].rearrange("(j p) c -> p j c", p=128), r=["VA"], w=[f"V1_{vb}"])
                        k.dma("sp", V1[vb][64:128, 0, 0:64], vsrc[0:64, :], r=["VA"], w=[f"V1_{vb}"])
                        k.dma("sp", V1[vb][0:64, 32, 0:64], vsrc[S - 64:S, :], r=["VA"], w=[f"V1_{vb}"])
                        for jc in range(33):
                            sb_ = ccount % 2
                            ccount += 1
                            bnd = (128 * jc) % L == 0
                            qlo = 128 if jc == 0 else 0
                            qhi = 128 if jc == 32 else 256
                            sv = bankSs[sb_][:, 0:256]
                            k.mm(sv[:, qlo:qhi], KTs[pb][bp:bp + 64, 128 * jc:128 * jc + 128],
                                 QTs[pb][bp:bp + 64, 128 * jc - 64 + qlo:128 * jc - 64 + qhi],
                                 True, True, r=[f"KTs{pb}", f"QTs{pb}"], w=[f"bS{sb_}"])
                            k.act(Eb[sb_][:, qlo:qhi], sv[:, qlo:qhi], AF.Exp, r=[f"bS{sb_}"], w=[f"Eb{sb_}"], scale=0.125)
                            mk = mbnd if bnd else mstd
                            k.tt("dve", Pm[sb_][:, qlo:qhi], Eb[sb_][:, qlo:qhi], mk[:, qlo:qhi], ALU.mult, r=[f"Eb{sb_}", "mstd", "mbnd"], w=[f"Pm{sb_}"])
                            for half in range(2):
                                if half * 128 < qlo or half * 128 >= qhi:
                                    continue
                                blk = jc - 1 + half
                                a = blk % 4
                                k.mm(bankOs[a][:, 0:65], Pm[sb_][:, half * 128:half * 128 + 128], V1[vb][:, jc, :],
                                     half == 1, half == 0, r=[f"Pm{sb_}", f"V1_{vb}"], w=[f"bO{a}"])
                            if jc >= 1:
                                blk = jc - 1
                                a = blk % 4
                                k.cp("dve", osb[a][:], bankOs[a][:, 0:65], r=[f"bO{a}"], w=[f"osb{a}"])
                                r_ = (128 * blk) // L
                                j0 = (128 * blk) % L
                                s0 = j0 * dil + r_
                                dst = ND[s0:s0 + 127 * dil + 1:dil, g * 4 + hs, :]
                                k.dma("sp", dst, osb[a][:], r=[f"osb{a}"], w=["ND"])
            P.emit(nc, semsets, phase_sem, 2, fin)

        with ExitStack() as es:
            P = Prog()
            k = K(P)
            banks, bankT = alloc_psum(es, 7, 1)
            KTs = [sbt(es, f"KTd{i}", [128, S], BF16) for i in range(2)]
            QTs = [sbt(es, f"QTd{i}", [128, S], BF16) for i in range(2)]
            V1 = [sbt(es, f"V1d{i}", [128, 32, 129], BF16) for i in range(2)]
            E = [[sbt(es, f"E{m}_{i}", [128, 512], BF16) for i in range(2)] for m in range(2)]
            osb = sbt(es, "osbd", [128, 8, 129], F32)
            rr = sbt(es, "rr", [128, 8], F32)
            o1 = sbt(es, "o1", [128, 4, 128], F32)
            o2 = sbt(es, "o2", [128, 4, 128], F32)
            ssd = sbt(es, "ssd", [128, 4], F32)
            obb = sbt(es, "obb", [128, 4, 128], BF16)
            obT = [sbt(es, f"obT{i}", [128, 512], BF16) for i in range(2)]
            for i in range(2):
                k.memset("pool", V1[i][:, :, 128:129], 1.0, w=[f"V1d{i}"])
            accb = [banks[4], banks[5], banks[6]]
            steps = [(h, qb, kc) for h in range(8) for qb in range(8) for kc in range(32)]

            def qk_exp(n):
                h, qb, kc = steps[n]
                hb = h % 2
                eb = n % 2
                if qb == 0 and kc == 0:
                    k.dma("sp", KTs[hb][:], KTB[h], r=["KB"], w=[f"KTd{hb}"])
                    k.dma("sp", QTs[hb][:], QTB[h], r=["QB"], w=[f"QTd{hb}"])
                    k.dma("sp", V1[hb][:, :, 0:128], VB[:, h * 128:(h + 1) * 128].rearrange("(j p) c -> p j c", p=128), r=["VB"], w=[f"V1d{hb}"])
                for m in range(2):
                    bi = m * 2 + eb
                    tok = f"bS{bi}"
                    k.mm(banks[bi][:], KTs[hb][64 * m:64 * m + 64, kc * 128:(kc + 1) * 128], QTs[hb][64 * m:64 * m + 64, qb * 512:(qb + 1) * 512],
                         True, True, r=[f"KTd{hb}", f"QTd{hb}"], w=[tok])
                    k.act(E[m][eb][:], banks[bi][:], AF.Exp, r=[tok], w=[f"E{m}_{eb}"], scale=0.125)

            deferred = []
            qk_exp(0)
            for n, (h, qb, kc) in enumerate(steps):
                hb = h % 2
                eb = n % 2
                if n + 1 < len(steps):
                    qk_exp(n + 1)
                for m in range(2):
                    for sub in range(4):
                        a = m * 4 + sub
                        k.mm(accb[a // 3][:, (a % 3) * 129:(a % 3) * 129 + 129], E[m][eb][:, sub * 128:(sub + 1) * 128], V1[hb][:, kc, :],
                             kc == 0 and a % 3 == 0, kc == 31, r=[f"E{m}_{eb}", f"V1d{hb}"], w=[f"accb{a // 3}"])
                if kc == 31:
                    def ep1(h=h, qb=qb):
                        for a in range(8):
                            src = accb[a // 3][:, (a % 3) * 129:(a % 3) * 129 + 129]
                            k.cp("dve", osb[:, a, :], src, r=[f"accb{a // 3}"], w=[f"osbd{a}"])
                        allo = [f"osbd{a_}" for a_ in range(8)]
                        k.recip(rr[:], osb[:, :, 128], r=allo, w=["rr"])
                        k.ts("dve", rr[:, 4:8], rr[:, 4:8], neglam[:, 0:1], None, ALU.mult, r=["rr"], w=["rr"])
                        k.tt("dve", o1[:], osb[:, 0:4, 0:128], bc(rr[:, 0:4].unsqueeze(2), [128, 4, 128]), ALU.mult, r=allo + ["rr"], w=["o1"])
                        k.tt("pool", o2[:], osb[:, 4:8, 0:128], bc(rr[:, 4:8].unsqueeze(2), [128, 4, 128]), ALU.mult, r=allo + ["rr"], w=["o2"])
                        k.tt("dve", o1[:], o1[:], o2[:], ALU.add, r=["o1", "o2"], w=["o1"])
                        k.tt("pool", o2[:], o1[:], o1[:], ALU.mult, r=["o1"], w=["o2"])
                        k.red("dve", ssd[:], o2[:], r=["o2"], w=["ssd"])

                    def ep2(h=h, qb=qb):
                        k.act(ssd[:], ssd[:], AF.Sqrt, r=["ssd", "epsb"], w=["ssd"], scale=1.0 / 128, bias=epsb[:])
                        k.recip(ssd[:], ssd[:], r=["ssd"], w=["ssd"])
                        k.tt("dve", o1[:], o1[:], bc(ssd[:].unsqueeze(2), [128, 4, 128]), ALU.mult, r=["o1", "ssd"], w=["o1"])
                        k.tt("pool", obb[:], o1[:], bc(sgain[:].unsqueeze(1), [128, 4, 128]), ALU.mult, r=["o1"], w=["obb"])

                    def ep3(h=h, qb=qb):
                        ob_i = (h * 8 + qb) % 2
                        for sub in range(4):
                            k.tr(bankT[0][:, sub * 128:(sub + 1) * 128], obb[:, sub, :], identb[:], r=["obb"], w=["bTd"])
                        k.cp("dve", obT[ob_i][:], bankT[0][:, 0:512], r=["bTd"], w=[f"obT{ob_i}"])
                        k.dma("sp", OBT[h, :, qb * 512:(qb + 1) * 512], obT[ob_i][:], r=[f"obT{ob_i}"], w=["OBT"])
                    ep1()
                    deferred.append((n + 3, ep2))
                    deferred.append((n + 6, ep3))
                while deferred and (deferred[0][0] <= n or n == len(steps) - 1):
                    deferred.pop(0)[1]()
            P.emit(nc, semsets, phase_sem, 3, fin)

        with ExitStack() as es:
            P = Prog()
            k = K(P)
            banks, bankT = alloc_psum(es)
            Wc = sbt(es, "Wc", [128, 8, 2048], BF16)
            wpa = sbt(es, "wpa", [128, 2, D], BF16)
            wpb = sbt(es, "wpb", [128, 8, D], BF16)
            wo = sbt(es, "wo", [128, 8, D], BF16)
            wq = [sbt(es, f"wq{i}", [128, D], F32) for i in range(2)]
            sk = [sbt(es, f"sk{i}", [128, 128], F32) for i in range(2)]
            nd = [sbt(es, f"nd{i}", [128, 12, 65], F32) for i in range(2)]
            obt = [sbt(es, f"obt{i}", [128, 8, 128], BF16) for i in range(2)]
            gts = [sbt(es, f"gts{i}", [128, 2048], BF16) for i in range(2)]
            xts = [sbt(es, f"xts{i}", [128, D], F32) for i in range(2)]
            nsum = sbt(es, "nsum", [128, 4, 65], F32)
            rden = sbt(es, "rden", [128, 4], F32)
            oab = sbt(es, "oab", [128, 4, 64], BF16)
            oaT = sbt(es, "oaT", [128, 2, 128], BF16)
            bra = sbt(es, "bra", [128, D], F32)
            t1 = sbt(es, "t1", [128, D], F32)
            t2 = sbt(es, "t2", [128, D], F32)
            mixb = sbt(es, "mixb", [128, D], BF16)
            mixT = sbt(es, "mixT", [128, 8, 128], BF16)
            x1s = sbt(es, "x1s", [128, D], F32)
            sq2 = sbt(es, "sq2", [128, D], F32)
            ss2 = sbt(es, "ss2", [128, 1], F32)
            xn2 = sbt(es, "xn2", [128, D], BF16)
            h2T = sbt(es, "h2T", [128, 8, 128], BF16)
            Ssbs = [sbt(es, f"Ssb{i}", [128, 8, 2, 128], F32) for i in range(2)]
            mr = sbt(es, "mr", [128, 128], F32)
            t16 = sbt(es, "t16", [128, 8, 2, 16], F32)
            cand = sbt(es, "cand", [128, 8, 16, 16], F32)
            mr2 = sbt(es, "mr2", [128, 256], F32)
            c16 = sbt(es, "c16", [128, 8, 16], F32)
            dd = sbt(es, "dd", [128, 8, 16], F32)
            zz = sbt(es, "zz", [128, 8], F32)
            tau = sbt(es, "tau", [128, 8], F32)
            cf = sbt(es, "cf", [128, 8], F32)
            k.dma("pool", wpa[:], w_pa.rearrange("(c p) n -> p c n", p=128), w=["wpa"])
            k.dma("pool", wpb[:], w_pb.rearrange("(c p) n -> p c n", p=128), w=["wpb"])
            k.dma("pool", wo[:], w_out.rearrange("(c p) n -> p c n", p=128), w=["wo"])
            for kk in range(16):
                b = kk % 2
                k.dma("sp", wq[b][:], wqT[kk], w=[f"wq{b}"])
                k.dma("sp", sk[b][:], skT[kk], w=[f"sk{b}"])
                for dc in range(8):
                    k.mm(banks[dc // 4][:, (dc % 4) * 128:(dc % 4 + 1) * 128], wq[b][:, dc * 128:(dc + 1) * 128], sk[b][:], True, True,
                         r=[f"wq{b}", f"sk{b}"], w=[f"wcp{dc // 4}"])
                for hh in range(2):
                    k.act(Wc[:, 4 * hh:4 * hh + 4, kk * 128:(kk + 1) * 128], banks[hh][:].rearrange("p (a n) -> p a n", a=4), AF.Copy,
                          r=[f"wcp{hh}"], w=["Wc"])
            def stageA(t):
                b = t % 2
                t0 = t * 128
                Ssb = Ssbs[b]
                sS = f"Ssb{b}"
                k.dma("sp", nd[b][:], ND[t0:t0 + 128], r=["ND"], w=[f"nd{b}"])
                k.dma("sp", obt[b][:], OBT[:, :, t0:t0 + 128].rearrange("h c t -> c h t"), r=["OBT"], w=[f"obt{b}"])
                k.dma("sp", gts[b][:], GT[t0:t0 + 128, :], r=["GT"], w=[f"gts{b}"])
                k.dma("sp", xts[b][:], x[t0:t0 + 128, :], w=[f"xts{b}"])
                k.tt("pool", nsum[:], nd[b][:, 0:4, :], nd[b][:, 4:8, :], ALU.add, r=[f"nd{b}"], w=["nsum"])
                k.tt("pool", nsum[:], nsum[:], nd[b][:, 8:12, :], ALU.add, r=[f"nd{b}", "nsum"], w=["nsum"])
                k.recip(rden[:], nsum[:, :, 64], r=["nsum"], w=["rden"])
                k.tt("dve", oab[:], nsum[:, :, 0:64], bc(rden[:].unsqueeze(2), [128, 4, 64]), ALU.mult, r=["nsum", "rden"], w=["oab"])
                oaf = oab[:].rearrange("p a c -> p (a c)")
                for c in range(2):
                    k.tr(bankT[0][:, c * 128:(c + 1) * 128], oaf[:, c * 128:(c + 1) * 128], identb[:], r=["oab"], w=["bT0"])
                k.act(oaT[:].rearrange("p a t -> p (a t)"), bankT[0][:, 0:256], AF.Copy, r=["bT0"], w=["oaT"])
                for hf in range(2):
                    for c in range(2):
                        k.mm(banks[hf][:], oaT[:, c, :], wpa[:, c, hf * 512:(hf + 1) * 512], c == 0, c == 1, r=["oaT", "wpa"], w=[f"bk{hf}"])
                for hf in range(2):
                    for c in range(8):
                        k.mm(banks[2 + hf][:], obt[b][:, c, :], wpb[:, c, hf * 512:(hf + 1) * 512], c == 0, c == 7, r=[f"obt{b}", "wpb"], w=[f"bk{2 + hf}"])
                for hf in range(2):
                    sl = slice(hf * 512, (hf + 1) * 512)
                    k.act(bra[:, sl], banks[hf][:], AF.Copy, r=[f"bk{hf}"], w=["bra"])
                    k.tt("dve", t2[:, sl], banks[2 + hf][:], gts[b][:, 1024 + hf * 512:1024 + (hf + 1) * 512], ALU.mult, r=[f"bk{2 + hf}", f"gts{b}"], w=["t2"])
                k.tt("pool", t1[:], bra[:], gts[b][:, 0:1024], ALU.mult, r=["bra", f"gts{b}"], w=["t1"])
                k.tt("pool", mixb[:], t1[:], t2[:], ALU.add, r=["t1", "t2"], w=["mixb"])
                for c in range(8):
                    k.tr(bankT[1][:, c * 128:(c + 1) * 128], mixb[:, c * 128:(c + 1) * 128], identb[:], r=["mixb"], w=["bT1"])
                k.act(mixT[:].rearrange("p a t -> p (a t)"), bankT[1][:], AF.Copy, r=["bT1"], w=["mixT"])
                for hf in range(2):
                    for c in range(8):
                        k.mm(banks[4 + hf][:], mixT[:, c, :], wo[:, c, hf * 512:(hf + 1) * 512], c == 0, c == 7, r=["mixT", "wo"], w=[f"bk{4 + hf}"])
                for hf in range(2):
                    sl = slice(hf * 512, (hf + 1) * 512)
                    k.tt("dve", t2[:, sl], banks[4 + hf][:], G1row[:, sl], ALU.mult, r=[f"bk{4 + hf}"], w=["t2"])
                k.tt("pool", x1s[:], t2[:], xts[b][:], ALU.add, r=["t2", f"xts{b}"], w=["x1s"])
                k.dma("sp", X1[t0:t0 + 128, :], x1s[:], r=["x1s"], w=["X1"])
                k.act(sq2[:], x1s[:], AF.Square, r=["x1s"], w=["sq2"])
                k.red("dve", ss2[:], sq2[:], r=["sq2"], w=["ss2"])
                k.act(ss2[:], ss2[:], AF.Sqrt, r=["ss2", "epsb"], w=["ss2"], scale=1.0 / D, bias=epsb[:])
                k.recip(ss2[:], ss2[:], r=["ss2"], w=["ss2"])
                k.ts("dve", xn2[:], x1s[:], ss2[:, 0:1], None, ALU.mult, r=["x1s", "ss2"], w=["xn2"])
                for c in range(8):
                    k.tr(bankT[0][:, c * 128:(c + 1) * 128], xn2[:, c * 128:(c + 1) * 128], identb[:], r=["xn2"], w=["bT0"])
                for c in range(8):
                    k.act(h2T[:, c, :], bankT[0][:, c * 128:(c + 1) * 128], AF.Identity, r=["bT0"], w=["h2T"], scale=s2T[:, c:c + 1], bias=sh2T[:, c:c + 1])
                k.dma("sp", H2T[:, :, t0:t0 + 128].rearrange("c p t -> p c t"), h2T[:], r=["h2T"], w=["H2T"])
                for nb in range(4):
                    for c in range(8):
                        k.mm(banks[nb][:], h2T[:, c, :], Wc[:, c, nb * 512:(nb + 1) * 512], c == 0, c == 7, r=["h2T", "Wc"], w=[f"bk{nb}"])
                Sf = Ssb[:].rearrange("p h a n -> p (h a n)")
                for nb in range(4):
                    k.act(Sf[:, nb * 512:(nb + 1) * 512], banks[nb][:], AF.Copy, r=[f"bk{nb}"], w=[sS])
            def stageB(t):
                b = t % 2
                t0 = t * 128
                Ssb = Ssbs[b]
                sS = f"Ssb{b}"
                for h in range(8):
                    for a in range(2):
                        k.max8(t16[:, h, a, 0:8], Ssb[:, h, a, :], r=[sS], w=["t16"])
                        k.mrep(mr[:], t16[:, h, a, 0:8], Ssb[:, h, a, :], r=[sS, "t16"], w=["mr"])
                        k.max8(t16[:, h, a, 8:16], mr[:], r=["mr"], w=["t16"])
                k.tt("dve", cand[:], bc(t16[:, :, 0, :].unsqueeze(3), [128, 8, 16, 16]), bc(t16[:, :, 1, :].unsqueeze(2), [128, 8, 16, 16]), ALU.add,
                     r=["t16"], w=["cand"])
                for h in range(8):
                    cv = cand[:, h].rearrange("p a b -> p (a b)")
                    k.max8(c16[:, h, 0:8], cv, r=["cand"], w=["c16"])
                    k.mrep(mr2[:], c16[:, h, 0:8], cv, r=["cand", "c16"], w=["mr2"])
                    k.max8(c16[:, h, 8:16], mr2[:], r=["mr2"], w=["c16"])
                k.ts("dve", tau[:], c16[:, :, 15], -1e-3, None, ALU.add, r=["c16"], w=["tau"])
                k.tt("dve", dd[:], c16[:], bc(c16[:, :, 0:1], [128, 8, 16]), ALU.subtract, r=["c16"], w=["dd"])
                k.act(dd[:], dd[:], AF.Exp, r=["dd"], w=["dd"])
                k.red("dve", zz[:], dd[:], r=["dd"], w=["zz"])
                k.recip(zz[:], zz[:], r=["zz"], w=["zz"])
                k.tt("dve", cf[:], tau[:], c16[:, :, 0], ALU.subtract, r=["tau", "c16"], w=["cf"])
                k.act(cf[:], cf[:], AF.Exp, r=["cf"], w=["cf"])
                k.tt("dve", cf[:], cf[:], zz[:], ALU.mult, r=["cf", "zz"], w=["cf"])
                k.tt("dve", Ssb[:, :, 0, :], Ssb[:, :, 0, :], bc(tau[:].unsqueeze(2), [128, 8, 128]), ALU.subtract, r=[sS, "tau"], w=[sS])
                k.dma("sp", PS[t0:t0 + 128], Ssb[:], r=[sS], w=["PS"])
                k.dma("sp", CF[t0:t0 + 128, :], cf[:], r=["cf"], w=["CF"])
            stageA(0)
            for t in range(NT):
                if t + 1 < NT:
                    stageA(t + 1)
                stageB(t)
            P.emit(nc, semsets, phase_sem, 4, fin)

        with ExitStack() as es:
            P = Prog()
            k = K(P)
            banks, bankT = alloc_psum(es, 8, 0)
            NB = 4
            h2 = [sbt(es, f"h2_{i}", [128, 8, 256], BF16) for i in range(2)]
            pss = sbt(es, "pss", [128, 2, 8, 2, 128], F32)
            cfs = [sbt(es, f"cfs{i}", [128, 2, 8], F32) for i in range(2)]
            x1t = sbt(es, "x1t", [128, 2, D], F32)
            Dm = [sbt(es, f"Dm{i}", [128, 2, 8, 128], BF16) for i in range(2)]
            UTi = [sbt(es, f"UTi{i}", [128, 8, 128], BF16) for i in range(NB)]
            Vi = [sbt(es, f"Vi{i}", [128, D], BF16) for i in range(NB)]
            HgA = sbt(es, "HgA", [128, 128, 256], BF16)
            zt = [sbt(es, f"zt{i}", [128, 6, 128], F32) for i in range(3)]
            E32 = [sbt(es, f"E32_{i}", [128, 2, 8, 128], F32) for i in range(2)]
            Gt = [sbt(es, f"Gt{i}", [128, 2, 8, 128], BF16) for i in range(3)]
            WT = [sbt(es, f"WT{i}", [128, 256], BF16) for i in range(2)]
            yo = [sbt(es, f"yo{i}", [128, D], F32) for i in range(2)]
            P.add("sp", lambda e: e.wait_ge(conv_sem, 16 * 32))
            UTv = UTb.rearrange("(i p) (c e) -> i p c e", p=128, c=8)
            ustep = 0
            vstep = 0
            for tt_ in range(16):
                tb = tt_ % 2
                t0 = tt_ * 256
                k.dma("sp", h2[tb][:], H2T[:, :, t0:t0 + 256].rearrange("c p t -> p c t"), w=[f"h2_{tb}"])
                k.dma("sp", cfs[tb][:], CF[t0:t0 + 256, :].rearrange("(s p) h -> p s h", p=128), w=[f"cfs{tb}"])
                for s_ in range(2):
                    for h in range(8):
                        k.ts("pool", Dm[tb][:, s_, h, :], identf[:], cfs[tb][:, s_, h:h + 1], None, ALU.mult, r=[f"cfs{tb}"], w=[f"Dm{tb}"])
                k.dma("sp", pss[:], PS[t0:t0 + 256].rearrange("(s p) h a n -> p s h a n", p=128), w=["pss"])
                for i in range(128):
                    ub = ustep % NB
                    hb_ = ustep % 2
                    ustep += 1
                    k.dma("sp", UTi[ub][:], UTv[i], w=[f"UTi{ub}"])
                    hv = banks[4 + hb_][:, 0:256]
                    for c in range(8):
                        k.mm(hv, UTi[ub][:, c, :], h2[tb][:, c, :], c == 0, c == 7, r=[f"UTi{ub}", f"h2_{tb}"], w=[f"bH{hb_}"])
                    k.act(HgA[:, i, :], hv, AF.Gelu, r=[f"bH{hb_}"], w=[f"HgA{i}"])

                NA = 10

                def st_z(i):
                    zb = i % 3
                    h0 = NA - 8
                    k.tt("pool", zt[zb][:], pss[:, 1, h0:8, 1, :], bc(pss[:, 1, h0:8, 0, i:i + 1], [128, 8 - h0, 128]), ALU.add, r=["pss"], w=[f"zt{zb}"])

                def st_eg(i):
                    zb = i % 3
                    eb = i % 2
                    h0 = NA - 8
                    for g_ in range(NA):
                        s_, h = g_ // 8, g_ % 8
                        k.act(E32[eb][:, s_, h, :], pss[:, s_, h, 1, :], AF.Exp, r=["pss"], w=[f"E32_{eb}"], bias=pss[:, s_, h, 0, i:i + 1])
                    k.act(E32[eb][:, 1, h0:8, :], zt[zb][:], AF.Exp, r=[f"zt{zb}"], w=[f"E32_{eb}"])
                    k.stt("dve", Gt[zb][:], E32[eb][:], 1.0, E32[eb][:], ALU.is_ge, ALU.mult, r=[f"E32_{eb}"], w=[f"Gt{zb}"])

                def st_gt(i):
                    zb = i % 3
                    b2 = i % 2
                    gv = banks[6 + b2][:, 0:256]
                    for s_ in range(2):
                        for h in range(8):
                            k.mm(gv[:, s_ * 128:(s_ + 1) * 128], Gt[zb][:, s_, h, :], Dm[tb][:, s_, h, :], h == 0, h == 7,
                                 r=[f"Gt{zb}", f"Dm{tb}"], w=[f"bG{b2}"])

                def st_wt(i):
                    b2 = i % 2
                    gv = banks[6 + b2][:, 0:256]
                    k.tt("dve", WT[b2][:], gv, HgA[:, i, :], ALU.mult, r=[f"bG{b2}", f"HgA{i}"], w=[f"WT{b2}"])

                vbuf = {}

                def st_vload(i):
                    nonlocal vstep
                    vb_ = vstep % NB
                    vstep += 1
                    vbuf[i] = vb_
                    k.dma("sp", Vi[vb_][:], Vb[i * 128:(i + 1) * 128, :], w=[f"Vi{vb_}"])

                def st_y(i):
                    b2 = i % 2
                    vb_ = vbuf[i]
                    for s_ in range(2):
                        for hf in range(2):
                            k.mm(banks[s_ * 2 + hf][:], WT[b2][:, s_ * 128:(s_ + 1) * 128], Vi[vb_][:, hf * 512:(hf + 1) * 512], i == 0, i == 127,
                                 r=[f"WT{b2}", f"Vi{vb_}"], w=[f"bY{s_ * 2 + hf}"])

                for it in range(128 + 3):
                    if it < 128:
                        st_vload(it)
                        st_z(it)
                    if 1 <= it <= 128:
                        st_eg(it - 1)
                    if 2 <= it <= 129:
                        st_gt(it - 2)
                    if 3 <= it <= 130:
                        st_y(it - 3)
                    if 2 <= it <= 129:
                        st_wt(it - 2)
                k.dma("sp", x1t[:], X1[t0:t0 + 256, :].rearrange("(s p) d -> p s d", p=128), w=["x1t"])
                for s_ in range(2):
                    for hf in range(2):
                        sl = slice(hf * 512, (hf + 1) * 512)
                        k.tt("dve", yo[s_][:, sl], banks[s_ * 2 + hf][:], G2row[:, sl], ALU.mult, r=[f"bY{s_ * 2 + hf}"], w=[f"yo{s_}"])
                    k.tt("pool", yo[s_][:], yo[s_][:], x1t[:, s_, :], ALU.add, r=[f"yo{s_}", "x1t"], w=[f"yo{s_}"])
                    k.dma("sp", out[t0 + s_ * 128:t0 + (s_ + 1) * 128, :], yo[s_][:], r=[f"yo{s_}"], w=["out"])
            P.emit(nc, semsets, phase_sem, 5, fin)
    return nc


DEBUG_OUT = set()


def _masks():
    kk = np.arange(128)[:, None]
    qq = np.arange(256)[None, :]
    std = ((kk <= qq) & (qq <= kk + 128)).astype(np.float32)
    blk = (((kk < 64) & (qq < 128)) | ((kk >= 64) & (qq >= 128))).astype(np.float32)
    return std, std * blk


def prep_inputs(inputs):
    f = lambda a: np.ascontiguousarray(np.asarray(a), dtype=np.float32)
    x = f(inputs["x"])
    c = f(inputs["c"])
    pos = np.ascontiguousarray(np.asarray(inputs["positions"]), dtype=np.int32)
    L = 0
    rep = lambda v, n=128: np.ascontiguousarray(np.broadcast_to(f(v)[None], (n,) + f(v).shape))
    qn_a, kn_a = f(inputs["qn_a"])[L], f(inputs["kn_a"])[L]
    qn_b, kn_b = f(inputs["qn_b"])[L], f(inputs["kn_b"])[L]
    gainA = np.concatenate([np.tile(qn_a[None], (4, 1)), np.tile(kn_a[None], (4, 1))], 0)
    gainQB = np.tile(qn_b[None], (8, 1))
    gainKB = np.tile(kn_b[None], (8, 1))
    lam = np.stack([f(inputs["lam_q1"])[L], f(inputs["lam_k1"])[L], f(inputs["lam_q2"])[L], f(inputs["lam_k2"])[L]], 0)
    invf = (1.0 / (10000.0 ** (np.arange(0, 64, 2, dtype=np.float32) / 64))).astype(np.float32)
    hp = np.concatenate([np.zeros(32, np.float32), np.full(32, np.pi / 2, np.float32)])
    mstd, mbnd = _masks()
    wq = f(inputs["w_query"])[L]
    wqT = np.ascontiguousarray(wq.reshape(D, 16, 128).transpose(1, 2, 0))
    sk = f(inputs["sub_keys"])[L].reshape(16, 128, 128)
    skT = np.ascontiguousarray(sk.transpose(0, 2, 1))
    U = f(inputs["expert_u"])[L]
    UT = np.ascontiguousarray(U.reshape(128, 128, 8, 128).transpose(0, 3, 2, 1)).reshape(16384, 1024)
    shared = dict(
        w_ada=f(inputs["w_ada"])[L],
        b_adaT=np.ascontiguousarray(f(inputs["b_ada"])[L].reshape(48, 128).T),
        n1gT=np.ascontiguousarray(f(inputs["norm1_g"])[L].reshape(8, 128).T),
        n2gT=np.ascontiguousarray(f(inputs["norm2_g"])[L].reshape(8, 128).T),
        w_in=f(inputs["w_in"])[L],
        b_gate_bc=rep(f(inputs["b_gate"])[L]),
        gainA=rep(gainA), gainQB=rep(gainQB), gainKB=rep(gainKB),
        lam_bc=rep(lam), subln_bc=rep(f(inputs["subln_g"])[L]),
        invf_bc=rep(invf), halfpi=rep(hp), mask_std=mstd, mask_bnd=mbnd,
        w_pa=f(inputs["w_proj_a"])[L], w_pb=f(inputs["w_proj_b"])[L], w_out=f(inputs["w_out"])[L],
        wqT=wqT, skT=skT, UT=UT, Vx=f(inputs["expert_v"])[L],
    )
    in_maps = []
    for b in range(8):
        m = dict(shared)
        m["x"] = x[b]
        m["pos"] = pos[b]
        m["cT"] = np.ascontiguousarray(c[b].reshape(8, 128).T)
        in_maps.append(m)
    return in_maps


def kernel(**inputs):
    in_maps = prep_inputs(inputs)
    nc = build_program()
    res = run_bass_kernel_spmd(nc, in_maps, core_ids=list(range(8)))
    return np.stack([np.asarray(r["out"], dtype=np.float32) for r in res.results], 0)
```
